# Optimizing a Trainium2 kernel written in Bass

```python
import math
import jax, jax.numpy as jnp
from jax import lax
import numpy as np

D_MODEL = 1024
BATCH = 16
SEQ = 4096
DEPTH = 1

HEAD_DIM = 64
ROPE_THETA = 500000.0
NORM_EPS = 1e-6
Q_BLOCK = 128

MLA_HEADS = 8
MLA_Q_RANK = 256
MLA_KV_RANK = 128
MLA_NOPE_DIM = 64
MLA_ROPE_DIM = 32
MLA_V_DIM = 64
MLA_WIDTH = MLA_HEADS * MLA_V_DIM

NSA_HEADS = 8
NSA_KV_GROUPS = 2
NSA_HPG = NSA_HEADS // NSA_KV_GROUPS
NSA_ROPE_DIM = HEAD_DIM // 4
NSA_WIDTH = NSA_HEADS * HEAD_DIM
NSA_KV_WIDTH = NSA_KV_GROUPS * HEAD_DIM
CMP_BLOCK = 32
CMP_STRIDE = 16
CMP_HIDDEN = 2 * HEAD_DIM
SLC_BLOCK = 64
SLC_TOPK = 16
WINDOW = 512
FORCE_SCORE = 1e9

MIX_WIDTH = MLA_WIDTH + NSA_WIDTH
D_FF = 4 * D_MODEL

IN_SIZES = (MLA_Q_RANK, MLA_KV_RANK, MLA_ROPE_DIM, NSA_WIDTH, 6 * NSA_KV_WIDTH, 3 * NSA_HEADS)
IN_COLS = MLA_Q_RANK + MLA_KV_RANK + MLA_ROPE_DIM + NSA_WIDTH + 6 * NSA_KV_WIDTH + 3 * NSA_HEADS

kernel_name = 'hymba_mla_nsa_hybrid_layer'


def rmsnorm(x, g):
    xf = x.astype(jnp.float32)
    y = xf * lax.rsqrt(jnp.mean(xf * xf, axis=-1, keepdims=True) + NORM_EPS)
    return (y * g.astype(jnp.float32)).astype(x.dtype)


def rope_tables(pos, dim):
    inv_freq = jnp.exp(-math.log(ROPE_THETA) * jnp.arange(0, dim, 2, dtype=jnp.float32) / dim)
    ang = pos.astype(jnp.float32)[:, None] * inv_freq[None, :]
    return jnp.cos(ang), jnp.sin(ang)


def apply_rope(x, cos, sin):
    half = x.shape[-1] // 2
    xf = x.astype(jnp.float32)
    x1, x2 = xf[..., :half], xf[..., half:]
    c = cos[None, :, None, :]
    s = sin[None, :, None, :]
    return jnp.concatenate([x1 * c - x2 * s, x2 * c + x1 * s], axis=-1).astype(x.dtype)


def partial_rope(x, cos, sin):
    return jnp.concatenate([apply_rope(x[..., :NSA_ROPE_DIM], cos, sin), x[..., NSA_ROPE_DIM:]], axis=-1)


def masked_softmax(s, mask):
    s = jnp.where(mask, s, -jnp.inf)
    m = jnp.max(s, axis=-1, keepdims=True)
    m = jnp.where(jnp.isfinite(m), m, 0.0)
    p = jnp.exp(s - m)
    return p / jnp.maximum(jnp.sum(p, axis=-1, keepdims=True), 1e-30)


def to_blocks(a):
    b, s = a.shape[0], a.shape[1]
    return a.reshape(b, s // Q_BLOCK, Q_BLOCK, *a.shape[2:]).swapaxes(0, 1)


def from_blocks(a):
    nb, b, qb = a.shape[0], a.shape[1], a.shape[2]
    return a.swapaxes(0, 1).reshape(b, nb * qb, *a.shape[3:])


def mla_mixer(c_q, c_kv, k_rope, g_cq, w_uq, g_ckv, w_ukv, cos, sin):
    B, S, _ = c_q.shape
    q = (rmsnorm(c_q, g_cq) @ w_uq).reshape(B, S, MLA_HEADS, MLA_NOPE_DIM + MLA_ROPE_DIM)
    q_nope = q[..., :MLA_NOPE_DIM]
    q_pe = apply_rope(q[..., MLA_NOPE_DIM:], cos, sin)
    kv = (rmsnorm(c_kv, g_ckv) @ w_ukv).reshape(B, S, MLA_HEADS, MLA_NOPE_DIM + MLA_V_DIM)
    k_nope = kv[..., :MLA_NOPE_DIM]
    v = kv[..., MLA_NOPE_DIM:]
    k_pe = apply_rope(k_rope[:, :, None, :], cos, sin)[:, :, 0]
    scale = (MLA_NOPE_DIM + MLA_ROPE_DIM) ** -0.5
    kpos = jnp.arange(S)

    def attend(blk):
        qn, qp, qpos = blk
        s = (jnp.einsum('bqhd,bkhd->bhqk', qn, k_nope, preferred_element_type=jnp.float32)
             + jnp.einsum('bqhr,bkr->bhqk', qp, k_pe, preferred_element_type=jnp.float32)) * scale
        p = masked_softmax(s, kpos[None, :] <= qpos[:, None])
        return jnp.einsum('bhqk,bkhd->bqhd', p.astype(v.dtype), v)

    out = lax.map(attend, (to_blocks(q_nope), to_blocks(q_pe), kpos.reshape(-1, Q_BLOCK)))
    return from_blocks(out).reshape(B, S, MLA_WIDTH)


def compress_blocks(x, pe, w1, b1, w2, b2):
    B, S, G, dh = x.shape
    n_cmp = (S - CMP_BLOCK) // CMP_STRIDE + 1
    idx = jnp.arange(n_cmp)[:, None] * CMP_STRIDE + jnp.arange(CMP_BLOCK)[None, :]
    blocks = x[:, idx] + pe[None, None, :, None, :].astype(x.dtype)
    flat = blocks.transpose(0, 1, 3, 2, 4).reshape(B, n_cmp, G, CMP_BLOCK * dh)
    hid = jax.nn.gelu(flat @ w1 + b1)
    return hid @ w2 + b2


def nsa_mixer(q, kv_tok, gate_logits, cmp_pe_k, cmp_w1_k, cmp_b1_k, cmp_w2_k, cmp_b2_k,
              cmp_pe_v, cmp_w1_v, cmp_b1_v, cmp_w2_v, cmp_b2_v, cos, sin):
    B, S, H, dh = q.shape
    G = NSA_KV_GROUPS
    dt = q.dtype
    scale = dh ** -0.5
    q = partial_rope(q, cos, sin)
    k_slc = partial_rope(kv_tok[:, :, 2], cos, sin)
    v_slc = kv_tok[:, :, 3]
    k_win = partial_rope(kv_tok[:, :, 4], cos, sin)
    v_win = kv_tok[:, :, 5]

    n_cmp = (S - CMP_BLOCK) // CMP_STRIDE + 1
    cmp_end = jnp.arange(n_cmp) * CMP_STRIDE + CMP_BLOCK - 1
    k_cmp = compress_blocks(kv_tok[:, :, 0], cmp_pe_k, cmp_w1_k, cmp_b1_k, cmp_w2_k, cmp_b2_k)
    k_cmp = partial_rope(k_cmp, cos[cmp_end], sin[cmp_end])
    v_cmp = compress_blocks(kv_tok[:, :, 1], cmp_pe_v, cmp_w1_v, cmp_b1_v, cmp_w2_v, cmp_b2_v)

    n_slc = S // SLC_BLOCK
    k_top = min(SLC_TOPK, n_slc)
    cs = jnp.arange(n_cmp)[:, None] * CMP_STRIDE
    ss = jnp.arange(n_slc)[None, :] * SLC_BLOCK
    overlap = jnp.clip(jnp.minimum(cs + CMP_BLOCK, ss + SLC_BLOCK) - jnp.maximum(cs, ss), 0, None).astype(jnp.float32) / CMP_BLOCK
    k_slc_blocks = k_slc.reshape(B, n_slc, SLC_BLOCK, G, dh).transpose(0, 3, 1, 2, 4)
    v_slc_blocks = v_slc.reshape(B, n_slc, SLC_BLOCK, G, dh).transpose(0, 3, 1, 2, 4)
    gather = jax.vmap(jax.vmap(lambda blocks, ix: blocks[ix]))

    pad = ((0, 0), (WINDOW, 0), (0, 0), (0, 0))
    k_win_pad = jnp.pad(k_win, pad)
    v_win_pad = jnp.pad(v_win, pad)

    def attend(blk):
        qb, gb, bi = blk
        q0 = bi * Q_BLOCK
        qpos = q0 + jnp.arange(Q_BLOCK)
        qg = qb.reshape(B, Q_BLOCK, G, NSA_HPG, dh)

        s_c = jnp.einsum('bqghd,bngd->bghqn', qg, k_cmp, preferred_element_type=jnp.float32) * scale
        p_c = masked_softmax(s_c, cmp_end[None, :] <= qpos[:, None])
        o_c = jnp.einsum('bghqn,bngd->bqghd', p_c.astype(dt), v_cmp)

        imp = jnp.einsum('bghqn,nj->bgqj', p_c, overlap)
        j = jnp.arange(n_slc)[None, :]
        cur = (qpos // SLC_BLOCK)[:, None]
        forced = (j == 0) | (j == cur) | (j == cur - 1)
        causal = j * SLC_BLOCK <= qpos[:, None]
        score = jnp.where(forced, FORCE_SCORE, jnp.where(causal, imp, -FORCE_SCORE))
        _, sel = lax.top_k(score, k_top)
        kg = gather(k_slc_blocks, sel).reshape(B, G, Q_BLOCK, k_top * SLC_BLOCK, dh)
        vg = gather(v_slc_blocks, sel).reshape(B, G, Q_BLOCK, k_top * SLC_BLOCK, dh)
        tok = (sel[..., None] * SLC_BLOCK + jnp.arange(SLC_BLOCK)).reshape(B, G, Q_BLOCK, k_top * SLC_BLOCK)
        s_s = jnp.einsum('bqghd,bgqmd->bghqm', qg, kg, preferred_element_type=jnp.float32) * scale
        p_s = masked_softmax(s_s, (tok <= qpos[None, None, :, None])[:, :, None])
        o_s = jnp.einsum('bghqm,bgqmd->bqghd', p_s.astype(dt), vg)

        kw = lax.dynamic_slice_in_dim(k_win_pad, q0, Q_BLOCK + WINDOW, axis=1)
        vw = lax.dynamic_slice_in_dim(v_win_pad, q0, Q_BLOCK + WINDOW, axis=1)
        kpos = q0 - WINDOW + jnp.arange(Q_BLOCK + WINDOW)
        dist = qpos[:, None] - kpos[None, :]
        mask_w = (dist >= 0) & (dist < WINDOW) & (kpos[None, :] >= 0)
        s_w = jnp.einsum('bqghd,bkgd->bghqk', qg, kw, preferred_element_type=jnp.float32) * scale
        p_w = masked_softmax(s_w, mask_w)
        o_w = jnp.einsum('bghqk,bkgd->bqghd', p_w.astype(dt), vw)

        g = jax.nn.sigmoid(gb.astype(jnp.float32)).reshape(B, Q_BLOCK, 3, G, NSA_HPG)[..., None]
        o = g[:, :, 0] * o_c + g[:, :, 1] * o_s + g[:, :, 2] * o_w
        return o.reshape(B, Q_BLOCK, H * dh).astype(dt)

    nb = S // Q_BLOCK
    out = lax.map(attend, (to_blocks(q), to_blocks(gate_logits), jnp.arange(nb)))
    return from_blocks(out)


def setup_inputs(seed: int = 0) -> dict:
    key = jax.random.key(seed)
    keys = jax.random.split(key, 32)
    f32 = jnp.float32
    L = DEPTH
    flat = CMP_BLOCK * HEAD_DIM

    def w(i, shape, fan_in):
        return jax.random.normal(keys[i], shape, f32) * fan_in ** -0.5

    def gain(i, shape):
        return 1.0 + 0.02 * jax.random.normal(keys[i], shape, f32)

    def small(i, shape, s=0.01):
        return s * jax.random.normal(keys[i], shape, f32)

    return {
        'x': jax.random.normal(keys[0], (BATCH, SEQ, D_MODEL), f32),
        'g_mix_norm': gain(1, (L, D_MODEL)),
        'w_in': w(2, (L, D_MODEL, IN_COLS), D_MODEL),
        'g_cq': gain(3, (L, MLA_Q_RANK)),
        'w_uq': w(4, (L, MLA_Q_RANK, MLA_HEADS * (MLA_NOPE_DIM + MLA_ROPE_DIM)), MLA_Q_RANK),
        'g_ckv': gain(5, (L, MLA_KV_RANK)),
        'w_ukv': w(6, (L, MLA_KV_RANK, MLA_HEADS * (MLA_NOPE_DIM + MLA_V_DIM)), MLA_KV_RANK),
        'cmp_pe_k': small(7, (L, CMP_BLOCK, HEAD_DIM), 0.1),
        'cmp_w1_k': w(8, (L, flat, CMP_HIDDEN), flat),
        'cmp_b1_k': small(9, (L, CMP_HIDDEN)),
        'cmp_w2_k': w(10, (L, CMP_HIDDEN, HEAD_DIM), CMP_HIDDEN),
        'cmp_b2_k': small(11, (L, HEAD_DIM)),
        'cmp_pe_v': small(12, (L, CMP_BLOCK, HEAD_DIM), 0.1),
        'cmp_w1_v': w(13, (L, flat, CMP_HIDDEN), flat),
        'cmp_b1_v': small(14, (L, CMP_HIDDEN)),
        'cmp_w2_v': w(15, (L, CMP_HIDDEN, HEAD_DIM), CMP_HIDDEN),
        'cmp_b2_v': small(16, (L, HEAD_DIM)),
        'g_out_mla': gain(17, (L, MLA_WIDTH)),
        'g_out_nsa': gain(18, (L, NSA_WIDTH)),
        'w_o': w(19, (L, MIX_WIDTH, D_MODEL), MIX_WIDTH),
        'g_mlp_norm': gain(20, (L, D_MODEL)),
        'w_up': w(21, (L, D_MODEL, D_FF), D_MODEL),
        'w_down': w(22, (L, D_FF, D_MODEL), D_FF),
        'g_final': gain(23, (D_MODEL,)),
    }


def reference(x, g_mix_norm, w_in, g_cq, w_uq, g_ckv, w_ukv,
              cmp_pe_k, cmp_w1_k, cmp_b1_k, cmp_w2_k, cmp_b2_k,
              cmp_pe_v, cmp_w1_v, cmp_b1_v, cmp_w2_v, cmp_b2_v,
              g_out_mla, g_out_nsa, w_o, g_mlp_norm, w_up, w_down, g_final):
    B, S, _ = x.shape
    pos = jnp.arange(S)
    cos_m, sin_m = rope_tables(pos, MLA_ROPE_DIM)
    cos_n, sin_n = rope_tables(pos, NSA_ROPE_DIM)
    split_points = [int(v) for v in np.cumsum(IN_SIZES)[:-1]]
    h = x
    for l in range(DEPTH):
        u = rmsnorm(h, g_mix_norm[l]) @ w_in[l]
        c_q, c_kv, k_rope, q_nsa, kv_nsa, gate_nsa = jnp.split(u, split_points, axis=-1)
        y_mla = mla_mixer(c_q, c_kv, k_rope, g_cq[l], w_uq[l], g_ckv[l], w_ukv[l], cos_m, sin_m)
        y_nsa = nsa_mixer(q_nsa.reshape(B, S, NSA_HEADS, HEAD_DIM),
                          kv_nsa.reshape(B, S, 6, NSA_KV_GROUPS, HEAD_DIM),
                          gate_nsa.reshape(B, S, 3, NSA_HEADS),
                          cmp_pe_k[l], cmp_w1_k[l], cmp_b1_k[l], cmp_w2_k[l], cmp_b2_k[l],
                          cmp_pe_v[l], cmp_w1_v[l], cmp_b1_v[l], cmp_w2_v[l], cmp_b2_v[l],
                          cos_n, sin_n)
        mixed = jnp.concatenate([rmsnorm(y_mla, g_out_mla[l]), rmsnorm(y_nsa, g_out_nsa[l])], axis=-1)
        h = h + mixed @ w_o[l]
        a = jax.nn.relu(rmsnorm(h, g_mlp_norm[l]) @ w_up[l])
        h = h + (a * a) @ w_down[l]
    return rmsnorm(h, g_final)
```

```python
import math
import numpy as np
from contextlib import ExitStack
import concourse.bass as bass
import concourse.mybir as mybir
from concourse.bass_utils import run_bass_kernel_spmd

F32 = mybir.dt.float32
BF16 = mybir.dt.bfloat16
AF = mybir.ActivationFunctionType
ALU = mybir.AluOpType
AX = mybir.AxisListType

NCORES = 8
SEQ = 4096
D = 1024
NSEQ = 2
NT = SEQ // 128
EPS = 1e-6
IN_COLS = 1720
DFF = 4096
NEG = -30000.0
STRICT_SAME = True
OP_LIMIT = None
BG_OVERLAP = True


class Buf:
    __slots__ = ("name", "w", "r", "dsem", "dcnt", "excl")

    def __init__(self, name):
        self.name = name
        self.excl = False
        self.w = None
        self.r = {}
        self.dsem = None
        self.dcnt = 0


class Sched:
    ENG = ("pe", "act", "dve", "pool", "sp")

    def __init__(self, nc, ctx):
        self.nc = nc
        self.ctx = ctx
        self.sem = {e: ctx.enter_context(nc.semaphore("s_" + e)) for e in self.ENG}
        self.cnt = {e: 0 for e in self.ENG}
        self.seen = {e: {} for e in self.ENG}
        self.prog = {e: [] for e in self.ENG}
        self.dbufs = []
        self.nb = 0
        self.nops = 0
        self.fillregs = {}
        self.tick = None
        self.limit = OP_LIMIT

    def buf(self, name):
        self.nb += 1
        return Buf("%s_%d" % (name, self.nb))

    def bufs(self, name, n):
        return [self.buf(name) for _ in range(n)]

    def _dsem(self, b):
        if b.dsem is None:
            b.dsem = self.ctx.enter_context(self.nc.semaphore("d_" + b.name))
            self.dbufs.append(b)
        return b.dsem

    def _deps(self, e, reads, writes, strict):
        toks = []
        for b in reads:
            if b.w is not None:
                toks.append(b.w)
            if b.excl:
                toks.extend(b.r.values())
        for b in writes:
            if b.w is not None:
                toks.append(b.w)
            toks.extend(b.r.values())
        need = {}
        for (key, sem, val) in toks:
            if key == e and not strict and (e == "pe" or not STRICT_SAME):
                continue
            if self.seen[e].get(key, 0) >= val:
                continue
            if key not in need or need[key][1] < val:
                need[key] = (sem, val)
        for key, (sem, val) in need.items():
            self.seen[e][key] = val
            self.prog[e].append(("wait", sem, val))

    def op(self, e, meth, *args, R=(), W=(), **kw):
        self.nops += 1
        if self.limit is not None and self.nops > self.limit:
            return None
        self._deps(e, R, W, False)
        self.cnt[e] += 1
        tok = (e, self.sem[e], self.cnt[e])
        self.prog[e].append(("op", meth, args, kw))
        for b in R:
            b.r[e] = tok
        for b in W:
            b.w = tok
            b.r = {}
        if self.tick is not None:
            self.tick()
        return tok

    def dma(self, q, out, in_, R=(), W=(), **kw):
        self.nops += 1
        if self.limit is not None and self.nops > self.limit:
            return None
        self._deps(q, R, W, True)
        owner = W[0] if W else R[0]
        sem = self._dsem(owner)
        owner.dcnt += 16
        tok = ("d_" + owner.name, sem, owner.dcnt)
        self.prog[q].append(("dma", out, in_, kw, sem))
        for b in R:
            b.r[tok[0]] = tok
        for b in W:
            b.w = tok
            b.r = {}
        return tok

    def barrier(self):
        toks = [(e, self.sem[e], self.cnt[e]) for e in self.ENG if self.cnt[e] > 0]
        toks += [("d_" + b.name, b.dsem, b.dcnt) for b in self.dbufs if b.dcnt > 0]
        for e in self.ENG:
            for (key, sem, val) in toks:
                if self.seen[e].get(key, 0) >= val:
                    continue
                self.seen[e][key] = val
                self.prog[e].append(("wait", sem, val))

    def flush(self):
        nc = self.nc
        with nc.Block() as block:
            def replay(e):
                def f(eng):
                    sem_e = self.sem[e]
                    for it in self.prog[e]:
                        if it[0] == "wait":
                            eng.wait_ge(it[1], it[2])
                        elif it[0] == "op":
                            args = it[2]
                            if it[1] == "affine_select":
                                args = list(args)
                                if args[4] not in self.fillregs:
                                    self.fillregs[args[4]] = eng.to_reg(args[4])
                                args[4] = self.fillregs[args[4]]
                            getattr(eng, it[1])(*args, **it[3]).then_inc(sem_e, 1)
                        else:
                            eng.dma_start(out=it[1], in_=it[2], **it[3]).then_inc(it[4], 16)
                return f
            block.tensor(replay("pe"))
            block.scalar(replay("act"))
            block.vector(replay("dve"))
            block.gpsimd(replay("pool"))
            block.sync(replay("sp"))
        self.prog = {e: [] for e in self.ENG}

    def emit(self):
        self.barrier()
        self.flush()


def build_program(nseq=NSEQ, ntiles=NT, do_mlp=True, stages=("p1", "mla", "nsa", "out")):
    nc = bass.Bass("TRN2", target_bir_lowering=False)

    def din(name, shape):
        return nc.dram_tensor(name, list(shape), F32, kind="ExternalInput").ap()

    x_d = din("x", [nseq, SEQ, D])
    w_in_d = din("w_in", [D, IN_COLS])
    g_mix_d = din("g_mix", [128, 8])
    w_uq_d = din("w_uq", [256, 768])
    g_cq_d = din("g_cq", [128, 2])
    wukT_d = din("wukT", [64, 8, 128])
    wuv_d = din("wuv", [128, 8, 64])
    gckv_d = din("gckv_bc", [128, 128])
    w1_d = din("cmp_w1", [2, 64, 32, 128])
    peT_d = din("cmp_peT", [2, 64, 32])
    b1_d = din("cmp_b1", [128, 2])
    w2_d = din("cmp_w2", [128, 2, 64])
    b2k_d = din("cmp_b2k_bc", [128, 128])
    b2v_d = din("cmp_b2v", [64, 1])
    w_o_d = din("w_o", [D, D])
    g_out_d = din("g_out", [128, 8])
    w_up_d = din("w_up", [D, DFF])
    g_mlp_d = din("g_mlp", [128, 8])
    w_down_d = din("w_down", [DFF, D])
    gfin_d = din("gfin_bc", [128, D])
    cosm_d = din("cos_m", [128, NT, 16])
    sinm_d = din("sin_m", [128, NT, 16])
    cosn_d = din("cos_n", [128, NT, 8])
    sinn_d = din("sin_n", [128, NT, 8])
    cosn8_d = din("cos_n8", [128, NT, 8])
    sinn8_d = din("sin_n8", [128, NT, 8])
    cose_d = din("cos_e", [8, NT, 8])
    sine_d = din("sin_e", [8, NT, 8])
    tri_d = din("tri4", [128, 512])
    anti_d = din("anti4", [128, 512])
    ovl_d = din("ovl", [128, 2, 64])
    exp_d = din("expand", [64, SEQ])
    ident_d = din("ident", [128, 128])
    out_d = nc.dram_tensor("out", [nseq * SEQ, D], F32, kind="ExternalOutput").ap()
    hscr_d = nc.dram_tensor("hscr", [nseq * SEQ, D], F32, kind="Internal").ap()

    top = ExitStack()
    with top:
        S = Sched(nc, top)
        def psum(name, shape, dt):
            return top.enter_context(nc.psum_tensor(name, shape, dt))
        pS = [psum("pS%d" % i, [128, 512], F32) for i in range(2)]
        pA = [psum("pA%d" % i, [128, 512], F32) for i in range(2)]
        pM = [psum("pM%d" % i, [128, 512], F32) for i in range(2)]
        pT = [psum("pT%d" % i, [128, 1024], BF16) for i in range(2)]
        bpS = S.bufs("pS", 2)
        bpA = S.bufs("pA", 2)
        bpM = S.bufs("pM", 2)
        bpT = S.bufs("pT", 2)
        for b_ in bpS + bpA + bpM + bpT:
            b_.excl = True
        rr = {"S": 0, "M": 0, "T": 0, "P": 0, "A": 0}
        mode = {"bg": False, "B": False}

        def nxt(kind, n):
            if kind in ("M", "T") and not mode["B"]:
                return 0 if mode["bg"] else 1
            i = rr[kind] % n
            rr[kind] += 1
            return i

        A = ExitStack()
        with A:
            def sb(name, shape, dt):
                return A.enter_context(nc.sbuf_tensor("a_" + name, shape, dt))

            ident = sb("ident", [128, 128], BF16); b_ident = S.buf("ident")
            S.dma("pool", ident[:], ident_d, W=[b_ident])
            tri4 = sb("tri4", [128, 512], BF16); anti4 = sb("anti4", [128, 512], BF16); b_msk = S.buf("msk")
            S.dma("pool", tri4[:], tri_d, W=[b_msk])
            S.dma("pool", anti4[:], anti_d, W=[b_msk])
            tabs = {}
            b_tab = S.buf("tab")
            for nm, d_, w_ in (("cos_m", cosm_d, 16), ("sin_m", sinm_d, 16), ("cos_n", cosn_d, 8), ("sin_n", sinn_d, 8),
                               ("cos_n8", cosn8_d, 8), ("sin_n8", sinn8_d, 8)):
                tabs[nm] = sb(nm, [128, NT, w_], F32)
                S.dma("sp", tabs[nm][:], d_, W=[b_tab])
            cos_e = sb("cos_e", [8, NT, 8], F32); sin_e = sb("sin_e", [8, NT, 8], F32)
            S.dma("sp", cos_e[:], cose_d, W=[b_tab])
            S.dma("sp", sin_e[:], sine_d, W=[b_tab])
            gckv = sb("gckv", [128, 128], F32); b2k = sb("b2k", [128, 128], F32); b2v = sb("b2v", [64, 1], F32)
            b1 = sb("b1", [128, 2], F32)
            S.dma("sp", gckv[:], gckv_d, W=[b_tab])
            S.dma("sp", b2k[:], b2k_d, W=[b_tab])
            S.dma("sp", b2v[:], b2v_d, W=[b_tab])
            S.dma("sp", b1[:], b1_d, W=[b_tab])
            gvec = sb("gvec", [128, 24], F32)
            S.dma("sp", gvec[:, 0:8], g_mix_d, W=[b_tab])
            S.dma("sp", gvec[:, 8:10], g_cq_d, W=[b_tab])
            S.dma("sp", gvec[:, 10:18], g_out_d, W=[b_tab])

            w_in = sb("w_in", [128, 8, IN_COLS], BF16); b_win = S.buf("w_in")
            S.dma("pool", w_in[:], w_in_d.rearrange("(c p) n -> p c n", p=128), W=[b_win])
            for c in range(8):
                S.op("dve", "tensor_scalar", w_in[:, c, :], w_in[:, c, :], gvec[:, c:c + 1], None, ALU.mult,
                     R=[b_tab, b_win], W=[b_win])
            w_o = sb("w_o", [128, 8, D], BF16); b_wo = S.buf("w_o")
            S.dma("pool", w_o[:], w_o_d.rearrange("(c p) n -> p c n", p=128), W=[b_wo])
            for c in range(8):
                S.op("dve", "tensor_scalar", w_o[:, c, :], w_o[:, c, :], gvec[:, 10 + c:11 + c], None, ALU.mult,
                     R=[b_tab, b_wo], W=[b_wo])
            w_uq = sb("w_uq", [128, 2, 768], BF16); b_wuq = S.buf("w_uq")
            S.dma("pool", w_uq[:], w_uq_d.rearrange("(c p) n -> p c n", p=128), W=[b_wuq])
            for c in range(2):
                S.op("dve", "tensor_scalar", w_uq[:, c, :], w_uq[:, c, :], gvec[:, 8 + c:9 + c], 96.0 ** -0.5,
                     ALU.mult, ALU.mult, R=[b_tab, b_wuq], W=[b_wuq])
            wukT = sb("wukT", [64, 8, 128], BF16); wuv = sb("wuv", [128, 8, 64], BF16); b_wkv = S.buf("wkv")
            S.dma("pool", wukT[:], wukT_d, W=[b_wkv])
            S.dma("pool", wuv[:], wuv_d, W=[b_wkv])
            w1 = sb("w1", [64, 2, 32, 128], BF16); b_w1 = S.buf("w1")
            for kv in range(2):
                S.dma("pool", w1[:, kv], w1_d[kv], W=[b_w1])
            peT = sb("peT", [64, 2, 32], BF16)
            for kv in range(2):
                S.dma("pool", peT[:, kv], peT_d[kv], W=[b_w1])
            w2 = sb("w2", [128, 2, 64], BF16)
            S.dma("pool", w2[:], w2_d, W=[b_w1])

            bias_tot = sb("bias_tot", [128, 2], F32); b_bt = S.buf("bias_tot")
            for kv in range(2):
                for l in range(32):
                    S.op("pe", "matmul", pM[0][:, kv:kv + 1], lhsT=w1[:, kv, l, :], rhs=peT[:, kv, l:l + 1],
                         start=(l == 0), stop=(l == 31), R=[b_w1], W=[bpM[0]])
            S.op("dve", "tensor_tensor", bias_tot[:], pM[0][:, 0:2], b1[:], ALU.add, R=[bpM[0], b_tab], W=[b_bt])

            KlatT = sb("KlatT", [128, SEQ], BF16); bKlat = S.bufs("Klat", NT)
            KpeT = sb("KpeT", [32, SEQ], BF16); bKpe = S.bufs("Kpe", NT)
            Clat = sb("Clat", [128, NT, 130], BF16); bClat = S.bufs("Clat", NT)
            KsE = sb("KsE", [128, 2, SEQ], BF16); bKs = S.bufs("Ks", NT); b_exp = S.buf("expand")
            Vs = sb("Vs", [128, NT, 2, 65], BF16); bVs = S.bufs("Vs", NT)
            KwT = sb("KwT", [64, 2, 8 * 128], BF16); bKw = S.bufs("Kw", 8)
            Vw = sb("Vw", [128, 8, 2, 65], BF16); bVw = S.bufs("Vw", 8)
            KcT = sb("KcT", [64, 2, 256], BF16); b_Kc = S.buf("Kc")
            VcT = sb("VcT", [64, 2, 256], BF16); b_VcT = S.buf("VcT")
            VcO = sb("VcO", [128, 2, 2, 128], BF16); b_VcO = S.buf("VcO")
            for g in range(2):
                S.dma("pool", KsE[64:128, g, :], exp_d, W=[b_exp])
            for nt in range(2):
                for g in range(2):
                    S.dma("pool", VcO[:, nt, g, 64:128], ovl_d[:, nt, :], W=[b_VcO])
            S.op("pool", "memset", Clat[:, :, 128:130], 1.0, W=bClat)
            S.op("pool", "memset", Vs[:, :, :, 64:65], 1.0, W=bVs)
            S.op("pool", "memset", Vw[:, :, :, 64:65], 1.0, W=bVw)

            xs = [sb("xs%d" % i, [128, D], F32) for i in range(2)]; b_xs = S.bufs("xs", 2)
            st = sb("st", [128, 64], F32); b_st = S.buf("st")
            xn = sb("xn", [128, D], BF16); b_xn = S.buf("xn")
            xnT = sb("xnT", [128, 8, 128], BF16); b_xnT = S.buf("xnT")
            u = sb("u", [128, IN_COLS], F32); b_u = S.buf("u")
            cqn = sb("cqn", [128, 256], BF16); b_cqn = S.buf("cqn")
            cqnT = sb("cqnT", [128, 2, 128], BF16); b_cqnT = S.buf("cqnT")
            q_sb = sb("q_sb", [128, 8, 96], F32); b_q = S.buf("q")
            qn_sb = sb("qn_sb", [128, 8, 64], BF16); b_qn = S.buf("qn")
            qpe_sb = sb("qpe_sb", [128, 8, 32], BF16); b_qpe = S.buf("qpe")
            rt = [sb("rt%d" % i, [128, 8, 16], F32) for i in range(4)]; b_rt = S.buf("rt")
            QnT = sb("QnT", [64, 8, 128], BF16); b_QnT = S.buf("QnT")
            QpeT2 = [sb("QpeT%d" % i, [32, 8, 128], BF16) for i in range(2)]; b_QpeT2 = S.bufs("QpeT", 2)
            QabsT2 = [sb("QabsT%d" % i, [128, 8, 128], BF16) for i in range(2)]; b_Qabs2 = S.bufs("Qabs", 2)
            kpe_sb = sb("kpe_sb", [128, 32], BF16); b_kpe = S.buf("kpe")
            qnsa_sb = sb("qnsa_sb", [128, 8, 64], BF16); b_qnsa = S.buf("qnsa")
            QS2 = [sb("QS%d" % i, [128, 2, 4, 128], BF16) for i in range(2)]; b_QSq2 = S.bufs("QSq", 2); b_QSs2 = [S.bufs("QSs", 2) for _ in range(2)]
            ks_sb = sb("ks_sb", [128, 2, 64], BF16); b_ks = S.buf("ks")
            kw_sb = sb("kw_sb", [128, 2, 64], BF16); b_kw = S.buf("kw")
            raw_sb = sb("raw_sb", [128, 256], BF16); b_raw = S.buf("raw")
            rawT = [sb("rawT%d" % i, [64, 2, 2, 144], BF16) for i in range(2)]; b_rawT = S.bufs("rawT", 2)
            gate2 = [sb("gate%d" % i, [128, 3, 8], F32) for i in range(2)]; b_gate2 = S.bufs("gate", 2)
            z_sb = sb("z_sb", [128, 32], F32); z2_sb = sb("z2_sb", [128, 32], F32); b_z = S.buf("z")
            hid_sb = sb("hid_sb", [128, 32], BF16); b_hid = S.buf("hid")
            kc_f = sb("kc_f", [8, 2, 64], F32); kc_sb = sb("kc_sb", [8, 2, 64], BF16); b_kc = S.buf("kc")
            PT = [sb("PT%d" % i, [128, 512], BF16) for i in range(3)]; b_PT = S.bufs("PT", 3)
            PcT = [sb("PcT%d" % i, [128, 512], BF16) for i in range(2)]; b_PcT = S.bufs("PcT", 2)
            OlatT = sb("OlatT", [128, 4, 128], BF16); b_OlatT = S.buf("OlatT")
            y_sb = sb("y_sb", [128, D], F32); b_y = S.buf("y"); b_yn = S.buf("yn")
            imp = sb("imp", [128, 64], F32); sc1 = sb("sc1", [128, 64], F32); sc2 = sb("sc2", [128, 64], F32)
            sc3 = sb("sc3", [128, 64], F32); b_imp = S.buf("imp")
            selq = sb("selq", [128, 128], BF16); b_selq = S.buf("selq")
            mixed = sb("mixed", [128, D], BF16); b_mixed = S.buf("mixed")
            mixedT = sb("mixedT", [128, 8, 128], BF16); b_mixedT = S.buf("mixedT")
            h_sb = [sb("h_sb%d" % i, [128, D], F32) for i in range(2)]; b_h = S.bufs("h", 2)

            S.op("pool", "memset", selq[:], 0.0, W=[b_selq])
            for i in range(2):
                S.op("pool", "memset", rawT[i][:], 0.0, W=[b_rawT[i]])
            S.op("pool", "memset", KcT[:], 0.0, W=[b_Kc])
            S.op("pool", "memset", VcT[:], 0.0, W=[b_VcT])
            S.op("pool", "memset", VcO[:, :, :, 0:64], 0.0, W=[b_VcO])

            b_stc = {0: S.buf("st0"), 4: S.buf("st4"), 12: S.buf("st12")}
            b_stm = S.buf("stm")
            b_stn = S.buf("stn")
            rl = sb("rl", [128, 8], F32); b_rl = S.buf("rl")
            lacc = sb("lacc", [128, 512], F32); b_lacc = S.buf("lacc")
            ones_f = sb("ones_f", [128, 1], F32); b_ones = S.buf("ones")
            S.op("pool", "memset", ones_f[:], 1.0, W=[b_ones])

            def rstd_multi(items, col, Rb, jk, b_jk):
                b_st = b_stc[col]
                k = len(items)
                S.op("dve", "memset", st[:, col:col + k], 0.0, W=[b_st])
                for i_, (src_ap, n) in enumerate(items):
                    S.op("dve", "scalar_tensor_tensor", jk[:, 0:n], src_ap, 1.0, src_ap, ALU.mult, ALU.mult,
                         accum_out=st[:, col + i_:col + i_ + 1], R=Rb + [b_st], W=[b_jk, b_st])
                    S.op("dve", "tensor_scalar", st[:, col + k + i_:col + k + i_ + 1], st[:, col + i_:col + i_ + 1], 1.0 / n, EPS,
                         ALU.mult, ALU.add, R=[b_st], W=[b_st])
                S.op("act", "activation", st[:, col + 2 * k:col + 3 * k], st[:, col + k:col + 2 * k], AF.Sqrt, R=[b_st], W=[b_st])
                S.op("dve", "reciprocal", st[:, col + 3 * k:col + 4 * k], st[:, col + 2 * k:col + 3 * k], R=[b_st], W=[b_st])
                return [st[:, col + 3 * k + i_:col + 3 * k + i_ + 1] for i_ in range(k)]

            def rope(eng, out_ap, in_ap, cos_ap, sin_ap, nh, half, Rb, Wb):
                P = in_ap.shape[0]
                x1 = in_ap[:, :, 0:half]
                x2 = in_ap[:, :, half:2 * half]
                cb = cos_ap[:, None, :].to_broadcast([P, nh, half])
                sbb = sin_ap[:, None, :].to_broadcast([P, nh, half])
                t = [r_[0:P, 0:nh, 0:half] for r_ in rt]
                S.op(eng, "tensor_tensor", t[0], x1, cb, ALU.mult, R=Rb + [b_tab], W=[b_rt])
                S.op(eng, "tensor_tensor", t[1], x2, sbb, ALU.mult, R=Rb + [b_tab], W=[b_rt])
                S.op(eng, "tensor_tensor", t[2], x2, cb, ALU.mult, R=Rb + [b_tab], W=[b_rt])
                S.op(eng, "tensor_tensor", t[3], x1, sbb, ALU.mult, R=Rb + [b_tab], W=[b_rt])
                S.op(eng, "tensor_tensor", out_ap[:, :, 0:half], t[0], t[1], ALU.subtract, R=[b_rt], W=Wb)
                S.op(eng, "tensor_tensor", out_ap[:, :, half:2 * half], t[2], t[3], ALU.add, R=[b_rt], W=Wb)

            def transpose_to(ps_i, col0, in_ap, Rb):
                P, Fd = in_ap.shape[0], in_ap.shape[1]
                S.op("pe", "transpose", pT[ps_i][0:Fd, col0:col0 + P], in_ap, ident[0:P, 0:P],
                     R=Rb + [b_ident], W=[bpT[ps_i]])

            def phase1(s, t):
                par = t % 2
                QpeT, b_QpeT = QpeT2[par], b_QpeT2[par]
                QabsT, b_Qabs = QabsT2[par], b_Qabs2[par]
                QS, b_QSq, b_QSs = QS2[par], b_QSq2[par], b_QSs2[par]
                gate, b_gate = gate2[par], b_gate2[par]
                xb = t % 2
                S.dma("sp", xs[xb][:], x_d[s, t * 128:(t + 1) * 128, :], W=[b_xs[xb]])
                r0 = rstd_multi([(xs[xb][:], D)], 0, [b_xs[xb]], xn, b_xn)[0]
                S.op("dve", "tensor_scalar", xn[:], xs[xb][:], r0, None, ALU.mult, R=[b_xs[xb], b_stc[0]], W=[b_xn])
                ti = nxt("T", 2)
                for c in range(8):
                    transpose_to(ti, c * 128, xn[:, c * 128:(c + 1) * 128], [b_xn])
                S.op("dve", "tensor_copy", xnT[:], pT[ti][:, 0:1024].rearrange("p (c n) -> p c n", c=8), R=[bpT[ti]], W=[b_xnT])
                for cg, (c0, c1) in enumerate(((0, 512), (512, 1024), (1024, 1536), (1536, IN_COLS))):
                    mi = nxt("M", 2)
                    for c in range(8):
                        S.op("pe", "matmul", pM[mi][:, 0:c1 - c0], lhsT=xnT[:, c, :], rhs=w_in[:, c, c0:c1],
                             start=(c == 0), stop=(c == 7), R=[b_xnT, b_win], W=[bpM[mi]])
                    S.op("dve", "tensor_copy",
                         u[:, c0:c1], pM[mi][:, 0:c1 - c0], R=[bpM[mi]], W=[b_u])
                    yield

                yield
                r1, r2 = rstd_multi([(u[:, 0:256], 256), (u[:, 256:384], 128)], 4, [b_u], cqn, b_cqn)
                S.op("dve", "tensor_scalar", cqn[:], u[:, 0:256], r1, None, ALU.mult, R=[b_u, b_stc[4]], W=[b_cqn])
                ti = nxt("T", 2)
                for c in range(2):
                    transpose_to(ti, c * 128, cqn[:, c * 128:(c + 1) * 128], [b_cqn])
                S.op("dve", "tensor_copy", cqnT[:], pT[ti][:, 0:256].rearrange("p (c n) -> p c n", c=2), R=[bpT[ti]], W=[b_cqnT])
                yield
                for half in range(2):
                    mi = nxt("M", 2)
                    for c in range(2):
                        S.op("pe", "matmul", pM[mi][:, 0:384], lhsT=cqnT[:, c, :], rhs=w_uq[:, c, half * 384:(half + 1) * 384],
                             start=(c == 0), stop=(c == 1), R=[b_cqnT, b_wuq], W=[bpM[mi]])
                    S.op("dve", "tensor_copy", q_sb[:, half * 4:(half + 1) * 4, :],
                         pM[mi][:, 0:384].rearrange("p (h d) -> p h d", h=4), R=[bpM[mi]], W=[b_q])
                yield
                S.op("pool", "tensor_copy", qn_sb[:], q_sb[:, :, 0:64], R=[b_q], W=[b_qn])
                rope("pool", qpe_sb[:], q_sb[:, :, 64:96], tabs["cos_m"][:, t, :], tabs["sin_m"][:, t, :], 8, 16, [b_q], [b_qpe])
                ti = nxt("T", 2)
                for h in range(8):
                    transpose_to(ti, h * 128, qn_sb[:, h, :], [b_qn])
                S.op("dve", "tensor_copy", QnT[:], pT[ti][0:64, 0:1024].rearrange("p (c n) -> p c n", c=8), R=[bpT[ti]], W=[b_QnT])
                ti = nxt("T", 2)
                for h in range(8):
                    transpose_to(ti, h * 128, qpe_sb[:, h, :], [b_qpe])
                S.op("dve", "tensor_copy", QpeT[:], pT[ti][0:32, 0:1024].rearrange("p (c n) -> p c n", c=8), R=[bpT[ti]], W=[b_QpeT])
                yield
                for hg in range(2):
                    mi = nxt("M", 2)
                    for j in range(4):
                        h = hg * 4 + j
                        S.op("pe", "matmul", pM[mi][:, j * 128:(j + 1) * 128], lhsT=wukT[:, h, :],
                             rhs=QnT[:, h, :], start=True, stop=True, R=[b_wkv, b_QnT], W=[bpM[mi]])
                    S.op("dve", "tensor_copy", QabsT[:, hg * 4:(hg + 1) * 4, :],
                         pM[mi][:, 0:512].rearrange("p (c n) -> p c n", c=4), R=[bpM[mi]], W=[b_Qabs])

                yield
                S.op("dve", "scalar_tensor_tensor", Clat[:, t, 0:128], u[:, 256:384], r2, gckv[:], ALU.mult, ALU.mult,
                     R=[b_u, b_stc[4], b_tab], W=[bClat[t]])
                ti = nxt("T", 2)
                transpose_to(ti, 0, Clat[:, t, 0:128], [bClat[t]])
                S.op("dve", "tensor_copy", KlatT[:, t * 128:(t + 1) * 128], pT[ti][:, 0:128], R=[bpT[ti]], W=[bKlat[t]])
                rope("pool", kpe_sb[:, None, :], u[:, None, 384:416], tabs["cos_m"][:, t, :], tabs["sin_m"][:, t, :], 1, 16, [b_u], [b_kpe])
                ti = nxt("T", 2)
                transpose_to(ti, 0, kpe_sb[:], [b_kpe])
                S.op("dve", "tensor_copy", KpeT[:, t * 128:(t + 1) * 128], pT[ti][0:32, 0:128], R=[bpT[ti]], W=[bKpe[t]])

                yield
                qv = u[:, 416:928].rearrange("p (h d) -> p h d", h=8)
                S.op("pool", "tensor_scalar", qnsa_sb[:, :, 16:64], qv[:, :, 16:64], 0.125, None, ALU.mult, R=[b_u], W=[b_qnsa])
                rope("pool", qnsa_sb[:, :, 0:16], qv[:, :, 0:16], tabs["cos_n8"][:, t, :], tabs["sin_n8"][:, t, :], 8, 8, [b_u], [b_qnsa])
                ti = nxt("T", 2)
                for h in range(8):
                    transpose_to(ti, h * 128, qnsa_sb[:, h, :], [b_qnsa])
                S.op("dve", "tensor_copy", QS[0:64].rearrange("p g j n -> p (g j) n"),
                     pT[ti][0:64, 0:1024].rearrange("p (c n) -> p c n", c=8), R=[bpT[ti]], W=[b_QSq])
                yield
                kvv = u[:, 928:1696].rearrange("p (s g d) -> p s g d", s=6, g=2)
                S.op("pool", "tensor_copy", ks_sb[:, :, 16:64], kvv[:, 2, :, 16:64], R=[b_u], W=[b_ks])
                rope("pool", ks_sb[:, :, 0:16], kvv[:, 2, :, 0:16], tabs["cos_n"][:, t, :], tabs["sin_n"][:, t, :], 2, 8, [b_u], [b_ks])
                S.op("pool", "tensor_copy", kw_sb[:, :, 16:64], kvv[:, 4, :, 16:64], R=[b_u], W=[b_kw])
                rope("pool", kw_sb[:, :, 0:16], kvv[:, 4, :, 0:16], tabs["cos_n"][:, t, :], tabs["sin_n"][:, t, :], 2, 8, [b_u], [b_kw])
                S.op("dve", "tensor_copy", Vs[:, t, :, 0:64], kvv[:, 3], R=[b_u], W=[bVs[t]])
                S.op("dve", "tensor_copy", Vw[:, t % 8, :, 0:64], kvv[:, 5], R=[b_u], W=[bVw[t % 8]])
                ti = nxt("T", 2)
                for g in range(2):
                    transpose_to(ti, g * 128, ks_sb[:, g, :], [b_ks])
                    transpose_to(ti, 256 + g * 128, kw_sb[:, g, :], [b_kw])
                S.op("dve", "tensor_copy", KsE[0:64, :, t * 128:(t + 1) * 128],
                     pT[ti][0:64, 0:256].rearrange("p (g n) -> p g n", g=2), R=[bpT[ti]], W=[bKs[t]])
                S.op("dve", "tensor_copy", KwT[:, :, (t % 8) * 128:(t % 8 + 1) * 128],
                     pT[ti][0:64, 256:512].rearrange("p (g n) -> p g n", g=2), R=[bpT[ti]], W=[bKw[t % 8]])
                yield
                rb = t % 2
                S.op("pool", "tensor_copy", raw_sb[:], u[:, 928:1184], R=[b_u], W=[b_raw])
                if t == 0:
                    S.op("pool", "memset", rawT[rb][:, :, :, 0:16], 0.0, W=[b_rawT[rb]])
                else:
                    S.op("pool", "tensor_copy", rawT[rb][:, :, :, 0:16], rawT[1 - rb][:, :, :, 128:144],
                         R=[b_rawT[1 - rb]], W=[b_rawT[rb]])
                ti = nxt("T", 2)
                for c in range(4):
                    transpose_to(ti, c * 128, raw_sb[:, c * 64:(c + 1) * 64], [b_raw])
                S.op("dve", "tensor_copy", rawT[rb][:, :, :, 16:144],
                     pT[ti][0:64, 0:512].rearrange("p (k g n) -> p k g n", k=2, g=2), R=[bpT[ti]], W=[b_rawT[rb]])
                yield
                S.op("act", "activation", gate[:].rearrange("p b h -> p (b h)"), u[:, 1696:1720], AF.Tanh, scale=0.5, R=[b_u], W=[b_gate])
                S.op("dve", "tensor_scalar", gate[:].rearrange("p b h -> p (b h)"), gate[:].rearrange("p b h -> p (b h)"), 0.5, 0.5,
                     ALU.mult, ALU.add, R=[b_gate], W=[b_gate])

                yield
                mi = nxt("M", 2)
                for kv in range(2):
                    for g in range(2):
                        c0 = (kv * 2 + g) * 8
                        for l in range(32):
                            S.op("pe", "matmul", pM[mi][:, c0:c0 + 8], lhsT=w1[:, kv, l, :], rhs=rawT[rb][:, kv, g, l:l + 113:16],
                                 start=(l == 0), stop=(l == 31), R=[b_w1, b_rawT[rb]], W=[bpM[mi]])
                            if l % 8 == 7:
                                yield
                for kv in range(2):
                    S.op("dve", "tensor_scalar", z_sb[:, kv * 16:(kv + 1) * 16], pM[mi][:, kv * 16:(kv + 1) * 16],
                         bias_tot[:, kv:kv + 1], None, ALU.add, R=[bpM[mi], b_bt], W=[b_z])
                S.op("dve", "tensor_tensor", z2_sb[:], z_sb[:], z_sb[:], ALU.mult, R=[b_z], W=[b_z])
                S.op("dve", "tensor_scalar", z2_sb[:], z2_sb[:], 0.044715, 1.0, ALU.mult, ALU.add, R=[b_z], W=[b_z])
                S.op("dve", "tensor_tensor", z2_sb[:], z2_sb[:], z_sb[:], ALU.mult, R=[b_z], W=[b_z])
                S.op("act", "activation", z2_sb[:], z2_sb[:], AF.Tanh, scale=math.sqrt(2.0 / math.pi), R=[b_z], W=[b_z])
                S.op("dve", "tensor_scalar", z2_sb[:], z2_sb[:], 0.5, 0.5, ALU.mult, ALU.add, R=[b_z], W=[b_z])
                S.op("dve", "tensor_tensor", hid_sb[:], z_sb[:], z2_sb[:], ALU.mult, R=[b_z], W=[b_hid])
                yield
                n0 = 8 * t - 1
                m0 = 1 if t == 0 else 0
                mi = nxt("M", 2)
                for g in range(2):
                    S.op("pe", "matmul", pM[mi][0:8, g * 64:(g + 1) * 64], lhsT=hid_sb[:, g * 8:(g + 1) * 8], rhs=w2[:, 0, :],
                         start=True, stop=True, R=[b_hid, b_w1], W=[bpM[mi]])
                for g in range(2):
                    S.op("pe", "matmul", pM[mi][0:64, 128 + g * 8:136 + g * 8], lhsT=w2[:, 1, :], rhs=hid_sb[:, 16 + g * 8:24 + g * 8],
                         start=True, stop=True, R=[b_hid, b_w1], W=[bpM[mi]])
                S.op("dve", "tensor_tensor", kc_f[:].rearrange("p g d -> p (g d)"), pM[mi][0:8, 0:128], b2k[0:8, :], ALU.add,
                     R=[bpM[mi], b_tab], W=[b_kc])
                S.op("dve", "tensor_scalar", VcT[:, :, n0 + m0:n0 + 8], pM[mi][0:64, 128:144].rearrange("p (g m) -> p g m", g=2)[:, :, m0:8],
                     b2v[:, 0:1], None, ALU.add, R=[bpM[mi], b_tab], W=[b_VcT])
                S.op("pool", "tensor_copy", kc_sb[:, :, 16:64], kc_f[:, :, 16:64], R=[b_kc], W=[b_kc])
                rope("pool", kc_sb[:, :, 0:16], kc_f[:, :, 0:16], cos_e[:, t, :], sin_e[:, t, :], 2, 8, [b_kc], [b_kc])
                ti = nxt("T", 2)
                for g in range(2):
                    transpose_to(ti, g * 8, kc_sb[:, g, :], [b_kc])
                S.op("dve", "tensor_copy", KcT[:, :, n0 + m0:n0 + 8],
                     pT[ti][0:64, 0:16].rearrange("p (g m) -> p g m", g=2)[:, :, m0:8], R=[bpT[ti]], W=[b_Kc])
                yield
                nts = sorted(set([max(n0, 0) // 128, (n0 + 7) // 128]))
                for nt in nts:
                    ti = nxt("T", 2)
                    for g in range(2):
                        S.op("pe", "transpose", pT[ti][:, g * 64:(g + 1) * 64], VcT[:, g, nt * 128:(nt + 1) * 128], ident[0:64, 0:64],
                             R=[b_VcT, b_ident], W=[bpT[ti]])
                    S.op("dve", "tensor_copy", VcO[:, nt, :, 0:64], pT[ti][:, 0:128].rearrange("p (g d) -> p g d", g=2), R=[bpT[ti]], W=[b_VcO])

            def attn_loop(kts, qk_fn, post_fn):
                if not kts:
                    return
                si_next = qk_fn(kts[0])
                for i_, kt in enumerate(kts):
                    si = si_next
                    if i_ + 1 < len(kts):
                        si_next = qk_fn(kts[i_ + 1])
                    post_fn(kt, si)

            def mla(s, t):
                par = t % 2
                QpeT, b_QpeT = QpeT2[par], b_QpeT2[par]
                QabsT, b_Qabs = QabsT2[par], b_Qabs2[par]
                QS, b_QSq, b_QSs = QS2[par], b_QSq2[par], b_QSs2[par]
                gate, b_gate = gate2[par], b_gate2[par]
                mo = nxt("M", 2)
                for hg in range(2):
                    qa = QabsT[:, hg * 4:(hg + 1) * 4, :].rearrange("p c n -> p (c n)")
                    qp = QpeT[:, hg * 4:(hg + 1) * 4, :].rearrange("p c n -> p (c n)")

                    def qk(kt):
                        si = nxt("S", 2)
                        S.op("pe", "matmul", pS[si][:], lhsT=KlatT[:, kt * 128:(kt + 1) * 128], rhs=qa,
                             start=True, stop=False, R=[bKlat[kt], b_Qabs], W=[bpS[si]])
                        S.op("pe", "matmul", pS[si][:], lhsT=KpeT[:, kt * 128:(kt + 1) * 128], rhs=qp,
                             start=False, stop=True, R=[bKpe[kt], b_QpeT], W=[bpS[si]])
                        return si

                    def post(kt, si):
                        pi = nxt("P", 3)
                        S.op("act", "activation", PT[pi][:], pS[si][:], AF.Exp, R=[bpS[si]], W=[b_PT[pi]])
                        if kt == t:
                            S.op("pool", "tensor_tensor", PT[pi][:], PT[pi][:], tri4[:], ALU.mult, R=[b_PT[pi], b_msk], W=[b_PT[pi]])
                        S.op("pe", "matmul", pA[0][:], lhsT=Clat[:, kt, 0:128], rhs=PT[pi][:], start=(kt == 0), stop=(kt == t),
                             R=[b_PT[pi], bClat[kt]], W=[bpA[0]])
                        if kt == 0:
                            S.op("dve", "tensor_copy", lacc[:], PT[pi][:], R=[b_PT[pi]], W=[b_lacc])
                        else:
                            S.op("dve", "tensor_tensor", lacc[:], lacc[:], PT[pi][:], ALU.add, R=[b_PT[pi], b_lacc], W=[b_lacc])

                    attn_loop(list(range(t + 1)), qk, post)
                    for j in range(4):
                        S.op("pe", "matmul", pA[1][:, j:j + 1], lhsT=lacc[:, j * 128:(j + 1) * 128], rhs=ones_f[:, 0:1],
                             start=True, stop=True, R=[b_lacc, b_ones], W=[bpA[1]])
                    S.op("dve", "tensor_copy", OlatT[:], pA[0][:].rearrange("p (c n) -> p c n", c=4), R=[bpA[0]], W=[b_OlatT])
                    S.op("dve", "reciprocal", rl[:, hg * 4:(hg + 1) * 4], pA[1][:, 0:4], R=[bpA[1]], W=[b_rl])
                    for j in range(4):
                        h = hg * 4 + j
                        S.op("pe", "matmul", pM[mo][:, h * 64:(h + 1) * 64], lhsT=OlatT[:, j, :], rhs=wuv[:, h, :],
                             start=True, stop=True, R=[b_OlatT, b_wkv], W=[bpM[mo]])
                S.op("dve", "tensor_tensor", y_sb[:, 0:512].rearrange("p (h d) -> p h d", h=8),
                     pM[mo][:, 0:512].rearrange("p (h d) -> p h d", h=8), rl[:, 0:8, None].to_broadcast([128, 8, 64]), ALU.mult,
                     R=[bpM[mo], b_rl], W=[b_y])

            def nsa_sel(s, t, g):
                par = t % 2
                QpeT, b_QpeT = QpeT2[par], b_QpeT2[par]
                QabsT, b_Qabs = QabsT2[par], b_Qabs2[par]
                QS, b_QSq, b_QSs = QS2[par], b_QSq2[par], b_QSs2[par]
                gate, b_gate = gate2[par], b_gate2[par]
                QSg = QS[:, g].rearrange("p j n -> p (j n)")
                QSg = QS[:, g].rearrange("p j n -> p (j n)")
                nts = [0] + ([1] if t >= 16 else [])
                for nt in nts:
                    si = nxt("M", 2)
                    S.op("pe", "matmul", pM[si][:], lhsT=KcT[:, g, nt * 128:(nt + 1) * 128], rhs=QSg[0:64, :],
                         start=True, stop=True, R=[b_Kc, b_QSq], W=[bpM[si]])
                    S.op("act", "activation", PcT[nt][:], pM[si][:], AF.Exp, R=[bpM[si]], W=[b_PcT[nt]])
                    S.op("pool", "affine_select", PcT[nt][:].rearrange("p (j n) -> p j n", j=4),
                         PcT[nt][:].rearrange("p (j n) -> p j n", j=4), [[0, 4], [1, 128]], ALU.is_ge, 0.0,
                         base=128 * t - 31 - 2048 * nt, channel_multiplier=-16, R=[b_PcT[nt]], W=[b_PcT[nt]])
                yield
                mc = nxt("M", 2)
                for j in range(4):
                    for i_, nt in enumerate(nts):
                        S.op("pe", "matmul", pM[mc][:, j * 128:(j + 1) * 128], lhsT=PcT[nt][:, j * 128:(j + 1) * 128],
                             rhs=VcO[:, nt, g, :], start=(i_ == 0), stop=(i_ == len(nts) - 1),
                             R=[b_PcT[nt], b_VcO], W=[bpM[mc]])
                yield
                pc = pM[mc][:].rearrange("p (j c) -> p j c", j=4)
                S.op("dve", "tensor_reduce", st[:, 24:28], pc[:, :, 64:128], AX.X, ALU.add, R=[bpM[mc]], W=[b_stn])
                S.op("dve", "tensor_scalar", st[:, 24:28], st[:, 24:28], 1e-30, None, ALU.max, R=[b_stn], W=[b_stn])
                S.op("dve", "reciprocal", st[:, 28:32], st[:, 24:28], R=[b_stn], W=[b_stn])
                S.op("dve", "tensor_scalar", imp[:], pc[:, 0, 64:128], st[:, 28:29], None, ALU.mult, R=[bpM[mc], b_stn], W=[b_imp])
                for j in range(1, 4):
                    S.op("dve", "scalar_tensor_tensor", imp[:], pc[:, j, 64:128], st[:, 28 + j:29 + j], imp[:], ALU.mult, ALU.add,
                         R=[bpM[mc], b_stn, b_imp], W=[b_imp])
                yield
                S.op("dve", "tensor_tensor", st[:, 32:36], st[:, 28:32], gate[:, 0, g * 4:(g + 1) * 4], ALU.mult,
                     R=[b_stn, b_gate], W=[b_stn])
                for j in range(4):
                    h = g * 4 + j
                    S.op("dve", "tensor_scalar", y_sb[:, 512 + h * 64:576 + h * 64], pc[:, j, 0:64], st[:, 32 + j:33 + j], None, ALU.mult,
                         R=[bpM[mc], b_stn], W=[b_yn])
                yield
                S.op("pool", "affine_select", sc1[:], imp[:], [[-64, 64]], ALU.is_ge, 1e9, base=128 * t - 128, channel_multiplier=1,
                     R=[b_imp], W=[b_imp])
                S.op("pool", "affine_select", sc2[:], sc1[:], [[-64, 64]], ALU.is_ge, -1e9, base=128 * t, channel_multiplier=1,
                     R=[b_imp], W=[b_imp])
                S.op("pool", "memset", sc2[:, 0:1], 1e9, R=[b_imp], W=[b_imp])
                yield
                S.op("dve", "max", st[:, 40:48], sc2[:], R=[b_imp], W=[b_stn])
                S.op("dve", "match_replace", sc3[:], st[:, 40:48], sc2[:], -3e38, R=[b_imp, b_stn], W=[b_imp])
                S.op("dve", "max", st[:, 48:56], sc3[:], R=[b_imp], W=[b_stn])
                S.op("dve", "tensor_scalar", selq[:, 64:128], sc2[:], st[:, 55:56], NEG, ALU.is_lt, ALU.mult,
                     R=[b_imp, b_stn], W=[b_selq])
                yield
                ti = nxt("T", 2)
                transpose_to(ti, 0, selq[:], [b_selq])
                S.op("dve", "tensor_copy", QS[64:128, g], pT[ti][64:128, None, 0:128].to_broadcast([64, 4, 128]),
                     R=[bpT[ti]], W=[b_QSs[g]])
                yield

            def nsa_attn(s, t, g):
                par = t % 2
                QpeT, b_QpeT = QpeT2[par], b_QpeT2[par]
                QabsT, b_Qabs = QabsT2[par], b_Qabs2[par]
                QS, b_QSq, b_QSs = QS2[par], b_QSq2[par], b_QSs2[par]
                gate, b_gate = gate2[par], b_gate2[par]
                QSg = QS[:, g].rearrange("p j n -> p (j n)")
                S.op("dve", "memset", pA[0][:, 0:260], 0.0, W=[bpA[0]])
                S.op("dve", "memset", pA[1][:, 0:260], 0.0, W=[bpA[1]])
                def qk_s(kt):
                    si = nxt("S", 2)
                    S.op("pe", "matmul", pS[si][:], lhsT=KsE[:, g, kt * 128:(kt + 1) * 128], rhs=QSg,
                         start=True, stop=True, R=[bKs[kt], b_exp, b_QSq, b_QSs[g]], W=[bpS[si]])
                    return si

                def post_s(kt, si):
                    pi = nxt("P", 3)
                    S.op("act", "activation", PT[pi][:], pS[si][:], AF.Exp, R=[bpS[si]], W=[b_PT[pi]])
                    if kt == t:
                        S.op("pool", "tensor_tensor", PT[pi][:], PT[pi][:], tri4[:], ALU.mult, R=[b_PT[pi], b_msk], W=[b_PT[pi]])
                    for j in range(4):
                        S.op("pe", "matmul", pA[0][:, j * 65:j * 65 + 65], lhsT=PT[pi][:, j * 128:(j + 1) * 128],
                             rhs=Vs[:, kt, g, :], start=False, stop=(kt == t), skip_group_check=True,
                             R=[b_PT[pi], bVs[kt]], W=[bpA[0]])

                def qk_w(kt):
                    si = nxt("S", 2)
                    sl = kt % 8
                    S.op("pe", "matmul", pS[si][:], lhsT=KwT[:, g, sl * 128:(sl + 1) * 128], rhs=QSg[0:64, :],
                         start=True, stop=True, R=[bKw[sl], b_QSq], W=[bpS[si]])
                    return si

                def post_w(kt, si):
                    sl = kt % 8
                    pi = nxt("P", 3)
                    S.op("act", "activation", PT[pi][:], pS[si][:], AF.Exp, R=[bpS[si]], W=[b_PT[pi]])
                    if kt == t:
                        S.op("pool", "tensor_tensor", PT[pi][:], PT[pi][:], tri4[:], ALU.mult, R=[b_PT[pi], b_msk], W=[b_PT[pi]])
                    if kt == t - 4:
                        S.op("pool", "tensor_tensor", PT[pi][:], PT[pi][:], anti4[:], ALU.mult, R=[b_PT[pi], b_msk], W=[b_PT[pi]])
                    for j in range(4):
                        S.op("pe", "matmul", pA[1][:, j * 65:j * 65 + 65], lhsT=PT[pi][:, j * 128:(j + 1) * 128],
                             rhs=Vw[:, sl, g, :], start=False, stop=(kt == t), skip_group_check=True,
                             R=[b_PT[pi], bVw[sl]], W=[bpA[1]])

                attn_loop(list(range(t + 1)), qk_s, post_s)
                attn_loop(list(range(max(0, t - 4), t + 1)), qk_w, post_w)
                for br in range(2):
                    pa = pA[br][:, 0:260].rearrange("p (j c) -> p j c", j=4)
                    S.op("dve", "reciprocal", st[:, 56:60], pa[:, :, 64], R=[bpA[br]], W=[b_stn])
                    S.op("dve", "tensor_tensor", st[:, 60:64], st[:, 56:60], gate[:, 1 + br, g * 4:(g + 1) * 4], ALU.mult,
                         R=[b_stn, b_gate], W=[b_stn])
                    for j in range(4):
                        h = g * 4 + j
                        S.op("dve", "scalar_tensor_tensor", y_sb[:, 512 + h * 64:576 + h * 64], pa[:, j, 0:64], st[:, 60 + j:61 + j],
                             y_sb[:, 512 + h * 64:576 + h * 64], ALU.mult, ALU.add, R=[bpA[br], b_stn, b_yn], W=[b_yn])


            def outproj(s, t):
                xb = t % 2
                ra, rb_ = rstd_multi([(y_sb[:, 0:512], 512), (y_sb[:, 512:1024], 512)], 12, [b_y, b_yn], mixed, b_mixed)
                S.op("dve", "tensor_scalar", mixed[:, 0:512], y_sb[:, 0:512], ra, None, ALU.mult, R=[b_y, b_stc[12]], W=[b_mixed])
                S.op("dve", "tensor_scalar", mixed[:, 512:1024], y_sb[:, 512:1024], rb_, None, ALU.mult, R=[b_yn, b_stc[12]], W=[b_mixed])
                ti = nxt("T", 2)
                for c in range(8):
                    transpose_to(ti, c * 128, mixed[:, c * 128:(c + 1) * 128], [b_mixed])
                S.op("dve", "tensor_copy", mixedT[:], pT[ti][:, 0:1024].rearrange("p (c n) -> p c n", c=8), R=[bpT[ti]], W=[b_mixedT])
                for dh in range(2):
                    mi = nxt("S", 2)
                    for c in range(8):
                        S.op("pe", "matmul", pS[mi][:], lhsT=mixedT[:, c, :], rhs=w_o[:, c, dh * 512:(dh + 1) * 512],
                             start=(c == 0), stop=(c == 7), R=[b_mixedT, b_wo], W=[bpS[mi]])
                    S.op("dve", "tensor_tensor", h_sb[xb][:, dh * 512:(dh + 1) * 512], pS[mi][:], xs[xb][:, dh * 512:(dh + 1) * 512],
                         ALU.add, R=[bpS[mi], b_xs[xb]], W=[b_h[xb]])
                row = (s * SEQ + t * 128)
                S.dma("sp", hscr_d[row:row + 128, :], h_sb[xb][:], R=[b_h[xb]])

            bgst = {"gen": None, "credit": 0.0, "rate": 0.0}

            def bg_run(n=None):
                if bgst["gen"] is None:
                    return
                mode["bg"] = True
                try:
                    k = 0
                    while n is None or k < n:
                        next(bgst["gen"])
                        k += 1
                except StopIteration:
                    bgst["gen"] = None
                mode["bg"] = False

            def tick():
                if mode["bg"] or bgst["gen"] is None:
                    return
                bgst["credit"] += bgst["rate"]
                if bgst["credit"] >= 1.0:
                    n = int(bgst["credit"])
                    bgst["credit"] -= n
                    bg_run(n)

            S.tick = tick
            def chain(*gens):
                for g_ in gens:
                    yield from g_

            def set_bg(gen, nchunks, fg_ops):
                bgst["gen"] = gen
                bgst["credit"] = 0.0
                bgst["rate"] = nchunks / (0.7 * fg_ops)

            for s in range(nseq):
                bgst["gen"] = phase1(s, 0) if "p1" in stages else None
                bg_run(None)
                for t in range(ntiles):
                    if "nsa" in stages:
                        set_bg(chain(nsa_sel(s, t, 0), nsa_sel(s, t, 1)), 16.0, 30.0 + 18.0 * (t + 1))
                    if "mla" in stages:
                        mla(s, t)
                    bg_run(None)
                    if t + 1 < ntiles and "p1" in stages:
                        set_bg(phase1(s, t + 1), 40.0, 100.0 + 14.0 * (t + 1 + min(t + 1, 5)))
                    if "nsa" in stages:
                        nsa_attn(s, t, 0)
                        nsa_attn(s, t, 1)
                    if "out" in stages:
                        outproj(s, t)
                    bg_run(None)
            S.tick = None
            S.barrier()
        mode["B"] = True
        B = ExitStack()
        with B:
            def sb2(name, shape, dt):
                return B.enter_context(nc.sbuf_tensor("b_" + name, shape, dt))
            gb = sb2("gb", [128, 8], F32); b_gb = S.buf("gb")
            S.dma("sp", gb[:], g_mlp_d, W=[b_gb])
            gfin = sb2("gfin", [128, D], F32)
            S.dma("sp", gfin[:], gfin_d, W=[b_gb])
            ident2 = sb2("ident2", [128, 128], BF16); b_id2 = S.buf("ident2")
            S.dma("pool", ident2[:], ident_d, W=[b_id2])
            w_up = sb2("w_up", [128, 8, DFF], BF16); b_wup = S.buf("w_up")
            for c in range(8):
                S.dma("pool", w_up[:, c, :], w_up_d[c * 128:(c + 1) * 128, :], W=[b_wup])
                S.op("dve", "tensor_scalar", w_up[:, c, :], w_up[:, c, :], gb[:, c:c + 1], None, ALU.mult, R=[b_gb, b_wup], W=[b_wup])
            w_dn = sb2("w_dn", [128, 32, D], BF16); b_wdn = S.buf("w_dn")
            wdv = w_down_d.rearrange("(f p) n -> p f n", p=128)
            for f4 in range(8):
                S.dma("pool", w_dn[:, f4 * 4:(f4 + 1) * 4, :], wdv[:, f4 * 4:(f4 + 1) * 4, :], W=[b_wdn])
            hin = [sb2("hin%d" % i, [128, D], F32) for i in range(4)]; b_hin = S.bufs("hin", 4)
            st2 = sb2("st2", [128, 16], F32); b_st2 = S.buf("st2")
            junk2 = sb2("junk2", [128, D], BF16); b_junk2 = S.buf("junk2")
            hn = sb2("hn", [128, D], BF16); b_hn = S.buf("hn")
            hnT = sb2("hnT", [128, 8, 512], BF16); b_hnT = S.buf("hnT")
            rl = [sb2("rl%d" % i, [128, 512], BF16) for i in range(2)]; b_rl = S.bufs("rl", 2)
            aT = sb2("aT", [128, 32, 512], BF16); b_aT = S.buf("aT")
            yo = [sb2("yo%d" % i, [128, D], F32) for i in range(2)]; b_yo = S.bufs("yo", 2)
            S.op("pool", "memset", st2[:], 0.0, W=[b_st2])
            nT = (nseq * ntiles * 128) // 512 if do_mlp else 0

            def rstd2(src_ap, Rb):
                S.op("pool", "memset", st2[:, 0:1], 0.0, W=[b_st2])
                S.op("act", "activation", junk2[:], src_ap, AF.Square, accum_out=st2[:, 0:1], R=Rb, W=[b_junk2, b_st2])
                S.op("dve", "tensor_scalar", st2[:, 1:2], st2[:, 0:1], 1.0 / D, EPS, ALU.mult, ALU.add, R=[b_st2], W=[b_st2])
                S.op("act", "activation", st2[:, 2:3], st2[:, 1:2], AF.Sqrt, R=[b_st2], W=[b_st2])
                S.op("dve", "reciprocal", st2[:, 3:4], st2[:, 2:3], R=[b_st2], W=[b_st2])
                return st2[:, 3:4]

            oc = 0
            for T in range(nT):
                hb = T % 2
                row = T * 512 if nseq * ntiles * 128 == nseq * SEQ else None
                base = (T * 512 // (ntiles * 128)) * SEQ + (T * 512) % (ntiles * 128)
                for i in range(4):
                    S.dma("sp", hin[i][:], hscr_d[base + i * 128:base + (i + 1) * 128, :], W=[b_hin[i]])
                for i in range(4):
                    r = rstd2(hin[i][:], [b_hin[i]])
                    S.op("dve", "tensor_scalar", hn[:], hin[i][:], r, None, ALU.mult, R=[b_hin[i], b_st2], W=[b_hn])
                    ti = nxt("T", 2)
                    for c in range(8):
                        S.op("pe", "transpose", pT[ti][:, c * 128:(c + 1) * 128], hn[:, c * 128:(c + 1) * 128], ident2[:],
                             R=[b_hn, b_id2], W=[bpT[ti]])
                    S.op("act", "copy", hnT[:, :, i * 128:(i + 1) * 128], pT[ti][:, 0:1024].rearrange("p (c n) -> p c n", c=8),
                         R=[bpT[ti]], W=[b_hnT])
                for f in range(32):
                    si = nxt("S", 2)
                    for c in range(8):
                        S.op("pe", "matmul", pS[si][:], lhsT=w_up[:, c, f * 128:(f + 1) * 128], rhs=hnT[:, c, :],
                             start=(c == 0), stop=(c == 7), R=[b_wup, b_hnT], W=[bpS[si]])
                    ri = f % 2
                    S.op("act", "activation", rl[ri][:], pS[si][:], AF.Relu, R=[bpS[si]], W=[b_rl[ri]])
                    S.op("pool", "tensor_tensor", aT[:, f, :], rl[ri][:], rl[ri][:], ALU.mult, R=[b_rl[ri]], W=[b_aT])
                for i in range(4):
                    ob = oc % 2
                    oc += 1
                    for dh in range(2):
                        mi = nxt("M", 2)
                        for f in range(32):
                            S.op("pe", "matmul", pM[mi][:], lhsT=aT[:, f, i * 128:(i + 1) * 128], rhs=w_dn[:, f, dh * 512:(dh + 1) * 512],
                                 start=(f == 0), stop=(f == 31), R=[b_aT, b_wdn], W=[bpM[mi]])
                        S.op("dve", "tensor_tensor", yo[ob][:, dh * 512:(dh + 1) * 512], pM[mi][:], hin[i][:, dh * 512:(dh + 1) * 512],
                             ALU.add, R=[bpM[mi], b_hin[i]], W=[b_yo[ob]])
                    r = rstd2(yo[ob][:], [b_yo[ob]])
                    S.op("dve", "scalar_tensor_tensor", yo[ob][:], yo[ob][:], r, gfin[:], ALU.mult, ALU.mult,
                         R=[b_yo[ob], b_st2, b_gb], W=[b_yo[ob]])
                    S.dma("sp", out_d[base + i * 128:base + (i + 1) * 128, :], yo[ob][:], R=[b_yo[ob]])
            S.emit()
            print('NOPS', S.nops)
    return nc


def _rope_tab(pos, dim):
    inv = np.exp(np.float32(-math.log(500000.0)) * np.arange(0, dim, 2, dtype=np.float32) / np.float32(dim)).astype(np.float32)
    ang = pos.astype(np.float32)[:, None] * inv[None, :]
    return np.cos(ang).astype(np.float32), np.sin(ang).astype(np.float32)


def _tok_major(a):
    return np.ascontiguousarray(a.reshape(NT, 128, -1).transpose(1, 0, 2))


def host_consts():
    pos = np.arange(SEQ)
    cm, sm = _rope_tab(pos, 32)
    cn, sn = _rope_tab(pos, 16)
    c = {}
    c["cos_m"], c["sin_m"] = _tok_major(cm), _tok_major(sm)
    c["cos_n"], c["sin_n"] = _tok_major(cn), _tok_major(sn)
    c["cos_n8"], c["sin_n8"] = _tok_major(cn * np.float32(0.125)), _tok_major(sn * np.float32(0.125))
    ce = np.zeros((8, NT, 8), np.float32)
    se = np.zeros((8, NT, 8), np.float32)
    for t in range(NT):
        for m in range(8):
            n = 8 * t - 1 + m
            if 0 <= n < 255:
                p = 16 * n + 31
                ce[m, t], se[m, t] = cn[p], sn[p]
    c["cos_e"], c["sin_e"] = ce, se
    k = np.arange(128)[:, None]
    q = np.arange(128)[None, :]
    tri = (q >= k).astype(np.float32)
    anti = (q < k).astype(np.float32)
    c["tri4"] = np.ascontiguousarray(np.tile(tri, (1, 4)))
    c["anti4"] = np.ascontiguousarray(np.tile(anti, (1, 4)))
    n = np.arange(256)[:, None] * 16
    j = np.arange(64)[None, :] * 64
    ov = np.clip(np.minimum(n + 32, j + 64) - np.maximum(n, j), 0, None).astype(np.float32) / 32.0
    ov[255] = 0.0
    c["ovl"] = np.ascontiguousarray(ov.reshape(2, 128, 64).transpose(1, 0, 2))
    c["expand"] = (np.arange(SEQ)[None, :] // 64 == np.arange(64)[:, None]).astype(np.float32)
    c["ident"] = np.eye(128, dtype=np.float32)
    return c


def host_weights(inp):
    f = lambda a: np.ascontiguousarray(np.asarray(a, dtype=np.float32))
    pc = lambda v: np.ascontiguousarray(np.asarray(v, np.float32).reshape(-1, 128).T)
    w = {}
    w["w_in"] = f(inp["w_in"][0])
    w["g_mix"] = pc(inp["g_mix_norm"][0])
    w["w_uq"] = f(inp["w_uq"][0])
    w["g_cq"] = pc(inp["g_cq"][0])
    wukv = np.asarray(inp["w_ukv"][0], np.float32).reshape(128, 8, 2, 64)
    wuk = wukv[:, :, 0, :]
    w["wukT"] = np.ascontiguousarray(wuk.transpose(2, 1, 0))
    w["wuv"] = np.ascontiguousarray(wukv[:, :, 1, :])
    w["gckv_bc"] = np.ascontiguousarray(np.broadcast_to(np.asarray(inp["g_ckv"][0], np.float32)[None, :], (128, 128)))
    w1 = np.stack([np.asarray(inp["cmp_w1_k"][0], np.float32), np.asarray(inp["cmp_w1_v"][0], np.float32)])
    w["cmp_w1"] = np.ascontiguousarray(w1.reshape(2, 32, 64, 128).transpose(0, 2, 1, 3))
    pe = np.stack([np.asarray(inp["cmp_pe_k"][0], np.float32), np.asarray(inp["cmp_pe_v"][0], np.float32)])
    w["cmp_peT"] = np.ascontiguousarray(pe.transpose(0, 2, 1))
    w["cmp_b1"] = np.ascontiguousarray(np.stack([inp["cmp_b1_k"][0], inp["cmp_b1_v"][0]], axis=1).astype(np.float32))
    w["cmp_w2"] = np.ascontiguousarray(np.stack([inp["cmp_w2_k"][0], inp["cmp_w2_v"][0]], axis=1).astype(np.float32))
    b2k = np.asarray(inp["cmp_b2_k"][0], np.float32)
    w["cmp_b2k_bc"] = np.ascontiguousarray(np.broadcast_to(np.concatenate([b2k, b2k])[None, :], (128, 128)))
    w["cmp_b2v"] = np.ascontiguousarray(np.asarray(inp["cmp_b2_v"][0], np.float32).reshape(64, 1))
    w["w_o"] = f(inp["w_o"][0])
    w["g_out"] = pc(np.concatenate([np.asarray(inp["g_out_mla"][0]), np.asarray(inp["g_out_nsa"][0])]))
    w["w_up"] = f(inp["w_up"][0])
    w["g_mlp"] = pc(inp["g_mlp_norm"][0])
    w["w_down"] = f(inp["w_down"][0])
    w["gfin_bc"] = np.ascontiguousarray(np.broadcast_to(np.asarray(inp["g_final"], np.float32)[None, :], (128, D)))
    return w


_NC_CACHE = {}


def kernel(**inputs):
    x = np.asarray(inputs["x"], dtype=np.float32)
    shared = host_consts()
    shared.update(host_weights(inputs))
    if "full" not in _NC_CACHE:
        _NC_CACHE["full"] = build_program()
    nc = _NC_CACHE["full"]
    in_maps = []
    for c in range(NCORES):
        m = dict(shared)
        m["x"] = np.ascontiguousarray(x[c * NSEQ:(c + 1) * NSEQ])
        in_maps.append(m)
    res = run_bass_kernel_spmd(nc, in_maps, core_ids=list(range(NCORES)))
    outs = [np.asarray(r["out"]).reshape(NSEQ, SEQ, D) for r in res.results]
    return np.concatenate(outs, axis=0).astype(np.float32)
```

```python
import math
import numpy as np
from contextlib import ExitStack
import concourse.bass as bass
import concourse.mybir as mybir
from concourse.bass_utils import run_bass_kernel_spmd

F32 = mybir.dt.float32
BF16 = mybir.dt.bfloat16
I32 = mybir.dt.int32
AF = mybir.ActivationFunctionType
ALU = mybir.AluOpType
AX = mybir.AxisListType

NCORES = 8
SEQ = 4096
D = 1024
NSEQ = 2
NT = SEQ // 128
EPS = 1e-6
IN_COLS = 1720
DFF = 4096
NEG = -30000.0
STRICT_SAME = True
OP_LIMIT = None
BG_OVERLAP = True
USE_MAGIC = True


class Buf:
    __slots__ = ("name", "w", "r", "dsem", "dcnt", "excl")

    def __init__(self, name):
        self.name = name
        self.excl = False
        self.w = None
        self.r = {}
        self.dsem = None
        self.dcnt = 0


class Sched:
    ENG = ("pe", "act", "dve", "pool", "sp")

    def __init__(self, nc, ctx):
        self.nc = nc
        self.ctx = ctx
        self.sem = {e: ctx.enter_context(nc.semaphore("s_" + e)) for e in self.ENG}
        self.cnt = {e: 0 for e in self.ENG}
        self.seen = {e: {} for e in self.ENG}
        self.prog = {e: [] for e in self.ENG}
        self.dbufs = []
        self.nb = 0
        self.nops = 0
        self.fillregs = {}
        self.tick = None
        self.limit = OP_LIMIT

    def buf(self, name):
        self.nb += 1
        return Buf("%s_%d" % (name, self.nb))

    def bufs(self, name, n):
        return [self.buf(name) for _ in range(n)]

    def _dsem(self, b):
        if b.dsem is None:
            b.dsem = self.ctx.enter_context(self.nc.semaphore("d_" + b.name))
            self.dbufs.append(b)
        return b.dsem

    def _deps(self, e, reads, writes, strict):
        toks = []
        for b in reads:
            if b.w is not None:
                toks.append(b.w)
            if b.excl:
                toks.extend(b.r.values())
        for b in writes:
            if b.w is not None:
                toks.append(b.w)
            toks.extend(b.r.values())
        need = {}
        for (key, sem, val) in toks:
            if key == e and not strict and (e == "pe" or not STRICT_SAME):
                continue
            if self.seen[e].get(key, 0) >= val:
                continue
            if key not in need or need[key][1] < val:
                need[key] = (sem, val)
        for key, (sem, val) in need.items():
            self.seen[e][key] = val
            self.prog[e].append(("wait", sem, val))

    def op(self, e, meth, *args, R=(), W=(), **kw):
        self.nops += 1
        if self.limit is not None and self.nops > self.limit:
            return None
        self._deps(e, R, W, False)
        self.cnt[e] += 1
        tok = (e, self.sem[e], self.cnt[e])
        self.prog[e].append(("op", meth, args, kw))
        for b in R:
            b.r[e] = tok
        for b in W:
            b.w = tok
            b.r = {}
        if self.tick is not None:
            self.tick()
        return tok

    def dma(self, q, out, in_, R=(), W=(), **kw):
        self.nops += 1
        if self.limit is not None and self.nops > self.limit:
            return None
        self._deps(q, R, W, True)
        owner = W[0] if W else R[0]
        sem = self._dsem(owner)
        owner.dcnt += 16
        tok = ("d_" + owner.name, sem, owner.dcnt)
        self.prog[q].append(("dma", out, in_, kw, sem))
        for b in R:
            b.r[tok[0]] = tok
        for b in W:
            b.w = tok
            b.r = {}
        return tok

    def barrier(self):
        toks = [(e, self.sem[e], self.cnt[e]) for e in self.ENG if self.cnt[e] > 0]
        toks += [("d_" + b.name, b.dsem, b.dcnt) for b in self.dbufs if b.dcnt > 0]
        for e in self.ENG:
            for (key, sem, val) in toks:
                if self.seen[e].get(key, 0) >= val:
                    continue
                self.seen[e][key] = val
                self.prog[e].append(("wait", sem, val))

    def flush(self):
        nc = self.nc
        with nc.Block() as block:
            def replay(e):
                def f(eng):
                    sem_e = self.sem[e]
                    for it in self.prog[e]:
                        if it[0] == "wait":
                            eng.wait_ge(it[1], it[2])
                        elif it[0] == "op":
                            args = it[2]
                            if it[1] == "affine_select":
                                args = list(args)
                                if args[4] not in self.fillregs:
                                    self.fillregs[args[4]] = eng.to_reg(args[4])
                                args[4] = self.fillregs[args[4]]
                            getattr(eng, it[1])(*args, **it[3]).then_inc(sem_e, 1)
                        else:
                            eng.dma_start(out=it[1], in_=it[2], **it[3]).then_inc(it[4], 16)
                return f
            block.tensor(replay("pe"))
            block.scalar(replay("act"))
            block.vector(replay("dve"))
            block.gpsimd(replay("pool"))
            block.sync(replay("sp"))
        self.prog = {e: [] for e in self.ENG}

    def emit(self):
        self.barrier()
        self.flush()


def build_program(nseq=NSEQ, ntiles=NT, do_mlp=True, stages=("p1", "mla", "nsa", "out")):
    nc = bass.Bass("TRN2", target_bir_lowering=False)

    def din(name, shape):
        return nc.dram_tensor(name, list(shape), F32, kind="ExternalInput").ap()

    x_d = din("x", [nseq, SEQ, D])
    w_in_d = din("w_in", [D, IN_COLS])
    g_mix_d = din("g_mix", [128, 8])
    w_uq_d = din("w_uq", [256, 768])
    g_cq_d = din("g_cq", [128, 2])
    wukT_d = din("wukT", [64, 8, 128])
    wuv_d = din("wuv", [128, 8, 64])
    gckv_d = din("gckv_bc", [128, 128])
    w1_d = din("cmp_w1", [2, 64, 32, 128])
    peT_d = din("cmp_peT", [2, 64, 32])
    b1_d = din("cmp_b1", [128, 2])
    w2_d = din("cmp_w2", [128, 2, 64])
    b2k_d = din("cmp_b2k_bc", [128, 128])
    b2v_d = din("cmp_b2v", [64, 1])
    w_o_d = din("w_o", [D, D])
    g_out_d = din("g_out", [128, 8])
    w_up_d = din("w_up", [D, DFF])
    g_mlp_d = din("g_mlp", [128, 8])
    w_down_d = din("w_down", [DFF, D])
    gfin_d = din("gfin_bc", [128, D])
    cosm_d = din("cos_m", [128, NT, 16])
    sinm_d = din("sin_m", [128, NT, 16])
    cosn_d = din("cos_n", [128, NT, 8])
    sinn_d = din("sin_n", [128, NT, 8])
    cosn8_d = din("cos_n8", [128, NT, 8])
    sinn8_d = din("sin_n8", [128, NT, 8])
    cose_d = din("cos_e", [8, NT, 8])
    sine_d = din("sin_e", [8, NT, 8])
    tri_d = din("tri4", [128, 512])
    anti_d = din("anti4", [128, 512])
    ovl_d = din("ovl", [128, 2, 64])
    exp_d = din("expand", [64, SEQ])
    ident_d = din("ident", [128, 128])
    out_d = nc.dram_tensor("out", [nseq * SEQ, D], F32, kind="ExternalOutput").ap()
    hscr_d = nc.dram_tensor("hscr", [nseq * SEQ, D], F32, kind="Internal").ap()

    top = ExitStack()
    with top:
        S = Sched(nc, top)
        def psum(name, shape, dt):
            return top.enter_context(nc.psum_tensor(name, shape, dt))
        pS = [psum("pS%d" % i, [128, 512], F32) for i in range(2)]
        pA = [psum("pA%d" % i, [128, 512], F32) for i in range(2)]
        pM = [psum("pM%d" % i, [128, 512], F32) for i in range(2)]
        pT = [psum("pT%d" % i, [128, 1024], BF16) for i in range(2)]
        bpS = S.bufs("pS", 2)
        bpA = S.bufs("pA", 2)
        bpM = S.bufs("pM", 2)
        bpT = S.bufs("pT", 2)
        for b_ in bpS + bpA + bpM + bpT:
            b_.excl = True
        rr = {"S": 0, "M": 0, "T": 0, "P": 0, "A": 0}
        mode = {"bg": False, "B": False}

        def nxt(kind, n):
            if kind in ("M", "T") and not mode["B"]:
                return 0 if mode["bg"] else 1
            i = rr[kind] % n
            rr[kind] += 1
            return i

        A = ExitStack()
        with A:
            def sb(name, shape, dt):
                return A.enter_context(nc.sbuf_tensor("a_" + name, shape, dt))

            ident = sb("ident", [128, 128], BF16); b_ident = S.buf("ident")
            S.dma("pool", ident[:], ident_d, W=[b_ident])
            tri4 = sb("tri4", [128, 512], BF16); anti4 = sb("anti4", [128, 512], BF16); b_msk = S.buf("msk")
            S.dma("pool", tri4[:], tri_d, W=[b_msk])
            S.dma("pool", anti4[:], anti_d, W=[b_msk])
            tabs = {}
            b_tab = S.buf("tab")
            for nm, d_, w_ in (("cos_m", cosm_d, 16), ("sin_m", sinm_d, 16), ("cos_n", cosn_d, 8), ("sin_n", sinn_d, 8)):
                tabs[nm] = sb(nm, [128, NT, w_], F32)
                S.dma("sp", tabs[nm][:], d_, W=[b_tab])
            cos_e = sb("cos_e", [8, NT, 8], F32); sin_e = sb("sin_e", [8, NT, 8], F32)
            S.dma("sp", cos_e[:], cose_d, W=[b_tab])
            S.dma("sp", sin_e[:], sine_d, W=[b_tab])
            gckv = sb("gckv", [128, 128], F32); b2k = sb("b2k", [128, 128], F32); b2v = sb("b2v", [64, 1], F32)
            b1 = sb("b1", [128, 2], F32)
            S.dma("sp", gckv[:], gckv_d, W=[b_tab])
            S.dma("sp", b2k[:], b2k_d, W=[b_tab])
            S.dma("sp", b2v[:], b2v_d, W=[b_tab])
            S.dma("sp", b1[:], b1_d, W=[b_tab])
            gvec = sb("gvec", [128, 24], F32)
            S.dma("sp", gvec[:, 0:8], g_mix_d, W=[b_tab])
            S.dma("sp", gvec[:, 8:10], g_cq_d, W=[b_tab])
            S.dma("sp", gvec[:, 10:18], g_out_d, W=[b_tab])

            w_in = sb("w_in", [128, 8, IN_COLS], BF16); b_win = S.buf("w_in")
            S.dma("pool", w_in[:], w_in_d.rearrange("(c p) n -> p c n", p=128), W=[b_win])
            for c in range(8):
                S.op("dve", "tensor_scalar", w_in[:, c, :], w_in[:, c, :], gvec[:, c:c + 1], None, ALU.mult,
                     R=[b_tab, b_win], W=[b_win])
                S.op("dve", "tensor_scalar", w_in[:, c, 416:928], w_in[:, c, 416:928], 0.125, None, ALU.mult,
                     R=[b_win], W=[b_win])
            w_o = sb("w_o", [128, 8, D], BF16); b_wo = S.buf("w_o")
            S.dma("pool", w_o[:], w_o_d.rearrange("(c p) n -> p c n", p=128), W=[b_wo])
            for c in range(8):
                S.op("dve", "tensor_scalar", w_o[:, c, :], w_o[:, c, :], gvec[:, 10 + c:11 + c], None, ALU.mult,
                     R=[b_tab, b_wo], W=[b_wo])
            w_uq = sb("w_uq", [128, 2, 768], BF16); b_wuq = S.buf("w_uq")
            S.dma("pool", w_uq[:], w_uq_d.rearrange("(c p) n -> p c n", p=128), W=[b_wuq])
            for c in range(2):
                S.op("dve", "tensor_scalar", w_uq[:, c, :], w_uq[:, c, :], gvec[:, 8 + c:9 + c], 96.0 ** -0.5,
                     ALU.mult, ALU.mult, R=[b_tab, b_wuq], W=[b_wuq])
            wukT = sb("wukT", [64, 8, 128], BF16); wuv = sb("wuv", [128, 8, 64], BF16); b_wkv = S.buf("wkv")
            S.dma("pool", wukT[:], wukT_d, W=[b_wkv])
            S.dma("pool", wuv[:], wuv_d, W=[b_wkv])
            w1 = sb("w1", [64, 2, 32, 128], BF16); b_w1 = S.buf("w1")
            for kv in range(2):
                S.dma("pool", w1[:, kv], w1_d[kv], W=[b_w1])
            peT = sb("peT", [64, 2, 32], BF16)
            for kv in range(2):
                S.dma("pool", peT[:, kv], peT_d[kv], W=[b_w1])
            w2 = sb("w2", [128, 2, 64], BF16)
            S.dma("pool", w2[:], w2_d, W=[b_w1])

            bias_tot = sb("bias_tot", [128, 2], F32); b_bt = S.buf("bias_tot")
            for kv in range(2):
                for l in range(32):
                    S.op("pe", "matmul", pM[0][:, kv:kv + 1], lhsT=w1[:, kv, l, :], rhs=peT[:, kv, l:l + 1],
                         start=(l == 0), stop=(l == 31), R=[b_w1], W=[bpM[0]])
            S.op("dve", "tensor_tensor", bias_tot[:], pM[0][:, 0:2], b1[:], ALU.add, R=[bpM[0], b_tab], W=[b_bt])

            KlatT = sb("KlatT", [128, SEQ], BF16); bKlat = S.bufs("Klat", NT)
            KpeT = sb("KpeT", [32, SEQ], BF16); bKpe = S.bufs("Kpe", NT)
            Clat = sb("Clat", [128, NT, 130], BF16); bClat = S.bufs("Clat", NT)
            KsE = sb("KsE", [128, 2, SEQ], BF16); bKs = S.bufs("Ks", NT); b_exp = S.buf("expand")
            Vs = sb("Vs", [128, NT, 2, 65], BF16); bVs = S.bufs("Vs", NT)
            KwT = sb("KwT", [64, 2, 8 * 128], BF16); bKw = S.bufs("Kw", 8)
            Vw = sb("Vw", [128, 8, 2, 65], BF16); bVw = S.bufs("Vw", 8)
            KcT = sb("KcT", [64, 2, 256], BF16); b_Kc = S.buf("Kc")
            VcT = sb("VcT", [64, 2, 256], BF16); b_VcT = S.buf("VcT")
            VcO = sb("VcO", [128, 2, 2, 128], BF16); b_VcO = S.buf("VcO")
            for g in range(2):
                S.dma("pool", KsE[64:128, g, :], exp_d, W=[b_exp])
            for nt in range(2):
                for g in range(2):
                    S.dma("pool", VcO[:, nt, g, 64:128], ovl_d[:, nt, :], W=[b_VcO])
            S.op("pool", "memset", Clat[:, :, 128:130], 1.0, W=bClat)
            S.op("pool", "memset", Vs[:, :, :, 64:65], 1.0, W=bVs)
            S.op("pool", "memset", Vw[:, :, :, 64:65], 1.0, W=bVw)

            xs = [sb("xs%d" % i, [128, D], F32) for i in range(2)]; b_xs = S.bufs("xs", 2)
            st = sb("st", [128, 64], F32); b_st = S.buf("st")
            xn = sb("xn", [128, D], BF16); b_xn = S.buf("xn")
            xnT = sb("xnT", [128, 8, 128], BF16); b_xnT = S.buf("xnT")
            u = sb("u", [128, IN_COLS], F32); b_u = S.buf("u")
            cqn = sb("cqn", [128, 256], BF16); b_cqn = S.buf("cqn")
            cqnT = sb("cqnT", [128, 2, 128], BF16); b_cqnT = S.buf("cqnT")
            q_sb = sb("q_sb", [128, 9, 96], F32); b_q = S.buf("q")
            qn_sb = sb("qn_sb", [128, 8, 64], BF16); b_qn = S.buf("qn")
            qpe_sb = sb("qpe_sb", [128, 9, 32], BF16); b_qpe = S.buf("qpe")
            rt = [sb("rt%d" % i, [128, 160], F32) for i in range(4)]; b_rt = S.buf("rt")
            QnT = sb("QnT", [64, 8, 128], BF16); b_QnT = S.buf("QnT")
            QpeT2 = [sb("QpeT%d" % i, [32, 8, 128], BF16) for i in range(2)]; b_QpeT2 = S.bufs("QpeT", 2)
            QabsT2 = [sb("QabsT%d" % i, [128, 8, 128], BF16) for i in range(2)]; b_Qabs2 = S.bufs("Qabs", 2)
            ub = sb("ub", [128, 20, 64], BF16); b_ub = S.buf("ub")
            QS2 = [sb("QS%d" % i, [128, 2, 4, 128], BF16) for i in range(2)]; b_QSq2 = S.bufs("QSq", 2); b_QSs2 = [S.bufs("QSs", 2) for _ in range(2)]
            rawT = [sb("rawT%d" % i, [64, 2, 2, 144], BF16) for i in range(2)]; b_rawT = S.bufs("rawT", 2)
            gate2 = [sb("gate%d" % i, [128, 3, 8], F32) for i in range(2)]; b_gate2 = S.bufs("gate", 2)
            z_sb = sb("z_sb", [128, 32], F32); z2_sb = sb("z2_sb", [128, 32], F32); b_z = S.buf("z")
            hid_sb = sb("hid_sb", [128, 32], BF16); b_hid = S.buf("hid")
            kc_f = sb("kc_f", [8, 2, 64], F32); kc_sb = sb("kc_sb", [8, 2, 64], BF16); b_kc = S.buf("kc")
            PT = [sb("PT%d" % i, [128, 512], BF16) for i in range(3)]; b_PT = S.bufs("PT", 3)
            PcT = [sb("PcT%d" % i, [128, 512], BF16) for i in range(2)]; b_PcT = S.bufs("PcT", 2)
            OlatT = sb("OlatT", [128, 4, 128], BF16); b_OlatT = S.buf("OlatT")
            y_sb = sb("y_sb", [128, D], F32); b_y = S.buf("y"); b_yn = S.buf("yn")
            imp = sb("imp", [128, 64], F32); sc1 = sb("sc1", [128, 64], F32); sc2 = sb("sc2", [128, 64], F32)
            sc3 = sb("sc3", [128, 64], F32); b_imp = S.buf("imp")
            selq = sb("selq", [128, 128], BF16); b_selq = S.buf("selq")
            mixed = sb("mixed", [128, D], BF16); b_mixed = S.buf("mixed")
            mixedT = sb("mixedT", [128, 8, 128], BF16); b_mixedT = S.buf("mixedT")
            h_sb = [sb("h_sb%d" % i, [128, D], F32) for i in range(2)]; b_h = S.bufs("h", 2)

            S.op("pool", "memset", selq[:], 0.0, W=[b_selq])
            for i in range(2):
                S.op("pool", "memset", rawT[i][:], 0.0, W=[b_rawT[i]])
            S.op("pool", "memset", KcT[:], 0.0, W=[b_Kc])
            S.op("pool", "memset", VcT[:], 0.0, W=[b_VcT])
            S.op("pool", "memset", VcO[:, :, :, 0:64], 0.0, W=[b_VcO])

            b_stc = {0: S.buf("st0"), 4: S.buf("st4"), 12: S.buf("st12")}
            b_stm = S.buf("stm")
            b_stn = S.buf("stn")
            rl = sb("rl", [128, 8], F32); b_rl = S.buf("rl")
            lacc = sb("lacc", [128, 512], F32); b_lacc = S.buf("lacc")
            ones_f = sb("ones_f", [128, 1], F32); b_ones = S.buf("ones")
            S.op("pool", "memset", ones_f[:], 1.0, W=[b_ones])

            def rstd_multi(items, col, Rb, jk, b_jk):
                b_st = b_stc[col]
                k = len(items)
                S.op("dve", "memset", st[:, col:col + k], 0.0, W=[b_st])
                for i_, (src_ap, n) in enumerate(items):
                    S.op("dve", "scalar_tensor_tensor", jk[:, 0:n], src_ap, 1.0, src_ap, ALU.mult, ALU.mult,
                         accum_out=st[:, col + i_:col + i_ + 1], R=Rb + [b_st], W=[b_jk, b_st])
                    S.op("dve", "tensor_scalar", st[:, col + k + i_:col + k + i_ + 1], st[:, col + i_:col + i_ + 1], 1.0 / n, EPS,
                         ALU.mult, ALU.add, R=[b_st], W=[b_st])
                v_ = st[:, col + k:col + 2 * k]
                y_ = st[:, col + 2 * k:col + 3 * k]
                w_ = st[:, col + 3 * k:col + 4 * k]
                if not USE_MAGIC:
                    S.op("act", "activation", w_, v_, AF.Sqrt, R=[b_st], W=[b_st])
                    S.op("dve", "reciprocal", y_, w_, R=[b_st], W=[b_st])
                    return [st[:, col + 2 * k + i_:col + 2 * k + i_ + 1] for i_ in range(k)]
                S.op("dve", "tensor_scalar", y_.bitcast(I32), v_.bitcast(I32), -0.5, 1597463007.0, ALU.mult, ALU.add, R=[b_st], W=[b_st])
                for _it in range(2):
                    S.op("dve", "tensor_tensor", w_, y_, y_, ALU.mult, R=[b_st], W=[b_st])
                    S.op("dve", "tensor_tensor", w_, w_, v_, ALU.mult, R=[b_st], W=[b_st])
                    S.op("dve", "tensor_scalar", w_, w_, -0.5, 1.5, ALU.mult, ALU.add, R=[b_st], W=[b_st])
                    S.op("dve", "tensor_tensor", y_, y_, w_, ALU.mult, R=[b_st], W=[b_st])
                return [st[:, col + 2 * k + i_:col + 2 * k + i_ + 1] for i_ in range(k)]

            def rope(eng, out_ap, in_ap, cos_ap, sin_ap, nh, half, Rb, Wb):
                P = in_ap.shape[0]
                x1 = in_ap[:, :, 0:half]
                x2 = in_ap[:, :, half:2 * half]
                cb = cos_ap[:, None, :].to_broadcast([P, nh, half])
                sbb = sin_ap[:, None, :].to_broadcast([P, nh, half])
                t = [r_[0:P, 0:nh * half].rearrange("p (h d) -> p h d", h=nh) for r_ in rt]
                S.op(eng, "tensor_tensor", t[0], x1, cb, ALU.mult, R=Rb + [b_tab], W=[b_rt])
                S.op(eng, "tensor_tensor", t[1], x2, sbb, ALU.mult, R=Rb + [b_tab], W=[b_rt])
                S.op(eng, "tensor_tensor", t[2], x2, cb, ALU.mult, R=Rb + [b_tab], W=[b_rt])
                S.op(eng, "tensor_tensor", t[3], x1, sbb, ALU.mult, R=Rb + [b_tab], W=[b_rt])
                S.op(eng, "tensor_tensor", out_ap[:, :, 0:half], t[0], t[1], ALU.subtract, R=[b_rt], W=Wb)
                S.op(eng, "tensor_tensor", out_ap[:, :, half:2 * half], t[2], t[3], ALU.add, R=[b_rt], W=Wb)

            def transpose_to(ps_i, col0, in_ap, Rb):
                P, Fd = in_ap.shape[0], in_ap.shape[1]
                S.op("pe", "transpose", pT[ps_i][0:Fd, col0:col0 + P], in_ap, ident[0:P, 0:P],
                     R=Rb + [b_ident], W=[bpT[ps_i]])

            def phase1(s, t):
                par = t % 2
                QpeT, b_QpeT = QpeT2[par], b_QpeT2[par]
                QabsT, b_Qabs = QabsT2[par], b_Qabs2[par]
                QS, b_QSq, b_QSs = QS2[par], b_QSq2[par], b_QSs2[par]
                gate, b_gate = gate2[par], b_gate2[par]
                xb = t % 2
                S.dma("sp", xs[xb][:], x_d[s, t * 128:(t + 1) * 128, :], W=[b_xs[xb]])
                r0 = rstd_multi([(xs[xb][:], D)], 0, [b_xs[xb]], xn, b_xn)[0]
                S.op("dve", "tensor_scalar", xn[:], xs[xb][:], r0, None, ALU.mult, R=[b_xs[xb], b_stc[0]], W=[b_xn])
                ti = nxt("T", 2)
                for c in range(8):
                    transpose_to(ti, c * 128, xn[:, c * 128:(c + 1) * 128], [b_xn])
                S.op("dve", "tensor_copy", xnT[:], pT[ti][:, 0:1024].rearrange("p (c n) -> p c n", c=8), R=[bpT[ti]], W=[b_xnT])
                for cg, (c0, c1) in enumerate(((0, 512), (512, 1024), (1024, 1536), (1536, IN_COLS))):
                    mi = nxt("M", 2)
                    for c in range(8):
                        S.op("pe", "matmul", pM[mi][:, 0:c1 - c0], lhsT=xnT[:, c, :], rhs=w_in[:, c, c0:c1],
                             start=(c == 0), stop=(c == 7), R=[b_xnT, b_win], W=[bpM[mi]])
                    S.op("dve", "tensor_copy",
                         u[:, c0:c1], pM[mi][:, 0:c1 - c0], R=[bpM[mi]], W=[b_u])
                    yield

                yield
                r1, r2 = rstd_multi([(u[:, 0:256], 256), (u[:, 256:384], 128)], 4, [b_u], cqn, b_cqn)
                S.op("dve", "tensor_scalar", cqn[:], u[:, 0:256], r1, None, ALU.mult, R=[b_u, b_stc[4]], W=[b_cqn])
                ti = nxt("T", 2)
                for c in range(2):
                    transpose_to(ti, c * 128, cqn[:, c * 128:(c + 1) * 128], [b_cqn])
                S.op("dve", "tensor_copy", cqnT[:], pT[ti][:, 0:256].rearrange("p (c n) -> p c n", c=2), R=[bpT[ti]], W=[b_cqnT])
                yield
                for half in range(2):
                    mi = nxt("M", 2)
                    for c in range(2):
                        S.op("pe", "matmul", pM[mi][:, 0:384], lhsT=cqnT[:, c, :], rhs=w_uq[:, c, half * 384:(half + 1) * 384],
                             start=(c == 0), stop=(c == 1), R=[b_cqnT, b_wuq], W=[bpM[mi]])
                    S.op("dve", "tensor_copy", q_sb[:, half * 4:(half + 1) * 4, :],
                         pM[mi][:, 0:384].rearrange("p (h d) -> p h d", h=4), R=[bpM[mi]], W=[b_q])
                yield
                S.op("dve", "tensor_copy", q_sb[:, 8, 64:96], u[:, 384:416], R=[b_u], W=[b_q])
                S.op("dve", "tensor_copy", qn_sb[:], q_sb[:, 0:8, 0:64], R=[b_q], W=[b_qn])
                rope("dve", qpe_sb[:], q_sb[:, :, 64:96], tabs["cos_m"][:, t, :], tabs["sin_m"][:, t, :], 9, 16, [b_q], [b_qpe])
                ti = nxt("T", 2)
                for h in range(8):
                    transpose_to(ti, h * 128, qn_sb[:, h, :], [b_qn])
                S.op("dve", "tensor_copy", QnT[:], pT[ti][0:64, 0:1024].rearrange("p (c n) -> p c n", c=8), R=[bpT[ti]], W=[b_QnT])
                ti = nxt("T", 2)
                for h in range(8):
                    transpose_to(ti, h * 128, qpe_sb[:, h, :], [b_qpe])
                S.op("dve", "tensor_copy", QpeT[:], pT[ti][0:32, 0:1024].rearrange("p (c n) -> p c n", c=8), R=[bpT[ti]], W=[b_QpeT])
                yield
                for hg in range(2):
                    mi = nxt("M", 2)
                    for j in range(4):
                        h = hg * 4 + j
                        S.op("pe", "matmul", pM[mi][:, j * 128:(j + 1) * 128], lhsT=wukT[:, h, :],
                             rhs=QnT[:, h, :], start=True, stop=True, R=[b_wkv, b_QnT], W=[bpM[mi]])
                    S.op("dve", "tensor_copy", QabsT[:, hg * 4:(hg + 1) * 4, :],
                         pM[mi][:, 0:512].rearrange("p (c n) -> p c n", c=4), R=[bpM[mi]], W=[b_Qabs])

                yield
                S.op("dve", "scalar_tensor_tensor", Clat[:, t, 0:128], u[:, 256:384], r2, gckv[:], ALU.mult, ALU.mult,
                     R=[b_u, b_stc[4], b_tab], W=[bClat[t]])
                ti = nxt("T", 2)
                transpose_to(ti, 0, Clat[:, t, 0:128], [bClat[t]])
                S.op("dve", "tensor_copy", KlatT[:, t * 128:(t + 1) * 128], pT[ti][:, 0:128], R=[bpT[ti]], W=[bKlat[t]])
                ti = nxt("T", 2)
                transpose_to(ti, 0, qpe_sb[:, 8, :], [b_qpe])
                S.op("dve", "tensor_copy", KpeT[:, t * 128:(t + 1) * 128], pT[ti][0:32, 0:128], R=[bpT[ti]], W=[bKpe[t]])

                yield
                uv = u[:, 416:1696].rearrange("p (b d) -> p b d", b=20)
                S.op("dve", "tensor_copy", ub[:, :, 16:64], uv[:, :, 16:64], R=[b_u], W=[b_ub])
                rope("dve", ub[:, :, 0:16], uv[:, :, 0:16], tabs["cos_n"][:, t, :], tabs["sin_n"][:, t, :], 20, 8, [b_u], [b_ub])
                S.op("dve", "tensor_copy", ub[:, 8:12, 0:16], uv[:, 8:12, 0:16], R=[b_u, b_ub], W=[b_ub])
                ti = nxt("T", 2)
                for h in range(8):
                    transpose_to(ti, h * 128, ub[:, h, :], [b_ub])
                S.op("dve", "tensor_copy", QS[0:64].rearrange("p g j n -> p (g j) n"),
                     pT[ti][0:64, 0:1024].rearrange("p (c n) -> p c n", c=8), R=[bpT[ti]], W=[b_QSq])
                yield
                kvv = u[:, 928:1696].rearrange("p (s g d) -> p s g d", s=6, g=2)
                S.op("dve", "tensor_copy", Vs[:, t, :, 0:64], kvv[:, 3], R=[b_u], W=[bVs[t]])
                S.op("dve", "tensor_copy", Vw[:, t % 8, :, 0:64], kvv[:, 5], R=[b_u], W=[bVw[t % 8]])
                ti = nxt("T", 2)
                for g in range(2):
                    transpose_to(ti, g * 128, ub[:, 12 + g, :], [b_ub])
                    transpose_to(ti, 256 + g * 128, ub[:, 16 + g, :], [b_ub])
                S.op("dve", "tensor_copy", KsE[0:64, :, t * 128:(t + 1) * 128],
                     pT[ti][0:64, 0:256].rearrange("p (g n) -> p g n", g=2), R=[bpT[ti]], W=[bKs[t]])
                S.op("dve", "tensor_copy", KwT[:, :, (t % 8) * 128:(t % 8 + 1) * 128],
                     pT[ti][0:64, 256:512].rearrange("p (g n) -> p g n", g=2), R=[bpT[ti]], W=[bKw[t % 8]])
                yield
                rb = t % 2
                if t == 0:
                    S.op("pool", "memset", rawT[rb][:, :, :, 0:16], 0.0, W=[b_rawT[rb]])
                else:
                    S.op("dve", "tensor_copy", rawT[rb][:, :, :, 0:16], rawT[1 - rb][:, :, :, 128:144],
                         R=[b_rawT[1 - rb]], W=[b_rawT[rb]])
                ti = nxt("T", 2)
                for c in range(4):
                    transpose_to(ti, c * 128, ub[:, 8 + c, :], [b_ub])
                S.op("dve", "tensor_copy", rawT[rb][:, :, :, 16:144],
                     pT[ti][0:64, 0:512].rearrange("p (k g n) -> p k g n", k=2, g=2), R=[bpT[ti]], W=[b_rawT[rb]])
                yield
                S.op("act", "activation", gate[:].rearrange("p b h -> p (b h)"), u[:, 1696:1720], AF.Tanh, scale=0.5, R=[b_u], W=[b_gate])
                S.op("dve", "tensor_scalar", gate[:].rearrange("p b h -> p (b h)"), gate[:].rearrange("p b h -> p (b h)"), 0.5, 0.5,
                     ALU.mult, ALU.add, R=[b_gate], W=[b_gate])

                yield
                mi = nxt("M", 2)
                for kv in range(2):
                    for g in range(2):
                        c0 = (kv * 2 + g) * 8
                        for l in range(32):
                            S.op("pe", "matmul", pM[mi][:, c0:c0 + 8], lhsT=w1[:, kv, l, :], rhs=rawT[rb][:, kv, g, l:l + 113:16],
                                 start=(l == 0), stop=(l == 31), R=[b_w1, b_rawT[rb]], W=[bpM[mi]])
                            if l % 8 == 7:
                                yield
                for kv in range(2):
                    S.op("dve", "tensor_scalar", z_sb[:, kv * 16:(kv + 1) * 16], pM[mi][:, kv * 16:(kv + 1) * 16],
                         bias_tot[:, kv:kv + 1], None, ALU.add, R=[bpM[mi], b_bt], W=[b_z])
                S.op("dve", "tensor_tensor", z2_sb[:], z_sb[:], z_sb[:], ALU.mult, R=[b_z], W=[b_z])
                S.op("dve", "tensor_scalar", z2_sb[:], z2_sb[:], 0.044715, 1.0, ALU.mult, ALU.add, R=[b_z], W=[b_z])
                S.op("dve", "tensor_tensor", z2_sb[:], z2_sb[:], z_sb[:], ALU.mult, R=[b_z], W=[b_z])
                S.op("act", "activation", z2_sb[:], z2_sb[:], AF.Tanh, scale=math.sqrt(2.0 / math.pi), R=[b_z], W=[b_z])
                S.op("dve", "tensor_scalar", z2_sb[:], z2_sb[:], 0.5, 0.5, ALU.mult, ALU.add, R=[b_z], W=[b_z])
                S.op("dve", "tensor_tensor", hid_sb[:], z_sb[:], z2_sb[:], ALU.mult, R=[b_z], W=[b_hid])
                yield
                n0 = 8 * t - 1
                m0 = 1 if t == 0 else 0
                mi = nxt("M", 2)
                for g in range(2):
                    S.op("pe", "matmul", pM[mi][0:8, g * 64:(g + 1) * 64], lhsT=hid_sb[:, g * 8:(g + 1) * 8], rhs=w2[:, 0, :],
                         start=True, stop=True, R=[b_hid, b_w1], W=[bpM[mi]])
                for g in range(2):
                    S.op("pe", "matmul", pM[mi][0:64, 128 + g * 8:136 + g * 8], lhsT=w2[:, 1, :], rhs=hid_sb[:, 16 + g * 8:24 + g * 8],
                         start=True, stop=True, R=[b_hid, b_w1], W=[bpM[mi]])
                S.op("dve", "tensor_tensor", kc_f[:].rearrange("p g d -> p (g d)"), pM[mi][0:8, 0:128], b2k[0:8, :], ALU.add,
                     R=[bpM[mi], b_tab], W=[b_kc])
                S.op("dve", "tensor_scalar", VcT[:, :, n0 + m0:n0 + 8], pM[mi][0:64, 128:144].rearrange("p (g m) -> p g m", g=2)[:, :, m0:8],
                     b2v[:, 0:1], None, ALU.add, R=[bpM[mi], b_tab], W=[b_VcT])
                S.op("dve", "tensor_copy", kc_sb[:, :, 16:64], kc_f[:, :, 16:64], R=[b_kc], W=[b_kc])
                rope("dve", kc_sb[:, :, 0:16], kc_f[:, :, 0:16], cos_e[:, t, :], sin_e[:, t, :], 2, 8, [b_kc], [b_kc])
                ti = nxt("T", 2)
                for g in range(2):
                    transpose_to(ti, g * 8, kc_sb[:, g, :], [b_kc])
                S.op("dve", "tensor_copy", KcT[:, :, n0 + m0:n0 + 8],
                     pT[ti][0:64, 0:16].rearrange("p (g m) -> p g m", g=2)[:, :, m0:8], R=[bpT[ti]], W=[b_Kc])
                yield
                nts = sorted(set([max(n0, 0) // 128, (n0 + 7) // 128]))
                for nt in nts:
                    ti = nxt("T", 2)
                    for g in range(2):
                        S.op("pe", "transpose", pT[ti][:, g * 64:(g + 1) * 64], VcT[:, g, nt * 128:(nt + 1) * 128], ident[0:64, 0:64],
                             R=[b_VcT, b_ident], W=[bpT[ti]])
                    S.op("dve", "tensor_copy", VcO[:, nt, :, 0:64], pT[ti][:, 0:128].rearrange("p (g d) -> p g d", g=2), R=[bpT[ti]], W=[b_VcO])

            def attn_loop(kts, qk_fn, post_fn):
                if not kts:
                    return
                si_next = qk_fn(kts[0])
                for i_, kt in enumerate(kts):
                    si = si_next
                    if i_ + 1 < len(kts):
                        si_next = qk_fn(kts[i_ + 1])
                    post_fn(kt, si)

            def mla(s, t):
                par = t % 2
                QpeT, b_QpeT = QpeT2[par], b_QpeT2[par]
                QabsT, b_Qabs = QabsT2[par], b_Qabs2[par]
                QS, b_QSq, b_QSs = QS2[par], b_QSq2[par], b_QSs2[par]
                gate, b_gate = gate2[par], b_gate2[par]
                mo = nxt("M", 2)
                for hg in range(2):
                    qa = QabsT[:, hg * 4:(hg + 1) * 4, :].rearrange("p c n -> p (c n)")
                    qp = QpeT[:, hg * 4:(hg + 1) * 4, :].rearrange("p c n -> p (c n)")

                    def qk(kt):
                        si = nxt("S", 2)
                        S.op("pe", "matmul", pS[si][:], lhsT=KlatT[:, kt * 128:(kt + 1) * 128], rhs=qa,
                             start=True, stop=False, R=[bKlat[kt], b_Qabs], W=[bpS[si]])
                        S.op("pe", "matmul", pS[si][:], lhsT=KpeT[:, kt * 128:(kt + 1) * 128], rhs=qp,
                             start=False, stop=True, R=[bKpe[kt], b_QpeT], W=[bpS[si]])
                        return si

                    def post(kt, si):
                        pi = nxt("P", 3)
                        S.op("act", "activation", PT[pi][:], pS[si][:], AF.Exp, R=[bpS[si]], W=[b_PT[pi]])
                        if kt == t:
                            S.op("dve", "tensor_tensor", PT[pi][:], PT[pi][:], tri4[:], ALU.mult, R=[b_PT[pi], b_msk], W=[b_PT[pi]])
                        S.op("pe", "matmul", pA[0][:], lhsT=Clat[:, kt, 0:128], rhs=PT[pi][:], start=(kt == 0), stop=(kt == t),
                             R=[b_PT[pi], bClat[kt]], W=[bpA[0]])
                        if kt == 0:
                            S.op("dve", "tensor_copy", lacc[:], PT[pi][:], R=[b_PT[pi]], W=[b_lacc])
                        else:
                            S.op("dve", "tensor_tensor", lacc[:], lacc[:], PT[pi][:], ALU.add, R=[b_PT[pi], b_lacc], W=[b_lacc])

                    attn_loop(list(range(t + 1)), qk, post)
                    for j in range(4):
                        S.op("pe", "matmul", pA[1][:, j:j + 1], lhsT=lacc[:, j * 128:(j + 1) * 128], rhs=ones_f[:, 0:1],
                             start=True, stop=True, R=[b_lacc, b_ones], W=[bpA[1]])
                    S.op("dve", "tensor_copy", OlatT[:], pA[0][:].rearrange("p (c n) -> p c n", c=4), R=[bpA[0]], W=[b_OlatT])
                    S.op("dve", "reciprocal", rl[:, hg * 4:(hg + 1) * 4], pA[1][:, 0:4], R=[bpA[1]], W=[b_rl])
                    for j in range(4):
                        h = hg * 4 + j
                        S.op("pe", "matmul", pM[mo][:, h * 64:(h + 1) * 64], lhsT=OlatT[:, j, :], rhs=wuv[:, h, :],
                             start=True, stop=True, R=[b_OlatT, b_wkv], W=[bpM[mo]])
                S.op("dve", "tensor_tensor", y_sb[:, 0:512].rearrange("p (h d) -> p h d", h=8),
                     pM[mo][:, 0:512].rearrange("p (h d) -> p h d", h=8), rl[:, 0:8, None].to_broadcast([128, 8, 64]), ALU.mult,
                     R=[bpM[mo], b_rl], W=[b_y])

            def nsa_sel(s, t, g):
                par = t % 2
                QpeT, b_QpeT = QpeT2[par], b_QpeT2[par]
                QabsT, b_Qabs = QabsT2[par], b_Qabs2[par]
                QS, b_QSq, b_QSs = QS2[par], b_QSq2[par], b_QSs2[par]
                gate, b_gate = gate2[par], b_gate2[par]
                QSg = QS[:, g].rearrange("p j n -> p (j n)")
                QSg = QS[:, g].rearrange("p j n -> p (j n)")
                nts = [0] + ([1] if t >= 16 else [])
                for nt in nts:
                    si = nxt("M", 2)
                    S.op("pe", "matmul", pM[si][:], lhsT=KcT[:, g, nt * 128:(nt + 1) * 128], rhs=QSg[0:64, :],
                         start=True, stop=True, R=[b_Kc, b_QSq], W=[bpM[si]])
                    S.op("act", "activation", PcT[nt][:], pM[si][:], AF.Exp, R=[bpM[si]], W=[b_PcT[nt]])
                    S.op("pool", "affine_select", PcT[nt][:].rearrange("p (j n) -> p j n", j=4),
                         PcT[nt][:].rearrange("p (j n) -> p j n", j=4), [[0, 4], [1, 128]], ALU.is_ge, 0.0,
                         base=128 * t - 31 - 2048 * nt, channel_multiplier=-16, R=[b_PcT[nt]], W=[b_PcT[nt]])
                yield
                mc = nxt("M", 2)
                for j in range(4):
                    for i_, nt in enumerate(nts):
                        S.op("pe", "matmul", pM[mc][:, j * 128:(j + 1) * 128], lhsT=PcT[nt][:, j * 128:(j + 1) * 128],
                             rhs=VcO[:, nt, g, :], start=(i_ == 0), stop=(i_ == len(nts) - 1),
                             R=[b_PcT[nt], b_VcO], W=[bpM[mc]])
                yield
                pc = pM[mc][:].rearrange("p (j c) -> p j c", j=4)
                S.op("dve", "tensor_reduce", st[:, 24:28], pc[:, :, 64:128], AX.X, ALU.add, R=[bpM[mc]], W=[b_stn])
                S.op("dve", "tensor_scalar", st[:, 24:28], st[:, 24:28], 1e-30, None, ALU.max, R=[b_stn], W=[b_stn])
                S.op("dve", "reciprocal", st[:, 28:32], st[:, 24:28], R=[b_stn], W=[b_stn])
                S.op("dve", "tensor_scalar", imp[:], pc[:, 0, 64:128], st[:, 28:29], None, ALU.mult, R=[bpM[mc], b_stn], W=[b_imp])
                for j in range(1, 4):
                    S.op("dve", "scalar_tensor_tensor", imp[:], pc[:, j, 64:128], st[:, 28 + j:29 + j], imp[:], ALU.mult, ALU.add,
                         R=[bpM[mc], b_stn, b_imp], W=[b_imp])
                yield
                S.op("dve", "tensor_tensor", st[:, 32:36], st[:, 28:32], gate[:, 0, g * 4:(g + 1) * 4], ALU.mult,
                     R=[b_stn, b_gate], W=[b_stn])
                for j in range(4):
                    h = g * 4 + j
                    S.op("dve", "tensor_scalar", y_sb[:, 512 + h * 64:576 + h * 64], pc[:, j, 0:64], st[:, 32 + j:33 + j], None, ALU.mult,
                         R=[bpM[mc], b_stn], W=[b_yn])
                yield
                S.op("pool", "affine_select", sc1[:], imp[:], [[-64, 64]], ALU.is_ge, 1e9, base=128 * t - 128, channel_multiplier=1,
                     R=[b_imp], W=[b_imp])
                S.op("pool", "affine_select", sc2[:], sc1[:], [[-64, 64]], ALU.is_ge, -1e9, base=128 * t, channel_multiplier=1,
                     R=[b_imp], W=[b_imp])
                S.op("pool", "memset", sc2[:, 0:1], 1e9, R=[b_imp], W=[b_imp])
                yield
                S.op("dve", "max", st[:, 40:48], sc2[:], R=[b_imp], W=[b_stn])
                S.op("dve", "match_replace", sc3[:], st[:, 40:48], sc2[:], -3e38, R=[b_imp, b_stn], W=[b_imp])
                S.op("dve", "max", st[:, 48:56], sc3[:], R=[b_imp], W=[b_stn])
                S.op("dve", "tensor_scalar", selq[:, 64:128], sc2[:], st[:, 55:56], NEG, ALU.is_lt, ALU.mult,
                     R=[b_imp, b_stn], W=[b_selq])
                yield
                ti = nxt("T", 2)
                transpose_to(ti, 0, selq[:], [b_selq])
                S.op("dve", "tensor_copy", QS[64:128, g], pT[ti][64:128, None, 0:128].to_broadcast([64, 4, 128]),
                     R=[bpT[ti]], W=[b_QSs[g]])
                yield

            def nsa_attn(s, t, g):
                par = t % 2
                QpeT, b_QpeT = QpeT2[par], b_QpeT2[par]
                QabsT, b_Qabs = QabsT2[par], b_Qabs2[par]
                QS, b_QSq, b_QSs = QS2[par], b_QSq2[par], b_QSs2[par]
                gate, b_gate = gate2[par], b_gate2[par]
                QSg = QS[:, g].rearrange("p j n -> p (j n)")
                S.op("dve", "memset", pA[0][:, 0:260], 0.0, W=[bpA[0]])
                S.op("dve", "memset", pA[1][:, 0:260], 0.0, W=[bpA[1]])
                def qk_s(kt):
                    si = nxt("S", 2)
                    S.op("pe", "matmul", pS[si][:], lhsT=KsE[:, g, kt * 128:(kt + 1) * 128], rhs=QSg,
                         start=True, stop=True, R=[bKs[kt], b_exp, b_QSq, b_QSs[g]], W=[bpS[si]])
                    return si

                def post_s(kt, si):
                    pi = nxt("P", 3)
                    S.op("act", "activation", PT[pi][:], pS[si][:], AF.Exp, R=[bpS[si]], W=[b_PT[pi]])
                    if kt == t:
                        S.op("dve", "tensor_tensor", PT[pi][:], PT[pi][:], tri4[:], ALU.mult, R=[b_PT[pi], b_msk], W=[b_PT[pi]])
                    for j in range(4):
                        S.op("pe", "matmul", pA[0][:, j * 65:j * 65 + 65], lhsT=PT[pi][:, j * 128:(j + 1) * 128],
                             rhs=Vs[:, kt, g, :], start=False, stop=(kt == t), skip_group_check=True,
                             R=[b_PT[pi], bVs[kt]], W=[bpA[0]])

                def qk_w(kt):
                    si = nxt("S", 2)
                    sl = kt % 8
                    S.op("pe", "matmul", pS[si][:], lhsT=KwT[:, g, sl * 128:(sl + 1) * 128], rhs=QSg[0:64, :],
                         start=True, stop=True, R=[bKw[sl], b_QSq], W=[bpS[si]])
                    return si

                def post_w(kt, si):
                    sl = kt % 8
                    pi = nxt("P", 3)
                    S.op("act", "activation", PT[pi][:], pS[si][:], AF.Exp, R=[bpS[si]], W=[b_PT[pi]])
                    if kt == t:
                        S.op("dve", "tensor_tensor", PT[pi][:], PT[pi][:], tri4[:], ALU.mult, R=[b_PT[pi], b_msk], W=[b_PT[pi]])
                    if kt == t - 4:
                        S.op("dve", "tensor_tensor", PT[pi][:], PT[pi][:], anti4[:], ALU.mult, R=[b_PT[pi], b_msk], W=[b_PT[pi]])
                    for j in range(4):
                        S.op("pe", "matmul", pA[1][:, j * 65:j * 65 + 65], lhsT=PT[pi][:, j * 128:(j + 1) * 128],
                             rhs=Vw[:, sl, g, :], start=False, stop=(kt == t), skip_group_check=True,
                             R=[b_PT[pi], bVw[sl]], W=[bpA[1]])

                attn_loop(list(range(t + 1)), qk_s, post_s)
                attn_loop(list(range(max(0, t - 4), t + 1)), qk_w, post_w)
                for br in range(2):
                    pa = pA[br][:, 0:260].rearrange("p (j c) -> p j c", j=4)
                    S.op("dve", "reciprocal", st[:, 56:60], pa[:, :, 64], R=[bpA[br]], W=[b_stn])
                    S.op("dve", "tensor_tensor", st[:, 60:64], st[:, 56:60], gate[:, 1 + br, g * 4:(g + 1) * 4], ALU.mult,
                         R=[b_stn, b_gate], W=[b_stn])
                    for j in range(4):
                        h = g * 4 + j
                        S.op("dve", "scalar_tensor_tensor", y_sb[:, 512 + h * 64:576 + h * 64], pa[:, j, 0:64], st[:, 60 + j:61 + j],
                             y_sb[:, 512 + h * 64:576 + h * 64], ALU.mult, ALU.add, R=[bpA[br], b_stn, b_yn], W=[b_yn])


            def outproj(s, t):
                xb = t % 2
                ra, rb_ = rstd_multi([(y_sb[:, 0:512], 512), (y_sb[:, 512:1024], 512)], 12, [b_y, b_yn], mixed, b_mixed)
                S.op("dve", "tensor_scalar", mixed[:, 0:512], y_sb[:, 0:512], ra, None, ALU.mult, R=[b_y, b_stc[12]], W=[b_mixed])
                S.op("dve", "tensor_scalar", mixed[:, 512:1024], y_sb[:, 512:1024], rb_, None, ALU.mult, R=[b_yn, b_stc[12]], W=[b_mixed])
                ti = nxt("T", 2)
                for c in range(8):
                    transpose_to(ti, c * 128, mixed[:, c * 128:(c + 1) * 128], [b_mixed])
                S.op("dve", "tensor_copy", mixedT[:], pT[ti][:, 0:1024].rearrange("p (c n) -> p c n", c=8), R=[bpT[ti]], W=[b_mixedT])
                for dh in range(2):
                    mi = nxt("S", 2)
                    for c in range(8):
                        S.op("pe", "matmul", pS[mi][:], lhsT=mixedT[:, c, :], rhs=w_o[:, c, dh * 512:(dh + 1) * 512],
                             start=(c == 0), stop=(c == 7), R=[b_mixedT, b_wo], W=[bpS[mi]])
                    S.op("dve", "tensor_tensor", h_sb[xb][:, dh * 512:(dh + 1) * 512], pS[mi][:], xs[xb][:, dh * 512:(dh + 1) * 512],
                         ALU.add, R=[bpS[mi], b_xs[xb]], W=[b_h[xb]])
                row = (s * SEQ + t * 128)
                S.dma("sp", hscr_d[row:row + 128, :], h_sb[xb][:], R=[b_h[xb]])

            bgst = {"gen": None, "credit": 0.0, "rate": 0.0}

            def bg_run(n=None):
                if bgst["gen"] is None:
                    return
                mode["bg"] = True
                try:
                    k = 0
                    while n is None or k < n:
                        next(bgst["gen"])
                        k += 1
                except StopIteration:
                    bgst["gen"] = None
                mode["bg"] = False

            def tick():
                if mode["bg"] or bgst["gen"] is None:
                    return
                bgst["credit"] += bgst["rate"]
                if bgst["credit"] >= 1.0:
                    n = int(bgst["credit"])
                    bgst["credit"] -= n
                    bg_run(n)

            S.tick = tick
            def chain(*gens):
                for g_ in gens:
                    yield from g_

            def set_bg(gen, nchunks, fg_ops):
                bgst["gen"] = gen
                bgst["credit"] = 0.0
                bgst["rate"] = nchunks / (0.7 * fg_ops)

            for s in range(nseq):
                bgst["gen"] = phase1(s, 0) if "p1" in stages else None
                bg_run(None)
                for t in range(ntiles):
                    if "nsa" in stages:
                        set_bg(chain(nsa_sel(s, t, 0), nsa_sel(s, t, 1)), 16.0, 30.0 + 18.0 * (t + 1))
                    if "mla" in stages:
                        mla(s, t)
                    bg_run(None)
                    if t + 1 < ntiles and "p1" in stages:
                        set_bg(phase1(s, t + 1), 40.0, 100.0 + 14.0 * (t + 1 + min(t + 1, 5)))
                    if "nsa" in stages:
                        nsa_attn(s, t, 0)
                        nsa_attn(s, t, 1)
                    if "out" in stages:
                        outproj(s, t)
                    bg_run(None)
            S.tick = None
            S.barrier()
        mode["B"] = True
        B = ExitStack()
        with B:
            def sb2(name, shape, dt):
                return B.enter_context(nc.sbuf_tensor("b_" + name, shape, dt))
            gb = sb2("gb", [128, 8], F32); b_gb = S.buf("gb")
            S.dma("sp", gb[:], g_mlp_d, W=[b_gb])
            gfin = sb2("gfin", [128, D], F32)
            S.dma("sp", gfin[:], gfin_d, W=[b_gb])
            ident2 = sb2("ident2", [128, 128], BF16); b_id2 = S.buf("ident2")
            S.dma("pool", ident2[:], ident_d, W=[b_id2])
            w_up = sb2("w_up", [128, 8, DFF], BF16); b_wup = S.buf("w_up")
            for c in range(8):
                S.dma("pool", w_up[:, c, :], w_up_d[c * 128:(c + 1) * 128, :], W=[b_wup])
                S.op("dve", "tensor_scalar", w_up[:, c, :], w_up[:, c, :], gb[:, c:c + 1], None, ALU.mult, R=[b_gb, b_wup], W=[b_wup])
            w_dn = sb2("w_dn", [128, 32, D], BF16); b_wdn = S.buf("w_dn")
            wdv = w_down_d.rearrange("(f p) n -> p f n", p=128)
            for f4 in range(8):
                S.dma("pool", w_dn[:, f4 * 4:(f4 + 1) * 4, :], wdv[:, f4 * 4:(f4 + 1) * 4, :], W=[b_wdn])
            hin = [sb2("hin%d" % i, [128, D], F32) for i in range(4)]; b_hin = S.bufs("hin", 4)
            st2 = sb2("st2", [128, 16], F32); b_st2 = S.buf("st2")
            junk2 = sb2("junk2", [128, D], BF16); b_junk2 = S.buf("junk2")
            hn = sb2("hn", [128, D], BF16); b_hn = S.buf("hn")
            hnT = sb2("hnT", [128, 8, 512], BF16); b_hnT = S.buf("hnT")
            rl = [sb2("rl%d" % i, [128, 512], BF16) for i in range(2)]; b_rl = S.bufs("rl", 2)
            aT = sb2("aT", [128, 32, 512], BF16); b_aT = S.buf("aT")
            yo = [sb2("yo%d" % i, [128, D], F32) for i in range(2)]; b_yo = S.bufs("yo", 2)
            S.op("pool", "memset", st2[:], 0.0, W=[b_st2])
            nT = (nseq * ntiles * 128) // 512 if do_mlp else 0

            def rstd2(src_ap, Rb):
                S.op("pool", "memset", st2[:, 0:1], 0.0, W=[b_st2])
                S.op("act", "activation", junk2[:], src_ap, AF.Square, accum_out=st2[:, 0:1], R=Rb, W=[b_junk2, b_st2])
                S.op("dve", "tensor_scalar", st2[:, 1:2], st2[:, 0:1], 1.0 / D, EPS, ALU.mult, ALU.add, R=[b_st2], W=[b_st2])
                S.op("act", "activation", st2[:, 2:3], st2[:, 1:2], AF.Sqrt, R=[b_st2], W=[b_st2])
                S.op("dve", "reciprocal", st2[:, 3:4], st2[:, 2:3], R=[b_st2], W=[b_st2])
                return st2[:, 3:4]

            oc = 0
            for T in range(nT):
                hb = T % 2
                row = T * 512 if nseq * ntiles * 128 == nseq * SEQ else None
                base = (T * 512 // (ntiles * 128)) * SEQ + (T * 512) % (ntiles * 128)
                for i in range(4):
                    S.dma("sp", hin[i][:], hscr_d[base + i * 128:base + (i + 1) * 128, :], W=[b_hin[i]])
                for i in range(4):
                    r = rstd2(hin[i][:], [b_hin[i]])
                    S.op("dve", "tensor_scalar", hn[:], hin[i][:], r, None, ALU.mult, R=[b_hin[i], b_st2], W=[b_hn])
                    ti = nxt("T", 2)
                    for c in range(8):
                        S.op("pe", "transpose", pT[ti][:, c * 128:(c + 1) * 128], hn[:, c * 128:(c + 1) * 128], ident2[:],
                             R=[b_hn, b_id2], W=[bpT[ti]])
                    S.op("act", "copy", hnT[:, :, i * 128:(i + 1) * 128], pT[ti][:, 0:1024].rearrange("p (c n) -> p c n", c=8),
                         R=[bpT[ti]], W=[b_hnT])
                for f in range(32):
                    si = nxt("S", 2)
                    for c in range(8):
                        S.op("pe", "matmul", pS[si][:], lhsT=w_up[:, c, f * 128:(f + 1) * 128], rhs=hnT[:, c, :],
                             start=(c == 0), stop=(c == 7), R=[b_wup, b_hnT], W=[bpS[si]])
                    ri = f % 2
                    S.op("act", "activation", rl[ri][:], pS[si][:], AF.Relu, R=[bpS[si]], W=[b_rl[ri]])
                    S.op("pool", "tensor_tensor", aT[:, f, :], rl[ri][:], rl[ri][:], ALU.mult, R=[b_rl[ri]], W=[b_aT])
                for i in range(4):
                    ob = oc % 2
                    oc += 1
                    for dh in range(2):
                        mi = nxt("M", 2)
                        for f in range(32):
                            S.op("pe", "matmul", pM[mi][:], lhsT=aT[:, f, i * 128:(i + 1) * 128], rhs=w_dn[:, f, dh * 512:(dh + 1) * 512],
                                 start=(f == 0), stop=(f == 31), R=[b_aT, b_wdn], W=[bpM[mi]])
                        S.op("dve", "tensor_tensor", yo[ob][:, dh * 512:(dh + 1) * 512], pM[mi][:], hin[i][:, dh * 512:(dh + 1) * 512],
                             ALU.add, R=[bpM[mi], b_hin[i]], W=[b_yo[ob]])
                    r = rstd2(yo[ob][:], [b_yo[ob]])
                    S.op("dve", "scalar_tensor_tensor", yo[ob][:], yo[ob][:], r, gfin[:], ALU.mult, ALU.mult,
                         R=[b_yo[ob], b_st2, b_gb], W=[b_yo[ob]])
                    S.dma("sp", out_d[base + i * 128:base + (i + 1) * 128, :], yo[ob][:], R=[b_yo[ob]])
            S.emit()
            print('NOPS', S.nops)
    return nc


def _rope_tab(pos, dim):
    inv = np.exp(np.float32(-math.log(500000.0)) * np.arange(0, dim, 2, dtype=np.float32) / np.float32(dim)).astype(np.float32)
    ang = pos.astype(np.float32)[:, None] * inv[None, :]
    return np.cos(ang).astype(np.float32), np.sin(ang).astype(np.float32)


def _tok_major(a):
    return np.ascontiguousarray(a.reshape(NT, 128, -1).transpose(1, 0, 2))


def host_consts():
    pos = np.arange(SEQ)
    cm, sm = _rope_tab(pos, 32)
    cn, sn = _rope_tab(pos, 16)
    c = {}
    c["cos_m"], c["sin_m"] = _tok_major(cm), _tok_major(sm)
    c["cos_n"], c["sin_n"] = _tok_major(cn), _tok_major(sn)
    c["cos_n8"], c["sin_n8"] = _tok_major(cn * np.float32(0.125)), _tok_major(sn * np.float32(0.125))
    ce = np.zeros((8, NT, 8), np.float32)
    se = np.zeros((8, NT, 8), np.float32)
    for t in range(NT):
        for m in range(8):
            n = 8 * t - 1 + m
            if 0 <= n < 255:
                p = 16 * n + 31
                ce[m, t], se[m, t] = cn[p], sn[p]
    c["cos_e"], c["sin_e"] = ce, se
    k = np.arange(128)[:, None]
    q = np.arange(128)[None, :]
    tri = (q >= k).astype(np.float32)
    anti = (q < k).astype(np.float32)
    c["tri4"] = np.ascontiguousarray(np.tile(tri, (1, 4)))
    c["anti4"] = np.ascontiguousarray(np.tile(anti, (1, 4)))
    n = np.arange(256)[:, None] * 16
    j = np.arange(64)[None, :] * 64
    ov = np.clip(np.minimum(n + 32, j + 64) - np.maximum(n, j), 0, None).astype(np.float32) / 32.0
    ov[255] = 0.0
    c["ovl"] = np.ascontiguousarray(ov.reshape(2, 128, 64).transpose(1, 0, 2))
    c["expand"] = (np.arange(SEQ)[None, :] // 64 == np.arange(64)[:, None]).astype(np.float32)
    c["ident"] = np.eye(128, dtype=np.float32)
    return c


def host_weights(inp):
    f = lambda a: np.ascontiguousarray(np.asarray(a, dtype=np.float32))
    pc = lambda v: np.ascontiguousarray(np.asarray(v, np.float32).reshape(-1, 128).T)
    w = {}
    w["w_in"] = f(inp["w_in"][0])
    w["g_mix"] = pc(inp["g_mix_norm"][0])
    w["w_uq"] = f(inp["w_uq"][0])
    w["g_cq"] = pc(inp["g_cq"][0])
    wukv = np.asarray(inp["w_ukv"][0], np.float32).reshape(128, 8, 2, 64)
    wuk = wukv[:, :, 0, :]
    w["wukT"] = np.ascontiguousarray(wuk.transpose(2, 1, 0))
    w["wuv"] = np.ascontiguousarray(wukv[:, :, 1, :])
    w["gckv_bc"] = np.ascontiguousarray(np.broadcast_to(np.asarray(inp["g_ckv"][0], np.float32)[None, :], (128, 128)))
    w1 = np.stack([np.asarray(inp["cmp_w1_k"][0], np.float32), np.asarray(inp["cmp_w1_v"][0], np.float32)])
    w["cmp_w1"] = np.ascontiguousarray(w1.reshape(2, 32, 64, 128).transpose(0, 2, 1, 3))
    pe = np.stack([np.asarray(inp["cmp_pe_k"][0], np.float32), np.asarray(inp["cmp_pe_v"][0], np.float32)])
    w["cmp_peT"] = np.ascontiguousarray(pe.transpose(0, 2, 1))
    w["cmp_b1"] = np.ascontiguousarray(np.stack([inp["cmp_b1_k"][0], inp["cmp_b1_v"][0]], axis=1).astype(np.float32))
    w["cmp_w2"] = np.ascontiguousarray(np.stack([inp["cmp_w2_k"][0], inp["cmp_w2_v"][0]], axis=1).astype(np.float32))
    b2k = np.asarray(inp["cmp_b2_k"][0], np.float32)
    w["cmp_b2k_bc"] = np.ascontiguousarray(np.broadcast_to(np.concatenate([b2k, b2k])[None, :], (128, 128)))
    w["cmp_b2v"] = np.ascontiguousarray(np.asarray(inp["cmp_b2_v"][0], np.float32).reshape(64, 1))
    w["w_o"] = f(inp["w_o"][0])
    w["g_out"] = pc(np.concatenate([np.asarray(inp["g_out_mla"][0]), np.asarray(inp["g_out_nsa"][0])]))
    w["w_up"] = f(inp["w_up"][0])
    w["g_mlp"] = pc(inp["g_mlp_norm"][0])
    w["w_down"] = f(inp["w_down"][0])
    w["gfin_bc"] = np.ascontiguousarray(np.broadcast_to(np.asarray(inp["g_final"], np.float32)[None, :], (128, D)))
    return w


_NC_CACHE = {}


def kernel(**inputs):
    x = np.asarray(inputs["x"], dtype=np.float32)
    shared = host_consts()
    shared.update(host_weights(inputs))
    if "full" not in _NC_CACHE:
        _NC_CACHE["full"] = build_program()
    nc = _NC_CACHE["full"]
    in_maps = []
    for c in range(NCORES):
        m = dict(shared)
        m["x"] = np.ascontiguousarray(x[c * NSEQ:(c + 1) * NSEQ])
        in_maps.append(m)
    res = run_bass_kernel_spmd(nc, in_maps, core_ids=list(range(NCORES)))
    outs = [np.asarray(r["out"]).reshape(NSEQ, SEQ, D) for r in res.results]
    return np.concatenate(outs, axis=0).astype(np.float32)
```

```python
import math
import numpy as np
from contextlib import ExitStack
import concourse.bass as bass
import concourse.mybir as mybir
from concourse.bass_utils import run_bass_kernel_spmd

F32 = mybir.dt.float32
BF16 = mybir.dt.bfloat16
I32 = mybir.dt.int32
AF = mybir.ActivationFunctionType
ALU = mybir.AluOpType
AX = mybir.AxisListType

NCORES = 8
SEQ = 4096
D = 1024
NSEQ = 2
NT = SEQ // 128
EPS = 1e-6
IN_COLS = 1720
DFF = 4096
NEG = -30000.0
STRICT_SAME = True
OP_LIMIT = None
BG_OVERLAP = True
USE_MAGIC = True
ACT_COPY_T = 20


class Buf:
    __slots__ = ("name", "w", "r", "dsem", "dcnt", "excl")

    def __init__(self, name):
        self.name = name
        self.excl = False
        self.w = None
        self.r = {}
        self.dsem = None
        self.dcnt = 0


class Sched:
    ENG = ("pe", "act", "dve", "pool", "sp")

    def __init__(self, nc, ctx):
        self.nc = nc
        self.ctx = ctx
        self.sem = {e: ctx.enter_context(nc.semaphore("s_" + e)) for e in self.ENG}
        self.cnt = {e: 0 for e in self.ENG}
        self.seen = {e: {} for e in self.ENG}
        self.prog = {e: [] for e in self.ENG}
        self.dbufs = []
        self.nb = 0
        self.nops = 0
        self.fillregs = {}
        self.tick = None
        self.limit = OP_LIMIT

    def buf(self, name):
        self.nb += 1
        return Buf("%s_%d" % (name, self.nb))

    def bufs(self, name, n):
        return [self.buf(name) for _ in range(n)]

    def _dsem(self, b):
        if b.dsem is None:
            b.dsem = self.ctx.enter_context(self.nc.semaphore("d_" + b.name))
            self.dbufs.append(b)
        return b.dsem

    def _deps(self, e, reads, writes, strict):
        toks = []
        for b in reads:
            if b.w is not None:
                toks.append(b.w)
            if b.excl:
                toks.extend(b.r.values())
        for b in writes:
            if b.w is not None:
                toks.append(b.w)
            toks.extend(b.r.values())
        need = {}
        for (key, sem, val) in toks:
            if key == e and not strict and (e == "pe" or not STRICT_SAME):
                continue
            if self.seen[e].get(key, 0) >= val:
                continue
            if key not in need or need[key][1] < val:
                need[key] = (sem, val)
        for key, (sem, val) in need.items():
            self.seen[e][key] = val
            self.prog[e].append(("wait", sem, val))

    def op(self, e, meth, *args, R=(), W=(), **kw):
        self.nops += 1
        if self.limit is not None and self.nops > self.limit:
            return None
        self._deps(e, R, W, False)
        self.cnt[e] += 1
        tok = (e, self.sem[e], self.cnt[e])
        self.prog[e].append(("op", meth, args, kw))
        for b in R:
            b.r[e] = tok
        for b in W:
            b.w = tok
            b.r = {}
        if self.tick is not None:
            self.tick()
        return tok

    def dma(self, q, out, in_, R=(), W=(), **kw):
        self.nops += 1
        if self.limit is not None and self.nops > self.limit:
            return None
        self._deps(q, R, W, True)
        owner = W[0] if W else R[0]
        sem = self._dsem(owner)
        owner.dcnt += 16
        tok = ("d_" + owner.name, sem, owner.dcnt)
        self.prog[q].append(("dma", out, in_, kw, sem))
        for b in R:
            b.r[tok[0]] = tok
        for b in W:
            b.w = tok
            b.r = {}
        return tok

    def barrier(self):
        toks = [(e, self.sem[e], self.cnt[e]) for e in self.ENG if self.cnt[e] > 0]
        toks += [("d_" + b.name, b.dsem, b.dcnt) for b in self.dbufs if b.dcnt > 0]
        for e in self.ENG:
            for (key, sem, val) in toks:
                if self.seen[e].get(key, 0) >= val:
                    continue
                self.seen[e][key] = val
                self.prog[e].append(("wait", sem, val))

    def flush(self):
        nc = self.nc
        with nc.Block() as block:
            def replay(e):
                def f(eng):
                    sem_e = self.sem[e]
                    for it in self.prog[e]:
                        if it[0] == "wait":
                            eng.wait_ge(it[1], it[2])
                        elif it[0] == "op":
                            args = it[2]
                            if it[1] == "affine_select":
                                args = list(args)
                                if args[4] not in self.fillregs:
                                    self.fillregs[args[4]] = eng.to_reg(args[4])
                                args[4] = self.fillregs[args[4]]
                            getattr(eng, it[1])(*args, **it[3]).then_inc(sem_e, 1)
                        else:
                            eng.dma_start(out=it[1], in_=it[2], **it[3]).then_inc(it[4], 16)
                return f
            block.tensor(replay("pe"))
            block.scalar(replay("act"))
            block.vector(replay("dve"))
            block.gpsimd(replay("pool"))
            block.sync(replay("sp"))
        self.prog = {e: [] for e in self.ENG}

    def emit(self):
        self.barrier()
        self.flush()


def build_program(nseq=NSEQ, ntiles=NT, do_mlp=True, stages=("p1", "mla", "nsa", "out")):
    nc = bass.Bass("TRN2", target_bir_lowering=False)

    def din(name, shape):
        return nc.dram_tensor(name, list(shape), F32, kind="ExternalInput").ap()

    x_d = din("x", [nseq, SEQ, D])
    w_in_d = din("w_in", [D, IN_COLS])
    g_mix_d = din("g_mix", [128, 8])
    w_uq_d = din("w_uq", [256, 768])
    g_cq_d = din("g_cq", [128, 2])
    wukT_d = din("wukT", [64, 8, 128])
    wuv_d = din("wuv", [128, 8, 64])
    gckv_d = din("gckv_bc", [128, 128])
    w1_d = din("cmp_w1", [2, 64, 32, 128])
    peT_d = din("cmp_peT", [2, 64, 32])
    b1_d = din("cmp_b1", [128, 2])
    w2_d = din("cmp_w2", [128, 2, 64])
    b2k_d = din("cmp_b2k_bc", [128, 128])
    b2v_d = din("cmp_b2v", [64, 1])
    w_o_d = din("w_o", [D, D])
    g_out_d = din("g_out", [128, 8])
    w_up_d = din("w_up", [D, DFF])
    g_mlp_d = din("g_mlp", [128, 8])
    w_down_d = din("w_down", [DFF, D])
    gfin_d = din("gfin_bc", [128, D])
    cosm_d = din("cos_m", [128, NT, 16])
    sinm_d = din("sin_m", [128, NT, 16])
    cosn_d = din("cos_n", [128, NT, 8])
    sinn_d = din("sin_n", [128, NT, 8])
    cosn8_d = din("cos_n8", [128, NT, 8])
    sinn8_d = din("sin_n8", [128, NT, 8])
    cose_d = din("cos_e", [8, NT, 8])
    sine_d = din("sin_e", [8, NT, 8])
    tri_d = din("tri4", [128, 512])
    anti_d = din("anti4", [128, 512])
    ovl_d = din("ovl", [128, 2, 64])
    exp_d = din("expand", [64, SEQ])
    ident_d = din("ident", [128, 128])
    out_d = nc.dram_tensor("out", [nseq * SEQ, D], F32, kind="ExternalOutput").ap()
    hscr_d = nc.dram_tensor("hscr", [nseq * SEQ, D], F32, kind="Internal").ap()

    top = ExitStack()
    with top:
        S = Sched(nc, top)
        def psum(name, shape, dt):
            return top.enter_context(nc.psum_tensor(name, shape, dt))
        pS = [psum("pS%d" % i, [128, 512], F32) for i in range(2)]
        pA = [psum("pA%d" % i, [128, 512], F32) for i in range(2)]
        pM = [psum("pM%d" % i, [128, 512], F32) for i in range(2)]
        pT = [psum("pT%d" % i, [128, 1024], BF16) for i in range(2)]
        bpS = S.bufs("pS", 2)
        bpA = S.bufs("pA", 2)
        bpM = S.bufs("pM", 2)
        bpT = S.bufs("pT", 2)
        for b_ in bpS + bpA + bpM + bpT:
            b_.excl = True
        rr = {"S": 0, "M": 0, "T": 0, "P": 0, "A": 0}
        mode = {"bg": False, "B": False}

        def nxt(kind, n):
            if kind in ("M", "T") and not mode["B"]:
                return 0 if mode["bg"] else 1
            i = rr[kind] % n
            rr[kind] += 1
            return i

        A = ExitStack()
        with A:
            def sb(name, shape, dt):
                return A.enter_context(nc.sbuf_tensor("a_" + name, shape, dt))

            ident = sb("ident", [128, 128], BF16); b_ident = S.buf("ident")
            S.dma("pool", ident[:], ident_d, W=[b_ident])
            tri4 = sb("tri4", [128, 512], BF16); anti4 = sb("anti4", [128, 512], BF16); b_msk = S.buf("msk")
            S.dma("pool", tri4[:], tri_d, W=[b_msk])
            S.dma("pool", anti4[:], anti_d, W=[b_msk])
            tabs = {}
            b_tab = S.buf("tab")
            for nm, d_, w_ in (("cos_m", cosm_d, 16), ("sin_m", sinm_d, 16), ("cos_n", cosn_d, 8), ("sin_n", sinn_d, 8)):
                tabs[nm] = sb(nm, [128, NT, w_], F32)
                S.dma("sp", tabs[nm][:], d_, W=[b_tab])
            cos_e = sb("cos_e", [8, NT, 8], F32); sin_e = sb("sin_e", [8, NT, 8], F32)
            S.dma("sp", cos_e[:], cose_d, W=[b_tab])
            S.dma("sp", sin_e[:], sine_d, W=[b_tab])
            gckv = sb("gckv", [128, 128], F32); b2k = sb("b2k", [128, 128], F32); b2v = sb("b2v", [64, 1], F32)
            b1 = sb("b1", [128, 2], F32)
            S.dma("sp", gckv[:], gckv_d, W=[b_tab])
            S.dma("sp", b2k[:], b2k_d, W=[b_tab])
            S.dma("sp", b2v[:], b2v_d, W=[b_tab])
            S.dma("sp", b1[:], b1_d, W=[b_tab])
            gvec = sb("gvec", [128, 24], F32)
            S.dma("sp", gvec[:, 0:8], g_mix_d, W=[b_tab])
            S.dma("sp", gvec[:, 8:10], g_cq_d, W=[b_tab])
            S.dma("sp", gvec[:, 10:18], g_out_d, W=[b_tab])

            w_in = sb("w_in", [128, 8, IN_COLS], BF16); b_win = S.buf("w_in")
            S.dma("pool", w_in[:], w_in_d.rearrange("(c p) n -> p c n", p=128), W=[b_win])
            for c in range(8):
                S.op("dve", "tensor_scalar", w_in[:, c, :], w_in[:, c, :], gvec[:, c:c + 1], None, ALU.mult,
                     R=[b_tab, b_win], W=[b_win])
                S.op("dve", "tensor_scalar", w_in[:, c, 416:928], w_in[:, c, 416:928], 0.125, None, ALU.mult,
                     R=[b_win], W=[b_win])
            w_o = sb("w_o", [128, 8, D], BF16); b_wo = S.buf("w_o")
            S.dma("pool", w_o[:], w_o_d.rearrange("(c p) n -> p c n", p=128), W=[b_wo])
            for c in range(8):
                S.op("dve", "tensor_scalar", w_o[:, c, :], w_o[:, c, :], gvec[:, 10 + c:11 + c], None, ALU.mult,
                     R=[b_tab, b_wo], W=[b_wo])
            w_uq = sb("w_uq", [128, 2, 768], BF16); b_wuq = S.buf("w_uq")
            S.dma("pool", w_uq[:], w_uq_d.rearrange("(c p) n -> p c n", p=128), W=[b_wuq])
            for c in range(2):
                S.op("dve", "tensor_scalar", w_uq[:, c, :], w_uq[:, c, :], gvec[:, 8 + c:9 + c], 96.0 ** -0.5,
                     ALU.mult, ALU.mult, R=[b_tab, b_wuq], W=[b_wuq])
            wukT = sb("wukT", [64, 8, 128], BF16); wuv = sb("wuv", [128, 8, 64], BF16); b_wkv = S.buf("wkv")
            S.dma("pool", wukT[:], wukT_d, W=[b_wkv])
            S.dma("pool", wuv[:], wuv_d, W=[b_wkv])
            w1 = sb("w1", [64, 2, 32, 128], BF16); b_w1 = S.buf("w1")
            for kv in range(2):
                S.dma("pool", w1[:, kv], w1_d[kv], W=[b_w1])
            peT = sb("peT", [64, 2, 32], BF16)
            for kv in range(2):
                S.dma("pool", peT[:, kv], peT_d[kv], W=[b_w1])
            w2 = sb("w2", [128, 2, 64], BF16)
            S.dma("pool", w2[:], w2_d, W=[b_w1])

            bias_tot = sb("bias_tot", [128, 2], F32); b_bt = S.buf("bias_tot")
            for kv in range(2):
                for l in range(32):
                    S.op("pe", "matmul", pM[0][:, kv:kv + 1], lhsT=w1[:, kv, l, :], rhs=peT[:, kv, l:l + 1],
                         start=(l == 0), stop=(l == 31), R=[b_w1], W=[bpM[0]])
            S.op("dve", "tensor_tensor", bias_tot[:], pM[0][:, 0:2], b1[:], ALU.add, R=[bpM[0], b_tab], W=[b_bt])

            KlatT = sb("KlatT", [128, SEQ], BF16); bKlat = S.bufs("Klat", NT)
            KpeT = sb("KpeT", [32, SEQ], BF16); bKpe = S.bufs("Kpe", NT)
            Clat = sb("Clat", [128, NT, 130], BF16); bClat = S.bufs("Clat", NT)
            KsE = sb("KsE", [128, 2, SEQ], BF16); bKs = S.bufs("Ks", NT); b_exp = S.buf("expand")
            Vs = sb("Vs", [128, NT, 2, 65], BF16); bVs = S.bufs("Vs", NT)
            KwT = sb("KwT", [64, 2, 8 * 128], BF16); bKw = S.bufs("Kw", 8)
            Vw = sb("Vw", [128, 8, 2, 65], BF16); bVw = S.bufs("Vw", 8)
            KcT = sb("KcT", [64, 2, 256], BF16); b_Kc = S.buf("Kc")
            VcT = sb("VcT", [64, 2, 256], BF16); b_VcT = S.buf("VcT")
            VcO = sb("VcO", [128, 2, 2, 128], BF16); b_VcO = S.buf("VcO")
            for g in range(2):
                S.dma("pool", KsE[64:128, g, :], exp_d, W=[b_exp])
            for nt in range(2):
                for g in range(2):
                    S.dma("pool", VcO[:, nt, g, 64:128], ovl_d[:, nt, :], W=[b_VcO])
            S.op("pool", "memset", Clat[:, :, 128:130], 1.0, W=bClat)
            S.op("pool", "memset", Vs[:, :, :, 64:65], 1.0, W=bVs)
            S.op("pool", "memset", Vw[:, :, :, 64:65], 1.0, W=bVw)

            xs = [sb("xs%d" % i, [128, D], F32) for i in range(2)]; b_xs = S.bufs("xs", 2)
            st = sb("st", [128, 64], F32); b_st = S.buf("st")
            xn = sb("xn", [128, D], BF16); b_xn = S.buf("xn")
            xnT = sb("xnT", [128, 8, 128], BF16); b_xnT = S.buf("xnT")
            u = sb("u", [128, IN_COLS], F32); b_u = S.buf("u")
            cqn = sb("cqn", [128, 256], BF16); b_cqn = S.buf("cqn")
            cqnT = sb("cqnT", [128, 2, 128], BF16); b_cqnT = S.buf("cqnT")
            q_sb = sb("q_sb", [128, 9, 96], F32); b_q = S.buf("q")
            qn_sb = sb("qn_sb", [128, 8, 64], BF16); b_qn = S.buf("qn")
            qpe_sb = sb("qpe_sb", [128, 9, 32], BF16); b_qpe = S.buf("qpe")
            rt = [sb("rt%d" % i, [128, 160], F32) for i in range(4)]; b_rt = S.buf("rt")
            QnT = sb("QnT", [64, 8, 128], BF16); b_QnT = S.buf("QnT")
            QpeT2 = [sb("QpeT%d" % i, [32, 8, 128], BF16) for i in range(2)]; b_QpeT2 = S.bufs("QpeT", 2)
            QabsT2 = [sb("QabsT%d" % i, [128, 8, 128], BF16) for i in range(2)]; b_Qabs2 = S.bufs("Qabs", 2)
            ub = sb("ub", [128, 20, 64], BF16); b_ub = S.buf("ub")
            QS2 = [sb("QS%d" % i, [128, 2, 4, 128], BF16) for i in range(2)]; b_QSq2 = S.bufs("QSq", 2); b_QSs2 = [S.bufs("QSs", 2) for _ in range(2)]
            rawT = [sb("rawT%d" % i, [64, 2, 2, 144], BF16) for i in range(2)]; b_rawT = S.bufs("rawT", 2)
            gate2 = [sb("gate%d" % i, [128, 3, 8], F32) for i in range(2)]; b_gate2 = S.bufs("gate", 2)
            z_sb = sb("z_sb", [128, 32], F32); z2_sb = sb("z2_sb", [128, 32], F32); b_z = S.buf("z")
            hid_sb = sb("hid_sb", [128, 32], BF16); b_hid = S.buf("hid")
            kc_f = sb("kc_f", [8, 2, 64], F32); kc_sb = sb("kc_sb", [8, 2, 64], BF16); b_kc = S.buf("kc")
            PT = [sb("PT%d" % i, [128, 512], BF16) for i in range(3)]; b_PT = S.bufs("PT", 3)
            PcT = [sb("PcT%d" % i, [128, 512], BF16) for i in range(2)]; b_PcT = S.bufs("PcT", 2)
            OlatT = sb("OlatT", [128, 4, 128], BF16); b_OlatT = S.buf("OlatT")
            y_sb = sb("y_sb", [128, D], F32); b_y = S.buf("y"); b_yn = S.buf("yn")
            imp = sb("imp", [128, 64], F32); sc1 = sb("sc1", [128, 64], F32); sc2 = sb("sc2", [128, 64], F32)
            sc3 = sb("sc3", [128, 64], F32); b_imp = S.buf("imp")
            selq = sb("selq", [128, 128], BF16); b_selq = S.buf("selq")
            tmp4 = sb("tmp4", [128, 4, 64], F32); b_tmp4 = S.buf("tmp4")
            mixed = sb("mixed", [128, D], BF16); b_mixed = S.buf("mixed")
            mixedT = sb("mixedT", [128, 8, 128], BF16); b_mixedT = S.buf("mixedT")
            h_sb = [sb("h_sb%d" % i, [128, D], F32) for i in range(2)]; b_h = S.bufs("h", 2)

            S.op("pool", "memset", selq[:], 0.0, W=[b_selq])
            for i in range(2):
                S.op("pool", "memset", rawT[i][:], 0.0, W=[b_rawT[i]])
            S.op("pool", "memset", KcT[:], 0.0, W=[b_Kc])
            S.op("pool", "memset", VcT[:], 0.0, W=[b_VcT])
            S.op("pool", "memset", VcO[:, :, :, 0:64], 0.0, W=[b_VcO])

            b_stc = {0: S.buf("st0"), 4: S.buf("st4"), 12: S.buf("st12")}
            b_stm = S.buf("stm")
            b_stn = S.buf("stn")
            rl = sb("rl", [128, 8], F32); b_rl = S.buf("rl")
            lacc = sb("lacc", [128, 512], F32); b_lacc = S.buf("lacc")
            ones_f = sb("ones_f", [128, 1], F32); b_ones = S.buf("ones")
            S.op("pool", "memset", ones_f[:], 1.0, W=[b_ones])

            def rstd_multi(items, col, Rb, jk, b_jk):
                b_st = b_stc[col]
                k = len(items)
                S.op("dve", "memset", st[:, col:col + k], 0.0, W=[b_st])
                for i_, (src_ap, n) in enumerate(items):
                    S.op("dve", "scalar_tensor_tensor", jk[:, 0:n], src_ap, 1.0, src_ap, ALU.mult, ALU.mult,
                         accum_out=st[:, col + i_:col + i_ + 1], R=Rb + [b_st], W=[b_jk, b_st])
                    S.op("dve", "tensor_scalar", st[:, col + k + i_:col + k + i_ + 1], st[:, col + i_:col + i_ + 1], 1.0 / n, EPS,
                         ALU.mult, ALU.add, R=[b_st], W=[b_st])
                v_ = st[:, col + k:col + 2 * k]
                y_ = st[:, col + 2 * k:col + 3 * k]
                w_ = st[:, col + 3 * k:col + 4 * k]
                if not USE_MAGIC:
                    S.op("act", "activation", w_, v_, AF.Sqrt, R=[b_st], W=[b_st])
                    S.op("dve", "reciprocal", y_, w_, R=[b_st], W=[b_st])
                    return [st[:, col + 2 * k + i_:col + 2 * k + i_ + 1] for i_ in range(k)]
                S.op("dve", "tensor_scalar", y_.bitcast(I32), v_.bitcast(I32), -0.5, 1597463007.0, ALU.mult, ALU.add, R=[b_st], W=[b_st])
                for _it in range(2):
                    S.op("pool", "tensor_tensor", w_, y_, y_, ALU.mult, R=[b_st], W=[b_st])
                    S.op("pool", "tensor_tensor", w_, w_, v_, ALU.mult, R=[b_st], W=[b_st])
                    S.op("pool", "tensor_scalar", w_, w_, -0.5, 1.5, ALU.mult, ALU.add, R=[b_st], W=[b_st])
                    S.op("pool", "tensor_tensor", y_, y_, w_, ALU.mult, R=[b_st], W=[b_st])
                return [st[:, col + 2 * k + i_:col + 2 * k + i_ + 1] for i_ in range(k)]

            def rope(eng, out_ap, in_ap, cos_ap, sin_ap, nh, half, Rb, Wb):
                P = in_ap.shape[0]
                x1 = in_ap[:, :, 0:half]
                x2 = in_ap[:, :, half:2 * half]
                cb = cos_ap[:, None, :].to_broadcast([P, nh, half])
                sbb = sin_ap[:, None, :].to_broadcast([P, nh, half])
                t = [r_[0:P, 0:nh * half].rearrange("p (h d) -> p h d", h=nh) for r_ in rt]
                S.op(eng, "tensor_tensor", t[0], x1, cb, ALU.mult, R=Rb + [b_tab], W=[b_rt])
                S.op(eng, "tensor_tensor", t[1], x2, sbb, ALU.mult, R=Rb + [b_tab], W=[b_rt])
                S.op(eng, "tensor_tensor", t[2], x2, cb, ALU.mult, R=Rb + [b_tab], W=[b_rt])
                S.op(eng, "tensor_tensor", t[3], x1, sbb, ALU.mult, R=Rb + [b_tab], W=[b_rt])
                S.op(eng, "tensor_tensor", out_ap[:, :, 0:half], t[0], t[1], ALU.subtract, R=[b_rt], W=Wb)
                S.op(eng, "tensor_tensor", out_ap[:, :, half:2 * half], t[2], t[3], ALU.add, R=[b_rt], W=Wb)

            def transpose_to(ps_i, col0, in_ap, Rb):
                P, Fd = in_ap.shape[0], in_ap.shape[1]
                S.op("pe", "transpose", pT[ps_i][0:Fd, col0:col0 + P], in_ap, ident[0:P, 0:P],
                     R=Rb + [b_ident], W=[bpT[ps_i]])

            def phase1(s, t):
                par = t % 2
                QpeT, b_QpeT = QpeT2[par], b_QpeT2[par]
                QabsT, b_Qabs = QabsT2[par], b_Qabs2[par]
                QS, b_QSq, b_QSs = QS2[par], b_QSq2[par], b_QSs2[par]
                gate, b_gate = gate2[par], b_gate2[par]
                xb = t % 2
                ce, cm = ("act", "copy") if t < ACT_COPY_T else ("dve", "tensor_copy")
                S.dma("sp", xs[xb][:], x_d[s, t * 128:(t + 1) * 128, :], W=[b_xs[xb]])
                r0 = rstd_multi([(xs[xb][:], D)], 0, [b_xs[xb]], xn, b_xn)[0]
                S.op("dve", "tensor_scalar", xn[:], xs[xb][:], r0, None, ALU.mult, R=[b_xs[xb], b_stc[0]], W=[b_xn])
                ti = nxt("T", 2)
                for c in range(8):
                    transpose_to(ti, c * 128, xn[:, c * 128:(c + 1) * 128], [b_xn])
                S.op(ce, cm, xnT[:], pT[ti][:, 0:1024].rearrange("p (c n) -> p c n", c=8), R=[bpT[ti]], W=[b_xnT])
                for cg, (c0, c1) in enumerate(((0, 512), (512, 1024), (1024, 1536), (1536, IN_COLS))):
                    mi = nxt("M", 2)
                    for c in range(8):
                        S.op("pe", "matmul", pM[mi][:, 0:c1 - c0], lhsT=xnT[:, c, :], rhs=w_in[:, c, c0:c1],
                             start=(c == 0), stop=(c == 7), R=[b_xnT, b_win], W=[bpM[mi]])
                    S.op(ce, cm,
                         u[:, c0:c1], pM[mi][:, 0:c1 - c0], R=[bpM[mi]], W=[b_u])
                    yield

                yield
                r1, r2 = rstd_multi([(u[:, 0:256], 256), (u[:, 256:384], 128)], 4, [b_u], cqn, b_cqn)
                S.op("dve", "tensor_scalar", cqn[:], u[:, 0:256], r1, None, ALU.mult, R=[b_u, b_stc[4]], W=[b_cqn])
                ti = nxt("T", 2)
                for c in range(2):
                    transpose_to(ti, c * 128, cqn[:, c * 128:(c + 1) * 128], [b_cqn])
                S.op("dve", "tensor_copy", cqnT[:], pT[ti][:, 0:256].rearrange("p (c n) -> p c n", c=2), R=[bpT[ti]], W=[b_cqnT])
                yield
                for half in range(2):
                    mi = nxt("M", 2)
                    for c in range(2):
                        S.op("pe", "matmul", pM[mi][:, 0:384], lhsT=cqnT[:, c, :], rhs=w_uq[:, c, half * 384:(half + 1) * 384],
                             start=(c == 0), stop=(c == 1), R=[b_cqnT, b_wuq], W=[bpM[mi]])
                    S.op(ce, cm, q_sb[:, half * 4:(half + 1) * 4, :],
                         pM[mi][:, 0:384].rearrange("p (h d) -> p h d", h=4), R=[bpM[mi]], W=[b_q])
                yield
                S.op("dve", "tensor_copy", q_sb[:, 8, 64:96], u[:, 384:416], R=[b_u], W=[b_q])
                S.op("dve", "tensor_copy", qn_sb[:], q_sb[:, 0:8, 0:64], R=[b_q], W=[b_qn])
                rope("dve", qpe_sb[:], q_sb[:, :, 64:96], tabs["cos_m"][:, t, :], tabs["sin_m"][:, t, :], 9, 16, [b_q], [b_qpe])
                ti = nxt("T", 2)
                for h in range(8):
                    transpose_to(ti, h * 128, qn_sb[:, h, :], [b_qn])
                S.op(ce, cm, QnT[:], pT[ti][0:64, 0:1024].rearrange("p (c n) -> p c n", c=8), R=[bpT[ti]], W=[b_QnT])
                ti = nxt("T", 2)
                for h in range(8):
                    transpose_to(ti, h * 128, qpe_sb[:, h, :], [b_qpe])
                S.op(ce, cm, QpeT[:], pT[ti][0:32, 0:1024].rearrange("p (c n) -> p c n", c=8), R=[bpT[ti]], W=[b_QpeT])
                yield
                for hg in range(2):
                    mi = nxt("M", 2)
                    for j in range(4):
                        h = hg * 4 + j
                        S.op("pe", "matmul", pM[mi][:, j * 128:(j + 1) * 128], lhsT=wukT[:, h, :],
                             rhs=QnT[:, h, :], start=True, stop=True, R=[b_wkv, b_QnT], W=[bpM[mi]])
                    S.op(ce, cm, QabsT[:, hg * 4:(hg + 1) * 4, :],
                         pM[mi][:, 0:512].rearrange("p (c n) -> p c n", c=4), R=[bpM[mi]], W=[b_Qabs])

                yield
                S.op("dve", "scalar_tensor_tensor", Clat[:, t, 0:128], u[:, 256:384], r2, gckv[:], ALU.mult, ALU.mult,
                     R=[b_u, b_stc[4], b_tab], W=[bClat[t]])
                ti = nxt("T", 2)
                transpose_to(ti, 0, Clat[:, t, 0:128], [bClat[t]])
                S.op("dve", "tensor_copy", KlatT[:, t * 128:(t + 1) * 128], pT[ti][:, 0:128], R=[bpT[ti]], W=[bKlat[t]])
                ti = nxt("T", 2)
                transpose_to(ti, 0, qpe_sb[:, 8, :], [b_qpe])
                S.op("dve", "tensor_copy", KpeT[:, t * 128:(t + 1) * 128], pT[ti][0:32, 0:128], R=[bpT[ti]], W=[bKpe[t]])

                yield
                uv = u[:, 416:1696].rearrange("p (b d) -> p b d", b=20)
                S.op("dve", "tensor_copy", ub[:, :, 16:64], uv[:, :, 16:64], R=[b_u], W=[b_ub])
                rope("dve", ub[:, :, 0:16], uv[:, :, 0:16], tabs["cos_n"][:, t, :], tabs["sin_n"][:, t, :], 20, 8, [b_u], [b_ub])
                S.op("dve", "tensor_copy", ub[:, 8:12, 0:16], uv[:, 8:12, 0:16], R=[b_u, b_ub], W=[b_ub])
                ti = nxt("T", 2)
                for h in range(8):
                    transpose_to(ti, h * 128, ub[:, h, :], [b_ub])
                S.op(ce, cm, QS[0:64].rearrange("p g j n -> p (g j) n"),
                     pT[ti][0:64, 0:1024].rearrange("p (c n) -> p c n", c=8), R=[bpT[ti]], W=[b_QSq])
                yield
                kvv = u[:, 928:1696].rearrange("p (s g d) -> p s g d", s=6, g=2)
                S.op("dve", "tensor_copy", Vs[:, t, :, 0:64], kvv[:, 3], R=[b_u], W=[bVs[t]])
                S.op("dve", "tensor_copy", Vw[:, t % 8, :, 0:64], kvv[:, 5], R=[b_u], W=[bVw[t % 8]])
                ti = nxt("T", 2)
                for g in range(2):
                    transpose_to(ti, g * 128, ub[:, 12 + g, :], [b_ub])
                    transpose_to(ti, 256 + g * 128, ub[:, 16 + g, :], [b_ub])
                S.op(ce, cm, KsE[0:64, :, t * 128:(t + 1) * 128],
                     pT[ti][0:64, 0:256].rearrange("p (g n) -> p g n", g=2), R=[bpT[ti]], W=[bKs[t]])
                S.op("dve", "tensor_copy", KwT[:, :, (t % 8) * 128:(t % 8 + 1) * 128],
                     pT[ti][0:64, 256:512].rearrange("p (g n) -> p g n", g=2), R=[bpT[ti]], W=[bKw[t % 8]])
                yield
                rb = t % 2
                if t == 0:
                    S.op("pool", "memset", rawT[rb][:, :, :, 0:16], 0.0, W=[b_rawT[rb]])
                else:
                    S.op("dve", "tensor_copy", rawT[rb][:, :, :, 0:16], rawT[1 - rb][:, :, :, 128:144],
                         R=[b_rawT[1 - rb]], W=[b_rawT[rb]])
                ti = nxt("T", 2)
                for c in range(4):
                    transpose_to(ti, c * 128, ub[:, 8 + c, :], [b_ub])
                S.op(ce, cm, rawT[rb][:, :, :, 16:144],
                     pT[ti][0:64, 0:512].rearrange("p (k g n) -> p k g n", k=2, g=2), R=[bpT[ti]], W=[b_rawT[rb]])
                yield
                S.op("act", "activation", gate[:].rearrange("p b h -> p (b h)"), u[:, 1696:1720], AF.Tanh, scale=0.5, R=[b_u], W=[b_gate])
                S.op("dve", "tensor_scalar", gate[:].rearrange("p b h -> p (b h)"), gate[:].rearrange("p b h -> p (b h)"), 0.5, 0.5,
                     ALU.mult, ALU.add, R=[b_gate], W=[b_gate])

                yield
                mi = nxt("M", 2)
                for kv in range(2):
                    for g in range(2):
                        c0 = (kv * 2 + g) * 8
                        for l in range(32):
                            S.op("pe", "matmul", pM[mi][:, c0:c0 + 8], lhsT=w1[:, kv, l, :], rhs=rawT[rb][:, kv, g, l:l + 113:16],
                                 start=(l == 0), stop=(l == 31), R=[b_w1, b_rawT[rb]], W=[bpM[mi]])
                            if l % 8 == 7:
                                yield
                for kv in range(2):
                    S.op("dve", "tensor_scalar", z_sb[:, kv * 16:(kv + 1) * 16], pM[mi][:, kv * 16:(kv + 1) * 16],
                         bias_tot[:, kv:kv + 1], None, ALU.add, R=[bpM[mi], b_bt], W=[b_z])
                S.op("dve", "tensor_tensor", z2_sb[:], z_sb[:], z_sb[:], ALU.mult, R=[b_z], W=[b_z])
                S.op("dve", "tensor_scalar", z2_sb[:], z2_sb[:], 0.044715, 1.0, ALU.mult, ALU.add, R=[b_z], W=[b_z])
                S.op("dve", "tensor_tensor", z2_sb[:], z2_sb[:], z_sb[:], ALU.mult, R=[b_z], W=[b_z])
                S.op("act", "activation", z2_sb[:], z2_sb[:], AF.Tanh, scale=math.sqrt(2.0 / math.pi), R=[b_z], W=[b_z])
                S.op("dve", "tensor_scalar", z2_sb[:], z2_sb[:], 0.5, 0.5, ALU.mult, ALU.add, R=[b_z], W=[b_z])
                S.op("dve", "tensor_tensor", hid_sb[:], z_sb[:], z2_sb[:], ALU.mult, R=[b_z], W=[b_hid])
                yield
                n0 = 8 * t - 1
                m0 = 1 if t == 0 else 0
                mi = nxt("M", 2)
                for g in range(2):
                    S.op("pe", "matmul", pM[mi][0:8, g * 64:(g + 1) * 64], lhsT=hid_sb[:, g * 8:(g + 1) * 8], rhs=w2[:, 0, :],
                         start=True, stop=True, R=[b_hid, b_w1], W=[bpM[mi]])
                for g in range(2):
                    S.op("pe", "matmul", pM[mi][0:64, 128 + g * 8:136 + g * 8], lhsT=w2[:, 1, :], rhs=hid_sb[:, 16 + g * 8:24 + g * 8],
                         start=True, stop=True, R=[b_hid, b_w1], W=[bpM[mi]])
                S.op("dve", "tensor_tensor", kc_f[:].rearrange("p g d -> p (g d)"), pM[mi][0:8, 0:128], b2k[0:8, :], ALU.add,
                     R=[bpM[mi], b_tab], W=[b_kc])
                S.op("dve", "tensor_scalar", VcT[:, :, n0 + m0:n0 + 8], pM[mi][0:64, 128:144].rearrange("p (g m) -> p g m", g=2)[:, :, m0:8],
                     b2v[:, 0:1], None, ALU.add, R=[bpM[mi], b_tab], W=[b_VcT])
                S.op("dve", "tensor_copy", kc_sb[:, :, 16:64], kc_f[:, :, 16:64], R=[b_kc], W=[b_kc])
                rope("dve", kc_sb[:, :, 0:16], kc_f[:, :, 0:16], cos_e[:, t, :], sin_e[:, t, :], 2, 8, [b_kc], [b_kc])
                ti = nxt("T", 2)
                for g in range(2):
                    transpose_to(ti, g * 8, kc_sb[:, g, :], [b_kc])
                S.op("dve", "tensor_copy", KcT[:, :, n0 + m0:n0 + 8],
                     pT[ti][0:64, 0:16].rearrange("p (g m) -> p g m", g=2)[:, :, m0:8], R=[bpT[ti]], W=[b_Kc])
                yield
                nts = sorted(set([max(n0, 0) // 128, (n0 + 7) // 128]))
                for nt in nts:
                    ti = nxt("T", 2)
                    for g in range(2):
                        S.op("pe", "transpose", pT[ti][:, g * 64:(g + 1) * 64], VcT[:, g, nt * 128:(nt + 1) * 128], ident[0:64, 0:64],
                             R=[b_VcT, b_ident], W=[bpT[ti]])
                    S.op("dve", "tensor_copy", VcO[:, nt, :, 0:64], pT[ti][:, 0:128].rearrange("p (g d) -> p g d", g=2), R=[bpT[ti]], W=[b_VcO])

            def attn_loop(kts, qk_fn, post_fn):
                if not kts:
                    return
                si_next = qk_fn(kts[0])
                for i_, kt in enumerate(kts):
                    si = si_next
                    if i_ + 1 < len(kts):
                        si_next = qk_fn(kts[i_ + 1])
                    post_fn(kt, si)

            def mla(s, t):
                par = t % 2
                QpeT, b_QpeT = QpeT2[par], b_QpeT2[par]
                QabsT, b_Qabs = QabsT2[par], b_Qabs2[par]
                QS, b_QSq, b_QSs = QS2[par], b_QSq2[par], b_QSs2[par]
                gate, b_gate = gate2[par], b_gate2[par]
                mo = nxt("M", 2)
                for hg in range(2):
                    qa = QabsT[:, hg * 4:(hg + 1) * 4, :].rearrange("p c n -> p (c n)")
                    qp = QpeT[:, hg * 4:(hg + 1) * 4, :].rearrange("p c n -> p (c n)")

                    def qk(kt):
                        si = nxt("S", 2)
                        S.op("pe", "matmul", pS[si][:], lhsT=KlatT[:, kt * 128:(kt + 1) * 128], rhs=qa,
                             start=True, stop=False, R=[bKlat[kt], b_Qabs], W=[bpS[si]])
                        S.op("pe", "matmul", pS[si][:], lhsT=KpeT[:, kt * 128:(kt + 1) * 128], rhs=qp,
                             start=False, stop=True, R=[bKpe[kt], b_QpeT], W=[bpS[si]])
                        return si

                    def post(kt, si):
                        pi = nxt("P", 3)
                        S.op("act", "activation", PT[pi][:], pS[si][:], AF.Exp, R=[bpS[si]], W=[b_PT[pi]])
                        if kt == t:
                            S.op("dve", "tensor_tensor", PT[pi][:], PT[pi][:], tri4[:], ALU.mult, R=[b_PT[pi], b_msk], W=[b_PT[pi]])
                        S.op("pe", "matmul", pA[0][:], lhsT=Clat[:, kt, 0:128], rhs=PT[pi][:], start=(kt == 0), stop=(kt == t),
                             R=[b_PT[pi], bClat[kt]], W=[bpA[0]])
                        if kt == 0:
                            S.op("dve", "tensor_copy", lacc[:], PT[pi][:], R=[b_PT[pi]], W=[b_lacc])
                        else:
                            S.op("dve", "tensor_tensor", lacc[:], lacc[:], PT[pi][:], ALU.add, R=[b_PT[pi], b_lacc], W=[b_lacc])

                    attn_loop(list(range(t + 1)), qk, post)
                    for j in range(4):
                        S.op("pe", "matmul", pA[1][:, j:j + 1], lhsT=lacc[:, j * 128:(j + 1) * 128], rhs=ones_f[:, 0:1],
                             start=True, stop=True, R=[b_lacc, b_ones], W=[bpA[1]])
                    S.op("dve", "tensor_copy", OlatT[:], pA[0][:].rearrange("p (c n) -> p c n", c=4), R=[bpA[0]], W=[b_OlatT])
                    S.op("dve", "reciprocal", rl[:, hg * 4:(hg + 1) * 4], pA[1][:, 0:4], R=[bpA[1]], W=[b_rl])
                    for j in range(4):
                        h = hg * 4 + j
                        S.op("pe", "matmul", pM[mo][:, h * 64:(h + 1) * 64], lhsT=OlatT[:, j, :], rhs=wuv[:, h, :],
                             start=True, stop=True, R=[b_OlatT, b_wkv], W=[bpM[mo]])
                S.op("dve", "tensor_tensor", y_sb[:, 0:512].rearrange("p (h d) -> p h d", h=8),
                     pM[mo][:, 0:512].rearrange("p (h d) -> p h d", h=8), rl[:, 0:8, None].to_broadcast([128, 8, 64]), ALU.mult,
                     R=[bpM[mo], b_rl], W=[b_y])

            def nsa_sel(s, t, g):
                par = t % 2
                QpeT, b_QpeT = QpeT2[par], b_QpeT2[par]
                QabsT, b_Qabs = QabsT2[par], b_Qabs2[par]
                QS, b_QSq, b_QSs = QS2[par], b_QSq2[par], b_QSs2[par]
                gate, b_gate = gate2[par], b_gate2[par]
                QSg = QS[:, g].rearrange("p j n -> p (j n)")
                QSg = QS[:, g].rearrange("p j n -> p (j n)")
                nts = [0] + ([1] if t >= 16 else [])
                for nt in nts:
                    si = nxt("M", 2)
                    S.op("pe", "matmul", pM[si][:], lhsT=KcT[:, g, nt * 128:(nt + 1) * 128], rhs=QSg[0:64, :],
                         start=True, stop=True, R=[b_Kc, b_QSq], W=[bpM[si]])
                    S.op("act", "activation", PcT[nt][:], pM[si][:], AF.Exp, R=[bpM[si]], W=[b_PcT[nt]])
                    S.op("pool", "affine_select", PcT[nt][:].rearrange("p (j n) -> p j n", j=4),
                         PcT[nt][:].rearrange("p (j n) -> p j n", j=4), [[0, 4], [1, 128]], ALU.is_ge, 0.0,
                         base=128 * t - 31 - 2048 * nt, channel_multiplier=-16, R=[b_PcT[nt]], W=[b_PcT[nt]])
                yield
                mc = nxt("M", 2)
                for j in range(4):
                    for i_, nt in enumerate(nts):
                        S.op("pe", "matmul", pM[mc][:, j * 128:(j + 1) * 128], lhsT=PcT[nt][:, j * 128:(j + 1) * 128],
                             rhs=VcO[:, nt, g, :], start=(i_ == 0), stop=(i_ == len(nts) - 1),
                             R=[b_PcT[nt], b_VcO], W=[bpM[mc]])
                yield
                pc = pM[mc][:].rearrange("p (j c) -> p j c", j=4)
                S.op("dve", "tensor_reduce", st[:, 24:28], pc[:, :, 64:128], AX.X, ALU.add, R=[bpM[mc]], W=[b_stn])
                S.op("dve", "tensor_scalar", st[:, 24:28], st[:, 24:28], 1e-30, None, ALU.max, R=[b_stn], W=[b_stn])
                S.op("dve", "reciprocal", st[:, 28:32], st[:, 24:28], R=[b_stn], W=[b_stn])
                S.op("dve", "tensor_tensor", tmp4[:], pc[:, :, 64:128], st[:, 28:32, None].to_broadcast([128, 4, 64]), ALU.mult,
                     R=[bpM[mc], b_stn], W=[b_tmp4])
                S.op("dve", "tensor_reduce", imp[:], tmp4[:].rearrange("p j c -> p c j"), AX.X, ALU.add, R=[b_tmp4], W=[b_imp])
                yield
                S.op("dve", "tensor_tensor", st[:, 32:36], st[:, 28:32], gate[:, 0, g * 4:(g + 1) * 4], ALU.mult,
                     R=[b_stn, b_gate], W=[b_stn])
                S.op("dve", "tensor_tensor", y_sb[:, 512 + g * 256:768 + g * 256].rearrange("p (j c) -> p j c", j=4), pc[:, :, 0:64],
                     st[:, 32:36, None].to_broadcast([128, 4, 64]), ALU.mult, R=[bpM[mc], b_stn], W=[b_yn])
                yield
                S.op("pool", "affine_select", sc1[:], imp[:], [[-64, 64]], ALU.is_ge, 1e9, base=128 * t - 128, channel_multiplier=1,
                     R=[b_imp], W=[b_imp])
                S.op("pool", "affine_select", sc2[:], sc1[:], [[-64, 64]], ALU.is_ge, -1e9, base=128 * t, channel_multiplier=1,
                     R=[b_imp], W=[b_imp])
                S.op("pool", "memset", sc2[:, 0:1], 1e9, R=[b_imp], W=[b_imp])
                yield
                S.op("dve", "max", st[:, 40:48], sc2[:], R=[b_imp], W=[b_stn])
                S.op("dve", "match_replace", sc3[:], st[:, 40:48], sc2[:], -3e38, R=[b_imp, b_stn], W=[b_imp])
                S.op("dve", "max", st[:, 48:56], sc3[:], R=[b_imp], W=[b_stn])
                S.op("dve", "tensor_scalar", selq[:, 64:128], sc2[:], st[:, 55:56], NEG, ALU.is_lt, ALU.mult,
                     R=[b_imp, b_stn], W=[b_selq])
                yield
                ti = nxt("T", 2)
                transpose_to(ti, 0, selq[:], [b_selq])
                S.op("dve", "tensor_copy", QS[64:128, g], pT[ti][64:128, None, 0:128].to_broadcast([64, 4, 128]),
                     R=[bpT[ti]], W=[b_QSs[g]])
                yield

            def nsa_attn(s, t, g):
                par = t % 2
                QpeT, b_QpeT = QpeT2[par], b_QpeT2[par]
                QabsT, b_Qabs = QabsT2[par], b_Qabs2[par]
                QS, b_QSq, b_QSs = QS2[par], b_QSq2[par], b_QSs2[par]
                gate, b_gate = gate2[par], b_gate2[par]
                QSg = QS[:, g].rearrange("p j n -> p (j n)")
                S.op("dve", "memset", pA[0][:, 0:260], 0.0, W=[bpA[0]])
                S.op("dve", "memset", pA[1][:, 0:260], 0.0, W=[bpA[1]])
                def qk_s(kt):
                    si = nxt("S", 2)
                    S.op("pe", "matmul", pS[si][:], lhsT=KsE[:, g, kt * 128:(kt + 1) * 128], rhs=QSg,
                         start=True, stop=True, R=[bKs[kt], b_exp, b_QSq, b_QSs[g]], W=[bpS[si]])
                    return si

                def post_s(kt, si):
                    pi = nxt("P", 3)
                    S.op("act", "activation", PT[pi][:], pS[si][:], AF.Exp, R=[bpS[si]], W=[b_PT[pi]])
                    if kt == t:
                        S.op("dve", "tensor_tensor", PT[pi][:], PT[pi][:], tri4[:], ALU.mult, R=[b_PT[pi], b_msk], W=[b_PT[pi]])
                    for j in range(4):
                        S.op("pe", "matmul", pA[0][:, j * 65:j * 65 + 65], lhsT=PT[pi][:, j * 128:(j + 1) * 128],
                             rhs=Vs[:, kt, g, :], start=False, stop=(kt == t), skip_group_check=True,
                             R=[b_PT[pi], bVs[kt]], W=[bpA[0]])

                def qk_w(kt):
                    si = nxt("S", 2)
                    sl = kt % 8
                    S.op("pe", "matmul", pS[si][:], lhsT=KwT[:, g, sl * 128:(sl + 1) * 128], rhs=QSg[0:64, :],
                         start=True, stop=True, R=[bKw[sl], b_QSq], W=[bpS[si]])
                    return si

                def post_w(kt, si):
                    sl = kt % 8
                    pi = nxt("P", 3)
                    S.op("act", "activation", PT[pi][:], pS[si][:], AF.Exp, R=[bpS[si]], W=[b_PT[pi]])
                    if kt == t:
                        S.op("dve", "tensor_tensor", PT[pi][:], PT[pi][:], tri4[:], ALU.mult, R=[b_PT[pi], b_msk], W=[b_PT[pi]])
                    if kt == t - 4:
                        S.op("dve", "tensor_tensor", PT[pi][:], PT[pi][:], anti4[:], ALU.mult, R=[b_PT[pi], b_msk], W=[b_PT[pi]])
                    for j in range(4):
                        S.op("pe", "matmul", pA[1][:, j * 65:j * 65 + 65], lhsT=PT[pi][:, j * 128:(j + 1) * 128],
                             rhs=Vw[:, sl, g, :], start=False, stop=(kt == t), skip_group_check=True,
                             R=[b_PT[pi], bVw[sl]], W=[bpA[1]])

                attn_loop(list(range(t + 1)), qk_s, post_s)
                attn_loop(list(range(max(0, t - 4), t + 1)), qk_w, post_w)
                for br in range(2):
                    pa = pA[br][:, 0:260].rearrange("p (j c) -> p j c", j=4)
                    S.op("dve", "reciprocal", st[:, 56:60], pa[:, :, 64], R=[bpA[br]], W=[b_stn])
                    S.op("dve", "tensor_tensor", st[:, 60:64], st[:, 56:60], gate[:, 1 + br, g * 4:(g + 1) * 4], ALU.mult,
                         R=[b_stn, b_gate], W=[b_stn])
                    yv = y_sb[:, 512 + g * 256:768 + g * 256].rearrange("p (j c) -> p j c", j=4)
                    S.op("dve", "tensor_tensor", tmp4[:], pa[:, :, 0:64], st[:, 60:64, None].to_broadcast([128, 4, 64]), ALU.mult,
                         R=[bpA[br], b_stn], W=[b_tmp4])
                    S.op("dve", "tensor_tensor", yv, yv, tmp4[:], ALU.add, R=[b_tmp4, b_yn], W=[b_yn])


            def outproj(s, t):
                xb = t % 2
                ra, rb_ = rstd_multi([(y_sb[:, 0:512], 512), (y_sb[:, 512:1024], 512)], 12, [b_y, b_yn], mixed, b_mixed)
                S.op("dve", "tensor_scalar", mixed[:, 0:512], y_sb[:, 0:512], ra, None, ALU.mult, R=[b_y, b_stc[12]], W=[b_mixed])
                S.op("dve", "tensor_scalar", mixed[:, 512:1024], y_sb[:, 512:1024], rb_, None, ALU.mult, R=[b_yn, b_stc[12]], W=[b_mixed])
                ti = nxt("T", 2)
                for c in range(8):
                    transpose_to(ti, c * 128, mixed[:, c * 128:(c + 1) * 128], [b_mixed])
                S.op("dve", "tensor_copy", mixedT[:], pT[ti][:, 0:1024].rearrange("p (c n) -> p c n", c=8), R=[bpT[ti]], W=[b_mixedT])
                for dh in range(2):
                    mi = nxt("S", 2)
                    for c in range(8):
                        S.op("pe", "matmul", pS[mi][:], lhsT=mixedT[:, c, :], rhs=w_o[:, c, dh * 512:(dh + 1) * 512],
                             start=(c == 0), stop=(c == 7), R=[b_mixedT, b_wo], W=[bpS[mi]])
                    S.op("dve", "tensor_tensor", h_sb[xb][:, dh * 512:(dh + 1) * 512], pS[mi][:], xs[xb][:, dh * 512:(dh + 1) * 512],
                         ALU.add, R=[bpS[mi], b_xs[xb]], W=[b_h[xb]])
                row = (s * SEQ + t * 128)
                S.dma("sp", hscr_d[row:row + 128, :], h_sb[xb][:], R=[b_h[xb]])

            bgst = {"gen": None, "credit": 0.0, "rate": 0.0}

            def bg_run(n=None):
                if bgst["gen"] is None:
                    return
                mode["bg"] = True
                try:
                    k = 0
                    while n is None or k < n:
                        next(bgst["gen"])
                        k += 1
                except StopIteration:
                    bgst["gen"] = None
                mode["bg"] = False

            def tick():
                if mode["bg"] or bgst["gen"] is None:
                    return
                bgst["credit"] += bgst["rate"]
                if bgst["credit"] >= 1.0:
                    n = int(bgst["credit"])
                    bgst["credit"] -= n
                    bg_run(n)

            S.tick = tick
            def chain(*gens):
                for g_ in gens:
                    yield from g_

            def set_bg(gen, nchunks, fg_ops):
                bgst["gen"] = gen
                bgst["credit"] = 0.0
                bgst["rate"] = nchunks / (0.7 * fg_ops)

            for s in range(nseq):
                bgst["gen"] = phase1(s, 0) if "p1" in stages else None
                bg_run(None)
                for t in range(ntiles):
                    if "nsa" in stages:
                        set_bg(chain(nsa_sel(s, t, 0), nsa_sel(s, t, 1)), 16.0, 30.0 + 18.0 * (t + 1))
                    if "mla" in stages:
                        mla(s, t)
                    bg_run(None)
                    if t + 1 < ntiles and "p1" in stages:
                        set_bg(phase1(s, t + 1), 40.0, 100.0 + 14.0 * (t + 1 + min(t + 1, 5)))
                    if "nsa" in stages:
                        nsa_attn(s, t, 0)
                        nsa_attn(s, t, 1)
                    if "out" in stages:
                        outproj(s, t)
                    bg_run(None)
            S.tick = None
            S.barrier()
        mode["B"] = True
        B = ExitStack()
        with B:
            def sb2(name, shape, dt):
                return B.enter_context(nc.sbuf_tensor("b_" + name, shape, dt))
            gb = sb2("gb", [128, 8], F32); b_gb = S.buf("gb")
            S.dma("sp", gb[:], g_mlp_d, W=[b_gb])
            gfin = sb2("gfin", [128, D], F32)
            S.dma("sp", gfin[:], gfin_d, W=[b_gb])
            ident2 = sb2("ident2", [128, 128], BF16); b_id2 = S.buf("ident2")
            S.dma("pool", ident2[:], ident_d, W=[b_id2])
            w_up = sb2("w_up", [128, 8, DFF], BF16); b_wup = S.buf("w_up")
            for c in range(8):
                S.dma("pool", w_up[:, c, :], w_up_d[c * 128:(c + 1) * 128, :], W=[b_wup])
                S.op("dve", "tensor_scalar", w_up[:, c, :], w_up[:, c, :], gb[:, c:c + 1], None, ALU.mult, R=[b_gb, b_wup], W=[b_wup])
            w_dn = sb2("w_dn", [128, 32, D], BF16); b_wdn = S.buf("w_dn")
            wdv = w_down_d.rearrange("(f p) n -> p f n", p=128)
            for f4 in range(8):
                S.dma("pool", w_dn[:, f4 * 4:(f4 + 1) * 4, :], wdv[:, f4 * 4:(f4 + 1) * 4, :], W=[b_wdn])
            hin = [sb2("hin%d" % i, [128, D], F32) for i in range(4)]; b_hin = S.bufs("hin", 4)
            st2 = sb2("st2", [128, 16], F32); b_st2 = S.buf("st2")
            junk2 = sb2("junk2", [128, D], BF16); b_junk2 = S.buf("junk2")
            hn = sb2("hn", [128, D], BF16); b_hn = S.buf("hn")
            hnT = sb2("hnT", [128, 8, 512], BF16); b_hnT = S.buf("hnT")
            rl = [sb2("rl%d" % i, [128, 512], BF16) for i in range(2)]; b_rl = S.bufs("rl", 2)
            aT = sb2("aT", [128, 32, 512], BF16); b_aT = S.buf("aT")
            yo = [sb2("yo%d" % i, [128, D], F32) for i in range(2)]; b_yo = S.bufs("yo", 2)
            S.op("pool", "memset", st2[:], 0.0, W=[b_st2])
            nT = (nseq * ntiles * 128) // 512 if do_mlp else 0

            def rstd2(src_ap, Rb):
                S.op("pool", "memset", st2[:, 0:1], 0.0, W=[b_st2])
                S.op("act", "activation", junk2[:], src_ap, AF.Square, accum_out=st2[:, 0:1], R=Rb, W=[b_junk2, b_st2])
                S.op("dve", "tensor_scalar", st2[:, 1:2], st2[:, 0:1], 1.0 / D, EPS, ALU.mult, ALU.add, R=[b_st2], W=[b_st2])
                S.op("act", "activation", st2[:, 2:3], st2[:, 1:2], AF.Sqrt, R=[b_st2], W=[b_st2])
                S.op("dve", "reciprocal", st2[:, 3:4], st2[:, 2:3], R=[b_st2], W=[b_st2])
                return st2[:, 3:4]

            oc = 0
            for T in range(nT):
                hb = T % 2
                row = T * 512 if nseq * ntiles * 128 == nseq * SEQ else None
                base = (T * 512 // (ntiles * 128)) * SEQ + (T * 512) % (ntiles * 128)
                for i in range(4):
                    S.dma("sp", hin[i][:], hscr_d[base + i * 128:base + (i + 1) * 128, :], W=[b_hin[i]])
                for i in range(4):
                    r = rstd2(hin[i][:], [b_hin[i]])
                    S.op("dve", "tensor_scalar", hn[:], hin[i][:], r, None, ALU.mult, R=[b_hin[i], b_st2], W=[b_hn])
                    ti = nxt("T", 2)
                    for c in range(8):
                        S.op("pe", "transpose", pT[ti][:, c * 128:(c + 1) * 128], hn[:, c * 128:(c + 1) * 128], ident2[:],
                             R=[b_hn, b_id2], W=[bpT[ti]])
                    S.op("act", "copy", hnT[:, :, i * 128:(i + 1) * 128], pT[ti][:, 0:1024].rearrange("p (c n) -> p c n", c=8),
                         R=[bpT[ti]], W=[b_hnT])
                for f in range(32):
                    si = nxt("S", 2)
                    for c in range(8):
                        S.op("pe", "matmul", pS[si][:], lhsT=w_up[:, c, f * 128:(f + 1) * 128], rhs=hnT[:, c, :],
                             start=(c == 0), stop=(c == 7), R=[b_wup, b_hnT], W=[bpS[si]])
                    ri = f % 2
                    S.op("act", "activation", rl[ri][:], pS[si][:], AF.Relu, R=[bpS[si]], W=[b_rl[ri]])
                    S.op("pool", "tensor_tensor", aT[:, f, :], rl[ri][:], rl[ri][:], ALU.mult, R=[b_rl[ri]], W=[b_aT])
                for i in range(4):
                    ob = oc % 2
                    oc += 1
                    for dh in range(2):
                        mi = nxt("M", 2)
                        for f in range(32):
                            S.op("pe", "matmul", pM[mi][:], lhsT=aT[:, f, i * 128:(i + 1) * 128], rhs=w_dn[:, f, dh * 512:(dh + 1) * 512],
                                 start=(f == 0), stop=(f == 31), R=[b_aT, b_wdn], W=[bpM[mi]])
                        S.op("dve", "tensor_tensor", yo[ob][:, dh * 512:(dh + 1) * 512], pM[mi][:], hin[i][:, dh * 512:(dh + 1) * 512],
                             ALU.add, R=[bpM[mi], b_hin[i]], W=[b_yo[ob]])
                    r = rstd2(yo[ob][:], [b_yo[ob]])
                    S.op("dve", "scalar_tensor_tensor", yo[ob][:], yo[ob][:], r, gfin[:], ALU.mult, ALU.mult,
                         R=[b_yo[ob], b_st2, b_gb], W=[b_yo[ob]])
                    S.dma("sp", out_d[base + i * 128:base + (i + 1) * 128, :], yo[ob][:], R=[b_yo[ob]])
            S.emit()
            print('NOPS', S.nops)
    return nc


def _rope_tab(pos, dim):
    inv = np.exp(np.float32(-math.log(500000.0)) * np.arange(0, dim, 2, dtype=np.float32) / np.float32(dim)).astype(np.float32)
    ang = pos.astype(np.float32)[:, None] * inv[None, :]
    return np.cos(ang).astype(np.float32), np.sin(ang).astype(np.float32)


def _tok_major(a):
    return np.ascontiguousarray(a.reshape(NT, 128, -1).transpose(1, 0, 2))


def host_consts():
    pos = np.arange(SEQ)
    cm, sm = _rope_tab(pos, 32)
    cn, sn = _rope_tab(pos, 16)
    c = {}
    c["cos_m"], c["sin_m"] = _tok_major(cm), _tok_major(sm)
    c["cos_n"], c["sin_n"] = _tok_major(cn), _tok_major(sn)
    c["cos_n8"], c["sin_n8"] = _tok_major(cn * np.float32(0.125)), _tok_major(sn * np.float32(0.125))
    ce = np.zeros((8, NT, 8), np.float32)
    se = np.zeros((8, NT, 8), np.float32)
    for t in range(NT):
        for m in range(8):
            n = 8 * t - 1 + m
            if 0 <= n < 255:
                p = 16 * n + 31
                ce[m, t], se[m, t] = cn[p], sn[p]
    c["cos_e"], c["sin_e"] = ce, se
    k = np.arange(128)[:, None]
    q = np.arange(128)[None, :]
    tri = (q >= k).astype(np.float32)
    anti = (q < k).astype(np.float32)
    c["tri4"] = np.ascontiguousarray(np.tile(tri, (1, 4)))
    c["anti4"] = np.ascontiguousarray(np.tile(anti, (1, 4)))
    n = np.arange(256)[:, None] * 16
    j = np.arange(64)[None, :] * 64
    ov = np.clip(np.minimum(n + 32, j + 64) - np.maximum(n, j), 0, None).astype(np.float32) / 32.0
    ov[255] = 0.0
    c["ovl"] = np.ascontiguousarray(ov.reshape(2, 128, 64).transpose(1, 0, 2))
    c["expand"] = (np.arange(SEQ)[None, :] // 64 == np.arange(64)[:, None]).astype(np.float32)
    c["ident"] = np.eye(128, dtype=np.float32)
    return c


def host_weights(inp):
    f = lambda a: np.ascontiguousarray(np.asarray(a, dtype=np.float32))
    pc = lambda v: np.ascontiguousarray(np.asarray(v, np.float32).reshape(-1, 128).T)
    w = {}
    w["w_in"] = f(inp["w_in"][0])
    w["g_mix"] = pc(inp["g_mix_norm"][0])
    w["w_uq"] = f(inp["w_uq"][0])
    w["g_cq"] = pc(inp["g_cq"][0])
    wukv = np.asarray(inp["w_ukv"][0], np.float32).reshape(128, 8, 2, 64)
    wuk = wukv[:, :, 0, :]
    w["wukT"] = np.ascontiguousarray(wuk.transpose(2, 1, 0))
    w["wuv"] = np.ascontiguousarray(wukv[:, :, 1, :])
    w["gckv_bc"] = np.ascontiguousarray(np.broadcast_to(np.asarray(inp["g_ckv"][0], np.float32)[None, :], (128, 128)))
    w1 = np.stack([np.asarray(inp["cmp_w1_k"][0], np.float32), np.asarray(inp["cmp_w1_v"][0], np.float32)])
    w["cmp_w1"] = np.ascontiguousarray(w1.reshape(2, 32, 64, 128).transpose(0, 2, 1, 3))
    pe = np.stack([np.asarray(inp["cmp_pe_k"][0], np.float32), np.asarray(inp["cmp_pe_v"][0], np.float32)])
    w["cmp_peT"] = np.ascontiguousarray(pe.transpose(0, 2, 1))
    w["cmp_b1"] = np.ascontiguousarray(np.stack([inp["cmp_b1_k"][0], inp["cmp_b1_v"][0]], axis=1).astype(np.float32))
    w["cmp_w2"] = np.ascontiguousarray(np.stack([inp["cmp_w2_k"][0], inp["cmp_w2_v"][0]], axis=1).astype(np.float32))
    b2k = np.asarray(inp["cmp_b2_k"][0], np.float32)
    w["cmp_b2k_bc"] = np.ascontiguousarray(np.broadcast_to(np.concatenate([b2k, b2k])[None, :], (128, 128)))
    w["cmp_b2v"] = np.ascontiguousarray(np.asarray(inp["cmp_b2_v"][0], np.float32).reshape(64, 1))
    w["w_o"] = f(inp["w_o"][0])
    w["g_out"] = pc(np.concatenate([np.asarray(inp["g_out_mla"][0]), np.asarray(inp["g_out_nsa"][0])]))
    w["w_up"] = f(inp["w_up"][0])
    w["g_mlp"] = pc(inp["g_mlp_norm"][0])
    w["w_down"] = f(inp["w_down"][0])
    w["gfin_bc"] = np.ascontiguousarray(np.broadcast_to(np.asarray(inp["g_final"], np.float32)[None, :], (128, D)))
    return w


_NC_CACHE = {}


def kernel(**inputs):
    x = np.asarray(inputs["x"], dtype=np.float32)
    shared = host_consts()
    shared.update(host_weights(inputs))
    if "full" not in _NC_CACHE:
        _NC_CACHE["full"] = build_program()
    nc = _NC_CACHE["full"]
    in_maps = []
    for c in range(NCORES):
        m = dict(shared)
        m["x"] = np.ascontiguousarray(x[c * NSEQ:(c + 1) * NSEQ])
        in_maps.append(m)
    res = run_bass_kernel_spmd(nc, in_maps, core_ids=list(range(NCORES)))
    outs = [np.asarray(r["out"]).reshape(NSEQ, SEQ, D) for r in res.results]
    return np.concatenate(outs, axis=0).astype(np.float32)
```

```python
import math
import numpy as np
from contextlib import ExitStack
import concourse.bass as bass
import concourse.mybir as mybir
from concourse.bass_utils import run_bass_kernel_spmd

F32 = mybir.dt.float32
BF16 = mybir.dt.bfloat16
I32 = mybir.dt.int32
AF = mybir.ActivationFunctionType
ALU = mybir.AluOpType
AX = mybir.AxisListType

NCORES = 8
SEQ = 4096
D = 1024
NSEQ = 2
NT = SEQ // 128
EPS = 1e-6
IN_COLS = 1720
DFF = 4096
NEG = -30000.0
STRICT_SAME = True
OP_LIMIT = None
BG_OVERLAP = True
USE_MAGIC = True
ACT_COPY_T = 20


class Buf:
    __slots__ = ("name", "w", "r", "dsem", "dcnt", "excl")

    def __init__(self, name):
        self.name = name
        self.excl = False
        self.w = None
        self.r = {}
        self.dsem = None
        self.dcnt = 0


class Sched:
    ENG = ("pe", "act", "dve", "pool", "sp")

    def __init__(self, nc, ctx):
        self.nc = nc
        self.ctx = ctx
        self.sem = {e: ctx.enter_context(nc.semaphore("s_" + e)) for e in self.ENG}
        self.cnt = {e: 0 for e in self.ENG}
        self.seen = {e: {} for e in self.ENG}
        self.prog = {e: [] for e in self.ENG}
        self.dbufs = []
        self.nb = 0
        self.nops = 0
        self.fillregs = {}
        self.tick = None
        self.limit = OP_LIMIT

    def buf(self, name):
        self.nb += 1
        return Buf("%s_%d" % (name, self.nb))

    def bufs(self, name, n):
        return [self.buf(name) for _ in range(n)]

    def _dsem(self, b):
        if b.dsem is None:
            b.dsem = self.ctx.enter_context(self.nc.semaphore("d_" + b.name))
            self.dbufs.append(b)
        return b.dsem

    def _deps(self, e, reads, writes, strict):
        toks = []
        for b in reads:
            if b.w is not None:
                toks.append(b.w)
            if b.excl:
                toks.extend(b.r.values())
        for b in writes:
            if b.w is not None:
                toks.append(b.w)
            toks.extend(b.r.values())
        need = {}
        for (key, sem, val) in toks:
            if key == e and not strict and (e == "pe" or not STRICT_SAME):
                continue
            if self.seen[e].get(key, 0) >= val:
                continue
            if key not in need or need[key][1] < val:
                need[key] = (sem, val)
        for key, (sem, val) in need.items():
            self.seen[e][key] = val
            self.prog[e].append(("wait", sem, val))

    def op(self, e, meth, *args, R=(), W=(), **kw):
        self.nops += 1
        if self.limit is not None and self.nops > self.limit:
            return None
        self._deps(e, R, W, False)
        self.cnt[e] += 1
        tok = (e, self.sem[e], self.cnt[e])
        self.prog[e].append(("op", meth, args, kw))
        for b in R:
            b.r[e] = tok
        for b in W:
            b.w = tok
            b.r = {}
        if self.tick is not None:
            self.tick()
        return tok

    def dma(self, q, out, in_, R=(), W=(), **kw):
        self.nops += 1
        if self.limit is not None and self.nops > self.limit:
            return None
        self._deps(q, R, W, True)
        owner = W[0] if W else R[0]
        sem = self._dsem(owner)
        owner.dcnt += 16
        tok = ("d_" + owner.name, sem, owner.dcnt)
        self.prog[q].append(("dma", out, in_, kw, sem))
        for b in R:
            b.r[tok[0]] = tok
        for b in W:
            b.w = tok
            b.r = {}
        return tok

    def barrier(self):
        toks = [(e, self.sem[e], self.cnt[e]) for e in self.ENG if self.cnt[e] > 0]
        toks += [("d_" + b.name, b.dsem, b.dcnt) for b in self.dbufs if b.dcnt > 0]
        for e in self.ENG:
            for (key, sem, val) in toks:
                if self.seen[e].get(key, 0) >= val:
                    continue
                self.seen[e][key] = val
                self.prog[e].append(("wait", sem, val))

    def flush(self):
        nc = self.nc
        with nc.Block() as block:
            def replay(e):
                def f(eng):
                    sem_e = self.sem[e]
                    for it in self.prog[e]:
                        if it[0] == "wait":
                            eng.wait_ge(it[1], it[2])
                        elif it[0] == "op":
                            args = it[2]
                            if it[1] == "affine_select":
                                args = list(args)
                                if args[4] not in self.fillregs:
                                    self.fillregs[args[4]] = eng.to_reg(args[4])
                                args[4] = self.fillregs[args[4]]
                            getattr(eng, it[1])(*args, **it[3]).then_inc(sem_e, 1)
                        else:
                            eng.dma_start(out=it[1], in_=it[2], **it[3]).then_inc(it[4], 16)
                return f
            block.tensor(replay("pe"))
            block.scalar(replay("act"))
            block.vector(replay("dve"))
            block.gpsimd(replay("pool"))
            block.sync(replay("sp"))
        self.prog = {e: [] for e in self.ENG}

    def emit(self):
        self.barrier()
        self.flush()


def build_program(nseq=NSEQ, ntiles=NT, do_mlp=True, stages=("p1", "mla", "nsa", "out")):
    nc = bass.Bass("TRN2", target_bir_lowering=False)

    def din(name, shape):
        return nc.dram_tensor(name, list(shape), F32, kind="ExternalInput").ap()

    x_d = din("x", [nseq, SEQ, D])
    w_in_d = din("w_in", [D, IN_COLS])
    g_mix_d = din("g_mix", [128, 8])
    w_uq_d = din("w_uq", [256, 768])
    g_cq_d = din("g_cq", [128, 2])
    wukT_d = din("wukT", [64, 8, 128])
    wuv_d = din("wuv", [128, 8, 64])
    gckv_d = din("gckv_bc", [128, 128])
    w1_d = din("cmp_w1", [2, 64, 32, 128])
    peT_d = din("cmp_peT", [2, 64, 32])
    b1_d = din("cmp_b1", [128, 2])
    w2_d = din("cmp_w2", [128, 2, 64])
    b2k_d = din("cmp_b2k_bc", [128, 128])
    b2v_d = din("cmp_b2v", [64, 1])
    w_o_d = din("w_o", [D, D])
    g_out_d = din("g_out", [128, 8])
    w_up_d = din("w_up", [D, DFF])
    g_mlp_d = din("g_mlp", [128, 8])
    w_down_d = din("w_down", [DFF, D])
    gfin_d = din("gfin_bc", [128, D])
    cosm_d = din("cos_m", [128, NT, 16])
    sinm_d = din("sin_m", [128, NT, 16])
    cosn_d = din("cos_n", [128, NT, 8])
    sinn_d = din("sin_n", [128, NT, 8])
    cosn8_d = din("cos_n8", [128, NT, 8])
    sinn8_d = din("sin_n8", [128, NT, 8])
    cose_d = din("cos_e", [8, NT, 8])
    sine_d = din("sin_e", [8, NT, 8])
    tri_d = din("tri4", [128, 512])
    anti_d = din("anti4", [128, 512])
    ovl_d = din("ovl", [128, 2, 64])
    exp_d = din("expand", [64, SEQ])
    ident_d = din("ident", [128, 128])
    out_d = nc.dram_tensor("out", [nseq * SEQ, D], F32, kind="ExternalOutput").ap()
    hscr_d = nc.dram_tensor("hscr", [nseq * SEQ, D], F32, kind="Internal").ap()

    top = ExitStack()
    with top:
        S = Sched(nc, top)
        def psum(name, shape, dt):
            return top.enter_context(nc.psum_tensor(name, shape, dt))
        pS = [psum("pS%d" % i, [128, 512], F32) for i in range(2)]
        pA = [psum("pA%d" % i, [128, 512], F32) for i in range(2)]
        pM = [psum("pM%d" % i, [128, 512], F32) for i in range(2)]
        pT = [psum("pT%d" % i, [128, 1024], BF16) for i in range(2)]
        bpS = S.bufs("pS", 2)
        bpA = S.bufs("pA", 2)
        bpM = S.bufs("pM", 2)
        bpT = S.bufs("pT", 2)
        for b_ in bpS + bpA + bpM + bpT:
            b_.excl = True
        rr = {"S": 0, "M": 0, "T": 0, "P": 0, "A": 0}
        mode = {"bg": False, "B": False}

        def nxt(kind, n):
            if kind in ("M", "T") and not mode["B"]:
                return 0 if mode["bg"] else 1
            i = rr[kind] % n
            rr[kind] += 1
            return i

        A = ExitStack()
        with A:
            def sb(name, shape, dt):
                return A.enter_context(nc.sbuf_tensor("a_" + name, shape, dt))

            ident = sb("ident", [128, 128], BF16); b_ident = S.buf("ident")
            S.dma("pool", ident[:], ident_d, W=[b_ident])
            tri4 = sb("tri4", [128, 512], BF16); anti4 = sb("anti4", [128, 512], BF16); b_msk = S.buf("msk")
            S.dma("pool", tri4[:], tri_d, W=[b_msk])
            S.dma("pool", anti4[:], anti_d, W=[b_msk])
            tabs = {}
            b_tab = S.buf("tab")
            for nm, d_, w_ in (("cos_m", cosm_d, 16), ("sin_m", sinm_d, 16), ("cos_n", cosn_d, 8), ("sin_n", sinn_d, 8)):
                tabs[nm] = sb(nm, [128, NT, w_], F32)
                S.dma("sp", tabs[nm][:], d_, W=[b_tab])
            cos_e = sb("cos_e", [8, NT, 8], F32); sin_e = sb("sin_e", [8, NT, 8], F32)
            S.dma("sp", cos_e[:], cose_d, W=[b_tab])
            S.dma("sp", sin_e[:], sine_d, W=[b_tab])
            gckv = sb("gckv", [128, 128], F32); b2k = sb("b2k", [128, 128], F32); b2v = sb("b2v", [64, 1], F32)
            b1 = sb("b1", [128, 2], F32)
            S.dma("sp", gckv[:], gckv_d, W=[b_tab])
            S.dma("sp", b2k[:], b2k_d, W=[b_tab])
            S.dma("sp", b2v[:], b2v_d, W=[b_tab])
            S.dma("sp", b1[:], b1_d, W=[b_tab])
            gvec = sb("gvec", [128, 24], F32)
            S.dma("sp", gvec[:, 0:8], g_mix_d, W=[b_tab])
            S.dma("sp", gvec[:, 8:10], g_cq_d, W=[b_tab])
            S.dma("sp", gvec[:, 10:18], g_out_d, W=[b_tab])

            w_in = sb("w_in", [128, 8, IN_COLS], BF16); b_win = S.buf("w_in")
            S.dma("pool", w_in[:], w_in_d.rearrange("(c p) n -> p c n", p=128), W=[b_win])
            for c in range(8):
                S.op("dve", "tensor_scalar", w_in[:, c, :], w_in[:, c, :], gvec[:, c:c + 1], None, ALU.mult,
                     R=[b_tab, b_win], W=[b_win])
                S.op("dve", "tensor_scalar", w_in[:, c, 416:928], w_in[:, c, 416:928], 0.125, None, ALU.mult,
                     R=[b_win], W=[b_win])
            w_o = sb("w_o", [128, 8, D], BF16); b_wo = S.buf("w_o")
            S.dma("pool", w_o[:], w_o_d.rearrange("(c p) n -> p c n", p=128), W=[b_wo])
            for c in range(8):
                S.op("dve", "tensor_scalar", w_o[:, c, :], w_o[:, c, :], gvec[:, 10 + c:11 + c], None, ALU.mult,
                     R=[b_tab, b_wo], W=[b_wo])
            w_uq = sb("w_uq", [128, 2, 768], BF16); b_wuq = S.buf("w_uq")
            S.dma("pool", w_uq[:], w_uq_d.rearrange("(c p) n -> p c n", p=128), W=[b_wuq])
            for c in range(2):
                S.op("dve", "tensor_scalar", w_uq[:, c, :], w_uq[:, c, :], gvec[:, 8 + c:9 + c], 96.0 ** -0.5,
                     ALU.mult, ALU.mult, R=[b_tab, b_wuq], W=[b_wuq])
            wukT = sb("wukT", [64, 8, 128], BF16); wuv = sb("wuv", [128, 8, 64], BF16); b_wkv = S.buf("wkv")
            S.dma("pool", wukT[:], wukT_d, W=[b_wkv])
            S.dma("pool", wuv[:], wuv_d, W=[b_wkv])
            w1 = sb("w1", [64, 2, 32, 128], BF16); b_w1 = S.buf("w1")
            for kv in range(2):
                S.dma("pool", w1[:, kv], w1_d[kv], W=[b_w1])
            peT = sb("peT", [64, 2, 32], BF16)
            for kv in range(2):
                S.dma("pool", peT[:, kv], peT_d[kv], W=[b_w1])
            w2 = sb("w2", [128, 2, 64], BF16)
            S.dma("pool", w2[:], w2_d, W=[b_w1])

            bias_tot = sb("bias_tot", [128, 2], F32); b_bt = S.buf("bias_tot")
            for kv in range(2):
                for l in range(32):
                    S.op("pe", "matmul", pM[0][:, kv:kv + 1], lhsT=w1[:, kv, l, :], rhs=peT[:, kv, l:l + 1],
                         start=(l == 0), stop=(l == 31), R=[b_w1], W=[bpM[0]])
            S.op("dve", "tensor_tensor", bias_tot[:], pM[0][:, 0:2], b1[:], ALU.add, R=[bpM[0], b_tab], W=[b_bt])

            KlatT = sb("KlatT", [128, SEQ], BF16); bKlat = S.bufs("Klat", NT)
            KpeT = sb("KpeT", [128, SEQ], BF16); bKpe = S.bufs("Kpe", NT)
            Clat = sb("Clat", [128, NT, 130], BF16); bClat = S.bufs("Clat", NT)
            KsE = sb("KsE", [128, 2, SEQ], BF16); bKs = S.bufs("Ks", NT); b_exp = S.buf("expand")
            Vs = sb("Vs", [128, NT, 2, 65], BF16); bVs = S.bufs("Vs", NT)
            KwT = sb("KwT", [64, 2, 8 * 128], BF16); bKw = S.bufs("Kw", 8)
            Vw = sb("Vw", [128, 8, 2, 65], BF16); bVw = S.bufs("Vw", 8)
            KcT = sb("KcT", [64, 2, 256], BF16); b_Kc = S.buf("Kc")
            VcT = sb("VcT", [64, 2, 256], BF16); b_VcT = S.buf("VcT")
            VcO = sb("VcO", [128, 2, 2, 128], BF16); b_VcO = S.buf("VcO")
            for g in range(2):
                S.dma("pool", KsE[64:128, g, :], exp_d, W=[b_exp])
            for nt in range(2):
                for g in range(2):
                    S.dma("pool", VcO[:, nt, g, 64:128], ovl_d[:, nt, :], W=[b_VcO])
            S.op("pool", "memset", Clat[:, :, 128:130], 1.0, W=bClat)
            S.op("pool", "memset", Vs[:, :, :, 64:65], 1.0, W=bVs)
            S.op("pool", "memset", Vw[:, :, :, 64:65], 1.0, W=bVw)

            xs = [sb("xs%d" % i, [128, D], F32) for i in range(2)]; b_xs = S.bufs("xs", 2)
            st = sb("st", [128, 64], F32); b_st = S.buf("st")
            xn = sb("xn", [128, D], BF16); b_xn = S.buf("xn")
            xnT = sb("xnT", [128, 8, 128], BF16); b_xnT = S.buf("xnT")
            u = sb("u", [128, IN_COLS], F32); b_u = S.buf("u")
            cqn = sb("cqn", [128, 256], BF16); b_cqn = S.buf("cqn")
            cqnT = sb("cqnT", [128, 2, 128], BF16); b_cqnT = S.buf("cqnT")
            q_sb = sb("q_sb", [128, 9, 96], F32); b_q = S.buf("q")
            qn_sb = sb("qn_sb", [128, 8, 64], BF16); b_qn = S.buf("qn")
            qpe_sb = sb("qpe_sb", [128, 9, 32], BF16); b_qpe = S.buf("qpe")
            rt = [sb("rt%d" % i, [128, 160], F32) for i in range(4)]; b_rt = S.buf("rt")
            QnT = sb("QnT", [64, 8, 128], BF16); b_QnT = S.buf("QnT")
            QpeT2 = [sb("QpeT%d" % i, [128, 8, 128], BF16) for i in range(2)]; b_QpeT2 = S.bufs("QpeT", 2)
            QabsT2 = [sb("QabsT%d" % i, [128, 8, 128], BF16) for i in range(2)]; b_Qabs2 = S.bufs("Qabs", 2)
            ub = sb("ub", [128, 20, 64], BF16); b_ub = S.buf("ub")
            QS2 = [sb("QS%d" % i, [128, 2, 4, 128], BF16) for i in range(2)]; b_QSq2 = S.bufs("QSq", 2); b_QSs2 = [S.bufs("QSs", 2) for _ in range(2)]
            rawT = [sb("rawT%d" % i, [64, 2, 2, 144], BF16) for i in range(2)]; b_rawT = S.bufs("rawT", 2)
            gate2 = [sb("gate%d" % i, [128, 3, 8], F32) for i in range(2)]; b_gate2 = S.bufs("gate", 2)
            z_sb = sb("z_sb", [128, 32], F32); z2_sb = sb("z2_sb", [128, 32], F32); b_z = S.buf("z")
            hid_sb = sb("hid_sb", [128, 32], BF16); b_hid = S.buf("hid")
            kc_f = sb("kc_f", [8, 2, 64], F32); kc_sb = sb("kc_sb", [8, 2, 64], BF16); b_kc = S.buf("kc")
            PT = [sb("PT%d" % i, [128, 512], BF16) for i in range(3)]; b_PT = S.bufs("PT", 3)
            PcT = [sb("PcT%d" % i, [128, 512], BF16) for i in range(2)]; b_PcT = S.bufs("PcT", 2)
            OlatT = sb("OlatT", [128, 4, 128], BF16); b_OlatT = S.buf("OlatT")
            y_sb = sb("y_sb", [128, D], F32); b_y = S.buf("y"); b_yn = S.buf("yn")
            imp = sb("imp", [128, 64], F32); sc1 = sb("sc1", [128, 64], F32); sc2 = sb("sc2", [128, 64], F32)
            sc3 = sb("sc3", [128, 64], F32); b_imp = S.buf("imp")
            selq = sb("selq", [128, 128], BF16); b_selq = S.buf("selq")
            tmp4 = sb("tmp4", [128, 4, 64], F32); b_tmp4 = S.buf("tmp4")
            mixed = sb("mixed", [128, D], BF16); b_mixed = S.buf("mixed")
            mixedT = sb("mixedT", [128, 8, 128], BF16); b_mixedT = S.buf("mixedT")
            h_sb = [sb("h_sb%d" % i, [128, D], F32) for i in range(2)]; b_h = S.bufs("h", 2)

            S.op("pool", "memset", selq[:], 0.0, W=[b_selq])
            S.op("pool", "memset", KpeT[:], 0.0, W=bKpe)
            for i in range(2):
                S.op("pool", "memset", QpeT2[i][:], 0.0, W=[b_QpeT2[i]])
            for i in range(2):
                S.op("pool", "memset", rawT[i][:], 0.0, W=[b_rawT[i]])
            S.op("pool", "memset", KcT[:], 0.0, W=[b_Kc])
            S.op("pool", "memset", VcT[:], 0.0, W=[b_VcT])
            S.op("pool", "memset", VcO[:, :, :, 0:64], 0.0, W=[b_VcO])

            b_stc = {0: S.buf("st0"), 4: S.buf("st4"), 12: S.buf("st12")}
            b_stm = S.buf("stm")
            b_stn = S.buf("stn")
            rl = sb("rl", [128, 8], F32); b_rl = S.buf("rl")
            lacc = sb("lacc", [128, 512], F32); b_lacc = S.buf("lacc")
            ones_f = sb("ones_f", [128, 1], F32); b_ones = S.buf("ones")
            S.op("pool", "memset", ones_f[:], 1.0, W=[b_ones])

            def rstd_multi(items, col, Rb, jk, b_jk):
                b_st = b_stc[col]
                k = len(items)
                S.op("dve", "memset", st[:, col:col + k], 0.0, W=[b_st])
                for i_, (src_ap, n) in enumerate(items):
                    S.op("dve", "scalar_tensor_tensor", jk[:, 0:n], src_ap, 1.0, src_ap, ALU.mult, ALU.mult,
                         accum_out=st[:, col + i_:col + i_ + 1], R=Rb + [b_st], W=[b_jk, b_st])
                    S.op("dve", "tensor_scalar", st[:, col + k + i_:col + k + i_ + 1], st[:, col + i_:col + i_ + 1], 1.0 / n, EPS,
                         ALU.mult, ALU.add, R=[b_st], W=[b_st])
                v_ = st[:, col + k:col + 2 * k]
                y_ = st[:, col + 2 * k:col + 3 * k]
                w_ = st[:, col + 3 * k:col + 4 * k]
                if not USE_MAGIC:
                    S.op("act", "activation", w_, v_, AF.Sqrt, R=[b_st], W=[b_st])
                    S.op("dve", "reciprocal", y_, w_, R=[b_st], W=[b_st])
                    return [st[:, col + 2 * k + i_:col + 2 * k + i_ + 1] for i_ in range(k)]
                S.op("dve", "tensor_scalar", y_.bitcast(I32), v_.bitcast(I32), -0.5, 1597463007.0, ALU.mult, ALU.add, R=[b_st], W=[b_st])
                for _it in range(2):
                    S.op("pool", "tensor_tensor", w_, y_, y_, ALU.mult, R=[b_st], W=[b_st])
                    S.op("pool", "tensor_tensor", w_, w_, v_, ALU.mult, R=[b_st], W=[b_st])
                    S.op("pool", "tensor_scalar", w_, w_, -0.5, 1.5, ALU.mult, ALU.add, R=[b_st], W=[b_st])
                    S.op("pool", "tensor_tensor", y_, y_, w_, ALU.mult, R=[b_st], W=[b_st])
                return [st[:, col + 2 * k + i_:col + 2 * k + i_ + 1] for i_ in range(k)]

            def rope(eng, out_ap, in_ap, cos_ap, sin_ap, nh, half, Rb, Wb):
                P = in_ap.shape[0]
                x1 = in_ap[:, :, 0:half]
                x2 = in_ap[:, :, half:2 * half]
                cb = cos_ap[:, None, :].to_broadcast([P, nh, half])
                sbb = sin_ap[:, None, :].to_broadcast([P, nh, half])
                t = [r_[0:P, 0:nh * half].rearrange("p (h d) -> p h d", h=nh) for r_ in rt]
                S.op(eng, "tensor_tensor", t[0], x1, cb, ALU.mult, R=Rb + [b_tab], W=[b_rt])
                S.op(eng, "tensor_tensor", t[1], x2, sbb, ALU.mult, R=Rb + [b_tab], W=[b_rt])
                S.op(eng, "tensor_tensor", t[2], x2, cb, ALU.mult, R=Rb + [b_tab], W=[b_rt])
                S.op(eng, "tensor_tensor", t[3], x1, sbb, ALU.mult, R=Rb + [b_tab], W=[b_rt])
                S.op(eng, "tensor_tensor", out_ap[:, :, 0:half], t[0], t[1], ALU.subtract, R=[b_rt], W=Wb)
                S.op(eng, "tensor_tensor", out_ap[:, :, half:2 * half], t[2], t[3], ALU.add, R=[b_rt], W=Wb)

            def transpose_to(ps_i, col0, in_ap, Rb):
                P, Fd = in_ap.shape[0], in_ap.shape[1]
                S.op("pe", "transpose", pT[ps_i][0:Fd, col0:col0 + P], in_ap, ident[0:P, 0:P],
                     R=Rb + [b_ident], W=[bpT[ps_i]])

            def phase1(s, t):
                par = t % 2
                QpeT, b_QpeT = QpeT2[par], b_QpeT2[par]
                QabsT, b_Qabs = QabsT2[par], b_Qabs2[par]
                QS, b_QSq, b_QSs = QS2[par], b_QSq2[par], b_QSs2[par]
                gate, b_gate = gate2[par], b_gate2[par]
                xb = t % 2
                ce, cm = ("act", "copy") if t < ACT_COPY_T else ("dve", "tensor_copy")
                S.dma("sp", xs[xb][:], x_d[s, t * 128:(t + 1) * 128, :], W=[b_xs[xb]])
                r0 = rstd_multi([(xs[xb][:], D)], 0, [b_xs[xb]], xn, b_xn)[0]
                S.op("dve", "tensor_scalar", xn[:], xs[xb][:], r0, None, ALU.mult, R=[b_xs[xb], b_stc[0]], W=[b_xn])
                ti = nxt("T", 2)
                for c in range(8):
                    transpose_to(ti, c * 128, xn[:, c * 128:(c + 1) * 128], [b_xn])
                S.op(ce, cm, xnT[:], pT[ti][:, 0:1024].rearrange("p (c n) -> p c n", c=8), R=[bpT[ti]], W=[b_xnT])
                for cg, (c0, c1) in enumerate(((0, 512), (512, 1024), (1024, 1536), (1536, IN_COLS))):
                    mi = nxt("M", 2)
                    for c in range(8):
                        S.op("pe", "matmul", pM[mi][:, 0:c1 - c0], lhsT=xnT[:, c, :], rhs=w_in[:, c, c0:c1],
                             start=(c == 0), stop=(c == 7), R=[b_xnT, b_win], W=[bpM[mi]])
                    S.op(ce, cm,
                         u[:, c0:c1], pM[mi][:, 0:c1 - c0], R=[bpM[mi]], W=[b_u])
                    yield

                yield
                r1, r2 = rstd_multi([(u[:, 0:256], 256), (u[:, 256:384], 128)], 4, [b_u], cqn, b_cqn)
                S.op("dve", "tensor_scalar", cqn[:], u[:, 0:256], r1, None, ALU.mult, R=[b_u, b_stc[4]], W=[b_cqn])
                ti = nxt("T", 2)
                for c in range(2):
                    transpose_to(ti, c * 128, cqn[:, c * 128:(c + 1) * 128], [b_cqn])
                S.op("dve", "tensor_copy", cqnT[:], pT[ti][:, 0:256].rearrange("p (c n) -> p c n", c=2), R=[bpT[ti]], W=[b_cqnT])
                yield
                for half in range(2):
                    mi = nxt("M", 2)
                    for c in range(2):
                        S.op("pe", "matmul", pM[mi][:, 0:384], lhsT=cqnT[:, c, :], rhs=w_uq[:, c, half * 384:(half + 1) * 384],
                             start=(c == 0), stop=(c == 1), R=[b_cqnT, b_wuq], W=[bpM[mi]])
                    S.op(ce, cm, q_sb[:, half * 4:(half + 1) * 4, :],
                         pM[mi][:, 0:384].rearrange("p (h d) -> p h d", h=4), R=[bpM[mi]], W=[b_q])
                yield
                S.op("dve", "tensor_copy", q_sb[:, 8, 64:96], u[:, 384:416], R=[b_u], W=[b_q])
                S.op("dve", "tensor_copy", qn_sb[:], q_sb[:, 0:8, 0:64], R=[b_q], W=[b_qn])
                rope("dve", qpe_sb[:], q_sb[:, :, 64:96], tabs["cos_m"][:, t, :], tabs["sin_m"][:, t, :], 9, 16, [b_q], [b_qpe])
                ti = nxt("T", 2)
                for h in range(8):
                    transpose_to(ti, h * 128, qn_sb[:, h, :], [b_qn])
                S.op(ce, cm, QnT[:], pT[ti][0:64, 0:1024].rearrange("p (c n) -> p c n", c=8), R=[bpT[ti]], W=[b_QnT])
                ti = nxt("T", 2)
                for h in range(8):
                    transpose_to(ti, h * 128, qpe_sb[:, h, :], [b_qpe])
                S.op(ce, cm, QpeT[0:32], pT[ti][0:32, 0:1024].rearrange("p (c n) -> p c n", c=8), R=[bpT[ti]], W=[b_QpeT])
                yield
                for hg in range(2):
                    mi = nxt("M", 2)
                    for j in range(4):
                        h = hg * 4 + j
                        S.op("pe", "matmul", pM[mi][:, j * 128:(j + 1) * 128], lhsT=wukT[:, h, :],
                             rhs=QnT[:, h, :], start=True, stop=True, R=[b_wkv, b_QnT], W=[bpM[mi]])
                    S.op(ce, cm, QabsT[:, hg * 4:(hg + 1) * 4, :],
                         pM[mi][:, 0:512].rearrange("p (c n) -> p c n", c=4), R=[bpM[mi]], W=[b_Qabs])

                yield
                S.op("dve", "scalar_tensor_tensor", Clat[:, t, 0:128], u[:, 256:384], r2, gckv[:], ALU.mult, ALU.mult,
                     R=[b_u, b_stc[4], b_tab], W=[bClat[t]])
                ti = nxt("T", 2)
                transpose_to(ti, 0, Clat[:, t, 0:128], [bClat[t]])
                S.op("dve", "tensor_copy", KlatT[:, t * 128:(t + 1) * 128], pT[ti][:, 0:128], R=[bpT[ti]], W=[bKlat[t]])
                ti = nxt("T", 2)
                transpose_to(ti, 0, qpe_sb[:, 8, :], [b_qpe])
                S.op("dve", "tensor_copy", KpeT[0:32, t * 128:(t + 1) * 128], pT[ti][0:32, 0:128], R=[bpT[ti]], W=[bKpe[t]])

                yield
                uv = u[:, 416:1696].rearrange("p (b d) -> p b d", b=20)
                S.op("dve", "tensor_copy", ub[:, :, 16:64], uv[:, :, 16:64], R=[b_u], W=[b_ub])
                rope("dve", ub[:, :, 0:16], uv[:, :, 0:16], tabs["cos_n"][:, t, :], tabs["sin_n"][:, t, :], 20, 8, [b_u], [b_ub])
                S.op("dve", "tensor_copy", ub[:, 8:12, 0:16], uv[:, 8:12, 0:16], R=[b_u, b_ub], W=[b_ub])
                ti = nxt("T", 2)
                for h in range(8):
                    transpose_to(ti, h * 128, ub[:, h, :], [b_ub])
                S.op(ce, cm, QS[0:64].rearrange("p g j n -> p (g j) n"),
                     pT[ti][0:64, 0:1024].rearrange("p (c n) -> p c n", c=8), R=[bpT[ti]], W=[b_QSq])
                yield
                kvv = u[:, 928:1696].rearrange("p (s g d) -> p s g d", s=6, g=2)
                S.op("dve", "tensor_copy", Vs[:, t, :, 0:64], kvv[:, 3], R=[b_u], W=[bVs[t]])
                S.op("dve", "tensor_copy", Vw[:, t % 8, :, 0:64], kvv[:, 5], R=[b_u], W=[bVw[t % 8]])
                ti = nxt("T", 2)
                for g in range(2):
                    transpose_to(ti, g * 128, ub[:, 12 + g, :], [b_ub])
                    transpose_to(ti, 256 + g * 128, ub[:, 16 + g, :], [b_ub])
                S.op(ce, cm, KsE[0:64, :, t * 128:(t + 1) * 128],
                     pT[ti][0:64, 0:256].rearrange("p (g n) -> p g n", g=2), R=[bpT[ti]], W=[bKs[t]])
                S.op("dve", "tensor_copy", KwT[:, :, (t % 8) * 128:(t % 8 + 1) * 128],
                     pT[ti][0:64, 256:512].rearrange("p (g n) -> p g n", g=2), R=[bpT[ti]], W=[bKw[t % 8]])
                yield
                rb = t % 2
                if t == 0:
                    S.op("pool", "memset", rawT[rb][:, :, :, 0:16], 0.0, W=[b_rawT[rb]])
                else:
                    S.op("dve", "tensor_copy", rawT[rb][:, :, :, 0:16], rawT[1 - rb][:, :, :, 128:144],
                         R=[b_rawT[1 - rb]], W=[b_rawT[rb]])
                ti = nxt("T", 2)
                for c in range(4):
                    transpose_to(ti, c * 128, ub[:, 8 + c, :], [b_ub])
                S.op(ce, cm, rawT[rb][:, :, :, 16:144],
                     pT[ti][0:64, 0:512].rearrange("p (k g n) -> p k g n", k=2, g=2), R=[bpT[ti]], W=[b_rawT[rb]])
                yield
                S.op("act", "activation", gate[:].rearrange("p b h -> p (b h)"), u[:, 1696:1720], AF.Tanh, scale=0.5, R=[b_u], W=[b_gate])
                S.op("dve", "tensor_scalar", gate[:].rearrange("p b h -> p (b h)"), gate[:].rearrange("p b h -> p (b h)"), 0.5, 0.5,
                     ALU.mult, ALU.add, R=[b_gate], W=[b_gate])

                yield
                mi = nxt("M", 2)
                for kv in range(2):
                    for g in range(2):
                        c0 = (kv * 2 + g) * 8
                        for l in range(32):
                            S.op("pe", "matmul", pM[mi][:, c0:c0 + 8], lhsT=w1[:, kv, l, :], rhs=rawT[rb][:, kv, g, l:l + 113:16],
                                 start=(l == 0), stop=(l == 31), R=[b_w1, b_rawT[rb]], W=[bpM[mi]])
                            if l % 8 == 7:
                                yield
                for kv in range(2):
                    S.op("dve", "tensor_scalar", z_sb[:, kv * 16:(kv + 1) * 16], pM[mi][:, kv * 16:(kv + 1) * 16],
                         bias_tot[:, kv:kv + 1], None, ALU.add, R=[bpM[mi], b_bt], W=[b_z])
                S.op("dve", "tensor_tensor", z2_sb[:], z_sb[:], z_sb[:], ALU.mult, R=[b_z], W=[b_z])
                S.op("dve", "tensor_scalar", z2_sb[:], z2_sb[:], 0.044715, 1.0, ALU.mult, ALU.add, R=[b_z], W=[b_z])
                S.op("dve", "tensor_tensor", z2_sb[:], z2_sb[:], z_sb[:], ALU.mult, R=[b_z], W=[b_z])
                S.op("act", "activation", z2_sb[:], z2_sb[:], AF.Tanh, scale=math.sqrt(2.0 / math.pi), R=[b_z], W=[b_z])
                S.op("dve", "tensor_scalar", z2_sb[:], z2_sb[:], 0.5, 0.5, ALU.mult, ALU.add, R=[b_z], W=[b_z])
                S.op("dve", "tensor_tensor", hid_sb[:], z_sb[:], z2_sb[:], ALU.mult, R=[b_z], W=[b_hid])
                yield
                n0 = 8 * t - 1
                m0 = 1 if t == 0 else 0
                mi = nxt("M", 2)
                for g in range(2):
                    S.op("pe", "matmul", pM[mi][0:8, g * 64:(g + 1) * 64], lhsT=hid_sb[:, g * 8:(g + 1) * 8], rhs=w2[:, 0, :],
                         start=True, stop=True, R=[b_hid, b_w1], W=[bpM[mi]])
                for g in range(2):
                    S.op("pe", "matmul", pM[mi][0:64, 128 + g * 8:136 + g * 8], lhsT=w2[:, 1, :], rhs=hid_sb[:, 16 + g * 8:24 + g * 8],
                         start=True, stop=True, R=[b_hid, b_w1], W=[bpM[mi]])
                S.op("dve", "tensor_tensor", kc_f[:].rearrange("p g d -> p (g d)"), pM[mi][0:8, 0:128], b2k[0:8, :], ALU.add,
                     R=[bpM[mi], b_tab], W=[b_kc])
                S.op("dve", "tensor_scalar", VcT[:, :, n0 + m0:n0 + 8], pM[mi][0:64, 128:144].rearrange("p (g m) -> p g m", g=2)[:, :, m0:8],
                     b2v[:, 0:1], None, ALU.add, R=[bpM[mi], b_tab], W=[b_VcT])
                S.op("dve", "tensor_copy", kc_sb[:, :, 16:64], kc_f[:, :, 16:64], R=[b_kc], W=[b_kc])
                rope("dve", kc_sb[:, :, 0:16], kc_f[:, :, 0:16], cos_e[:, t, :], sin_e[:, t, :], 2, 8, [b_kc], [b_kc])
                ti = nxt("T", 2)
                for g in range(2):
                    transpose_to(ti, g * 8, kc_sb[:, g, :], [b_kc])
                S.op("dve", "tensor_copy", KcT[:, :, n0 + m0:n0 + 8],
                     pT[ti][0:64, 0:16].rearrange("p (g m) -> p g m", g=2)[:, :, m0:8], R=[bpT[ti]], W=[b_Kc])
                yield
                nts = sorted(set([max(n0, 0) // 128, (n0 + 7) // 128]))
                for nt in nts:
                    ti = nxt("T", 2)
                    for g in range(2):
                        S.op("pe", "transpose", pT[ti][:, g * 64:(g + 1) * 64], VcT[:, g, nt * 128:(nt + 1) * 128], ident[0:64, 0:64],
                             R=[b_VcT, b_ident], W=[bpT[ti]])
                    S.op("dve", "tensor_copy", VcO[:, nt, :, 0:64], pT[ti][:, 0:128].rearrange("p (g d) -> p g d", g=2), R=[bpT[ti]], W=[b_VcO])

            def attn_loop(kts, qk_fn, post_fn):
                if not kts:
                    return
                si_next = qk_fn(kts[0])
                for i_, kt in enumerate(kts):
                    si = si_next
                    if i_ + 1 < len(kts):
                        si_next = qk_fn(kts[i_ + 1])
                    post_fn(kt, si)

            def mla(s, t):
                par = t % 2
                QpeT, b_QpeT = QpeT2[par], b_QpeT2[par]
                QabsT, b_Qabs = QabsT2[par], b_Qabs2[par]
                QS, b_QSq, b_QSs = QS2[par], b_QSq2[par], b_QSs2[par]
                gate, b_gate = gate2[par], b_gate2[par]
                mo = nxt("M", 2)
                for hg in range(2):
                    qa = QabsT[:, hg * 4:(hg + 1) * 4, :].rearrange("p c n -> p (c n)")
                    qp = QpeT[:, hg * 4:(hg + 1) * 4, :].rearrange("p c n -> p (c n)")

                    def qk(kt):
                        si = nxt("S", 2)
                        S.op("pe", "matmul", pS[si][:], lhsT=KlatT[:, kt * 128:(kt + 1) * 128], rhs=qa,
                             start=True, stop=False, R=[bKlat[kt], b_Qabs], W=[bpS[si]])
                        S.op("pe", "matmul", pS[si][:], lhsT=KpeT[:, kt * 128:(kt + 1) * 128], rhs=qp,
                             start=False, stop=True, R=[bKpe[kt], b_QpeT], W=[bpS[si]])
                        return si

                    def post(kt, si):
                        pi = nxt("P", 3)
                        S.op("act", "activation", PT[pi][:], pS[si][:], AF.Exp, R=[bpS[si]], W=[b_PT[pi]])
                        if kt == t:
                            S.op("dve", "tensor_tensor", PT[pi][:], PT[pi][:], tri4[:], ALU.mult, R=[b_PT[pi], b_msk], W=[b_PT[pi]])
                        S.op("pe", "matmul", pA[0][:], lhsT=Clat[:, kt, 0:128], rhs=PT[pi][:], start=(kt == 0), stop=(kt == t),
                             R=[b_PT[pi], bClat[kt]], W=[bpA[0]])
                        if kt == 0:
                            S.op("dve", "tensor_copy", lacc[:], PT[pi][:], R=[b_PT[pi]], W=[b_lacc])
                        else:
                            S.op("dve", "tensor_tensor", lacc[:], lacc[:], PT[pi][:], ALU.add, R=[b_PT[pi], b_lacc], W=[b_lacc])

                    attn_loop(list(range(t + 1)), qk, post)
                    for j in range(4):
                        S.op("pe", "matmul", pA[1][:, j:j + 1], lhsT=lacc[:, j * 128:(j + 1) * 128], rhs=ones_f[:, 0:1],
                             start=True, stop=True, R=[b_lacc, b_ones], W=[bpA[1]])
                    S.op("dve", "tensor_copy", OlatT[:], pA[0][:].rearrange("p (c n) -> p c n", c=4), R=[bpA[0]], W=[b_OlatT])
                    S.op("dve", "reciprocal", rl[:, hg * 4:(hg + 1) * 4], pA[1][:, 0:4], R=[bpA[1]], W=[b_rl])
                    for j in range(4):
                        h = hg * 4 + j
                        S.op("pe", "matmul", pM[mo][:, h * 64:(h + 1) * 64], lhsT=OlatT[:, j, :], rhs=wuv[:, h, :],
                             start=True, stop=True, R=[b_OlatT, b_wkv], W=[bpM[mo]])
                S.op("dve", "tensor_tensor", y_sb[:, 0:512].rearrange("p (h d) -> p h d", h=8),
                     pM[mo][:, 0:512].rearrange("p (h d) -> p h d", h=8), rl[:, 0:8, None].to_broadcast([128, 8, 64]), ALU.mult,
                     R=[bpM[mo], b_rl], W=[b_y])

            def nsa_sel(s, t, g):
                par = t % 2
                QpeT, b_QpeT = QpeT2[par], b_QpeT2[par]
                QabsT, b_Qabs = QabsT2[par], b_Qabs2[par]
                QS, b_QSq, b_QSs = QS2[par], b_QSq2[par], b_QSs2[par]
                gate, b_gate = gate2[par], b_gate2[par]
                QSg = QS[:, g].rearrange("p j n -> p (j n)")
                QSg = QS[:, g].rearrange("p j n -> p (j n)")
                nts = [0] + ([1] if t >= 16 else [])
                for nt in nts:
                    si = nxt("M", 2)
                    S.op("pe", "matmul", pM[si][:], lhsT=KcT[:, g, nt * 128:(nt + 1) * 128], rhs=QSg[0:64, :],
                         start=True, stop=True, R=[b_Kc, b_QSq], W=[bpM[si]])
                    S.op("act", "activation", PcT[nt][:], pM[si][:], AF.Exp, R=[bpM[si]], W=[b_PcT[nt]])
                    S.op("pool", "affine_select", PcT[nt][:].rearrange("p (j n) -> p j n", j=4),
                         PcT[nt][:].rearrange("p (j n) -> p j n", j=4), [[0, 4], [1, 128]], ALU.is_ge, 0.0,
                         base=128 * t - 31 - 2048 * nt, channel_multiplier=-16, R=[b_PcT[nt]], W=[b_PcT[nt]])
                yield
                mc = nxt("M", 2)
                for j in range(4):
                    for i_, nt in enumerate(nts):
                        S.op("pe", "matmul", pM[mc][:, j * 128:(j + 1) * 128], lhsT=PcT[nt][:, j * 128:(j + 1) * 128],
                             rhs=VcO[:, nt, g, :], start=(i_ == 0), stop=(i_ == len(nts) - 1),
                             R=[b_PcT[nt], b_VcO], W=[bpM[mc]])
                yield
                pc = pM[mc][:].rearrange("p (j c) -> p j c", j=4)
                S.op("dve", "tensor_reduce", st[:, 24:28], pc[:, :, 64:128], AX.X, ALU.add, R=[bpM[mc]], W=[b_stn])
                S.op("dve", "tensor_scalar", st[:, 24:28], st[:, 24:28], 1e-30, None, ALU.max, R=[b_stn], W=[b_stn])
                S.op("dve", "reciprocal", st[:, 28:32], st[:, 24:28], R=[b_stn], W=[b_stn])
                S.op("dve", "tensor_tensor", tmp4[:], pc[:, :, 64:128], st[:, 28:32, None].to_broadcast([128, 4, 64]), ALU.mult,
                     R=[bpM[mc], b_stn], W=[b_tmp4])
                S.op("dve", "tensor_reduce", imp[:], tmp4[:].rearrange("p j c -> p c j"), AX.X, ALU.add, R=[b_tmp4], W=[b_imp])
                yield
                S.op("dve", "tensor_tensor", st[:, 32:36], st[:, 28:32], gate[:, 0, g * 4:(g + 1) * 4], ALU.mult,
                     R=[b_stn, b_gate], W=[b_stn])
                S.op("dve", "tensor_tensor", y_sb[:, 512 + g * 256:768 + g * 256].rearrange("p (j c) -> p j c", j=4), pc[:, :, 0:64],
                     st[:, 32:36, None].to_broadcast([128, 4, 64]), ALU.mult, R=[bpM[mc], b_stn], W=[b_yn])
                yield
                S.op("pool", "affine_select", sc1[:], imp[:], [[-64, 64]], ALU.is_ge, 1e9, base=128 * t - 128, channel_multiplier=1,
                     R=[b_imp], W=[b_imp])
                S.op("pool", "affine_select", sc2[:], sc1[:], [[-64, 64]], ALU.is_ge, -1e9, base=128 * t, channel_multiplier=1,
                     R=[b_imp], W=[b_imp])
                S.op("pool", "memset", sc2[:, 0:1], 1e9, R=[b_imp], W=[b_imp])
                yield
                S.op("dve", "max", st[:, 40:48], sc2[:], R=[b_imp], W=[b_stn])
                S.op("dve", "match_replace", sc3[:], st[:, 40:48], sc2[:], -3e38, R=[b_imp, b_stn], W=[b_imp])
                S.op("dve", "max", st[:, 48:56], sc3[:], R=[b_imp], W=[b_stn])
                S.op("dve", "tensor_scalar", selq[:, 64:128], sc2[:], st[:, 55:56], NEG, ALU.is_lt, ALU.mult,
                     R=[b_imp, b_stn], W=[b_selq])
                yield
                ti = nxt("T", 2)
                transpose_to(ti, 0, selq[:], [b_selq])
                S.op("dve", "tensor_copy", QS[64:128, g], pT[ti][64:128, None, 0:128].to_broadcast([64, 4, 128]),
                     R=[bpT[ti]], W=[b_QSs[g]])
                yield

            def nsa_attn(s, t, g):
                par = t % 2
                QpeT, b_QpeT = QpeT2[par], b_QpeT2[par]
                QabsT, b_Qabs = QabsT2[par], b_Qabs2[par]
                QS, b_QSq, b_QSs = QS2[par], b_QSq2[par], b_QSs2[par]
                gate, b_gate = gate2[par], b_gate2[par]
                QSg = QS[:, g].rearrange("p j n -> p (j n)")
                S.op("dve", "memset", pA[0][:, 0:260], 0.0, W=[bpA[0]])
                S.op("dve", "memset", pA[1][:, 0:260], 0.0, W=[bpA[1]])
                def qk_s(kt):
                    si = nxt("S", 2)
                    S.op("pe", "matmul", pS[si][:], lhsT=KsE[:, g, kt * 128:(kt + 1) * 128], rhs=QSg,
                         start=True, stop=True, R=[bKs[kt], b_exp, b_QSq, b_QSs[g]], W=[bpS[si]])
                    return si

                def post_s(kt, si):
                    pi = nxt("P", 3)
                    S.op("act", "activation", PT[pi][:], pS[si][:], AF.Exp, R=[bpS[si]], W=[b_PT[pi]])
                    if kt == t:
                        S.op("dve", "tensor_tensor", PT[pi][:], PT[pi][:], tri4[:], ALU.mult, R=[b_PT[pi], b_msk], W=[b_PT[pi]])
                    for j in range(4):
                        S.op("pe", "matmul", pA[0][:, j * 65:j * 65 + 65], lhsT=PT[pi][:, j * 128:(j + 1) * 128],
                             rhs=Vs[:, kt, g, :], start=False, stop=(kt == t), skip_group_check=True,
                             R=[b_PT[pi], bVs[kt]], W=[bpA[0]])

                def qk_w(kt):
                    si = nxt("S", 2)
                    sl = kt % 8
                    S.op("pe", "matmul", pS[si][:], lhsT=KwT[:, g, sl * 128:(sl + 1) * 128], rhs=QSg[0:64, :],
                         start=True, stop=True, R=[bKw[sl], b_QSq], W=[bpS[si]])
                    return si

                def post_w(kt, si):
                    sl = kt % 8
                    pi = nxt("P", 3)
                    S.op("act", "activation", PT[pi][:], pS[si][:], AF.Exp, R=[bpS[si]], W=[b_PT[pi]])
                    if kt == t:
                        S.op("dve", "tensor_tensor", PT[pi][:], PT[pi][:], tri4[:], ALU.mult, R=[b_PT[pi], b_msk], W=[b_PT[pi]])
                    if kt == t - 4:
                        S.op("dve", "tensor_tensor", PT[pi][:], PT[pi][:], anti4[:], ALU.mult, R=[b_PT[pi], b_msk], W=[b_PT[pi]])
                    for j in range(4):
                        S.op("pe", "matmul", pA[1][:, j * 65:j * 65 + 65], lhsT=PT[pi][:, j * 128:(j + 1) * 128],
                             rhs=Vw[:, sl, g, :], start=False, stop=(kt == t), skip_group_check=True,
                             R=[b_PT[pi], bVw[sl]], W=[bpA[1]])

                attn_loop(list(range(t + 1)), qk_s, post_s)
                attn_loop(list(range(max(0, t - 4), t + 1)), qk_w, post_w)
                for br in range(2):
                    pa = pA[br][:, 0:260].rearrange("p (j c) -> p j c", j=4)
                    S.op("dve", "reciprocal", st[:, 56:60], pa[:, :, 64], R=[bpA[br]], W=[b_stn])
                    S.op("dve", "tensor_tensor", st[:, 60:64], st[:, 56:60], gate[:, 1 + br, g * 4:(g + 1) * 4], ALU.mult,
                         R=[b_stn, b_gate], W=[b_stn])
                    yv = y_sb[:, 512 + g * 256:768 + g * 256].rearrange("p (j c) -> p j c", j=4)
                    S.op("dve", "tensor_tensor", tmp4[:], pa[:, :, 0:64], st[:, 60:64, None].to_broadcast([128, 4, 64]), ALU.mult,
                         R=[bpA[br], b_stn], W=[b_tmp4])
                    S.op("dve", "tensor_tensor", yv, yv, tmp4[:], ALU.add, R=[b_tmp4, b_yn], W=[b_yn])


            def outproj(s, t):
                xb = t % 2
                ra, rb_ = rstd_multi([(y_sb[:, 0:512], 512), (y_sb[:, 512:1024], 512)], 12, [b_y, b_yn], mixed, b_mixed)
                S.op("dve", "tensor_scalar", mixed[:, 0:512], y_sb[:, 0:512], ra, None, ALU.mult, R=[b_y, b_stc[12]], W=[b_mixed])
                S.op("dve", "tensor_scalar", mixed[:, 512:1024], y_sb[:, 512:1024], rb_, None, ALU.mult, R=[b_yn, b_stc[12]], W=[b_mixed])
                ti = nxt("T", 2)
                for c in range(8):
                    transpose_to(ti, c * 128, mixed[:, c * 128:(c + 1) * 128], [b_mixed])
                S.op("dve", "tensor_copy", mixedT[:], pT[ti][:, 0:1024].rearrange("p (c n) -> p c n", c=8), R=[bpT[ti]], W=[b_mixedT])
                for dh in range(2):
                    mi = nxt("S", 2)
                    for c in range(8):
                        S.op("pe", "matmul", pS[mi][:], lhsT=mixedT[:, c, :], rhs=w_o[:, c, dh * 512:(dh + 1) * 512],
                             start=(c == 0), stop=(c == 7), R=[b_mixedT, b_wo], W=[bpS[mi]])
                    S.op("dve", "tensor_tensor", h_sb[xb][:, dh * 512:(dh + 1) * 512], pS[mi][:], xs[xb][:, dh * 512:(dh + 1) * 512],
                         ALU.add, R=[bpS[mi], b_xs[xb]], W=[b_h[xb]])
                row = (s * SEQ + t * 128)
                S.dma("sp", hscr_d[row:row + 128, :], h_sb[xb][:], R=[b_h[xb]])

            bgst = {"gen": None, "credit": 0.0, "rate": 0.0}

            def bg_run(n=None):
                if bgst["gen"] is None:
                    return
                mode["bg"] = True
                try:
                    k = 0
                    while n is None or k < n:
                        next(bgst["gen"])
                        k += 1
                except StopIteration:
                    bgst["gen"] = None
                mode["bg"] = False

            def tick():
                if mode["bg"] or bgst["gen"] is None:
                    return
                bgst["credit"] += bgst["rate"]
                if bgst["credit"] >= 1.0:
                    n = int(bgst["credit"])
                    bgst["credit"] -= n
                    bg_run(n)

            S.tick = tick
            def chain(*gens):
                for g_ in gens:
                    yield from g_

            def set_bg(gen, nchunks, fg_ops):
                bgst["gen"] = gen
                bgst["credit"] = 0.0
                bgst["rate"] = nchunks / (0.7 * fg_ops)

            for s in range(nseq):
                bgst["gen"] = phase1(s, 0) if "p1" in stages else None
                bg_run(None)
                for t in range(ntiles):
                    if "nsa" in stages:
                        set_bg(chain(nsa_sel(s, t, 0), nsa_sel(s, t, 1)), 16.0, 30.0 + 18.0 * (t + 1))
                    if "mla" in stages:
                        mla(s, t)
                    bg_run(None)
                    if t + 1 < ntiles and "p1" in stages:
                        set_bg(phase1(s, t + 1), 40.0, 100.0 + 14.0 * (t + 1 + min(t + 1, 5)))
                    if "nsa" in stages:
                        nsa_attn(s, t, 0)
                        nsa_attn(s, t, 1)
                    if "out" in stages:
                        outproj(s, t)
                    bg_run(None)
            S.tick = None
            S.barrier()
        mode["B"] = True
        B = ExitStack()
        with B:
            def sb2(name, shape, dt):
                return B.enter_context(nc.sbuf_tensor("b_" + name, shape, dt))
            gb = sb2("gb", [128, 8], F32); b_gb = S.buf("gb")
            S.dma("sp", gb[:], g_mlp_d, W=[b_gb])
            gfin = sb2("gfin", [128, D], F32)
            S.dma("sp", gfin[:], gfin_d, W=[b_gb])
            ident2 = sb2("ident2", [128, 128], BF16); b_id2 = S.buf("ident2")
            S.dma("pool", ident2[:], ident_d, W=[b_id2])
            w_up = sb2("w_up", [128, 8, DFF], BF16); b_wup = S.buf("w_up")
            for c in range(8):
                S.dma("pool", w_up[:, c, :], w_up_d[c * 128:(c + 1) * 128, :], W=[b_wup])
                S.op("dve", "tensor_scalar", w_up[:, c, :], w_up[:, c, :], gb[:, c:c + 1], None, ALU.mult, R=[b_gb, b_wup], W=[b_wup])
            w_dn = sb2("w_dn", [128, 32, D], BF16); b_wdn = S.buf("w_dn")
            wdv = w_down_d.rearrange("(f p) n -> p f n", p=128)
            for f4 in range(8):
                S.dma("pool", w_dn[:, f4 * 4:(f4 + 1) * 4, :], wdv[:, f4 * 4:(f4 + 1) * 4, :], W=[b_wdn])
            hin = [sb2("hin%d" % i, [128, D], F32) for i in range(4)]; b_hin = S.bufs("hin", 4)
            st2 = sb2("st2", [128, 16], F32); b_st2 = S.buf("st2")
            junk2 = sb2("junk2", [128, D], BF16); b_junk2 = S.buf("junk2")
            hn = sb2("hn", [128, D], BF16); b_hn = S.buf("hn")
            hnT = sb2("hnT", [128, 8, 512], BF16); b_hnT = S.buf("hnT")
            rl = [sb2("rl%d" % i, [128, 512], BF16) for i in range(2)]; b_rl = S.bufs("rl", 2)
            aT = sb2("aT", [128, 32, 512], BF16); b_aT = S.buf("aT")
            yo = [sb2("yo%d" % i, [128, D], F32) for i in range(2)]; b_yo = S.bufs("yo", 2)
            S.op("pool", "memset", st2[:], 0.0, W=[b_st2])
            nT = (nseq * ntiles * 128) // 512 if do_mlp else 0

            def rstd2(src_ap, Rb):
                S.op("pool", "memset", st2[:, 0:1], 0.0, W=[b_st2])
                S.op("act", "activation", junk2[:], src_ap, AF.Square, accum_out=st2[:, 0:1], R=Rb, W=[b_junk2, b_st2])
                S.op("dve", "tensor_scalar", st2[:, 1:2], st2[:, 0:1], 1.0 / D, EPS, ALU.mult, ALU.add, R=[b_st2], W=[b_st2])
                S.op("act", "activation", st2[:, 2:3], st2[:, 1:2], AF.Sqrt, R=[b_st2], W=[b_st2])
                S.op("dve", "reciprocal", st2[:, 3:4], st2[:, 2:3], R=[b_st2], W=[b_st2])
                return st2[:, 3:4]

            oc = 0
            for T in range(nT):
                hb = T % 2
                row = T * 512 if nseq * ntiles * 128 == nseq * SEQ else None
                base = (T * 512 // (ntiles * 128)) * SEQ + (T * 512) % (ntiles * 128)
                for i in range(4):
                    S.dma("sp", hin[i][:], hscr_d[base + i * 128:base + (i + 1) * 128, :], W=[b_hin[i]])
                for i in range(4):
                    r = rstd2(hin[i][:], [b_hin[i]])
                    S.op("dve", "tensor_scalar", hn[:], hin[i][:], r, None, ALU.mult, R=[b_hin[i], b_st2], W=[b_hn])
                    ti = nxt("T", 2)
                    for c in range(8):
                        S.op("pe", "transpose", pT[ti][:, c * 128:(c + 1) * 128], hn[:, c * 128:(c + 1) * 128], ident2[:],
                             R=[b_hn, b_id2], W=[bpT[ti]])
                    S.op("act", "copy", hnT[:, :, i * 128:(i + 1) * 128], pT[ti][:, 0:1024].rearrange("p (c n) -> p c n", c=8),
                         R=[bpT[ti]], W=[b_hnT])
                for f in range(32):
                    si = nxt("S", 2)
                    for c in range(8):
                        S.op("pe", "matmul", pS[si][:], lhsT=w_up[:, c, f * 128:(f + 1) * 128], rhs=hnT[:, c, :],
                             start=(c == 0), stop=(c == 7), R=[b_wup, b_hnT], W=[bpS[si]])
                    ri = f % 2
                    S.op("act", "activation", rl[ri][:], pS[si][:], AF.Relu, R=[bpS[si]], W=[b_rl[ri]])
                    S.op("pool", "tensor_tensor", aT[:, f, :], rl[ri][:], rl[ri][:], ALU.mult, R=[b_rl[ri]], W=[b_aT])
                for i in range(4):
                    ob = oc % 2
                    oc += 1
                    for dh in range(2):
                        mi = nxt("M", 2)
                        for f in range(32):
                            S.op("pe", "matmul", pM[mi][:], lhsT=aT[:, f, i * 128:(i + 1) * 128], rhs=w_dn[:, f, dh * 512:(dh + 1) * 512],
                                 start=(f == 0), stop=(f == 31), R=[b_aT, b_wdn], W=[bpM[mi]])
                        S.op("dve", "tensor_tensor", yo[ob][:, dh * 512:(dh + 1) * 512], pM[mi][:], hin[i][:, dh * 512:(dh + 1) * 512],
                             ALU.add, R=[bpM[mi], b_hin[i]], W=[b_yo[ob]])
                    r = rstd2(yo[ob][:], [b_yo[ob]])
                    S.op("dve", "scalar_tensor_tensor", yo[ob][:], yo[ob][:], r, gfin[:], ALU.mult, ALU.mult,
                         R=[b_yo[ob], b_st2, b_gb], W=[b_yo[ob]])
                    S.dma("sp", out_d[base + i * 128:base + (i + 1) * 128, :], yo[ob][:], R=[b_yo[ob]])
            S.emit()
            print('NOPS', S.nops)
    return nc


def _rope_tab(pos, dim):
    inv = np.exp(np.float32(-math.log(500000.0)) * np.arange(0, dim, 2, dtype=np.float32) / np.float32(dim)).astype(np.float32)
    ang = pos.astype(np.float32)[:, None] * inv[None, :]
    return np.cos(ang).astype(np.float32), np.sin(ang).astype(np.float32)


def _tok_major(a):
    return np.ascontiguousarray(a.reshape(NT, 128, -1).transpose(1, 0, 2))


def host_consts():
    pos = np.arange(SEQ)
    cm, sm = _rope_tab(pos, 32)
    cn, sn = _rope_tab(pos, 16)
    c = {}
    c["cos_m"], c["sin_m"] = _tok_major(cm), _tok_major(sm)
    c["cos_n"], c["sin_n"] = _tok_major(cn), _tok_major(sn)
    c["cos_n8"], c["sin_n8"] = _tok_major(cn * np.float32(0.125)), _tok_major(sn * np.float32(0.125))
    ce = np.zeros((8, NT, 8), np.float32)
    se = np.zeros((8, NT, 8), np.float32)
    for t in range(NT):
        for m in range(8):
            n = 8 * t - 1 + m
            if 0 <= n < 255:
                p = 16 * n + 31
                ce[m, t], se[m, t] = cn[p], sn[p]
    c["cos_e"], c["sin_e"] = ce, se
    k = np.arange(128)[:, None]
    q = np.arange(128)[None, :]
    tri = (q >= k).astype(np.float32)
    anti = (q < k).astype(np.float32)
    c["tri4"] = np.ascontiguousarray(np.tile(tri, (1, 4)))
    c["anti4"] = np.ascontiguousarray(np.tile(anti, (1, 4)))
    n = np.arange(256)[:, None] * 16
    j = np.arange(64)[None, :] * 64
    ov = np.clip(np.minimum(n + 32, j + 64) - np.maximum(n, j), 0, None).astype(np.float32) / 32.0
    ov[255] = 0.0
    c["ovl"] = np.ascontiguousarray(ov.reshape(2, 128, 64).transpose(1, 0, 2))
    c["expand"] = (np.arange(SEQ)[None, :] // 64 == np.arange(64)[:, None]).astype(np.float32)
    c["ident"] = np.eye(128, dtype=np.float32)
    return c


def host_weights(inp):
    f = lambda a: np.ascontiguousarray(np.asarray(a, dtype=np.float32))
    pc = lambda v: np.ascontiguousarray(np.asarray(v, np.float32).reshape(-1, 128).T)
    w = {}
    w["w_in"] = f(inp["w_in"][0])
    w["g_mix"] = pc(inp["g_mix_norm"][0])
    w["w_uq"] = f(inp["w_uq"][0])
    w["g_cq"] = pc(inp["g_cq"][0])
    wukv = np.asarray(inp["w_ukv"][0], np.float32).reshape(128, 8, 2, 64)
    wuk = wukv[:, :, 0, :]
    w["wukT"] = np.ascontiguousarray(wuk.transpose(2, 1, 0))
    w["wuv"] = np.ascontiguousarray(wukv[:, :, 1, :])
    w["gckv_bc"] = np.ascontiguousarray(np.broadcast_to(np.asarray(inp["g_ckv"][0], np.float32)[None, :], (128, 128)))
    w1 = np.stack([np.asarray(inp["cmp_w1_k"][0], np.float32), np.asarray(inp["cmp_w1_v"][0], np.float32)])
    w["cmp_w1"] = np.ascontiguousarray(w1.reshape(2, 32, 64, 128).transpose(0, 2, 1, 3))
    pe = np.stack([np.asarray(inp["cmp_pe_k"][0], np.float32), np.asarray(inp["cmp_pe_v"][0], np.float32)])
    w["cmp_peT"] = np.ascontiguousarray(pe.transpose(0, 2, 1))
    w["cmp_b1"] = np.ascontiguousarray(np.stack([inp["cmp_b1_k"][0], inp["cmp_b1_v"][0]], axis=1).astype(np.float32))
    w["cmp_w2"] = np.ascontiguousarray(np.stack([inp["cmp_w2_k"][0], inp["cmp_w2_v"][0]], axis=1).astype(np.float32))
    b2k = np.asarray(inp["cmp_b2_k"][0], np.float32)
    w["cmp_b2k_bc"] = np.ascontiguousarray(np.broadcast_to(np.concatenate([b2k, b2k])[None, :], (128, 128)))
    w["cmp_b2v"] = np.ascontiguousarray(np.asarray(inp["cmp_b2_v"][0], np.float32).reshape(64, 1))
    w["w_o"] = f(inp["w_o"][0])
    w["g_out"] = pc(np.concatenate([np.asarray(inp["g_out_mla"][0]), np.asarray(inp["g_out_nsa"][0])]))
    w["w_up"] = f(inp["w_up"][0])
    w["g_mlp"] = pc(inp["g_mlp_norm"][0])
    w["w_down"] = f(inp["w_down"][0])
    w["gfin_bc"] = np.ascontiguousarray(np.broadcast_to(np.asarray(inp["g_final"], np.float32)[None, :], (128, D)))
    return w


_NC_CACHE = {}


def kernel(**inputs):
    x = np.asarray(inputs["x"], dtype=np.float32)
    shared = host_consts()
    shared.update(host_weights(inputs))
    if "full" not in _NC_CACHE:
        _NC_CACHE["full"] = build_program()
    nc = _NC_CACHE["full"]
    in_maps = []
    for c in range(NCORES):
        m = dict(shared)
        m["x"] = np.ascontiguousarray(x[c * NSEQ:(c + 1) * NSEQ])
        in_maps.append(m)
    res = run_bass_kernel_spmd(nc, in_maps, core_ids=list(range(NCORES)))
    outs = [np.asarray(r["out"]).reshape(NSEQ, SEQ, D) for r in res.results]
    return np.concatenate(outs, axis=0).astype(np.float32)
```

```python
import math
import numpy as np
from contextlib import ExitStack
import concourse.bass as bass
import concourse.mybir as mybir
from concourse.bass_utils import run_bass_kernel_spmd

F32 = mybir.dt.float32
BF16 = mybir.dt.bfloat16
I32 = mybir.dt.int32
AF = mybir.ActivationFunctionType
ALU = mybir.AluOpType
AX = mybir.AxisListType

NCORES = 8
SEQ = 4096
D = 1024
NSEQ = 2
NT = SEQ // 128
EPS = 1e-6
IN_COLS = 1720
DFF = 4096
NEG = -30000.0
STRICT_SAME = True
OP_LIMIT = None
BG_OVERLAP = True
USE_MAGIC = True
ACT_COPY_T = 20


class Buf:
    __slots__ = ("name", "w", "r", "dsem", "dcnt", "excl")

    def __init__(self, name):
        self.name = name
        self.excl = False
        self.w = None
        self.r = {}
        self.dsem = None
        self.dcnt = 0


class Sched:
    ENG = ("pe", "act", "dve", "pool", "sp")

    def __init__(self, nc, ctx):
        self.nc = nc
        self.ctx = ctx
        self.sem = {e: ctx.enter_context(nc.semaphore("s_" + e)) for e in self.ENG}
        self.cnt = {e: 0 for e in self.ENG}
        self.seen = {e: {} for e in self.ENG}
        self.prog = {e: [] for e in self.ENG}
        self.dbufs = []
        self.nb = 0
        self.nops = 0
        self.fillregs = {}
        self.tick = None
        self.limit = OP_LIMIT

    def buf(self, name):
        self.nb += 1
        return Buf("%s_%d" % (name, self.nb))

    def bufs(self, name, n):
        return [self.buf(name) for _ in range(n)]

    def _dsem(self, b):
        if b.dsem is None:
            b.dsem = self.ctx.enter_context(self.nc.semaphore("d_" + b.name))
            self.dbufs.append(b)
        return b.dsem

    def _deps(self, e, reads, writes, strict):
        toks = []
        for b in reads:
            if b.w is not None:
                toks.append(b.w)
            if b.excl:
                toks.extend(b.r.values())
        for b in writes:
            if b.w is not None:
                toks.append(b.w)
            toks.extend(b.r.values())
        need = {}
        for (key, sem, val) in toks:
            if key == e and not strict and (e == "pe" or not STRICT_SAME):
                continue
            if self.seen[e].get(key, 0) >= val:
                continue
            if key not in need or need[key][1] < val:
                need[key] = (sem, val)
        for key, (sem, val) in need.items():
            self.seen[e][key] = val
            self.prog[e].append(("wait", sem, val))

    def op(self, e, meth, *args, R=(), W=(), **kw):
        self.nops += 1
        if self.limit is not None and self.nops > self.limit:
            return None
        self._deps(e, R, W, False)
        self.cnt[e] += 1
        tok = (e, self.sem[e], self.cnt[e])
        self.prog[e].append(("op", meth, args, kw))
        for b in R:
            b.r[e] = tok
        for b in W:
            b.w = tok
            b.r = {}
        if self.tick is not None:
            self.tick()
        return tok

    def dma(self, q, out, in_, R=(), W=(), **kw):
        self.nops += 1
        if self.limit is not None and self.nops > self.limit:
            return None
        self._deps(q, R, W, True)
        owner = W[0] if W else R[0]
        sem = self._dsem(owner)
        owner.dcnt += 16
        tok = ("d_" + owner.name, sem, owner.dcnt)
        self.prog[q].append(("dma", out, in_, kw, sem))
        for b in R:
            b.r[tok[0]] = tok
        for b in W:
            b.w = tok
            b.r = {}
        return tok

    def barrier(self):
        toks = [(e, self.sem[e], self.cnt[e]) for e in self.ENG if self.cnt[e] > 0]
        toks += [("d_" + b.name, b.dsem, b.dcnt) for b in self.dbufs if b.dcnt > 0]
        for e in self.ENG:
            for (key, sem, val) in toks:
                if self.seen[e].get(key, 0) >= val:
                    continue
                self.seen[e][key] = val
                self.prog[e].append(("wait", sem, val))

    def flush(self):
        nc = self.nc
        with nc.Block() as block:
            def replay(e):
                def f(eng):
                    sem_e = self.sem[e]
                    for it in self.prog[e]:
                        if it[0] == "wait":
                            eng.wait_ge(it[1], it[2])
                        elif it[0] == "op":
                            args = it[2]
                            if it[1] == "affine_select":
                                args = list(args)
                                if args[4] not in self.fillregs:
                                    self.fillregs[args[4]] = eng.to_reg(args[4])
                                args[4] = self.fillregs[args[4]]
                            getattr(eng, it[1])(*args, **it[3]).then_inc(sem_e, 1)
                        else:
                            eng.dma_start(out=it[1], in_=it[2], **it[3]).then_inc(it[4], 16)
                return f
            block.tensor(replay("pe"))
            block.scalar(replay("act"))
            block.vector(replay("dve"))
            block.gpsimd(replay("pool"))
            block.sync(replay("sp"))
        self.prog = {e: [] for e in self.ENG}

    def emit(self):
        self.barrier()
        self.flush()


def build_program(nseq=NSEQ, ntiles=NT, do_mlp=True, stages=("p1", "mla", "nsa", "out")):
    nc = bass.Bass("TRN2", target_bir_lowering=False)

    def din(name, shape):
        return nc.dram_tensor(name, list(shape), F32, kind="ExternalInput").ap()

    x_d = din("x", [nseq, SEQ, D])
    w_in_d = din("w_in", [D, IN_COLS])
    g_mix_d = din("g_mix", [128, 8])
    w_uq_d = din("w_uq", [256, 768])
    g_cq_d = din("g_cq", [128, 2])
    wukT_d = din("wukT", [64, 8, 128])
    wuv_d = din("wuv", [128, 8, 64])
    gckv_d = din("gckv_bc", [128, 128])
    w1_d = din("cmp_w1", [2, 64, 32, 128])
    peT_d = din("cmp_peT", [2, 64, 32])
    b1_d = din("cmp_b1", [128, 2])
    w2_d = din("cmp_w2", [128, 2, 64])
    b2k_d = din("cmp_b2k_bc", [128, 128])
    b2v_d = din("cmp_b2v", [64, 1])
    w_o_d = din("w_o", [D, D])
    g_out_d = din("g_out", [128, 8])
    w_up_d = din("w_up", [D, DFF])
    g_mlp_d = din("g_mlp", [128, 8])
    w_down_d = din("w_down", [DFF, D])
    gfin_d = din("gfin_bc", [128, D])
    cosm_d = din("cos_m", [128, NT, 16])
    sinm_d = din("sin_m", [128, NT, 16])
    cosn_d = din("cos_n", [128, NT, 8])
    sinn_d = din("sin_n", [128, NT, 8])
    cosn8_d = din("cos_n8", [128, NT, 8])
    sinn8_d = din("sin_n8", [128, NT, 8])
    cose_d = din("cos_e", [8, NT, 8])
    sine_d = din("sin_e", [8, NT, 8])
    tri_d = din("tri4", [128, 512])
    anti_d = din("anti4", [128, 512])
    ovl_d = din("ovl", [128, 2, 64])
    exp_d = din("expand", [64, SEQ])
    ident_d = din("ident", [128, 128])
    out_d = nc.dram_tensor("out", [nseq * SEQ, D], F32, kind="ExternalOutput").ap()
    hscr_d = nc.dram_tensor("hscr", [nseq * SEQ, D], F32, kind="Internal").ap()

    top = ExitStack()
    with top:
        S = Sched(nc, top)
        def psum(name, shape, dt):
            return top.enter_context(nc.psum_tensor(name, shape, dt))
        pS = [psum("pS%d" % i, [128, 512], F32) for i in range(2)]
        pA = [psum("pA%d" % i, [128, 512], F32) for i in range(2)]
        pM = [psum("pM%d" % i, [128, 512], F32) for i in range(2)]
        pT = [psum("pT%d" % i, [128, 1024], BF16) for i in range(2)]
        bpS = S.bufs("pS", 2)
        bpA = S.bufs("pA", 2)
        bpM = S.bufs("pM", 2)
        bpT = S.bufs("pT", 2)
        for b_ in bpS + bpA + bpM + bpT:
            b_.excl = True
        rr = {"S": 0, "M": 0, "T": 0, "P": 0, "A": 0}
        mode = {"bg": False, "B": False}

        def nxt(kind, n):
            if kind in ("M", "T") and not mode["B"]:
                return 0 if mode["bg"] else 1
            i = rr[kind] % n
            rr[kind] += 1
            return i

        A = ExitStack()
        with A:
            def sb(name, shape, dt):
                return A.enter_context(nc.sbuf_tensor("a_" + name, shape, dt))

            ident = sb("ident", [128, 128], BF16); b_ident = S.buf("ident")
            S.dma("pool", ident[:], ident_d, W=[b_ident])
            tri4 = sb("tri4", [128, 512], BF16); anti4 = sb("anti4", [128, 512], BF16); b_msk = S.buf("msk")
            S.dma("pool", tri4[:], tri_d, W=[b_msk])
            S.dma("pool", anti4[:], anti_d, W=[b_msk])
            tabs = {}
            b_tab = S.buf("tab")
            for nm, d_, w_ in (("cos_m", cosm_d, 16), ("sin_m", sinm_d, 16), ("cos_n", cosn_d, 8), ("sin_n", sinn_d, 8)):
                tabs[nm] = sb(nm, [128, NT, w_], F32)
                S.dma("sp", tabs[nm][:], d_, W=[b_tab])
            cos_e = sb("cos_e", [8, NT, 8], F32); sin_e = sb("sin_e", [8, NT, 8], F32)
            S.dma("sp", cos_e[:], cose_d, W=[b_tab])
            S.dma("sp", sin_e[:], sine_d, W=[b_tab])
            gckv = sb("gckv", [128, 128], F32); b2k = sb("b2k", [128, 128], F32); b2v = sb("b2v", [64, 1], F32)
            b1 = sb("b1", [128, 2], F32)
            S.dma("sp", gckv[:], gckv_d, W=[b_tab])
            S.dma("sp", b2k[:], b2k_d, W=[b_tab])
            S.dma("sp", b2v[:], b2v_d, W=[b_tab])
            S.dma("sp", b1[:], b1_d, W=[b_tab])
            gvec = sb("gvec", [128, 24], F32)
            S.dma("sp", gvec[:, 0:8], g_mix_d, W=[b_tab])
            S.dma("sp", gvec[:, 8:10], g_cq_d, W=[b_tab])
            S.dma("sp", gvec[:, 10:18], g_out_d, W=[b_tab])

            w_in = sb("w_in", [128, 8, IN_COLS], BF16); b_win = S.buf("w_in")
            S.dma("pool", w_in[:], w_in_d.rearrange("(c p) n -> p c n", p=128), W=[b_win])
            for c in range(8):
                S.op("dve", "tensor_scalar", w_in[:, c, :], w_in[:, c, :], gvec[:, c:c + 1], None, ALU.mult,
                     R=[b_tab, b_win], W=[b_win])
                S.op("dve", "tensor_scalar", w_in[:, c, 416:928], w_in[:, c, 416:928], 0.125, None, ALU.mult,
                     R=[b_win], W=[b_win])
            w_o = sb("w_o", [128, 8, D], BF16); b_wo = S.buf("w_o")
            S.dma("pool", w_o[:], w_o_d.rearrange("(c p) n -> p c n", p=128), W=[b_wo])
            for c in range(8):
                S.op("dve", "tensor_scalar", w_o[:, c, :], w_o[:, c, :], gvec[:, 10 + c:11 + c], None, ALU.mult,
                     R=[b_tab, b_wo], W=[b_wo])
            w_uq = sb("w_uq", [128, 2, 768], BF16); b_wuq = S.buf("w_uq")
            S.dma("pool", w_uq[:], w_uq_d.rearrange("(c p) n -> p c n", p=128), W=[b_wuq])
            for c in range(2):
                S.op("dve", "tensor_scalar", w_uq[:, c, :], w_uq[:, c, :], gvec[:, 8 + c:9 + c], 96.0 ** -0.5,
                     ALU.mult, ALU.mult, R=[b_tab, b_wuq], W=[b_wuq])
            wukT = sb("wukT", [64, 8, 128], BF16); wuv = sb("wuv", [128, 8, 64], BF16); b_wkv = S.buf("wkv")
            S.dma("pool", wukT[:], wukT_d, W=[b_wkv])
            S.dma("pool", wuv[:], wuv_d, W=[b_wkv])
            w1 = sb("w1", [64, 2, 32, 128], BF16); b_w1 = S.buf("w1")
            for kv in range(2):
                S.dma("pool", w1[:, kv], w1_d[kv], W=[b_w1])
            peT = sb("peT", [64, 2, 32], BF16)
            for kv in range(2):
                S.dma("pool", peT[:, kv], peT_d[kv], W=[b_w1])
            w2 = sb("w2", [128, 2, 64], BF16)
            S.dma("pool", w2[:], w2_d, W=[b_w1])

            bias_tot = sb("bias_tot", [128, 2], F32); b_bt = S.buf("bias_tot")
            for kv in range(2):
                for l in range(32):
                    S.op("pe", "matmul", pM[0][:, kv:kv + 1], lhsT=w1[:, kv, l, :], rhs=peT[:, kv, l:l + 1],
                         start=(l == 0), stop=(l == 31), R=[b_w1], W=[bpM[0]])
            S.op("dve", "tensor_tensor", bias_tot[:], pM[0][:, 0:2], b1[:], ALU.add, R=[bpM[0], b_tab], W=[b_bt])

            KlatT = sb("KlatT", [128, SEQ], BF16); bKlat = S.bufs("Klat", NT)
            KpeT = sb("KpeT", [128, SEQ], BF16); bKpe = S.bufs("Kpe", NT)
            Clat = sb("Clat", [128, NT, 128], BF16); bClat = S.bufs("Clat", NT)
            KsE = sb("KsE", [128, 2, SEQ], BF16); bKs = S.bufs("Ks", NT); b_exp = S.buf("expand")
            Vs = sb("Vs", [128, NT, 2, 65], BF16); bVs = S.bufs("Vs", NT)
            KwT = sb("KwT", [128, 2, 8 * 128], BF16); bKw = S.bufs("Kw", 8)
            Vw = sb("Vw", [128, 8, 2, 65], BF16); bVw = S.bufs("Vw", 8)
            KcT = sb("KcT", [64, 2, 256], BF16); b_Kc = S.buf("Kc")
            VcT = sb("VcT", [64, 2, 256], BF16); b_VcT = S.buf("VcT")
            VcO = sb("VcO", [128, 2, 2, 128], BF16); b_VcO = S.buf("VcO")
            for g in range(2):
                S.dma("pool", KsE[64:128, g, :], exp_d, W=[b_exp])
            for nt in range(2):
                for g in range(2):
                    S.dma("pool", VcO[:, nt, g, 64:128], ovl_d[:, nt, :], W=[b_VcO])
            S.op("pool", "memset", Vs[:, :, :, 64:65], 1.0, W=bVs)
            S.op("pool", "memset", Vw[:, :, :, 64:65], 1.0, W=bVw)

            xs = [sb("xs%d" % i, [128, D], F32) for i in range(2)]; b_xs = S.bufs("xs", 2)
            st = sb("st", [128, 64], F32); b_st = S.buf("st")
            xn = sb("xn", [128, D], BF16); b_xn = S.buf("xn")
            xnT = sb("xnT", [128, 8, 128], BF16); b_xnT = S.buf("xnT")
            u = sb("u", [128, IN_COLS], F32); b_u = S.buf("u")
            cqn = sb("cqn", [128, 256], BF16); b_cqn = S.buf("cqn")
            cqnT = sb("cqnT", [128, 2, 128], BF16); b_cqnT = S.buf("cqnT")
            q_sb = sb("q_sb", [128, 9, 96], F32); b_q = S.buf("q")
            qn_sb = sb("qn_sb", [128, 8, 64], BF16); b_qn = S.buf("qn")
            qpe_sb = sb("qpe_sb", [128, 9, 32], BF16); b_qpe = S.buf("qpe")
            rt = [sb("rt%d" % i, [128, 160], F32) for i in range(4)]; b_rt = S.buf("rt")
            QnT = sb("QnT", [64, 8, 128], BF16); b_QnT = S.buf("QnT")
            QpeT2 = [sb("QpeT%d" % i, [128, 8, 128], BF16) for i in range(2)]; b_QpeT2 = S.bufs("QpeT", 2)
            QabsT2 = [sb("QabsT%d" % i, [128, 8, 128], BF16) for i in range(2)]; b_Qabs2 = S.bufs("Qabs", 2)
            ub = sb("ub", [128, 20, 64], BF16); b_ub = S.buf("ub")
            QS2 = [sb("QS%d" % i, [128, 2, 4, 128], BF16) for i in range(2)]; b_QSq2 = S.bufs("QSq", 2); b_QSs2 = [S.bufs("QSs", 2) for _ in range(2)]
            rawT = [sb("rawT%d" % i, [64, 2, 2, 144], BF16) for i in range(2)]; b_rawT = S.bufs("rawT", 2)
            gate2 = [sb("gate%d" % i, [128, 3, 8], F32) for i in range(2)]; b_gate2 = S.bufs("gate", 2)
            z_sb = sb("z_sb", [128, 32], F32); z2_sb = sb("z2_sb", [128, 32], F32); b_z = S.buf("z")
            hid_sb = sb("hid_sb", [128, 32], BF16); b_hid = S.buf("hid")
            kc_f = sb("kc_f", [8, 2, 64], F32); kc_sb = sb("kc_sb", [8, 2, 64], BF16); b_kc = S.buf("kc")
            PT = [sb("PT%d" % i, [128, 512], BF16) for i in range(3)]; b_PT = S.bufs("PT", 3)
            PcT = [sb("PcT%d" % i, [128, 512], BF16) for i in range(2)]; b_PcT = S.bufs("PcT", 2)
            OlatT = sb("OlatT", [128, 4, 128], BF16); b_OlatT = S.buf("OlatT")
            y_sb = sb("y_sb", [128, D], F32); b_y = S.buf("y"); b_yn = S.buf("yn")
            imp = sb("imp", [128, 64], F32); sc1 = sb("sc1", [128, 64], F32); sc2 = sb("sc2", [128, 64], F32)
            sc3 = sb("sc3", [128, 64], F32); b_imp = S.buf("imp")
            selq = sb("selq", [128, 128], BF16); b_selq = S.buf("selq")
            tmp4 = sb("tmp4", [128, 4, 64], F32); b_tmp4 = S.buf("tmp4")
            mixed = sb("mixed", [128, D], BF16); b_mixed = S.buf("mixed")
            mixedT = sb("mixedT", [128, 8, 128], BF16); b_mixedT = S.buf("mixedT")
            h_sb = [sb("h_sb%d" % i, [128, D], F32) for i in range(2)]; b_h = S.bufs("h", 2)

            S.op("pool", "memset", selq[:], 0.0, W=[b_selq])
            S.op("pool", "memset", KpeT[:], 0.0, W=bKpe)
            S.op("pool", "memset", KwT[:], 0.0, W=bKw)
            for i in range(2):
                S.op("pool", "memset", QS2[i][:], 0.0, W=[b_QSq2[i]] + b_QSs2[i])
            for i in range(2):
                S.op("pool", "memset", QpeT2[i][:], 0.0, W=[b_QpeT2[i]])
            for i in range(2):
                S.op("pool", "memset", rawT[i][:], 0.0, W=[b_rawT[i]])
            S.op("pool", "memset", KcT[:], 0.0, W=[b_Kc])
            S.op("pool", "memset", VcT[:], 0.0, W=[b_VcT])
            S.op("pool", "memset", VcO[:, :, :, 0:64], 0.0, W=[b_VcO])

            b_stc = {0: S.buf("st0"), 4: S.buf("st4"), 12: S.buf("st12")}
            b_stm = S.buf("stm")
            b_stn = S.buf("stn")
            rl = sb("rl", [128, 8], F32); b_rl = S.buf("rl")
            lacc = sb("lacc", [128, 512], F32); b_lacc = S.buf("lacc")
            ones_f = sb("ones_f", [128, 1], F32); b_ones = S.buf("ones")
            S.op("pool", "memset", ones_f[:], 1.0, W=[b_ones])

            def rstd_multi(items, col, Rb, jk, b_jk):
                b_st = b_stc[col]
                k = len(items)
                S.op("dve", "memset", st[:, col:col + k], 0.0, W=[b_st])
                for i_, (src_ap, n) in enumerate(items):
                    S.op("dve", "scalar_tensor_tensor", jk[:, 0:n], src_ap, 1.0, src_ap, ALU.mult, ALU.mult,
                         accum_out=st[:, col + i_:col + i_ + 1], R=Rb + [b_st], W=[b_jk, b_st])
                    S.op("dve", "tensor_scalar", st[:, col + k + i_:col + k + i_ + 1], st[:, col + i_:col + i_ + 1], 1.0 / n, EPS,
                         ALU.mult, ALU.add, R=[b_st], W=[b_st])
                v_ = st[:, col + k:col + 2 * k]
                y_ = st[:, col + 2 * k:col + 3 * k]
                w_ = st[:, col + 3 * k:col + 4 * k]
                if not USE_MAGIC:
                    S.op("act", "activation", w_, v_, AF.Sqrt, R=[b_st], W=[b_st])
                    S.op("dve", "reciprocal", y_, w_, R=[b_st], W=[b_st])
                    return [st[:, col + 2 * k + i_:col + 2 * k + i_ + 1] for i_ in range(k)]
                S.op("dve", "tensor_scalar", y_.bitcast(I32), v_.bitcast(I32), -0.5, 1597463007.0, ALU.mult, ALU.add, R=[b_st], W=[b_st])
                for _it in range(2):
                    S.op("pool", "tensor_tensor", w_, y_, y_, ALU.mult, R=[b_st], W=[b_st])
                    S.op("pool", "tensor_tensor", w_, w_, v_, ALU.mult, R=[b_st], W=[b_st])
                    S.op("pool", "tensor_scalar", w_, w_, -0.5, 1.5, ALU.mult, ALU.add, R=[b_st], W=[b_st])
                    S.op("pool", "tensor_tensor", y_, y_, w_, ALU.mult, R=[b_st], W=[b_st])
                return [st[:, col + 2 * k + i_:col + 2 * k + i_ + 1] for i_ in range(k)]

            def rope(eng, out_ap, in_ap, cos_ap, sin_ap, nh, half, Rb, Wb):
                P = in_ap.shape[0]
                x1 = in_ap[:, :, 0:half]
                x2 = in_ap[:, :, half:2 * half]
                cb = cos_ap[:, None, :].to_broadcast([P, nh, half])
                sbb = sin_ap[:, None, :].to_broadcast([P, nh, half])
                t = [r_[0:P, 0:nh * half].rearrange("p (h d) -> p h d", h=nh) for r_ in rt]
                S.op(eng, "tensor_tensor", t[0], x1, cb, ALU.mult, R=Rb + [b_tab], W=[b_rt])
                S.op(eng, "tensor_tensor", t[1], x2, sbb, ALU.mult, R=Rb + [b_tab], W=[b_rt])
                S.op(eng, "tensor_tensor", t[2], x2, cb, ALU.mult, R=Rb + [b_tab], W=[b_rt])
                S.op(eng, "tensor_tensor", t[3], x1, sbb, ALU.mult, R=Rb + [b_tab], W=[b_rt])
                S.op(eng, "tensor_tensor", out_ap[:, :, 0:half], t[0], t[1], ALU.subtract, R=[b_rt], W=Wb)
                S.op(eng, "tensor_tensor", out_ap[:, :, half:2 * half], t[2], t[3], ALU.add, R=[b_rt], W=Wb)

            def transpose_to(ps_i, col0, in_ap, Rb):
                P, Fd = in_ap.shape[0], in_ap.shape[1]
                S.op("pe", "transpose", pT[ps_i][0:Fd, col0:col0 + P], in_ap, ident[0:P, 0:P],
                     R=Rb + [b_ident], W=[bpT[ps_i]])

            def phase1(s, t):
                par = t % 2
                QpeT, b_QpeT = QpeT2[par], b_QpeT2[par]
                QabsT, b_Qabs = QabsT2[par], b_Qabs2[par]
                QS, b_QSq, b_QSs = QS2[par], b_QSq2[par], b_QSs2[par]
                gate, b_gate = gate2[par], b_gate2[par]
                xb = t % 2
                ce, cm = ("act", "copy") if t < ACT_COPY_T else ("dve", "tensor_copy")
                S.dma("sp", xs[xb][:], x_d[s, t * 128:(t + 1) * 128, :], W=[b_xs[xb]])
                r0 = rstd_multi([(xs[xb][:], D)], 0, [b_xs[xb]], xn, b_xn)[0]
                S.op("dve", "tensor_scalar", xn[:], xs[xb][:], r0, None, ALU.mult, R=[b_xs[xb], b_stc[0]], W=[b_xn])
                ti = nxt("T", 2)
                for c in range(8):
                    transpose_to(ti, c * 128, xn[:, c * 128:(c + 1) * 128], [b_xn])
                S.op(ce, cm, xnT[:], pT[ti][:, 0:1024].rearrange("p (c n) -> p c n", c=8), R=[bpT[ti]], W=[b_xnT])
                for cg, (c0, c1) in enumerate(((0, 512), (512, 1024), (1024, 1536), (1536, IN_COLS))):
                    mi = nxt("M", 2)
                    for c in range(8):
                        S.op("pe", "matmul", pM[mi][:, 0:c1 - c0], lhsT=xnT[:, c, :], rhs=w_in[:, c, c0:c1],
                             start=(c == 0), stop=(c == 7), R=[b_xnT, b_win], W=[bpM[mi]])
                    S.op(ce, cm,
                         u[:, c0:c1], pM[mi][:, 0:c1 - c0], R=[bpM[mi]], W=[b_u])
                    yield

                yield
                r1, r2 = rstd_multi([(u[:, 0:256], 256), (u[:, 256:384], 128)], 4, [b_u], cqn, b_cqn)
                S.op("dve", "tensor_scalar", cqn[:], u[:, 0:256], r1, None, ALU.mult, R=[b_u, b_stc[4]], W=[b_cqn])
                ti = nxt("T", 2)
                for c in range(2):
                    transpose_to(ti, c * 128, cqn[:, c * 128:(c + 1) * 128], [b_cqn])
                S.op("dve", "tensor_copy", cqnT[:], pT[ti][:, 0:256].rearrange("p (c n) -> p c n", c=2), R=[bpT[ti]], W=[b_cqnT])
                yield
                for half in range(2):
                    mi = nxt("M", 2)
                    for c in range(2):
                        S.op("pe", "matmul", pM[mi][:, 0:384], lhsT=cqnT[:, c, :], rhs=w_uq[:, c, half * 384:(half + 1) * 384],
                             start=(c == 0), stop=(c == 1), R=[b_cqnT, b_wuq], W=[bpM[mi]])
                    S.op(ce, cm, q_sb[:, half * 4:(half + 1) * 4, :],
                         pM[mi][:, 0:384].rearrange("p (h d) -> p h d", h=4), R=[bpM[mi]], W=[b_q])
                yield
                S.op("dve", "tensor_copy", q_sb[:, 8, 64:96], u[:, 384:416], R=[b_u], W=[b_q])
                S.op("dve", "tensor_copy", qn_sb[:], q_sb[:, 0:8, 0:64], R=[b_q], W=[b_qn])
                rope("dve", qpe_sb[:], q_sb[:, :, 64:96], tabs["cos_m"][:, t, :], tabs["sin_m"][:, t, :], 9, 16, [b_q], [b_qpe])
                ti = nxt("T", 2)
                for h in range(8):
                    transpose_to(ti, h * 128, qn_sb[:, h, :], [b_qn])
                S.op(ce, cm, QnT[:], pT[ti][0:64, 0:1024].rearrange("p (c n) -> p c n", c=8), R=[bpT[ti]], W=[b_QnT])
                ti = nxt("T", 2)
                for h in range(8):
                    transpose_to(ti, h * 128, qpe_sb[:, h, :], [b_qpe])
                S.op(ce, cm, QpeT[0:32], pT[ti][0:32, 0:1024].rearrange("p (c n) -> p c n", c=8), R=[bpT[ti]], W=[b_QpeT])
                yield
                for hg in range(2):
                    mi = nxt("M", 2)
                    for j in range(4):
                        h = hg * 4 + j
                        S.op("pe", "matmul", pM[mi][:, j * 128:(j + 1) * 128], lhsT=wukT[:, h, :],
                             rhs=QnT[:, h, :], start=True, stop=True, R=[b_wkv, b_QnT], W=[bpM[mi]])
                    S.op(ce, cm, QabsT[:, hg * 4:(hg + 1) * 4, :],
                         pM[mi][:, 0:512].rearrange("p (c n) -> p c n", c=4), R=[bpM[mi]], W=[b_Qabs])

                yield
                S.op("dve", "scalar_tensor_tensor", Clat[:, t, 0:128], u[:, 256:384], r2, gckv[:], ALU.mult, ALU.mult,
                     R=[b_u, b_stc[4], b_tab], W=[bClat[t]])
                ti = nxt("T", 2)
                transpose_to(ti, 0, Clat[:, t, 0:128], [bClat[t]])
                S.op("dve", "tensor_copy", KlatT[:, t * 128:(t + 1) * 128], pT[ti][:, 0:128], R=[bpT[ti]], W=[bKlat[t]])
                ti = nxt("T", 2)
                transpose_to(ti, 0, qpe_sb[:, 8, :], [b_qpe])
                S.op("dve", "tensor_copy", KpeT[0:32, t * 128:(t + 1) * 128], pT[ti][0:32, 0:128], R=[bpT[ti]], W=[bKpe[t]])

                yield
                uv = u[:, 416:1696].rearrange("p (b d) -> p b d", b=20)
                S.op("dve", "tensor_copy", ub[:, :, 16:64], uv[:, :, 16:64], R=[b_u], W=[b_ub])
                rope("dve", ub[:, :, 0:16], uv[:, :, 0:16], tabs["cos_n"][:, t, :], tabs["sin_n"][:, t, :], 20, 8, [b_u], [b_ub])
                S.op("dve", "tensor_copy", ub[:, 8:12, 0:16], uv[:, 8:12, 0:16], R=[b_u, b_ub], W=[b_ub])
                ti = nxt("T", 2)
                for h in range(8):
                    transpose_to(ti, h * 128, ub[:, h, :], [b_ub])
                S.op(ce, cm, QS[0:64].rearrange("p g j n -> p (g j) n"),
                     pT[ti][0:64, 0:1024].rearrange("p (c n) -> p c n", c=8), R=[bpT[ti]], W=[b_QSq])
                yield
                kvv = u[:, 928:1696].rearrange("p (s g d) -> p s g d", s=6, g=2)
                S.op("dve", "tensor_copy", Vs[:, t, :, 0:64], kvv[:, 3], R=[b_u], W=[bVs[t]])
                S.op("dve", "tensor_copy", Vw[:, t % 8, :, 0:64], kvv[:, 5], R=[b_u], W=[bVw[t % 8]])
                ti = nxt("T", 2)
                for g in range(2):
                    transpose_to(ti, g * 128, ub[:, 12 + g, :], [b_ub])
                    transpose_to(ti, 256 + g * 128, ub[:, 16 + g, :], [b_ub])
                S.op(ce, cm, KsE[0:64, :, t * 128:(t + 1) * 128],
                     pT[ti][0:64, 0:256].rearrange("p (g n) -> p g n", g=2), R=[bpT[ti]], W=[bKs[t]])
                S.op("dve", "tensor_copy", KwT[0:64, :, (t % 8) * 128:(t % 8 + 1) * 128],
                     pT[ti][0:64, 256:512].rearrange("p (g n) -> p g n", g=2), R=[bpT[ti]], W=[bKw[t % 8]])
                yield
                rb = t % 2
                if t == 0:
                    S.op("pool", "memset", rawT[rb][:, :, :, 0:16], 0.0, W=[b_rawT[rb]])
                else:
                    S.op("dve", "tensor_copy", rawT[rb][:, :, :, 0:16], rawT[1 - rb][:, :, :, 128:144],
                         R=[b_rawT[1 - rb]], W=[b_rawT[rb]])
                ti = nxt("T", 2)
                for c in range(4):
                    transpose_to(ti, c * 128, ub[:, 8 + c, :], [b_ub])
                S.op(ce, cm, rawT[rb][:, :, :, 16:144],
                     pT[ti][0:64, 0:512].rearrange("p (k g n) -> p k g n", k=2, g=2), R=[bpT[ti]], W=[b_rawT[rb]])
                yield
                S.op("act", "activation", gate[:].rearrange("p b h -> p (b h)"), u[:, 1696:1720], AF.Tanh, scale=0.5, R=[b_u], W=[b_gate])
                S.op("dve", "tensor_scalar", gate[:].rearrange("p b h -> p (b h)"), gate[:].rearrange("p b h -> p (b h)"), 0.5, 0.5,
                     ALU.mult, ALU.add, R=[b_gate], W=[b_gate])

                yield
                mi = nxt("M", 2)
                for kv in range(2):
                    for g in range(2):
                        c0 = (kv * 2 + g) * 8
                        for l in range(32):
                            S.op("pe", "matmul", pM[mi][:, c0:c0 + 8], lhsT=w1[:, kv, l, :], rhs=rawT[rb][:, kv, g, l:l + 113:16],
                                 start=(l == 0), stop=(l == 31), R=[b_w1, b_rawT[rb]], W=[bpM[mi]])
                            if l % 8 == 7:
                                yield
                for kv in range(2):
                    S.op("dve", "tensor_scalar", z_sb[:, kv * 16:(kv + 1) * 16], pM[mi][:, kv * 16:(kv + 1) * 16],
                         bias_tot[:, kv:kv + 1], None, ALU.add, R=[bpM[mi], b_bt], W=[b_z])
                S.op("dve", "tensor_tensor", z2_sb[:], z_sb[:], z_sb[:], ALU.mult, R=[b_z], W=[b_z])
                S.op("dve", "tensor_scalar", z2_sb[:], z2_sb[:], 0.044715, 1.0, ALU.mult, ALU.add, R=[b_z], W=[b_z])
                S.op("dve", "tensor_tensor", z2_sb[:], z2_sb[:], z_sb[:], ALU.mult, R=[b_z], W=[b_z])
                S.op("act", "activation", z2_sb[:], z2_sb[:], AF.Tanh, scale=math.sqrt(2.0 / math.pi), R=[b_z], W=[b_z])
                S.op("dve", "tensor_scalar", z2_sb[:], z2_sb[:], 0.5, 0.5, ALU.mult, ALU.add, R=[b_z], W=[b_z])
                S.op("dve", "tensor_tensor", hid_sb[:], z_sb[:], z2_sb[:], ALU.mult, R=[b_z], W=[b_hid])
                yield
                n0 = 8 * t - 1
                m0 = 1 if t == 0 else 0
                mi = nxt("M", 2)
                for g in range(2):
                    S.op("pe", "matmul", pM[mi][0:8, g * 64:(g + 1) * 64], lhsT=hid_sb[:, g * 8:(g + 1) * 8], rhs=w2[:, 0, :],
                         start=True, stop=True, R=[b_hid, b_w1], W=[bpM[mi]])
                for g in range(2):
                    S.op("pe", "matmul", pM[mi][0:64, 128 + g * 8:136 + g * 8], lhsT=w2[:, 1, :], rhs=hid_sb[:, 16 + g * 8:24 + g * 8],
                         start=True, stop=True, R=[b_hid, b_w1], W=[bpM[mi]])
                S.op("dve", "tensor_tensor", kc_f[:].rearrange("p g d -> p (g d)"), pM[mi][0:8, 0:128], b2k[0:8, :], ALU.add,
                     R=[bpM[mi], b_tab], W=[b_kc])
                S.op("dve", "tensor_scalar", VcT[:, :, n0 + m0:n0 + 8], pM[mi][0:64, 128:144].rearrange("p (g m) -> p g m", g=2)[:, :, m0:8],
                     b2v[:, 0:1], None, ALU.add, R=[bpM[mi], b_tab], W=[b_VcT])
                S.op("dve", "tensor_copy", kc_sb[:, :, 16:64], kc_f[:, :, 16:64], R=[b_kc], W=[b_kc])
                rope("dve", kc_sb[:, :, 0:16], kc_f[:, :, 0:16], cos_e[:, t, :], sin_e[:, t, :], 2, 8, [b_kc], [b_kc])
                ti = nxt("T", 2)
                for g in range(2):
                    transpose_to(ti, g * 8, kc_sb[:, g, :], [b_kc])
                S.op("dve", "tensor_copy", KcT[:, :, n0 + m0:n0 + 8],
                     pT[ti][0:64, 0:16].rearrange("p (g m) -> p g m", g=2)[:, :, m0:8], R=[bpT[ti]], W=[b_Kc])
                yield
                nts = sorted(set([max(n0, 0) // 128, (n0 + 7) // 128]))
                for nt in nts:
                    ti = nxt("T", 2)
                    for g in range(2):
                        S.op("pe", "transpose", pT[ti][:, g * 64:(g + 1) * 64], VcT[:, g, nt * 128:(nt + 1) * 128], ident[0:64, 0:64],
                             R=[b_VcT, b_ident], W=[bpT[ti]])
                    S.op("dve", "tensor_copy", VcO[:, nt, :, 0:64], pT[ti][:, 0:128].rearrange("p (g d) -> p g d", g=2), R=[bpT[ti]], W=[b_VcO])

            def attn_loop(kts, qk_fn, post_fn):
                if not kts:
                    return
                si_next = qk_fn(kts[0])
                for i_, kt in enumerate(kts):
                    si = si_next
                    if i_ + 1 < len(kts):
                        si_next = qk_fn(kts[i_ + 1])
                    post_fn(kt, si)

            def mla(s, t):
                par = t % 2
                QpeT, b_QpeT = QpeT2[par], b_QpeT2[par]
                QabsT, b_Qabs = QabsT2[par], b_Qabs2[par]
                QS, b_QSq, b_QSs = QS2[par], b_QSq2[par], b_QSs2[par]
                gate, b_gate = gate2[par], b_gate2[par]
                mo = nxt("M", 2)
                for hg in range(2):
                    qa = QabsT[:, hg * 4:(hg + 1) * 4, :].rearrange("p c n -> p (c n)")
                    qp = QpeT[:, hg * 4:(hg + 1) * 4, :].rearrange("p c n -> p (c n)")

                    def qk(kt):
                        si = nxt("S", 2)
                        S.op("pe", "matmul", pS[si][:], lhsT=KlatT[:, kt * 128:(kt + 1) * 128], rhs=qa,
                             start=True, stop=False, R=[bKlat[kt], b_Qabs], W=[bpS[si]])
                        S.op("pe", "matmul", pS[si][:], lhsT=KpeT[:, kt * 128:(kt + 1) * 128], rhs=qp,
                             start=False, stop=True, R=[bKpe[kt], b_QpeT], W=[bpS[si]])
                        return si

                    def post(kt, si):
                        pi = nxt("P", 3)
                        S.op("act", "activation", PT[pi][:], pS[si][:], AF.Exp, R=[bpS[si]], W=[b_PT[pi]])
                        if kt == t:
                            S.op("dve", "tensor_tensor", PT[pi][:], PT[pi][:], tri4[:], ALU.mult, R=[b_PT[pi], b_msk], W=[b_PT[pi]])
                        S.op("pe", "matmul", pA[0][:], lhsT=Clat[:, kt, 0:128], rhs=PT[pi][:], start=(kt == 0), stop=(kt == t),
                             R=[b_PT[pi], bClat[kt]], W=[bpA[0]])
                        if kt == 0:
                            S.op("dve", "tensor_copy", lacc[:], PT[pi][:], R=[b_PT[pi]], W=[b_lacc])
                        else:
                            S.op("dve", "tensor_tensor", lacc[:], lacc[:], PT[pi][:], ALU.add, R=[b_PT[pi], b_lacc], W=[b_lacc])

                    attn_loop(list(range(t + 1)), qk, post)
                    for j in range(4):
                        S.op("pe", "matmul", pA[1][:, j:j + 1], lhsT=lacc[:, j * 128:(j + 1) * 128], rhs=ones_f[:, 0:1],
                             start=True, stop=True, R=[b_lacc, b_ones], W=[bpA[1]])
                    S.op("dve", "tensor_copy", OlatT[:], pA[0][:].rearrange("p (c n) -> p c n", c=4), R=[bpA[0]], W=[b_OlatT])
                    S.op("dve", "reciprocal", rl[:, hg * 4:(hg + 1) * 4], pA[1][:, 0:4], R=[bpA[1]], W=[b_rl])
                    for j in range(4):
                        h = hg * 4 + j
                        S.op("pe", "matmul", pM[mo][:, h * 64:(h + 1) * 64], lhsT=OlatT[:, j, :], rhs=wuv[:, h, :],
                             start=True, stop=True, R=[b_OlatT, b_wkv], W=[bpM[mo]])
                S.op("dve", "tensor_tensor", y_sb[:, 0:512].rearrange("p (h d) -> p h d", h=8),
                     pM[mo][:, 0:512].rearrange("p (h d) -> p h d", h=8), rl[:, 0:8, None].to_broadcast([128, 8, 64]), ALU.mult,
                     R=[bpM[mo], b_rl], W=[b_y])

            def nsa_sel(s, t, g):
                par = t % 2
                QpeT, b_QpeT = QpeT2[par], b_QpeT2[par]
                QabsT, b_Qabs = QabsT2[par], b_Qabs2[par]
                QS, b_QSq, b_QSs = QS2[par], b_QSq2[par], b_QSs2[par]
                gate, b_gate = gate2[par], b_gate2[par]
                QSg = QS[:, g].rearrange("p j n -> p (j n)")
                QSg = QS[:, g].rearrange("p j n -> p (j n)")
                nts = [0] + ([1] if t >= 16 else [])
                for nt in nts:
                    si = nxt("M", 2)
                    S.op("pe", "matmul", pM[si][:], lhsT=KcT[:, g, nt * 128:(nt + 1) * 128], rhs=QSg[0:64, :],
                         start=True, stop=True, R=[b_Kc, b_QSq], W=[bpM[si]])
                    S.op("act", "activation", PcT[nt][:], pM[si][:], AF.Exp, R=[bpM[si]], W=[b_PcT[nt]])
                    S.op("pool", "affine_select", PcT[nt][:].rearrange("p (j n) -> p j n", j=4),
                         PcT[nt][:].rearrange("p (j n) -> p j n", j=4), [[0, 4], [1, 128]], ALU.is_ge, 0.0,
                         base=128 * t - 31 - 2048 * nt, channel_multiplier=-16, R=[b_PcT[nt]], W=[b_PcT[nt]])
                yield
                mc = nxt("M", 2)
                for j in range(4):
                    for i_, nt in enumerate(nts):
                        S.op("pe", "matmul", pM[mc][:, j * 128:(j + 1) * 128], lhsT=PcT[nt][:, j * 128:(j + 1) * 128],
                             rhs=VcO[:, nt, g, :], start=(i_ == 0), stop=(i_ == len(nts) - 1),
                             R=[b_PcT[nt], b_VcO], W=[bpM[mc]])
                yield
                pc = pM[mc][:].rearrange("p (j c) -> p j c", j=4)
                S.op("dve", "tensor_reduce", st[:, 24:28], pc[:, :, 64:128], AX.X, ALU.add, R=[bpM[mc]], W=[b_stn])
                S.op("dve", "tensor_scalar", st[:, 24:28], st[:, 24:28], 1e-30, None, ALU.max, R=[b_stn], W=[b_stn])
                S.op("dve", "reciprocal", st[:, 28:32], st[:, 24:28], R=[b_stn], W=[b_stn])
                S.op("dve", "tensor_tensor", tmp4[:], pc[:, :, 64:128], st[:, 28:32, None].to_broadcast([128, 4, 64]), ALU.mult,
                     R=[bpM[mc], b_stn], W=[b_tmp4])
                S.op("dve", "tensor_reduce", imp[:], tmp4[:].rearrange("p j c -> p c j"), AX.X, ALU.add, R=[b_tmp4], W=[b_imp])
                yield
                S.op("dve", "tensor_tensor", st[:, 32:36], st[:, 28:32], gate[:, 0, g * 4:(g + 1) * 4], ALU.mult,
                     R=[b_stn, b_gate], W=[b_stn])
                S.op("dve", "tensor_tensor", y_sb[:, 512 + g * 256:768 + g * 256].rearrange("p (j c) -> p j c", j=4), pc[:, :, 0:64],
                     st[:, 32:36, None].to_broadcast([128, 4, 64]), ALU.mult, R=[bpM[mc], b_stn], W=[b_yn])
                yield
                S.op("pool", "affine_select", sc1[:], imp[:], [[-64, 64]], ALU.is_ge, 1e9, base=128 * t - 128, channel_multiplier=1,
                     R=[b_imp], W=[b_imp])
                S.op("pool", "affine_select", sc2[:], sc1[:], [[-64, 64]], ALU.is_ge, -1e9, base=128 * t, channel_multiplier=1,
                     R=[b_imp], W=[b_imp])
                S.op("pool", "memset", sc2[:, 0:1], 1e9, R=[b_imp], W=[b_imp])
                yield
                S.op("dve", "max", st[:, 40:48], sc2[:], R=[b_imp], W=[b_stn])
                S.op("dve", "match_replace", sc3[:], st[:, 40:48], sc2[:], -3e38, R=[b_imp, b_stn], W=[b_imp])
                S.op("dve", "max", st[:, 48:56], sc3[:], R=[b_imp], W=[b_stn])
                S.op("dve", "tensor_scalar", selq[:, 64:128], sc2[:], st[:, 55:56], NEG, ALU.is_lt, ALU.mult,
                     R=[b_imp, b_stn], W=[b_selq])
                yield
                ti = nxt("T", 2)
                transpose_to(ti, 0, selq[:], [b_selq])
                S.op("dve", "tensor_copy", QS[64:128, g], pT[ti][64:128, None, 0:128].to_broadcast([64, 4, 128]),
                     R=[bpT[ti]], W=[b_QSs[g]])
                yield

            def nsa_attn(s, t, g):
                par = t % 2
                QpeT, b_QpeT = QpeT2[par], b_QpeT2[par]
                QabsT, b_Qabs = QabsT2[par], b_Qabs2[par]
                QS, b_QSq, b_QSs = QS2[par], b_QSq2[par], b_QSs2[par]
                gate, b_gate = gate2[par], b_gate2[par]
                QSg = QS[:, g].rearrange("p j n -> p (j n)")
                S.op("dve", "memset", pA[0][:, 0:260], 0.0, W=[bpA[0]])
                S.op("dve", "memset", pA[1][:, 0:260], 0.0, W=[bpA[1]])
                def qk_s(kt):
                    si = nxt("S", 2)
                    S.op("pe", "matmul", pS[si][:], lhsT=KsE[:, g, kt * 128:(kt + 1) * 128], rhs=QSg,
                         start=True, stop=True, R=[bKs[kt], b_exp, b_QSq, b_QSs[g]], W=[bpS[si]])
                    return si

                def post_s(kt, si):
                    pi = nxt("P", 3)
                    S.op("act", "activation", PT[pi][:], pS[si][:], AF.Exp, R=[bpS[si]], W=[b_PT[pi]])
                    if kt == t:
                        S.op("dve", "tensor_tensor", PT[pi][:], PT[pi][:], tri4[:], ALU.mult, R=[b_PT[pi], b_msk], W=[b_PT[pi]])
                    for j in range(4):
                        S.op("pe", "matmul", pA[0][:, j * 65:j * 65 + 65], lhsT=PT[pi][:, j * 128:(j + 1) * 128],
                             rhs=Vs[:, kt, g, :], start=False, stop=(kt == t), skip_group_check=True,
                             R=[b_PT[pi], bVs[kt]], W=[bpA[0]])

                def qk_w(kt):
                    si = nxt("S", 2)
                    sl = kt % 8
                    S.op("pe", "matmul", pS[si][:], lhsT=KwT[:, g, sl * 128:(sl + 1) * 128], rhs=QSg,
                         start=True, stop=True, R=[bKw[sl], b_QSq, b_QSs[g]], W=[bpS[si]])
                    return si

                def post_w(kt, si):
                    sl = kt % 8
                    pi = nxt("P", 3)
                    S.op("act", "activation", PT[pi][:], pS[si][:], AF.Exp, R=[bpS[si]], W=[b_PT[pi]])
                    if kt == t:
                        S.op("dve", "tensor_tensor", PT[pi][:], PT[pi][:], tri4[:], ALU.mult, R=[b_PT[pi], b_msk], W=[b_PT[pi]])
                    if kt == t - 4:
                        S.op("dve", "tensor_tensor", PT[pi][:], PT[pi][:], anti4[:], ALU.mult, R=[b_PT[pi], b_msk], W=[b_PT[pi]])
                    for j in range(4):
                        S.op("pe", "matmul", pA[1][:, j * 65:j * 65 + 65], lhsT=PT[pi][:, j * 128:(j + 1) * 128],
                             rhs=Vw[:, sl, g, :], start=False, stop=(kt == t), skip_group_check=True,
                             R=[b_PT[pi], bVw[sl]], W=[bpA[1]])

                attn_loop(list(range(t + 1)), qk_s, post_s)
                attn_loop(list(range(max(0, t - 4), t + 1)), qk_w, post_w)
                for br in range(2):
                    pa = pA[br][:, 0:260].rearrange("p (j c) -> p j c", j=4)
                    S.op("dve", "reciprocal", st[:, 56:60], pa[:, :, 64], R=[bpA[br]], W=[b_stn])
                    S.op("dve", "tensor_tensor", st[:, 60:64], st[:, 56:60], gate[:, 1 + br, g * 4:(g + 1) * 4], ALU.mult,
                         R=[b_stn, b_gate], W=[b_stn])
                    yv = y_sb[:, 512 + g * 256:768 + g * 256].rearrange("p (j c) -> p j c", j=4)
                    S.op("dve", "tensor_tensor", tmp4[:], pa[:, :, 0:64], st[:, 60:64, None].to_broadcast([128, 4, 64]), ALU.mult,
                         R=[bpA[br], b_stn], W=[b_tmp4])
                    S.op("dve", "tensor_tensor", yv, yv, tmp4[:], ALU.add, R=[b_tmp4, b_yn], W=[b_yn])


            def outproj(s, t):
                xb = t % 2
                ra, rb_ = rstd_multi([(y_sb[:, 0:512], 512), (y_sb[:, 512:1024], 512)], 12, [b_y, b_yn], mixed, b_mixed)
                S.op("dve", "tensor_scalar", mixed[:, 0:512], y_sb[:, 0:512], ra, None, ALU.mult, R=[b_y, b_stc[12]], W=[b_mixed])
                S.op("dve", "tensor_scalar", mixed[:, 512:1024], y_sb[:, 512:1024], rb_, None, ALU.mult, R=[b_yn, b_stc[12]], W=[b_mixed])
                ti = nxt("T", 2)
                for c in range(8):
                    transpose_to(ti, c * 128, mixed[:, c * 128:(c + 1) * 128], [b_mixed])
                S.op("dve", "tensor_copy", mixedT[:], pT[ti][:, 0:1024].rearrange("p (c n) -> p c n", c=8), R=[bpT[ti]], W=[b_mixedT])
                for dh in range(2):
                    mi = nxt("S", 2)
                    for c in range(8):
                        S.op("pe", "matmul", pS[mi][:], lhsT=mixedT[:, c, :], rhs=w_o[:, c, dh * 512:(dh + 1) * 512],
                             start=(c == 0), stop=(c == 7), R=[b_mixedT, b_wo], W=[bpS[mi]])
                    S.op("dve", "tensor_tensor", h_sb[xb][:, dh * 512:(dh + 1) * 512], pS[mi][:], xs[xb][:, dh * 512:(dh + 1) * 512],
                         ALU.add, R=[bpS[mi], b_xs[xb]], W=[b_h[xb]])
                row = (s * SEQ + t * 128)
                S.dma("sp", hscr_d[row:row + 128, :], h_sb[xb][:], R=[b_h[xb]])

            bgst = {"gen": None, "credit": 0.0, "rate": 0.0}

            def bg_run(n=None):
                if bgst["gen"] is None:
                    return
                mode["bg"] = True
                try:
                    k = 0
                    while n is None or k < n:
                        next(bgst["gen"])
                        k += 1
                except StopIteration:
                    bgst["gen"] = None
                mode["bg"] = False

            def tick():
                if mode["bg"] or bgst["gen"] is None:
                    return
                bgst["credit"] += bgst["rate"]
                if bgst["credit"] >= 1.0:
                    n = int(bgst["credit"])
                    bgst["credit"] -= n
                    bg_run(n)

            S.tick = tick
            def chain(*gens):
                for g_ in gens:
                    yield from g_

            def set_bg(gen, nchunks, fg_ops):
                bgst["gen"] = gen
                bgst["credit"] = 0.0
                bgst["rate"] = nchunks / (0.7 * fg_ops)

            for s in range(nseq):
                bgst["gen"] = phase1(s, 0) if "p1" in stages else None
                bg_run(None)
                for t in range(ntiles):
                    if "nsa" in stages:
                        set_bg(chain(nsa_sel(s, t, 0), nsa_sel(s, t, 1)), 16.0, 30.0 + 18.0 * (t + 1))
                    if "mla" in stages:
                        mla(s, t)
                    bg_run(None)
                    if t + 1 < ntiles and "p1" in stages:
                        set_bg(phase1(s, t + 1), 40.0, 100.0 + 14.0 * (t + 1 + min(t + 1, 5)))
                    if "nsa" in stages:
                        nsa_attn(s, t, 0)
                        nsa_attn(s, t, 1)
                    if "out" in stages:
                        outproj(s, t)
                    bg_run(None)
            S.tick = None
            S.barrier()
        mode["B"] = True
        B = ExitStack()
        with B:
            def sb2(name, shape, dt):
                return B.enter_context(nc.sbuf_tensor("b_" + name, shape, dt))
            gb = sb2("gb", [128, 8], F32); b_gb = S.buf("gb")
            S.dma("sp", gb[:], g_mlp_d, W=[b_gb])
            gfin = sb2("gfin", [128, D], F32)
            S.dma("sp", gfin[:], gfin_d, W=[b_gb])
            ident2 = sb2("ident2", [128, 128], BF16); b_id2 = S.buf("ident2")
            S.dma("pool", ident2[:], ident_d, W=[b_id2])
            w_up = sb2("w_up", [128, 8, DFF], BF16); b_wup = S.buf("w_up")
            for c in range(8):
                S.dma("pool", w_up[:, c, :], w_up_d[c * 128:(c + 1) * 128, :], W=[b_wup])
                S.op("dve", "tensor_scalar", w_up[:, c, :], w_up[:, c, :], gb[:, c:c + 1], None, ALU.mult, R=[b_gb, b_wup], W=[b_wup])
            w_dn = sb2("w_dn", [128, 32, D], BF16); b_wdn = S.buf("w_dn")
            wdv = w_down_d.rearrange("(f p) n -> p f n", p=128)
            for f4 in range(8):
                S.dma("pool", w_dn[:, f4 * 4:(f4 + 1) * 4, :], wdv[:, f4 * 4:(f4 + 1) * 4, :], W=[b_wdn])
            hin = [sb2("hin%d" % i, [128, D], F32) for i in range(4)]; b_hin = S.bufs("hin", 4)
            st2 = sb2("st2", [128, 16], F32); b_st2 = S.buf("st2")
            junk2 = sb2("junk2", [128, D], BF16); b_junk2 = S.buf("junk2")
            hn = sb2("hn", [128, D], BF16); b_hn = S.buf("hn")
            hnT = sb2("hnT", [128, 8, 512], BF16); b_hnT = S.buf("hnT")
            rl = [sb2("rl%d" % i, [128, 512], BF16) for i in range(2)]; b_rl = S.bufs("rl", 2)
            aT = sb2("aT", [128, 32, 512], BF16); b_aT = S.buf("aT")
            yo = [sb2("yo%d" % i, [128, D], F32) for i in range(2)]; b_yo = S.bufs("yo", 2)
            S.op("pool", "memset", st2[:], 0.0, W=[b_st2])
            nT = (nseq * ntiles * 128) // 512 if do_mlp else 0

            def rstd2(src_ap, Rb):
                S.op("pool", "memset", st2[:, 0:1], 0.0, W=[b_st2])
                S.op("act", "activation", junk2[:], src_ap, AF.Square, accum_out=st2[:, 0:1], R=Rb, W=[b_junk2, b_st2])
                S.op("dve", "tensor_scalar", st2[:, 1:2], st2[:, 0:1], 1.0 / D, EPS, ALU.mult, ALU.add, R=[b_st2], W=[b_st2])
                S.op("act", "activation", st2[:, 2:3], st2[:, 1:2], AF.Sqrt, R=[b_st2], W=[b_st2])
                S.op("dve", "reciprocal", st2[:, 3:4], st2[:, 2:3], R=[b_st2], W=[b_st2])
                return st2[:, 3:4]

            oc = 0
            for T in range(nT):
                hb = T % 2
                row = T * 512 if nseq * ntiles * 128 == nseq * SEQ else None
                base = (T * 512 // (ntiles * 128)) * SEQ + (T * 512) % (ntiles * 128)
                for i in range(4):
                    S.dma("sp", hin[i][:], hscr_d[base + i * 128:base + (i + 1) * 128, :], W=[b_hin[i]])
                for i in range(4):
                    r = rstd2(hin[i][:], [b_hin[i]])
                    S.op("dve", "tensor_scalar", hn[:], hin[i][:], r, None, ALU.mult, R=[b_hin[i], b_st2], W=[b_hn])
                    ti = nxt("T", 2)
                    for c in range(8):
                        S.op("pe", "transpose", pT[ti][:, c * 128:(c + 1) * 128], hn[:, c * 128:(c + 1) * 128], ident2[:],
                             R=[b_hn, b_id2], W=[bpT[ti]])
                    S.op("act", "copy", hnT[:, :, i * 128:(i + 1) * 128], pT[ti][:, 0:1024].rearrange("p (c n) -> p c n", c=8),
                         R=[bpT[ti]], W=[b_hnT])
                for f in range(32):
                    si = nxt("S", 2)
                    for c in range(8):
                        S.op("pe", "matmul", pS[si][:], lhsT=w_up[:, c, f * 128:(f + 1) * 128], rhs=hnT[:, c, :],
                             start=(c == 0), stop=(c == 7), R=[b_wup, b_hnT], W=[bpS[si]])
                    ri = f % 2
                    S.op("act", "activation", rl[ri][:], pS[si][:], AF.Relu, R=[bpS[si]], W=[b_rl[ri]])
                    S.op("pool", "tensor_tensor", aT[:, f, :], rl[ri][:], rl[ri][:], ALU.mult, R=[b_rl[ri]], W=[b_aT])
                for i in range(4):
                    ob = oc % 2
                    oc += 1
                    for dh in range(2):
                        mi = nxt("M", 2)
                        for f in range(32):
                            S.op("pe", "matmul", pM[mi][:], lhsT=aT[:, f, i * 128:(i + 1) * 128], rhs=w_dn[:, f, dh * 512:(dh + 1) * 512],
                                 start=(f == 0), stop=(f == 31), R=[b_aT, b_wdn], W=[bpM[mi]])
                        S.op("dve", "tensor_tensor", yo[ob][:, dh * 512:(dh + 1) * 512], pM[mi][:], hin[i][:, dh * 512:(dh + 1) * 512],
                             ALU.add, R=[bpM[mi], b_hin[i]], W=[b_yo[ob]])
                    r = rstd2(yo[ob][:], [b_yo[ob]])
                    S.op("dve", "scalar_tensor_tensor", yo[ob][:], yo[ob][:], r, gfin[:], ALU.mult, ALU.mult,
                         R=[b_yo[ob], b_st2, b_gb], W=[b_yo[ob]])
                    S.dma("sp", out_d[base + i * 128:base + (i + 1) * 128, :], yo[ob][:], R=[b_yo[ob]])
            S.emit()
            print('NOPS', S.nops)
    return nc


def _rope_tab(pos, dim):
    inv = np.exp(np.float32(-math.log(500000.0)) * np.arange(0, dim, 2, dtype=np.float32) / np.float32(dim)).astype(np.float32)
    ang = pos.astype(np.float32)[:, None] * inv[None, :]
    return np.cos(ang).astype(np.float32), np.sin(ang).astype(np.float32)


def _tok_major(a):
    return np.ascontiguousarray(a.reshape(NT, 128, -1).transpose(1, 0, 2))


def host_consts():
    pos = np.arange(SEQ)
    cm, sm = _rope_tab(pos, 32)
    cn, sn = _rope_tab(pos, 16)
    c = {}
    c["cos_m"], c["sin_m"] = _tok_major(cm), _tok_major(sm)
    c["cos_n"], c["sin_n"] = _tok_major(cn), _tok_major(sn)
    c["cos_n8"], c["sin_n8"] = _tok_major(cn * np.float32(0.125)), _tok_major(sn * np.float32(0.125))
    ce = np.zeros((8, NT, 8), np.float32)
    se = np.zeros((8, NT, 8), np.float32)
    for t in range(NT):
        for m in range(8):
            n = 8 * t - 1 + m
            if 0 <= n < 255:
                p = 16 * n + 31
                ce[m, t], se[m, t] = cn[p], sn[p]
    c["cos_e"], c["sin_e"] = ce, se
    k = np.arange(128)[:, None]
    q = np.arange(128)[None, :]
    tri = (q >= k).astype(np.float32)
    anti = (q < k).astype(np.float32)
    c["tri4"] = np.ascontiguousarray(np.tile(tri, (1, 4)))
    c["anti4"] = np.ascontiguousarray(np.tile(anti, (1, 4)))
    n = np.arange(256)[:, None] * 16
    j = np.arange(64)[None, :] * 64
    ov = np.clip(np.minimum(n + 32, j + 64) - np.maximum(n, j), 0, None).astype(np.float32) / 32.0
    ov[255] = 0.0
    c["ovl"] = np.ascontiguousarray(ov.reshape(2, 128, 64).transpose(1, 0, 2))
    c["expand"] = (np.arange(SEQ)[None, :] // 64 == np.arange(64)[:, None]).astype(np.float32)
    c["ident"] = np.eye(128, dtype=np.float32)
    return c


def host_weights(inp):
    f = lambda a: np.ascontiguousarray(np.asarray(a, dtype=np.float32))
    pc = lambda v: np.ascontiguousarray(np.asarray(v, np.float32).reshape(-1, 128).T)
    w = {}
    w["w_in"] = f(inp["w_in"][0])
    w["g_mix"] = pc(inp["g_mix_norm"][0])
    w["w_uq"] = f(inp["w_uq"][0])
    w["g_cq"] = pc(inp["g_cq"][0])
    wukv = np.asarray(inp["w_ukv"][0], np.float32).reshape(128, 8, 2, 64)
    wuk = wukv[:, :, 0, :]
    w["wukT"] = np.ascontiguousarray(wuk.transpose(2, 1, 0))
    w["wuv"] = np.ascontiguousarray(wukv[:, :, 1, :])
    w["gckv_bc"] = np.ascontiguousarray(np.broadcast_to(np.asarray(inp["g_ckv"][0], np.float32)[None, :], (128, 128)))
    w1 = np.stack([np.asarray(inp["cmp_w1_k"][0], np.float32), np.asarray(inp["cmp_w1_v"][0], np.float32)])
    w["cmp_w1"] = np.ascontiguousarray(w1.reshape(2, 32, 64, 128).transpose(0, 2, 1, 3))
    pe = np.stack([np.asarray(inp["cmp_pe_k"][0], np.float32), np.asarray(inp["cmp_pe_v"][0], np.float32)])
    w["cmp_peT"] = np.ascontiguousarray(pe.transpose(0, 2, 1))
    w["cmp_b1"] = np.ascontiguousarray(np.stack([inp["cmp_b1_k"][0], inp["cmp_b1_v"][0]], axis=1).astype(np.float32))
    w["cmp_w2"] = np.ascontiguousarray(np.stack([inp["cmp_w2_k"][0], inp["cmp_w2_v"][0]], axis=1).astype(np.float32))
    b2k = np.asarray(inp["cmp_b2_k"][0], np.float32)
    w["cmp_b2k_bc"] = np.ascontiguousarray(np.broadcast_to(np.concatenate([b2k, b2k])[None, :], (128, 128)))
    w["cmp_b2v"] = np.ascontiguousarray(np.asarray(inp["cmp_b2_v"][0], np.float32).reshape(64, 1))
    w["w_o"] = f(inp["w_o"][0])
    w["g_out"] = pc(np.concatenate([np.asarray(inp["g_out_mla"][0]), np.asarray(inp["g_out_nsa"][0])]))
    w["w_up"] = f(inp["w_up"][0])
    w["g_mlp"] = pc(inp["g_mlp_norm"][0])
    w["w_down"] = f(inp["w_down"][0])
    w["gfin_bc"] = np.ascontiguousarray(np.broadcast_to(np.asarray(inp["g_final"], np.float32)[None, :], (128, D)))
    return w


_NC_CACHE = {}


def kernel(**inputs):
    x = np.asarray(inputs["x"], dtype=np.float32)
    shared = host_consts()
    shared.update(host_weights(inputs))
    if "full" not in _NC_CACHE:
        _NC_CACHE["full"] = build_program()
    nc = _NC_CACHE["full"]
    in_maps = []
    for c in range(NCORES):
        m = dict(shared)
        m["x"] = np.ascontiguousarray(x[c * NSEQ:(c + 1) * NSEQ])
        in_maps.append(m)
    res = run_bass_kernel_spmd(nc, in_maps, core_ids=list(range(NCORES)))
    outs = [np.asarray(r["out"]).reshape(NSEQ, SEQ, D) for r in res.results]
    return np.concatenate(outs, axis=0).astype(np.float32)
```

```python
import math
import numpy as np
from contextlib import ExitStack
import concourse.bass as bass
import concourse.mybir as mybir
from concourse.bass_utils import run_bass_kernel_spmd

F32 = mybir.dt.float32
BF16 = mybir.dt.bfloat16
I32 = mybir.dt.int32
AF = mybir.ActivationFunctionType
ALU = mybir.AluOpType
AX = mybir.AxisListType

NCORES = 8
SEQ = 4096
D = 1024
NSEQ = 2
NT = SEQ // 128
EPS = 1e-6
IN_COLS = 1720
DFF = 4096
NEG = -30000.0
STRICT_SAME = True
OP_LIMIT = None
BG_OVERLAP = True
USE_MAGIC = True
ACT_COPY_T = 20


class Buf:
    __slots__ = ("name", "w", "r", "dsem", "dcnt", "excl")

    def __init__(self, name):
        self.name = name
        self.excl = False
        self.w = None
        self.r = {}
        self.dsem = None
        self.dcnt = 0


class Sched:
    ENG = ("pe", "act", "dve", "pool", "sp")

    def __init__(self, nc, ctx):
        self.nc = nc
        self.ctx = ctx
        self.sem = {e: ctx.enter_context(nc.semaphore("s_" + e)) for e in self.ENG}
        self.cnt = {e: 0 for e in self.ENG}
        self.seen = {e: {} for e in self.ENG}
        self.prog = {e: [] for e in self.ENG}
        self.dbufs = []
        self.nb = 0
        self.nops = 0
        self.fillregs = {}
        self.tick = None
        self.limit = OP_LIMIT

    def buf(self, name):
        self.nb += 1
        return Buf("%s_%d" % (name, self.nb))

    def bufs(self, name, n):
        return [self.buf(name) for _ in range(n)]

    def _dsem(self, b):
        if b.dsem is None:
            b.dsem = self.ctx.enter_context(self.nc.semaphore("d_" + b.name))
            self.dbufs.append(b)
        return b.dsem

    def _deps(self, e, reads, writes, strict):
        toks = []
        for b in reads:
            if b.w is not None:
                toks.append(b.w)
            if b.excl:
                toks.extend(b.r.values())
        for b in writes:
            if b.w is not None:
                toks.append(b.w)
            toks.extend(b.r.values())
        need = {}
        for (key, sem, val) in toks:
            if key == e and not strict and (e == "pe" or not STRICT_SAME):
                continue
            if self.seen[e].get(key, 0) >= val:
                continue
            if key not in need or need[key][1] < val:
                need[key] = (sem, val)
        for key, (sem, val) in need.items():
            self.seen[e][key] = val
            self.prog[e].append(("wait", sem, val))

    def op(self, e, meth, *args, R=(), W=(), **kw):
        self.nops += 1
        if self.limit is not None and self.nops > self.limit:
            return None
        self._deps(e, R, W, False)
        self.cnt[e] += 1
        tok = (e, self.sem[e], self.cnt[e])
        self.prog[e].append(("op", meth, args, kw))
        for b in R:
            b.r[e] = tok
        for b in W:
            b.w = tok
            b.r = {}
        if self.tick is not None:
            self.tick()
        return tok

    def dma(self, q, out, in_, R=(), W=(), **kw):
        self.nops += 1
        if self.limit is not None and self.nops > self.limit:
            return None
        self._deps(q, R, W, True)
        owner = W[0] if W else R[0]
        sem = self._dsem(owner)
        owner.dcnt += 16
        tok = ("d_" + owner.name, sem, owner.dcnt)
        self.prog[q].append(("dma", out, in_, kw, sem))
        for b in R:
            b.r[tok[0]] = tok
        for b in W:
            b.w = tok
            b.r = {}
        return tok

    def barrier(self):
        toks = [(e, self.sem[e], self.cnt[e]) for e in self.ENG if self.cnt[e] > 0]
        toks += [("d_" + b.name, b.dsem, b.dcnt) for b in self.dbufs if b.dcnt > 0]
        for e in self.ENG:
            for (key, sem, val) in toks:
                if self.seen[e].get(key, 0) >= val:
                    continue
                self.seen[e][key] = val
                self.prog[e].append(("wait", sem, val))

    def flush(self):
        nc = self.nc
        with nc.Block() as block:
            def replay(e):
                def f(eng):
                    sem_e = self.sem[e]
                    for it in self.prog[e]:
                        if it[0] == "wait":
                            eng.wait_ge(it[1], it[2])
                        elif it[0] == "op":
                            args = it[2]
                            if it[1] == "affine_select":
                                args = list(args)
                                if args[4] not in self.fillregs:
                                    self.fillregs[args[4]] = eng.to_reg(args[4])
                                args[4] = self.fillregs[args[4]]
                            getattr(eng, it[1])(*args, **it[3]).then_inc(sem_e, 1)
                        else:
                            eng.dma_start(out=it[1], in_=it[2], **it[3]).then_inc(it[4], 16)
                return f
            block.tensor(replay("pe"))
            block.scalar(replay("act"))
            block.vector(replay("dve"))
            block.gpsimd(replay("pool"))
            block.sync(replay("sp"))
        self.prog = {e: [] for e in self.ENG}

    def emit(self):
        self.barrier()
        self.flush()


def build_program(nseq=NSEQ, ntiles=NT, do_mlp=True, stages=("p1", "mla", "nsa", "out")):
    nc = bass.Bass("TRN2", target_bir_lowering=False)

    def din(name, shape):
        return nc.dram_tensor(name, list(shape), F32, kind="ExternalInput").ap()

    x_d = din("x", [nseq, SEQ, D])
    w_in_d = din("w_in", [D, IN_COLS])
    g_mix_d = din("g_mix", [128, 8])
    w_uq_d = din("w_uq", [256, 768])
    g_cq_d = din("g_cq", [128, 2])
    wukT_d = din("wukT", [64, 8, 128])
    wuv_d = din("wuv", [128, 8, 64])
    gckv_d = din("gckv_bc", [128, 128])
    w1_d = din("cmp_w1", [2, 64, 32, 128])
    peT_d = din("cmp_peT", [2, 64, 32])
    b1_d = din("cmp_b1", [128, 2])
    w2_d = din("cmp_w2", [128, 2, 64])
    b2k_d = din("cmp_b2k_bc", [128, 128])
    b2v_d = din("cmp_b2v", [64, 1])
    w_o_d = din("w_o", [D, D])
    g_out_d = din("g_out", [128, 8])
    w_up_d = din("w_up", [D, DFF])
    g_mlp_d = din("g_mlp", [128, 8])
    w_down_d = din("w_down", [DFF, D])
    gfin_d = din("gfin_bc", [128, D])
    cosm_d = din("cos_m", [128, NT, 16])
    sinm_d = din("sin_m", [128, NT, 16])
    cosn_d = din("cos_n", [128, NT, 8])
    sinn_d = din("sin_n", [128, NT, 8])
    cosn8_d = din("cos_n8", [128, NT, 8])
    sinn8_d = din("sin_n8", [128, NT, 8])
    cose_d = din("cos_e", [8, NT, 8])
    sine_d = din("sin_e", [8, NT, 8])
    tri_d = din("tri4", [128, 512])
    anti_d = din("anti4", [128, 512])
    ovl_d = din("ovl", [128, 2, 64])
    exp_d = din("expand", [64, SEQ])
    ident_d = din("ident", [128, 128])
    out_d = nc.dram_tensor("out", [nseq * SEQ, D], F32, kind="ExternalOutput").ap()
    hscr_d = nc.dram_tensor("hscr", [nseq * SEQ, D], F32, kind="Internal").ap()

    top = ExitStack()
    with top:
        S = Sched(nc, top)
        def psum(name, shape, dt):
            return top.enter_context(nc.psum_tensor(name, shape, dt))
        pS = [psum("pS%d" % i, [128, 512], F32) for i in range(2)]
        pA = [psum("pA%d" % i, [128, 512], F32) for i in range(2)]
        pM = [psum("pM%d" % i, [128, 512], F32) for i in range(2)]
        pT = [psum("pT%d" % i, [128, 1024], BF16) for i in range(2)]
        bpS = S.bufs("pS", 2)
        bpA = S.bufs("pA", 2)
        bpM = S.bufs("pM", 2)
        bpT = S.bufs("pT", 2)
        for b_ in bpS + bpA + bpM + bpT:
            b_.excl = True
        rr = {"S": 0, "M": 0, "T": 0, "P": 0, "A": 0}
        mode = {"bg": False, "B": False}

        def nxt(kind, n):
            if kind in ("M", "T") and not mode["B"]:
                return 0 if mode["bg"] else 1
            i = rr[kind] % n
            rr[kind] += 1
            return i

        A = ExitStack()
        with A:
            def sb(name, shape, dt):
                return A.enter_context(nc.sbuf_tensor("a_" + name, shape, dt))

            ident = sb("ident", [128, 128], BF16); b_ident = S.buf("ident")
            S.dma("pool", ident[:], ident_d, W=[b_ident])
            tri4 = sb("tri4", [128, 512], BF16); anti4 = sb("anti4", [128, 512], BF16); b_msk = S.buf("msk")
            S.dma("pool", tri4[:], tri_d, W=[b_msk])
            S.dma("pool", anti4[:], anti_d, W=[b_msk])
            tabs = {}
            b_tab = S.buf("tab")
            for nm, d_, w_ in (("cos_m", cosm_d, 16), ("sin_m", sinm_d, 16), ("cos_n", cosn_d, 8), ("sin_n", sinn_d, 8)):
                tabs[nm] = sb(nm, [128, NT, w_], F32)
                S.dma("sp", tabs[nm][:], d_, W=[b_tab])
            cos_e = sb("cos_e", [8, NT, 8], F32); sin_e = sb("sin_e", [8, NT, 8], F32)
            S.dma("sp", cos_e[:], cose_d, W=[b_tab])
            S.dma("sp", sin_e[:], sine_d, W=[b_tab])
            gckv = sb("gckv", [128, 128], F32); b2k = sb("b2k", [128, 128], F32); b2v = sb("b2v", [64, 1], F32)
            b1 = sb("b1", [128, 2], F32)
            S.dma("sp", gckv[:], gckv_d, W=[b_tab])
            S.dma("sp", b2k[:], b2k_d, W=[b_tab])
            S.dma("sp", b2v[:], b2v_d, W=[b_tab])
            S.dma("sp", b1[:], b1_d, W=[b_tab])
            gvec = sb("gvec", [128, 24], F32)
            S.dma("sp", gvec[:, 0:8], g_mix_d, W=[b_tab])
            S.dma("sp", gvec[:, 8:10], g_cq_d, W=[b_tab])
            S.dma("sp", gvec[:, 10:18], g_out_d, W=[b_tab])

            w_in = sb("w_in", [128, 8, IN_COLS], BF16); b_win = S.buf("w_in")
            S.dma("pool", w_in[:], w_in_d.rearrange("(c p) n -> p c n", p=128), W=[b_win])
            for c in range(8):
                S.op("dve", "tensor_scalar", w_in[:, c, :], w_in[:, c, :], gvec[:, c:c + 1], None, ALU.mult,
                     R=[b_tab, b_win], W=[b_win])
                S.op("dve", "tensor_scalar", w_in[:, c, 416:928], w_in[:, c, 416:928], 0.125, None, ALU.mult,
                     R=[b_win], W=[b_win])
            w_o = sb("w_o", [128, 8, D], BF16); b_wo = S.buf("w_o")
            S.dma("pool", w_o[:], w_o_d.rearrange("(c p) n -> p c n", p=128), W=[b_wo])
            for c in range(8):
                S.op("dve", "tensor_scalar", w_o[:, c, :], w_o[:, c, :], gvec[:, 10 + c:11 + c], None, ALU.mult,
                     R=[b_tab, b_wo], W=[b_wo])
            w_uq = sb("w_uq", [128, 2, 768], BF16); b_wuq = S.buf("w_uq")
            S.dma("pool", w_uq[:], w_uq_d.rearrange("(c p) n -> p c n", p=128), W=[b_wuq])
            for c in range(2):
                S.op("dve", "tensor_scalar", w_uq[:, c, :], w_uq[:, c, :], gvec[:, 8 + c:9 + c], 96.0 ** -0.5,
                     ALU.mult, ALU.mult, R=[b_tab, b_wuq], W=[b_wuq])
            wukT = sb("wukT", [64, 8, 128], BF16); wuv = sb("wuv", [128, 8, 64], BF16); b_wkv = S.buf("wkv")
            S.dma("pool", wukT[:], wukT_d, W=[b_wkv])
            S.dma("pool", wuv[:], wuv_d, W=[b_wkv])
            w1 = sb("w1", [64, 2, 32, 128], BF16); b_w1 = S.buf("w1")
            for kv in range(2):
                S.dma("pool", w1[:, kv], w1_d[kv], W=[b_w1])
            peT = sb("peT", [64, 2, 32], BF16)
            for kv in range(2):
                S.dma("pool", peT[:, kv], peT_d[kv], W=[b_w1])
            w2 = sb("w2", [128, 2, 64], BF16)
            S.dma("pool", w2[:], w2_d, W=[b_w1])

            bias_tot = sb("bias_tot", [128, 2], F32); b_bt = S.buf("bias_tot")
            for kv in range(2):
                for l in range(32):
                    S.op("pe", "matmul", pM[0][:, kv:kv + 1], lhsT=w1[:, kv, l, :], rhs=peT[:, kv, l:l + 1],
                         start=(l == 0), stop=(l == 31), R=[b_w1], W=[bpM[0]])
            S.op("dve", "tensor_tensor", bias_tot[:], pM[0][:, 0:2], b1[:], ALU.add, R=[bpM[0], b_tab], W=[b_bt])

            KlatT = sb("KlatT", [128, SEQ], BF16); bKlat = S.bufs("Klat", NT)
            KpeT = sb("KpeT", [128, SEQ], BF16); bKpe = S.bufs("Kpe", NT)
            Clat = sb("Clat", [128, NT, 128], BF16); bClat = S.bufs("Clat", NT)
            KsE = sb("KsE", [128, 2, SEQ], BF16); bKs = S.bufs("Ks", NT); b_exp = S.buf("expand")
            Vs = sb("Vs", [128, NT, 2, 65], BF16); bVs = S.bufs("Vs", NT)
            KwT = sb("KwT", [128, 2, 8 * 128], BF16); bKw = S.bufs("Kw", 8)
            Vw = sb("Vw", [128, 8, 2, 65], BF16); bVw = S.bufs("Vw", 8)
            KcT = sb("KcT", [64, 2, 256], BF16); b_Kc = S.buf("Kc")
            VcT = sb("VcT", [64, 2, 256], BF16); b_VcT = S.buf("VcT")
            VcO = sb("VcO", [128, 2, 2, 128], BF16); b_VcO = S.buf("VcO")
            for g in range(2):
                S.dma("pool", KsE[64:128, g, :], exp_d, W=[b_exp])
            for nt in range(2):
                for g in range(2):
                    S.dma("pool", VcO[:, nt, g, 64:128], ovl_d[:, nt, :], W=[b_VcO])
            S.op("pool", "memset", Vs[:, :, :, 64:65], 1.0, W=bVs)
            S.op("pool", "memset", Vw[:, :, :, 64:65], 1.0, W=bVw)

            xs = [sb("xs%d" % i, [128, D], F32) for i in range(2)]; b_xs = S.bufs("xs", 2)
            st = sb("st", [128, 64], F32); b_st = S.buf("st")
            xn = sb("xn", [128, D], BF16); b_xn = S.buf("xn")
            xnT = sb("xnT", [128, 8, 128], BF16); b_xnT = S.buf("xnT")
            u = sb("u", [128, IN_COLS], F32); b_u = S.buf("u")
            cqn = sb("cqn", [128, 256], BF16); b_cqn = S.buf("cqn")
            cqnT = sb("cqnT", [128, 2, 128], BF16); b_cqnT = S.buf("cqnT")
            q_sb = sb("q_sb", [128, 9, 96], F32); b_q = S.buf("q")
            qn_sb = sb("qn_sb", [128, 8, 64], BF16); b_qn = S.buf("qn")
            qpe_sb = sb("qpe_sb", [128, 9, 32], BF16); b_qpe = S.buf("qpe")
            rt = [sb("rt%d" % i, [128, 160], F32) for i in range(4)]; b_rt = S.buf("rt")
            QnT = sb("QnT", [64, 8, 128], BF16); b_QnT = S.buf("QnT")
            QpeT2 = [sb("QpeT%d" % i, [128, 8, 128], BF16) for i in range(2)]; b_QpeT2 = S.bufs("QpeT", 2)
            QabsT2 = [sb("QabsT%d" % i, [128, 8, 128], BF16) for i in range(2)]; b_Qabs2 = S.bufs("Qabs", 2)
            ub = sb("ub", [128, 20, 64], BF16); b_ub = S.buf("ub")
            QS2 = [sb("QS%d" % i, [128, 2, 4, 128], BF16) for i in range(2)]; b_QSq2 = S.bufs("QSq", 2); b_QSs2 = [S.bufs("QSs", 2) for _ in range(2)]
            rawT = [sb("rawT%d" % i, [64, 2, 2, 144], BF16) for i in range(2)]; b_rawT = S.bufs("rawT", 2)
            gate2 = [sb("gate%d" % i, [128, 3, 8], F32) for i in range(2)]; b_gate2 = S.bufs("gate", 2)
            z_sb = sb("z_sb", [128, 32], F32); z2_sb = sb("z2_sb", [128, 32], F32); b_z = S.buf("z")
            hid_sb = sb("hid_sb", [128, 32], BF16); b_hid = S.buf("hid")
            kc_f = sb("kc_f", [8, 2, 64], F32); kc_sb = sb("kc_sb", [8, 2, 64], BF16); b_kc = S.buf("kc")
            PT = [sb("PT%d" % i, [128, 512], BF16) for i in range(3)]; b_PT = S.bufs("PT", 3)
            PcT = [sb("PcT%d" % i, [128, 512], BF16) for i in range(2)]; b_PcT = S.bufs("PcT", 2)
            OlatT = sb("OlatT", [128, 4, 128], BF16); b_OlatT = S.buf("OlatT")
            y_sb = sb("y_sb", [128, D], F32); b_y = S.buf("y"); b_yn = S.buf("yn")
            imp = sb("imp", [128, 64], F32); sc1 = sb("sc1", [128, 64], F32); sc2 = sb("sc2", [128, 64], F32)
            sc3 = sb("sc3", [128, 64], F32); b_imp = S.buf("imp")
            selq = sb("selq", [128, 128], BF16); b_selq = S.buf("selq")
            tmp4 = sb("tmp4", [128, 4, 64], F32); b_tmp4 = S.buf("tmp4")
            mixed = sb("mixed", [128, D], BF16); b_mixed = S.buf("mixed")
            mixedT = sb("mixedT", [128, 8, 128], BF16); b_mixedT = S.buf("mixedT")
            h_sb = [sb("h_sb%d" % i, [128, D], F32) for i in range(2)]; b_h = S.bufs("h", 2)

            S.op("pool", "memset", selq[:], 0.0, W=[b_selq])
            S.op("pool", "memset", KpeT[:], 0.0, W=bKpe)
            S.op("pool", "memset", KwT[:], 0.0, W=bKw)
            for i in range(2):
                S.op("pool", "memset", QS2[i][:], 0.0, W=[b_QSq2[i]] + b_QSs2[i])
            for i in range(2):
                S.op("pool", "memset", QpeT2[i][:], 0.0, W=[b_QpeT2[i]])
            for i in range(2):
                S.op("pool", "memset", rawT[i][:], 0.0, W=[b_rawT[i]])
            S.op("pool", "memset", KcT[:], 0.0, W=[b_Kc])
            S.op("pool", "memset", VcT[:], 0.0, W=[b_VcT])
            S.op("pool", "memset", VcO[:, :, :, 0:64], 0.0, W=[b_VcO])

            b_stc = {0: S.buf("st0"), 4: S.buf("st4"), 12: S.buf("st12")}
            b_stm = S.buf("stm")
            b_stn = S.buf("stn")
            rl = sb("rl", [128, 8], F32); b_rl = S.buf("rl")
            lacc = sb("lacc", [128, 512], F32); b_lacc = S.buf("lacc")
            ones_f = sb("ones_f", [128, 1], F32); b_ones = S.buf("ones")
            S.op("pool", "memset", ones_f[:], 1.0, W=[b_ones])

            def rstd_multi(items, col, Rb, jk, b_jk):
                b_st = b_stc[col]
                k = len(items)
                S.op("dve", "memset", st[:, col:col + k], 0.0, W=[b_st])
                for i_, (src_ap, n) in enumerate(items):
                    S.op("dve", "scalar_tensor_tensor", jk[:, 0:n], src_ap, 1.0, src_ap, ALU.mult, ALU.mult,
                         accum_out=st[:, col + i_:col + i_ + 1], R=Rb + [b_st], W=[b_jk, b_st])
                    S.op("dve", "tensor_scalar", st[:, col + k + i_:col + k + i_ + 1], st[:, col + i_:col + i_ + 1], 1.0 / n, EPS,
                         ALU.mult, ALU.add, R=[b_st], W=[b_st])
                v_ = st[:, col + k:col + 2 * k]
                y_ = st[:, col + 2 * k:col + 3 * k]
                w_ = st[:, col + 3 * k:col + 4 * k]
                if not USE_MAGIC:
                    S.op("act", "activation", w_, v_, AF.Sqrt, R=[b_st], W=[b_st])
                    S.op("dve", "reciprocal", y_, w_, R=[b_st], W=[b_st])
                    return [st[:, col + 2 * k + i_:col + 2 * k + i_ + 1] for i_ in range(k)]
                S.op("dve", "tensor_scalar", y_.bitcast(I32), v_.bitcast(I32), -0.5, 1597463007.0, ALU.mult, ALU.add, R=[b_st], W=[b_st])
                for _it in range(2):
                    S.op("pool", "tensor_tensor", w_, y_, y_, ALU.mult, R=[b_st], W=[b_st])
                    S.op("pool", "tensor_tensor", w_, w_, v_, ALU.mult, R=[b_st], W=[b_st])
                    S.op("pool", "tensor_scalar", w_, w_, -0.5, 1.5, ALU.mult, ALU.add, R=[b_st], W=[b_st])
                    S.op("pool", "tensor_tensor", y_, y_, w_, ALU.mult, R=[b_st], W=[b_st])
                return [st[:, col + 2 * k + i_:col + 2 * k + i_ + 1] for i_ in range(k)]

            def rope(eng, out_ap, in_ap, cos_ap, sin_ap, nh, half, Rb, Wb):
                P = in_ap.shape[0]
                x1 = in_ap[:, :, 0:half]
                x2 = in_ap[:, :, half:2 * half]
                cb = cos_ap[:, None, :].to_broadcast([P, nh, half])
                sbb = sin_ap[:, None, :].to_broadcast([P, nh, half])
                t = [r_[0:P, 0:nh * half].rearrange("p (h d) -> p h d", h=nh) for r_ in rt]
                S.op(eng, "tensor_tensor", t[0], x1, cb, ALU.mult, R=Rb + [b_tab], W=[b_rt])
                S.op(eng, "tensor_tensor", t[1], x2, sbb, ALU.mult, R=Rb + [b_tab], W=[b_rt])
                S.op(eng, "tensor_tensor", t[2], x2, cb, ALU.mult, R=Rb + [b_tab], W=[b_rt])
                S.op(eng, "tensor_tensor", t[3], x1, sbb, ALU.mult, R=Rb + [b_tab], W=[b_rt])
                S.op(eng, "tensor_tensor", out_ap[:, :, 0:half], t[0], t[1], ALU.subtract, R=[b_rt], W=Wb)
                S.op(eng, "tensor_tensor", out_ap[:, :, half:2 * half], t[2], t[3], ALU.add, R=[b_rt], W=Wb)

            def transpose_to(ps_i, col0, in_ap, Rb):
                P, Fd = in_ap.shape[0], in_ap.shape[1]
                S.op("pe", "transpose", pT[ps_i][0:Fd, col0:col0 + P], in_ap, ident[0:P, 0:P],
                     R=Rb + [b_ident], W=[bpT[ps_i]])

            def phase1(s, t):
                par = t % 2
                QpeT, b_QpeT = QpeT2[par], b_QpeT2[par]
                QabsT, b_Qabs = QabsT2[par], b_Qabs2[par]
                QS, b_QSq, b_QSs = QS2[par], b_QSq2[par], b_QSs2[par]
                gate, b_gate = gate2[par], b_gate2[par]
                xb = t % 2
                ce, cm = ("act", "copy") if t < ACT_COPY_T else ("dve", "tensor_copy")
                S.dma("sp", xs[xb][:], x_d[s, t * 128:(t + 1) * 128, :], W=[b_xs[xb]])
                r0 = rstd_multi([(xs[xb][:], D)], 0, [b_xs[xb]], xn, b_xn)[0]
                S.op("dve", "tensor_scalar", xn[:], xs[xb][:], r0, None, ALU.mult, R=[b_xs[xb], b_stc[0]], W=[b_xn])
                ti = nxt("T", 2)
                for c in range(8):
                    transpose_to(ti, c * 128, xn[:, c * 128:(c + 1) * 128], [b_xn])
                S.op(ce, cm, xnT[:], pT[ti][:, 0:1024].rearrange("p (c n) -> p c n", c=8), R=[bpT[ti]], W=[b_xnT])
                for cg, (c0, c1) in enumerate(((0, 512), (512, 1024), (1024, 1536), (1536, IN_COLS))):
                    mi = nxt("M", 2)
                    for c in range(8):
                        S.op("pe", "matmul", pM[mi][:, 0:c1 - c0], lhsT=xnT[:, c, :], rhs=w_in[:, c, c0:c1],
                             start=(c == 0), stop=(c == 7), R=[b_xnT, b_win], W=[bpM[mi]])
                    S.op(ce, cm,
                         u[:, c0:c1], pM[mi][:, 0:c1 - c0], R=[bpM[mi]], W=[b_u])
                    yield

                yield
                def branch_a():
                    r1, r2 = rstd_multi([(u[:, 0:256], 256), (u[:, 256:384], 128)], 4, [b_u], cqn, b_cqn)
                    S.op("dve", "tensor_scalar", cqn[:], u[:, 0:256], r1, None, ALU.mult, R=[b_u, b_stc[4]], W=[b_cqn])
                    ti = nxt("T", 2)
                    for c in range(2):
                        transpose_to(ti, c * 128, cqn[:, c * 128:(c + 1) * 128], [b_cqn])
                    S.op("dve", "tensor_copy", cqnT[:], pT[ti][:, 0:256].rearrange("p (c n) -> p c n", c=2), R=[bpT[ti]], W=[b_cqnT])
                    yield
                    for half in range(2):
                        mi = nxt("M", 2)
                        for c in range(2):
                            S.op("pe", "matmul", pM[mi][:, 0:384], lhsT=cqnT[:, c, :], rhs=w_uq[:, c, half * 384:(half + 1) * 384],
                                 start=(c == 0), stop=(c == 1), R=[b_cqnT, b_wuq], W=[bpM[mi]])
                        S.op(ce, cm, q_sb[:, half * 4:(half + 1) * 4, :],
                             pM[mi][:, 0:384].rearrange("p (h d) -> p h d", h=4), R=[bpM[mi]], W=[b_q])
                    yield
                    S.op("dve", "tensor_copy", q_sb[:, 8, 64:96], u[:, 384:416], R=[b_u], W=[b_q])
                    S.op("dve", "tensor_copy", qn_sb[:], q_sb[:, 0:8, 0:64], R=[b_q], W=[b_qn])
                    rope("dve", qpe_sb[:], q_sb[:, :, 64:96], tabs["cos_m"][:, t, :], tabs["sin_m"][:, t, :], 9, 16, [b_q], [b_qpe])
                    ti = nxt("T", 2)
                    for h in range(8):
                        transpose_to(ti, h * 128, qn_sb[:, h, :], [b_qn])
                    S.op(ce, cm, QnT[:], pT[ti][0:64, 0:1024].rearrange("p (c n) -> p c n", c=8), R=[bpT[ti]], W=[b_QnT])
                    ti = nxt("T", 2)
                    for h in range(8):
                        transpose_to(ti, h * 128, qpe_sb[:, h, :], [b_qpe])
                    S.op(ce, cm, QpeT[0:32], pT[ti][0:32, 0:1024].rearrange("p (c n) -> p c n", c=8), R=[bpT[ti]], W=[b_QpeT])
                    yield
                    for hg in range(2):
                        mi = nxt("M", 2)
                        for j in range(4):
                            h = hg * 4 + j
                            S.op("pe", "matmul", pM[mi][:, j * 128:(j + 1) * 128], lhsT=wukT[:, h, :],
                                 rhs=QnT[:, h, :], start=True, stop=True, R=[b_wkv, b_QnT], W=[bpM[mi]])
                        S.op(ce, cm, QabsT[:, hg * 4:(hg + 1) * 4, :],
                             pM[mi][:, 0:512].rearrange("p (c n) -> p c n", c=4), R=[bpM[mi]], W=[b_Qabs])

                    yield
                    S.op("dve", "scalar_tensor_tensor", Clat[:, t, 0:128], u[:, 256:384], r2, gckv[:], ALU.mult, ALU.mult,
                         R=[b_u, b_stc[4], b_tab], W=[bClat[t]])
                    ti = nxt("T", 2)
                    transpose_to(ti, 0, Clat[:, t, 0:128], [bClat[t]])
                    S.op("dve", "tensor_copy", KlatT[:, t * 128:(t + 1) * 128], pT[ti][:, 0:128], R=[bpT[ti]], W=[bKlat[t]])
                    ti = nxt("T", 2)
                    transpose_to(ti, 0, qpe_sb[:, 8, :], [b_qpe])
                    S.op("dve", "tensor_copy", KpeT[0:32, t * 128:(t + 1) * 128], pT[ti][0:32, 0:128], R=[bpT[ti]], W=[bKpe[t]])

                    yield
                    yield

                def branch_b():
                    uv = u[:, 416:1696].rearrange("p (b d) -> p b d", b=20)
                    S.op("dve", "tensor_copy", ub[:, :, 16:64], uv[:, :, 16:64], R=[b_u], W=[b_ub])
                    rope("dve", ub[:, :, 0:16], uv[:, :, 0:16], tabs["cos_n"][:, t, :], tabs["sin_n"][:, t, :], 20, 8, [b_u], [b_ub])
                    S.op("dve", "tensor_copy", ub[:, 8:12, 0:16], uv[:, 8:12, 0:16], R=[b_u, b_ub], W=[b_ub])
                    ti = nxt("T", 2)
                    for h in range(8):
                        transpose_to(ti, h * 128, ub[:, h, :], [b_ub])
                    S.op(ce, cm, QS[0:64].rearrange("p g j n -> p (g j) n"),
                         pT[ti][0:64, 0:1024].rearrange("p (c n) -> p c n", c=8), R=[bpT[ti]], W=[b_QSq])
                    yield
                    kvv = u[:, 928:1696].rearrange("p (s g d) -> p s g d", s=6, g=2)
                    S.op("dve", "tensor_copy", Vs[:, t, :, 0:64], kvv[:, 3], R=[b_u], W=[bVs[t]])
                    S.op("dve", "tensor_copy", Vw[:, t % 8, :, 0:64], kvv[:, 5], R=[b_u], W=[bVw[t % 8]])
                    ti = nxt("T", 2)
                    for g in range(2):
                        transpose_to(ti, g * 128, ub[:, 12 + g, :], [b_ub])
                        transpose_to(ti, 256 + g * 128, ub[:, 16 + g, :], [b_ub])
                    S.op(ce, cm, KsE[0:64, :, t * 128:(t + 1) * 128],
                         pT[ti][0:64, 0:256].rearrange("p (g n) -> p g n", g=2), R=[bpT[ti]], W=[bKs[t]])
                    S.op("dve", "tensor_copy", KwT[0:64, :, (t % 8) * 128:(t % 8 + 1) * 128],
                         pT[ti][0:64, 256:512].rearrange("p (g n) -> p g n", g=2), R=[bpT[ti]], W=[bKw[t % 8]])
                    yield
                    rb = t % 2
                    if t == 0:
                        S.op("pool", "memset", rawT[rb][:, :, :, 0:16], 0.0, W=[b_rawT[rb]])
                    else:
                        S.op("dve", "tensor_copy", rawT[rb][:, :, :, 0:16], rawT[1 - rb][:, :, :, 128:144],
                             R=[b_rawT[1 - rb]], W=[b_rawT[rb]])
                    ti = nxt("T", 2)
                    for c in range(4):
                        transpose_to(ti, c * 128, ub[:, 8 + c, :], [b_ub])
                    S.op(ce, cm, rawT[rb][:, :, :, 16:144],
                         pT[ti][0:64, 0:512].rearrange("p (k g n) -> p k g n", k=2, g=2), R=[bpT[ti]], W=[b_rawT[rb]])
                    yield
                    S.op("act", "activation", gate[:].rearrange("p b h -> p (b h)"), u[:, 1696:1720], AF.Tanh, scale=0.5, R=[b_u], W=[b_gate])
                    S.op("dve", "tensor_scalar", gate[:].rearrange("p b h -> p (b h)"), gate[:].rearrange("p b h -> p (b h)"), 0.5, 0.5,
                         ALU.mult, ALU.add, R=[b_gate], W=[b_gate])

                    yield
                    mi = nxt("M", 2)
                    for kv in range(2):
                        for g in range(2):
                            c0 = (kv * 2 + g) * 8
                            for l in range(32):
                                S.op("pe", "matmul", pM[mi][:, c0:c0 + 8], lhsT=w1[:, kv, l, :], rhs=rawT[rb][:, kv, g, l:l + 113:16],
                                     start=(l == 0), stop=(l == 31), R=[b_w1, b_rawT[rb]], W=[bpM[mi]])
                    for kv in range(2):
                        S.op("dve", "tensor_scalar", z_sb[:, kv * 16:(kv + 1) * 16], pM[mi][:, kv * 16:(kv + 1) * 16],
                             bias_tot[:, kv:kv + 1], None, ALU.add, R=[bpM[mi], b_bt], W=[b_z])
                    S.op("dve", "tensor_tensor", z2_sb[:], z_sb[:], z_sb[:], ALU.mult, R=[b_z], W=[b_z])
                    S.op("dve", "tensor_scalar", z2_sb[:], z2_sb[:], 0.044715, 1.0, ALU.mult, ALU.add, R=[b_z], W=[b_z])
                    S.op("dve", "tensor_tensor", z2_sb[:], z2_sb[:], z_sb[:], ALU.mult, R=[b_z], W=[b_z])
                    S.op("act", "activation", z2_sb[:], z2_sb[:], AF.Tanh, scale=math.sqrt(2.0 / math.pi), R=[b_z], W=[b_z])
                    S.op("dve", "tensor_scalar", z2_sb[:], z2_sb[:], 0.5, 0.5, ALU.mult, ALU.add, R=[b_z], W=[b_z])
                    S.op("dve", "tensor_tensor", hid_sb[:], z_sb[:], z2_sb[:], ALU.mult, R=[b_z], W=[b_hid])
                    yield
                    n0 = 8 * t - 1
                    m0 = 1 if t == 0 else 0
                    mi = nxt("M", 2)
                    for g in range(2):
                        S.op("pe", "matmul", pM[mi][0:8, g * 64:(g + 1) * 64], lhsT=hid_sb[:, g * 8:(g + 1) * 8], rhs=w2[:, 0, :],
                             start=True, stop=True, R=[b_hid, b_w1], W=[bpM[mi]])
                    for g in range(2):
                        S.op("pe", "matmul", pM[mi][0:64, 128 + g * 8:136 + g * 8], lhsT=w2[:, 1, :], rhs=hid_sb[:, 16 + g * 8:24 + g * 8],
                             start=True, stop=True, R=[b_hid, b_w1], W=[bpM[mi]])
                    S.op("dve", "tensor_tensor", kc_f[:].rearrange("p g d -> p (g d)"), pM[mi][0:8, 0:128], b2k[0:8, :], ALU.add,
                         R=[bpM[mi], b_tab], W=[b_kc])
                    S.op("dve", "tensor_scalar", VcT[:, :, n0 + m0:n0 + 8], pM[mi][0:64, 128:144].rearrange("p (g m) -> p g m", g=2)[:, :, m0:8],
                         b2v[:, 0:1], None, ALU.add, R=[bpM[mi], b_tab], W=[b_VcT])
                    S.op("dve", "tensor_copy", kc_sb[:, :, 16:64], kc_f[:, :, 16:64], R=[b_kc], W=[b_kc])
                    rope("dve", kc_sb[:, :, 0:16], kc_f[:, :, 0:16], cos_e[:, t, :], sin_e[:, t, :], 2, 8, [b_kc], [b_kc])
                    ti = nxt("T", 2)
                    for g in range(2):
                        transpose_to(ti, g * 8, kc_sb[:, g, :], [b_kc])
                    S.op("dve", "tensor_copy", KcT[:, :, n0 + m0:n0 + 8],
                         pT[ti][0:64, 0:16].rearrange("p (g m) -> p g m", g=2)[:, :, m0:8], R=[bpT[ti]], W=[b_Kc])
                    yield
                    nts = sorted(set([max(n0, 0) // 128, (n0 + 7) // 128]))
                    for nt in nts:
                        ti = nxt("T", 2)
                        for g in range(2):
                            S.op("pe", "transpose", pT[ti][:, g * 64:(g + 1) * 64], VcT[:, g, nt * 128:(nt + 1) * 128], ident[0:64, 0:64],
                                 R=[b_VcT, b_ident], W=[bpT[ti]])
                        S.op("dve", "tensor_copy", VcO[:, nt, :, 0:64], pT[ti][:, 0:128].rearrange("p (g d) -> p g d", g=2), R=[bpT[ti]], W=[b_VcO])

                    yield

                ga, gb = branch_a(), branch_b()
                live = [ga, gb]
                while live:
                    for g_ in list(live):
                        try:
                            next(g_)
                            yield
                        except StopIteration:
                            live.remove(g_)

            def attn_loop(kts, qk_fn, post_fn):
                if not kts:
                    return
                si_next = qk_fn(kts[0])
                for i_, kt in enumerate(kts):
                    si = si_next
                    if i_ + 1 < len(kts):
                        si_next = qk_fn(kts[i_ + 1])
                    post_fn(kt, si)

            def mla(s, t):
                par = t % 2
                QpeT, b_QpeT = QpeT2[par], b_QpeT2[par]
                QabsT, b_Qabs = QabsT2[par], b_Qabs2[par]
                QS, b_QSq, b_QSs = QS2[par], b_QSq2[par], b_QSs2[par]
                gate, b_gate = gate2[par], b_gate2[par]
                mo = nxt("M", 2)
                for hg in range(2):
                    qa = QabsT[:, hg * 4:(hg + 1) * 4, :].rearrange("p c n -> p (c n)")
                    qp = QpeT[:, hg * 4:(hg + 1) * 4, :].rearrange("p c n -> p (c n)")

                    def qk(kt):
                        si = nxt("S", 2)
                        S.op("pe", "matmul", pS[si][:], lhsT=KlatT[:, kt * 128:(kt + 1) * 128], rhs=qa,
                             start=True, stop=False, R=[bKlat[kt], b_Qabs], W=[bpS[si]])
                        S.op("pe", "matmul", pS[si][:], lhsT=KpeT[:, kt * 128:(kt + 1) * 128], rhs=qp,
                             start=False, stop=True, R=[bKpe[kt], b_QpeT], W=[bpS[si]])
                        return si

                    def post(kt, si):
                        pi = nxt("P", 3)
                        S.op("act", "activation", PT[pi][:], pS[si][:], AF.Exp, R=[bpS[si]], W=[b_PT[pi]])
                        if kt == t:
                            S.op("dve", "tensor_tensor", PT[pi][:], PT[pi][:], tri4[:], ALU.mult, R=[b_PT[pi], b_msk], W=[b_PT[pi]])
                        S.op("pe", "matmul", pA[0][:], lhsT=Clat[:, kt, 0:128], rhs=PT[pi][:], start=(kt == 0), stop=(kt == t),
                             R=[b_PT[pi], bClat[kt]], W=[bpA[0]])
                        if kt == 0:
                            S.op("dve", "tensor_copy", lacc[:], PT[pi][:], R=[b_PT[pi]], W=[b_lacc])
                        else:
                            S.op("dve", "tensor_tensor", lacc[:], lacc[:], PT[pi][:], ALU.add, R=[b_PT[pi], b_lacc], W=[b_lacc])

                    attn_loop(list(range(t + 1)), qk, post)
                    for j in range(4):
                        S.op("pe", "matmul", pA[1][:, j:j + 1], lhsT=lacc[:, j * 128:(j + 1) * 128], rhs=ones_f[:, 0:1],
                             start=True, stop=True, R=[b_lacc, b_ones], W=[bpA[1]])
                    S.op("dve", "tensor_copy", OlatT[:], pA[0][:].rearrange("p (c n) -> p c n", c=4), R=[bpA[0]], W=[b_OlatT])
                    S.op("dve", "reciprocal", rl[:, hg * 4:(hg + 1) * 4], pA[1][:, 0:4], R=[bpA[1]], W=[b_rl])
                    for j in range(4):
                        h = hg * 4 + j
                        S.op("pe", "matmul", pM[mo][:, h * 64:(h + 1) * 64], lhsT=OlatT[:, j, :], rhs=wuv[:, h, :],
                             start=True, stop=True, R=[b_OlatT, b_wkv], W=[bpM[mo]])
                S.op("dve", "tensor_tensor", y_sb[:, 0:512].rearrange("p (h d) -> p h d", h=8),
                     pM[mo][:, 0:512].rearrange("p (h d) -> p h d", h=8), rl[:, 0:8, None].to_broadcast([128, 8, 64]), ALU.mult,
                     R=[bpM[mo], b_rl], W=[b_y])

            def nsa_sel(s, t, g):
                par = t % 2
                QpeT, b_QpeT = QpeT2[par], b_QpeT2[par]
                QabsT, b_Qabs = QabsT2[par], b_Qabs2[par]
                QS, b_QSq, b_QSs = QS2[par], b_QSq2[par], b_QSs2[par]
                gate, b_gate = gate2[par], b_gate2[par]
                QSg = QS[:, g].rearrange("p j n -> p (j n)")
                QSg = QS[:, g].rearrange("p j n -> p (j n)")
                nts = [0] + ([1] if t >= 16 else [])
                for nt in nts:
                    si = nxt("M", 2)
                    S.op("pe", "matmul", pM[si][:], lhsT=KcT[:, g, nt * 128:(nt + 1) * 128], rhs=QSg[0:64, :],
                         start=True, stop=True, R=[b_Kc, b_QSq], W=[bpM[si]])
                    S.op("act", "activation", PcT[nt][:], pM[si][:], AF.Exp, R=[bpM[si]], W=[b_PcT[nt]])
                    S.op("pool", "affine_select", PcT[nt][:].rearrange("p (j n) -> p j n", j=4),
                         PcT[nt][:].rearrange("p (j n) -> p j n", j=4), [[0, 4], [1, 128]], ALU.is_ge, 0.0,
                         base=128 * t - 31 - 2048 * nt, channel_multiplier=-16, R=[b_PcT[nt]], W=[b_PcT[nt]])
                yield
                mc = nxt("M", 2)
                for j in range(4):
                    for i_, nt in enumerate(nts):
                        S.op("pe", "matmul", pM[mc][:, j * 128:(j + 1) * 128], lhsT=PcT[nt][:, j * 128:(j + 1) * 128],
                             rhs=VcO[:, nt, g, :], start=(i_ == 0), stop=(i_ == len(nts) - 1),
                             R=[b_PcT[nt], b_VcO], W=[bpM[mc]])
                yield
                pc = pM[mc][:].rearrange("p (j c) -> p j c", j=4)
                S.op("dve", "tensor_reduce", st[:, 24:28], pc[:, :, 64:128], AX.X, ALU.add, R=[bpM[mc]], W=[b_stn])
                S.op("dve", "tensor_scalar", st[:, 24:28], st[:, 24:28], 1e-30, None, ALU.max, R=[b_stn], W=[b_stn])
                S.op("dve", "reciprocal", st[:, 28:32], st[:, 24:28], R=[b_stn], W=[b_stn])
                S.op("dve", "tensor_tensor", tmp4[:], pc[:, :, 64:128], st[:, 28:32, None].to_broadcast([128, 4, 64]), ALU.mult,
                     R=[bpM[mc], b_stn], W=[b_tmp4])
                S.op("dve", "tensor_reduce", imp[:], tmp4[:].rearrange("p j c -> p c j"), AX.X, ALU.add, R=[b_tmp4], W=[b_imp])
                yield
                S.op("dve", "tensor_tensor", st[:, 32:36], st[:, 28:32], gate[:, 0, g * 4:(g + 1) * 4], ALU.mult,
                     R=[b_stn, b_gate], W=[b_stn])
                S.op("dve", "tensor_tensor", y_sb[:, 512 + g * 256:768 + g * 256].rearrange("p (j c) -> p j c", j=4), pc[:, :, 0:64],
                     st[:, 32:36, None].to_broadcast([128, 4, 64]), ALU.mult, R=[bpM[mc], b_stn], W=[b_yn])
                yield
                S.op("pool", "affine_select", sc1[:], imp[:], [[-64, 64]], ALU.is_ge, 1e9, base=128 * t - 128, channel_multiplier=1,
                     R=[b_imp], W=[b_imp])
                S.op("pool", "affine_select", sc2[:], sc1[:], [[-64, 64]], ALU.is_ge, -1e9, base=128 * t, channel_multiplier=1,
                     R=[b_imp], W=[b_imp])
                S.op("pool", "memset", sc2[:, 0:1], 1e9, R=[b_imp], W=[b_imp])
                yield
                S.op("dve", "max", st[:, 40:48], sc2[:], R=[b_imp], W=[b_stn])
                S.op("dve", "match_replace", sc3[:], st[:, 40:48], sc2[:], -3e38, R=[b_imp, b_stn], W=[b_imp])
                S.op("dve", "max", st[:, 48:56], sc3[:], R=[b_imp], W=[b_stn])
                S.op("dve", "tensor_scalar", selq[:, 64:128], sc2[:], st[:, 55:56], NEG, ALU.is_lt, ALU.mult,
                     R=[b_imp, b_stn], W=[b_selq])
                yield
                ti = nxt("T", 2)
                transpose_to(ti, 0, selq[:], [b_selq])
                S.op("dve", "tensor_copy", QS[64:128, g], pT[ti][64:128, None, 0:128].to_broadcast([64, 4, 128]),
                     R=[bpT[ti]], W=[b_QSs[g]])
                yield

            def nsa_attn(s, t, g):
                par = t % 2
                QpeT, b_QpeT = QpeT2[par], b_QpeT2[par]
                QabsT, b_Qabs = QabsT2[par], b_Qabs2[par]
                QS, b_QSq, b_QSs = QS2[par], b_QSq2[par], b_QSs2[par]
                gate, b_gate = gate2[par], b_gate2[par]
                QSg = QS[:, g].rearrange("p j n -> p (j n)")
                S.op("dve", "memset", pA[0][:, 0:260], 0.0, W=[bpA[0]])
                S.op("dve", "memset", pA[1][:, 0:260], 0.0, W=[bpA[1]])
                def qk_s(kt):
                    si = nxt("S", 2)
                    S.op("pe", "matmul", pS[si][:], lhsT=KsE[:, g, kt * 128:(kt + 1) * 128], rhs=QSg,
                         start=True, stop=True, R=[bKs[kt], b_exp, b_QSq, b_QSs[g]], W=[bpS[si]])
                    return si

                def post_s(kt, si):
                    pi = nxt("P", 3)
                    S.op("act", "activation", PT[pi][:], pS[si][:], AF.Exp, R=[bpS[si]], W=[b_PT[pi]])
                    if kt == t:
                        S.op("dve", "tensor_tensor", PT[pi][:], PT[pi][:], tri4[:], ALU.mult, R=[b_PT[pi], b_msk], W=[b_PT[pi]])
                    for j in range(4):
                        S.op("pe", "matmul", pA[0][:, j * 65:j * 65 + 65], lhsT=PT[pi][:, j * 128:(j + 1) * 128],
                             rhs=Vs[:, kt, g, :], start=False, stop=(kt == t), skip_group_check=True,
                             R=[b_PT[pi], bVs[kt]], W=[bpA[0]])

                def qk_w(kt):
                    si = nxt("S", 2)
                    sl = kt % 8
                    S.op("pe", "matmul", pS[si][:], lhsT=KwT[:, g, sl * 128:(sl + 1) * 128], rhs=QSg,
                         start=True, stop=True, R=[bKw[sl], b_QSq, b_QSs[g]], W=[bpS[si]])
                    return si

                def post_w(kt, si):
                    sl = kt % 8
                    pi = nxt("P", 3)
                    S.op("act", "activation", PT[pi][:], pS[si][:], AF.Exp, R=[bpS[si]], W=[b_PT[pi]])
                    if kt == t:
                        S.op("dve", "tensor_tensor", PT[pi][:], PT[pi][:], tri4[:], ALU.mult, R=[b_PT[pi], b_msk], W=[b_PT[pi]])
                    if kt == t - 4:
                        S.op("dve", "tensor_tensor", PT[pi][:], PT[pi][:], anti4[:], ALU.mult, R=[b_PT[pi], b_msk], W=[b_PT[pi]])
                    for j in range(4):
                        S.op("pe", "matmul", pA[1][:, j * 65:j * 65 + 65], lhsT=PT[pi][:, j * 128:(j + 1) * 128],
                             rhs=Vw[:, sl, g, :], start=False, stop=(kt == t), skip_group_check=True,
                             R=[b_PT[pi], bVw[sl]], W=[bpA[1]])

                attn_loop(list(range(t + 1)), qk_s, post_s)
                attn_loop(list(range(max(0, t - 4), t + 1)), qk_w, post_w)
                for br in range(2):
                    pa = pA[br][:, 0:260].rearrange("p (j c) -> p j c", j=4)
                    S.op("dve", "reciprocal", st[:, 56:60], pa[:, :, 64], R=[bpA[br]], W=[b_stn])
                    S.op("dve", "tensor_tensor", st[:, 60:64], st[:, 56:60], gate[:, 1 + br, g * 4:(g + 1) * 4], ALU.mult,
                         R=[b_stn, b_gate], W=[b_stn])
                    yv = y_sb[:, 512 + g * 256:768 + g * 256].rearrange("p (j c) -> p j c", j=4)
                    S.op("dve", "tensor_tensor", tmp4[:], pa[:, :, 0:64], st[:, 60:64, None].to_broadcast([128, 4, 64]), ALU.mult,
                         R=[bpA[br], b_stn], W=[b_tmp4])
                    S.op("dve", "tensor_tensor", yv, yv, tmp4[:], ALU.add, R=[b_tmp4, b_yn], W=[b_yn])


            def outproj(s, t):
                xb = t % 2
                ra, rb_ = rstd_multi([(y_sb[:, 0:512], 512), (y_sb[:, 512:1024], 512)], 12, [b_y, b_yn], mixed, b_mixed)
                S.op("dve", "tensor_scalar", mixed[:, 0:512], y_sb[:, 0:512], ra, None, ALU.mult, R=[b_y, b_stc[12]], W=[b_mixed])
                S.op("dve", "tensor_scalar", mixed[:, 512:1024], y_sb[:, 512:1024], rb_, None, ALU.mult, R=[b_yn, b_stc[12]], W=[b_mixed])
                ti = nxt("T", 2)
                for c in range(8):
                    transpose_to(ti, c * 128, mixed[:, c * 128:(c + 1) * 128], [b_mixed])
                S.op("dve", "tensor_copy", mixedT[:], pT[ti][:, 0:1024].rearrange("p (c n) -> p c n", c=8), R=[bpT[ti]], W=[b_mixedT])
                for dh in range(2):
                    mi = nxt("S", 2)
                    for c in range(8):
                        S.op("pe", "matmul", pS[mi][:], lhsT=mixedT[:, c, :], rhs=w_o[:, c, dh * 512:(dh + 1) * 512],
                             start=(c == 0), stop=(c == 7), R=[b_mixedT, b_wo], W=[bpS[mi]])
                    S.op("dve", "tensor_tensor", h_sb[xb][:, dh * 512:(dh + 1) * 512], pS[mi][:], xs[xb][:, dh * 512:(dh + 1) * 512],
                         ALU.add, R=[bpS[mi], b_xs[xb]], W=[b_h[xb]])
                row = (s * SEQ + t * 128)
                S.dma("sp", hscr_d[row:row + 128, :], h_sb[xb][:], R=[b_h[xb]])

            bgst = {"gen": None, "credit": 0.0, "rate": 0.0}

            def bg_run(n=None):
                if bgst["gen"] is None:
                    return
                mode["bg"] = True
                try:
                    k = 0
                    while n is None or k < n:
                        next(bgst["gen"])
                        k += 1
                except StopIteration:
                    bgst["gen"] = None
                mode["bg"] = False

            def tick():
                if mode["bg"] or bgst["gen"] is None:
                    return
                bgst["credit"] += bgst["rate"]
                if bgst["credit"] >= 1.0:
                    n = int(bgst["credit"])
                    bgst["credit"] -= n
                    bg_run(n)

            S.tick = tick
            def chain(*gens):
                for g_ in gens:
                    yield from g_

            def set_bg(gen, nchunks, fg_ops):
                bgst["gen"] = gen
                bgst["credit"] = 0.0
                bgst["rate"] = nchunks / (0.7 * fg_ops)

            for s in range(nseq):
                bgst["gen"] = phase1(s, 0) if "p1" in stages else None
                bg_run(None)
                for t in range(ntiles):
                    if "nsa" in stages:
                        set_bg(chain(nsa_sel(s, t, 0), nsa_sel(s, t, 1)), 16.0, 30.0 + 18.0 * (t + 1))
                    if "mla" in stages:
                        mla(s, t)
                    bg_run(None)
                    if t + 1 < ntiles and "p1" in stages:
                        set_bg(phase1(s, t + 1), 40.0, 100.0 + 14.0 * (t + 1 + min(t + 1, 5)))
                    if "nsa" in stages:
                        nsa_attn(s, t, 0)
                        nsa_attn(s, t, 1)
                    if "out" in stages:
                        outproj(s, t)
                    bg_run(None)
            S.tick = None
            S.barrier()
        mode["B"] = True
        B = ExitStack()
        with B:
            def sb2(name, shape, dt):
                return B.enter_context(nc.sbuf_tensor("b_" + name, shape, dt))
            gb = sb2("gb", [128, 8], F32); b_gb = S.buf("gb")
            S.dma("sp", gb[:], g_mlp_d, W=[b_gb])
            gfin = sb2("gfin", [128, D], F32)
            S.dma("sp", gfin[:], gfin_d, W=[b_gb])
            ident2 = sb2("ident2", [128, 128], BF16); b_id2 = S.buf("ident2")
            S.dma("pool", ident2[:], ident_d, W=[b_id2])
            w_up = sb2("w_up", [128, 8, DFF], BF16); b_wup = S.buf("w_up")
            for c in range(8):
                S.dma("pool", w_up[:, c, :], w_up_d[c * 128:(c + 1) * 128, :], W=[b_wup])
                S.op("dve", "tensor_scalar", w_up[:, c, :], w_up[:, c, :], gb[:, c:c + 1], None, ALU.mult, R=[b_gb, b_wup], W=[b_wup])
            w_dn = sb2("w_dn", [128, 32, D], BF16); b_wdn = S.buf("w_dn")
            wdv = w_down_d.rearrange("(f p) n -> p f n", p=128)
            for f4 in range(8):
                S.dma("pool", w_dn[:, f4 * 4:(f4 + 1) * 4, :], wdv[:, f4 * 4:(f4 + 1) * 4, :], W=[b_wdn])
            hin = [sb2("hin%d" % i, [128, D], F32) for i in range(4)]; b_hin = S.bufs("hin", 4)
            st2 = sb2("st2", [128, 16], F32); b_st2 = S.buf("st2")
            junk2 = sb2("junk2", [128, D], BF16); b_junk2 = S.buf("junk2")
            hn = sb2("hn", [128, D], BF16); b_hn = S.buf("hn")
            hnT = sb2("hnT", [128, 8, 512], BF16); b_hnT = S.buf("hnT")
            rl = [sb2("rl%d" % i, [128, 512], BF16) for i in range(2)]; b_rl = S.bufs("rl", 2)
            aT = sb2("aT", [128, 32, 512], BF16); b_aT = S.buf("aT")
            yo = [sb2("yo%d" % i, [128, D], F32) for i in range(2)]; b_yo = S.bufs("yo", 2)
            S.op("pool", "memset", st2[:], 0.0, W=[b_st2])
            nT = (nseq * ntiles * 128) // 512 if do_mlp else 0

            def rstd2(src_ap, Rb):
                S.op("pool", "memset", st2[:, 0:1], 0.0, W=[b_st2])
                S.op("act", "activation", junk2[:], src_ap, AF.Square, accum_out=st2[:, 0:1], R=Rb, W=[b_junk2, b_st2])
                S.op("dve", "tensor_scalar", st2[:, 1:2], st2[:, 0:1], 1.0 / D, EPS, ALU.mult, ALU.add, R=[b_st2], W=[b_st2])
                S.op("act", "activation", st2[:, 2:3], st2[:, 1:2], AF.Sqrt, R=[b_st2], W=[b_st2])
                S.op("dve", "reciprocal", st2[:, 3:4], st2[:, 2:3], R=[b_st2], W=[b_st2])
                return st2[:, 3:4]

            oc = 0
            for T in range(nT):
                hb = T % 2
                row = T * 512 if nseq * ntiles * 128 == nseq * SEQ else None
                base = (T * 512 // (ntiles * 128)) * SEQ + (T * 512) % (ntiles * 128)
                for i in range(4):
                    S.dma("sp", hin[i][:], hscr_d[base + i * 128:base + (i + 1) * 128, :], W=[b_hin[i]])
                for i in range(4):
                    r = rstd2(hin[i][:], [b_hin[i]])
                    S.op("dve", "tensor_scalar", hn[:], hin[i][:], r, None, ALU.mult, R=[b_hin[i], b_st2], W=[b_hn])
                    ti = nxt("T", 2)
                    for c in range(8):
                        S.op("pe", "transpose", pT[ti][:, c * 128:(c + 1) * 128], hn[:, c * 128:(c + 1) * 128], ident2[:],
                             R=[b_hn, b_id2], W=[bpT[ti]])
                    S.op("act", "copy", hnT[:, :, i * 128:(i + 1) * 128], pT[ti][:, 0:1024].rearrange("p (c n) -> p c n", c=8),
                         R=[bpT[ti]], W=[b_hnT])
                for f in range(32):
                    si = nxt("S", 2)
                    for c in range(8):
                        S.op("pe", "matmul", pS[si][:], lhsT=w_up[:, c, f * 128:(f + 1) * 128], rhs=hnT[:, c, :],
                             start=(c == 0), stop=(c == 7), R=[b_wup, b_hnT], W=[bpS[si]])
                    ri = f % 2
                    S.op("act", "activation", rl[ri][:], pS[si][:], AF.Relu, R=[bpS[si]], W=[b_rl[ri]])
                    S.op("pool", "tensor_tensor", aT[:, f, :], rl[ri][:], rl[ri][:], ALU.mult, R=[b_rl[ri]], W=[b_aT])
                for i in range(4):
                    ob = oc % 2
                    oc += 1
                    for dh in range(2):
                        mi = nxt("M", 2)
                        for f in range(32):
                            S.op("pe", "matmul", pM[mi][:], lhsT=aT[:, f, i * 128:(i + 1) * 128], rhs=w_dn[:, f, dh * 512:(dh + 1) * 512],
                                 start=(f == 0), stop=(f == 31), R=[b_aT, b_wdn], W=[bpM[mi]])
                        S.op("dve", "tensor_tensor", yo[ob][:, dh * 512:(dh + 1) * 512], pM[mi][:], hin[i][:, dh * 512:(dh + 1) * 512],
                             ALU.add, R=[bpM[mi], b_hin[i]], W=[b_yo[ob]])
                    r = rstd2(yo[ob][:], [b_yo[ob]])
                    S.op("dve", "scalar_tensor_tensor", yo[ob][:], yo[ob][:], r, gfin[:], ALU.mult, ALU.mult,
                         R=[b_yo[ob], b_st2, b_gb], W=[b_yo[ob]])
                    S.dma("sp", out_d[base + i * 128:base + (i + 1) * 128, :], yo[ob][:], R=[b_yo[ob]])
            S.emit()
            print('NOPS', S.nops)
    return nc


def _rope_tab(pos, dim):
    inv = np.exp(np.float32(-math.log(500000.0)) * np.arange(0, dim, 2, dtype=np.float32) / np.float32(dim)).astype(np.float32)
    ang = pos.astype(np.float32)[:, None] * inv[None, :]
    return np.cos(ang).astype(np.float32), np.sin(ang).astype(np.float32)


def _tok_major(a):
    return np.ascontiguousarray(a.reshape(NT, 128, -1).transpose(1, 0, 2))


def host_consts():
    pos = np.arange(SEQ)
    cm, sm = _rope_tab(pos, 32)
    cn, sn = _rope_tab(pos, 16)
    c = {}
    c["cos_m"], c["sin_m"] = _tok_major(cm), _tok_major(sm)
    c["cos_n"], c["sin_n"] = _tok_major(cn), _tok_major(sn)
    c["cos_n8"], c["sin_n8"] = _tok_major(cn * np.float32(0.125)), _tok_major(sn * np.float32(0.125))
    ce = np.zeros((8, NT, 8), np.float32)
    se = np.zeros((8, NT, 8), np.float32)
    for t in range(NT):
        for m in range(8):
            n = 8 * t - 1 + m
            if 0 <= n < 255:
                p = 16 * n + 31
                ce[m, t], se[m, t] = cn[p], sn[p]
    c["cos_e"], c["sin_e"] = ce, se
    k = np.arange(128)[:, None]
    q = np.arange(128)[None, :]
    tri = (q >= k).astype(np.float32)
    anti = (q < k).astype(np.float32)
    c["tri4"] = np.ascontiguousarray(np.tile(tri, (1, 4)))
    c["anti4"] = np.ascontiguousarray(np.tile(anti, (1, 4)))
    n = np.arange(256)[:, None] * 16
    j = np.arange(64)[None, :] * 64
    ov = np.clip(np.minimum(n + 32, j + 64) - np.maximum(n, j), 0, None).astype(np.float32) / 32.0
    ov[255] = 0.0
    c["ovl"] = np.ascontiguousarray(ov.reshape(2, 128, 64).transpose(1, 0, 2))
    c["expand"] = (np.arange(SEQ)[None, :] // 64 == np.arange(64)[:, None]).astype(np.float32)
    c["ident"] = np.eye(128, dtype=np.float32)
    return c


def host_weights(inp):
    f = lambda a: np.ascontiguousarray(np.asarray(a, dtype=np.float32))
    pc = lambda v: np.ascontiguousarray(np.asarray(v, np.float32).reshape(-1, 128).T)
    w = {}
    w["w_in"] = f(inp["w_in"][0])
    w["g_mix"] = pc(inp["g_mix_norm"][0])
    w["w_uq"] = f(inp["w_uq"][0])
    w["g_cq"] = pc(inp["g_cq"][0])
    wukv = np.asarray(inp["w_ukv"][0], np.float32).reshape(128, 8, 2, 64)
    wuk = wukv[:, :, 0, :]
    w["wukT"] = np.ascontiguousarray(wuk.transpose(2, 1, 0))
    w["wuv"] = np.ascontiguousarray(wukv[:, :, 1, :])
    w["gckv_bc"] = np.ascontiguousarray(np.broadcast_to(np.asarray(inp["g_ckv"][0], np.float32)[None, :], (128, 128)))
    w1 = np.stack([np.asarray(inp["cmp_w1_k"][0], np.float32), np.asarray(inp["cmp_w1_v"][0], np.float32)])
    w["cmp_w1"] = np.ascontiguousarray(w1.reshape(2, 32, 64, 128).transpose(0, 2, 1, 3))
    pe = np.stack([np.asarray(inp["cmp_pe_k"][0], np.float32), np.asarray(inp["cmp_pe_v"][0], np.float32)])
    w["cmp_peT"] = np.ascontiguousarray(pe.transpose(0, 2, 1))
    w["cmp_b1"] = np.ascontiguousarray(np.stack([inp["cmp_b1_k"][0], inp["cmp_b1_v"][0]], axis=1).astype(np.float32))
    w["cmp_w2"] = np.ascontiguousarray(np.stack([inp["cmp_w2_k"][0], inp["cmp_w2_v"][0]], axis=1).astype(np.float32))
    b2k = np.asarray(inp["cmp_b2_k"][0], np.float32)
    w["cmp_b2k_bc"] = np.ascontiguousarray(np.broadcast_to(np.concatenate([b2k, b2k])[None, :], (128, 128)))
    w["cmp_b2v"] = np.ascontiguousarray(np.asarray(inp["cmp_b2_v"][0], np.float32).reshape(64, 1))
    w["w_o"] = f(inp["w_o"][0])
    w["g_out"] = pc(np.concatenate([np.asarray(inp["g_out_mla"][0]), np.asarray(inp["g_out_nsa"][0])]))
    w["w_up"] = f(inp["w_up"][0])
    w["g_mlp"] = pc(inp["g_mlp_norm"][0])
    w["w_down"] = f(inp["w_down"][0])
    w["gfin_bc"] = np.ascontiguousarray(np.broadcast_to(np.asarray(inp["g_final"], np.float32)[None, :], (128, D)))
    return w


_NC_CACHE = {}


def kernel(**inputs):
    x = np.asarray(inputs["x"], dtype=np.float32)
    shared = host_consts()
    shared.update(host_weights(inputs))
    if "full" not in _NC_CACHE:
        _NC_CACHE["full"] = build_program()
    nc = _NC_CACHE["full"]
    in_maps = []
    for c in range(NCORES):
        m = dict(shared)
        m["x"] = np.ascontiguousarray(x[c * NSEQ:(c + 1) * NSEQ])
        in_maps.append(m)
    res = run_bass_kernel_spmd(nc, in_maps, core_ids=list(range(NCORES)))
    outs = [np.asarray(r["out"]).reshape(NSEQ, SEQ, D) for r in res.results]
    return np.concatenate(outs, axis=0).astype(np.float32)
```

```python
import math
import numpy as np
from contextlib import ExitStack
import concourse.bass as bass
import concourse.mybir as mybir
from concourse.bass_utils import run_bass_kernel_spmd

F32 = mybir.dt.float32
BF16 = mybir.dt.bfloat16
I32 = mybir.dt.int32
AF = mybir.ActivationFunctionType
ALU = mybir.AluOpType
AX = mybir.AxisListType

NCORES = 8
SEQ = 4096
D = 1024
NSEQ = 2
NT = SEQ // 128
EPS = 1e-6
IN_COLS = 1720
DFF = 4096
NEG = -30000.0
STRICT_SAME = True
OP_LIMIT = None
BG_OVERLAP = True
USE_MAGIC = True
ACT_COPY_T = 20


class Buf:
    __slots__ = ("name", "w", "r", "dsem", "dcnt", "excl")

    def __init__(self, name):
        self.name = name
        self.excl = False
        self.w = None
        self.r = {}
        self.dsem = None
        self.dcnt = 0


class Sched:
    ENG = ("pe", "act", "dve", "pool", "sp")

    def __init__(self, nc, ctx):
        self.nc = nc
        self.ctx = ctx
        self.sem = {e: ctx.enter_context(nc.semaphore("s_" + e)) for e in self.ENG}
        self.cnt = {e: 0 for e in self.ENG}
        self.seen = {e: {} for e in self.ENG}
        self.prog = {e: [] for e in self.ENG}
        self.dbufs = []
        self.nb = 0
        self.nops = 0
        self.fillregs = {}
        self.tick = None
        self.limit = OP_LIMIT

    def buf(self, name):
        self.nb += 1
        return Buf("%s_%d" % (name, self.nb))

    def bufs(self, name, n):
        return [self.buf(name) for _ in range(n)]

    def _dsem(self, b):
        if b.dsem is None:
            b.dsem = self.ctx.enter_context(self.nc.semaphore("d_" + b.name))
            self.dbufs.append(b)
        return b.dsem

    def _deps(self, e, reads, writes, strict):
        toks = []
        for b in reads:
            if b.w is not None:
                toks.append(b.w)
            if b.excl:
                toks.extend(b.r.values())
        for b in writes:
            if b.w is not None:
                toks.append(b.w)
            toks.extend(b.r.values())
        need = {}
        for (key, sem, val) in toks:
            if key == e and not strict and (e == "pe" or not STRICT_SAME):
                continue
            if self.seen[e].get(key, 0) >= val:
                continue
            if key not in need or need[key][1] < val:
                need[key] = (sem, val)
        for key, (sem, val) in need.items():
            self.seen[e][key] = val
            self.prog[e].append(("wait", sem, val))

    def op(self, e, meth, *args, R=(), W=(), **kw):
        self.nops += 1
        if self.limit is not None and self.nops > self.limit:
            return None
        self._deps(e, R, W, False)
        self.cnt[e] += 1
        tok = (e, self.sem[e], self.cnt[e])
        self.prog[e].append(("op", meth, args, kw))
        for b in R:
            b.r[e] = tok
        for b in W:
            b.w = tok
            b.r = {}
        if self.tick is not None:
            self.tick()
        return tok

    def dma(self, q, out, in_, R=(), W=(), **kw):
        self.nops += 1
        if self.limit is not None and self.nops > self.limit:
            return None
        self._deps(q, R, W, True)
        owner = W[0] if W else R[0]
        sem = self._dsem(owner)
        owner.dcnt += 16
        tok = ("d_" + owner.name, sem, owner.dcnt)
        self.prog[q].append(("dma", out, in_, kw, sem))
        for b in R:
            b.r[tok[0]] = tok
        for b in W:
            b.w = tok
            b.r = {}
        return tok

    def barrier(self):
        toks = [(e, self.sem[e], self.cnt[e]) for e in self.ENG if self.cnt[e] > 0]
        toks += [("d_" + b.name, b.dsem, b.dcnt) for b in self.dbufs if b.dcnt > 0]
        for e in self.ENG:
            for (key, sem, val) in toks:
                if self.seen[e].get(key, 0) >= val:
                    continue
                self.seen[e][key] = val
                self.prog[e].append(("wait", sem, val))

    def flush(self):
        nc = self.nc
        with nc.Block() as block:
            def replay(e):
                def f(eng):
                    sem_e = self.sem[e]
                    for it in self.prog[e]:
                        if it[0] == "wait":
                            eng.wait_ge(it[1], it[2])
                        elif it[0] == "op":
                            args = it[2]
                            if it[1] == "affine_select":
                                args = list(args)
                                if args[4] not in self.fillregs:
                                    self.fillregs[args[4]] = eng.to_reg(args[4])
                                args[4] = self.fillregs[args[4]]
                            getattr(eng, it[1])(*args, **it[3]).then_inc(sem_e, 1)
                        else:
                            eng.dma_start(out=it[1], in_=it[2], **it[3]).then_inc(it[4], 16)
                return f
            block.tensor(replay("pe"))
            block.scalar(replay("act"))
            block.vector(replay("dve"))
            block.gpsimd(replay("pool"))
            block.sync(replay("sp"))
        self.prog = {e: [] for e in self.ENG}

    def emit(self):
        self.barrier()
        self.flush()


def build_program(nseq=NSEQ, ntiles=NT, do_mlp=True, stages=("p1", "mla", "nsa", "out")):
    nc = bass.Bass("TRN2", target_bir_lowering=False)

    def din(name, shape):
        return nc.dram_tensor(name, list(shape), F32, kind="ExternalInput").ap()

    x_d = din("x", [nseq, SEQ, D])
    w_in_d = din("w_in", [D, IN_COLS])
    g_mix_d = din("g_mix", [128, 8])
    w_uq_d = din("w_uq", [256, 768])
    g_cq_d = din("g_cq", [128, 2])
    wukT_d = din("wukT", [64, 8, 128])
    wuv_d = din("wuv", [128, 8, 64])
    gckv_d = din("gckv_bc", [128, 128])
    w1_d = din("cmp_w1", [2, 64, 32, 128])
    peT_d = din("cmp_peT", [2, 64, 32])
    b1_d = din("cmp_b1", [128, 2])
    w2_d = din("cmp_w2", [128, 2, 64])
    b2k_d = din("cmp_b2k_bc", [128, 128])
    b2v_d = din("cmp_b2v", [64, 1])
    w_o_d = din("w_o", [D, D])
    g_out_d = din("g_out", [128, 8])
    w_up_d = din("w_up", [D, DFF])
    g_mlp_d = din("g_mlp", [128, 8])
    w_down_d = din("w_down", [DFF, D])
    gfin_d = din("gfin_bc", [128, D])
    cosm_d = din("cos_m", [128, NT, 16])
    sinm_d = din("sin_m", [128, NT, 16])
    cosn_d = din("cos_n", [128, NT, 8])
    sinn_d = din("sin_n", [128, NT, 8])
    cosn8_d = din("cos_n8", [128, NT, 8])
    sinn8_d = din("sin_n8", [128, NT, 8])
    cose_d = din("cos_e", [8, NT, 8])
    sine_d = din("sin_e", [8, NT, 8])
    tri_d = din("tri4", [128, 512])
    anti_d = din("anti4", [128, 512])
    ovl_d = din("ovl", [128, 2, 64])
    exp_d = din("expand", [64, SEQ])
    ident_d = din("ident", [128, 128])
    out_d = nc.dram_tensor("out", [nseq * SEQ, D], F32, kind="ExternalOutput").ap()
    hscr_d = nc.dram_tensor("hscr", [nseq * SEQ, D], F32, kind="Internal").ap()

    top = ExitStack()
    with top:
        S = Sched(nc, top)
        def psum(name, shape, dt):
            return top.enter_context(nc.psum_tensor(name, shape, dt))
        pS = [psum("pS%d" % i, [128, 512], F32) for i in range(2)]
        pA = [psum("pA%d" % i, [128, 512], F32) for i in range(2)]
        pM = [psum("pM%d" % i, [128, 512], F32) for i in range(2)]
        pT = [psum("pT%d" % i, [128, 1024], BF16) for i in range(2)]
        bpS = S.bufs("pS", 2)
        bpA = S.bufs("pA", 2)
        bpM = S.bufs("pM", 2)
        bpT = S.bufs("pT", 2)
        for b_ in bpS + bpA + bpM + bpT:
            b_.excl = True
        rr = {"S": 0, "M": 0, "T": 0, "P": 0, "A": 0}
        mode = {"bg": False, "B": False}

        def nxt(kind, n):
            if kind in ("M", "T") and not mode["B"]:
                return 0 if mode["bg"] else 1
            i = rr[kind] % n
            rr[kind] += 1
            return i

        A = ExitStack()
        with A:
            def sb(name, shape, dt):
                return A.enter_context(nc.sbuf_tensor("a_" + name, shape, dt))

            ident = sb("ident", [128, 128], BF16); b_ident = S.buf("ident")
            S.dma("pool", ident[:], ident_d, W=[b_ident])
            tri4 = sb("tri4", [128, 512], BF16); anti4 = sb("anti4", [128, 512], BF16); b_msk = S.buf("msk")
            S.dma("pool", tri4[:], tri_d, W=[b_msk])
            S.dma("pool", anti4[:], anti_d, W=[b_msk])
            tabs = {}
            b_tab = S.buf("tab")
            for nm, d_, w_ in (("cos_m", cosm_d, 16), ("sin_m", sinm_d, 16), ("cos_n", cosn_d, 8), ("sin_n", sinn_d, 8)):
                tabs[nm] = sb(nm, [128, NT, w_], F32)
                S.dma("sp", tabs[nm][:], d_, W=[b_tab])
            cos_e = sb("cos_e", [8, NT, 8], F32); sin_e = sb("sin_e", [8, NT, 8], F32)
            S.dma("sp", cos_e[:], cose_d, W=[b_tab])
            S.dma("sp", sin_e[:], sine_d, W=[b_tab])
            gckv = sb("gckv", [128, 128], F32); b2k = sb("b2k", [128, 128], F32); b2v = sb("b2v", [64, 1], F32)
            b1 = sb("b1", [128, 2], F32)
            S.dma("sp", gckv[:], gckv_d, W=[b_tab])
            S.dma("sp", b2k[:], b2k_d, W=[b_tab])
            S.dma("sp", b2v[:], b2v_d, W=[b_tab])
            S.dma("sp", b1[:], b1_d, W=[b_tab])
            gvec = sb("gvec", [128, 24], F32)
            S.dma("sp", gvec[:, 0:8], g_mix_d, W=[b_tab])
            S.dma("sp", gvec[:, 8:10], g_cq_d, W=[b_tab])
            S.dma("sp", gvec[:, 10:18], g_out_d, W=[b_tab])

            w_in = sb("w_in", [128, 8, IN_COLS], BF16); b_win = S.buf("w_in")
            S.dma("pool", w_in[:], w_in_d.rearrange("(c p) n -> p c n", p=128), W=[b_win])
            for c in range(8):
                S.op("dve", "tensor_scalar", w_in[:, c, :], w_in[:, c, :], gvec[:, c:c + 1], None, ALU.mult,
                     R=[b_tab, b_win], W=[b_win])
                S.op("dve", "tensor_scalar", w_in[:, c, 416:928], w_in[:, c, 416:928], 0.125, None, ALU.mult,
                     R=[b_win], W=[b_win])
            w_o = sb("w_o", [128, 8, D], BF16); b_wo = S.buf("w_o")
            S.dma("pool", w_o[:], w_o_d.rearrange("(c p) n -> p c n", p=128), W=[b_wo])
            for c in range(8):
                S.op("dve", "tensor_scalar", w_o[:, c, :], w_o[:, c, :], gvec[:, 10 + c:11 + c], None, ALU.mult,
                     R=[b_tab, b_wo], W=[b_wo])
            w_uq = sb("w_uq", [128, 2, 768], BF16); b_wuq = S.buf("w_uq")
            S.dma("pool", w_uq[:], w_uq_d.rearrange("(c p) n -> p c n", p=128), W=[b_wuq])
            for c in range(2):
                S.op("dve", "tensor_scalar", w_uq[:, c, :], w_uq[:, c, :], gvec[:, 8 + c:9 + c], 96.0 ** -0.5,
                     ALU.mult, ALU.mult, R=[b_tab, b_wuq], W=[b_wuq])
            wukT = sb("wukT", [64, 8, 128], BF16); wuv = sb("wuv", [128, 8, 64], BF16); b_wkv = S.buf("wkv")
            S.dma("pool", wukT[:], wukT_d, W=[b_wkv])
            S.dma("pool", wuv[:], wuv_d, W=[b_wkv])
            w1 = sb("w1", [64, 2, 32, 128], BF16); b_w1 = S.buf("w1")
            for kv in range(2):
                S.dma("pool", w1[:, kv], w1_d[kv], W=[b_w1])
            peT = sb("peT", [64, 2, 32], BF16)
            for kv in range(2):
                S.dma("pool", peT[:, kv], peT_d[kv], W=[b_w1])
            w2 = sb("w2", [128, 2, 64], BF16)
            S.dma("pool", w2[:], w2_d, W=[b_w1])

            bias_tot = sb("bias_tot", [128, 2], F32); b_bt = S.buf("bias_tot")
            for kv in range(2):
                for l in range(32):
                    S.op("pe", "matmul", pM[0][:, kv:kv + 1], lhsT=w1[:, kv, l, :], rhs=peT[:, kv, l:l + 1],
                         start=(l == 0), stop=(l == 31), R=[b_w1], W=[bpM[0]])
            S.op("dve", "tensor_tensor", bias_tot[:], pM[0][:, 0:2], b1[:], ALU.add, R=[bpM[0], b_tab], W=[b_bt])

            KlatT = sb("KlatT", [128, SEQ], BF16); bKlat = S.bufs("Klat", NT)
            KpeT = sb("KpeT", [128, SEQ], BF16); bKpe = S.bufs("Kpe", NT)
            Clat = sb("Clat", [128, NT, 128], BF16); bClat = S.bufs("Clat", NT)
            KsE = sb("KsE", [128, 2, SEQ], BF16); bKs = S.bufs("Ks", NT); b_exp = S.buf("expand")
            Vs = sb("Vs", [128, NT, 2, 65], BF16); bVs = S.bufs("Vs", NT)
            KwT = sb("KwT", [128, 2, 8 * 128], BF16); bKw = S.bufs("Kw", 8)
            Vw = sb("Vw", [128, 8, 2, 65], BF16); bVw = S.bufs("Vw", 8)
            KcT = sb("KcT", [64, 2, 256], BF16); b_Kc = S.buf("Kc")
            VcT = sb("VcT", [64, 2, 256], BF16); b_VcT = S.buf("VcT")
            VcO = sb("VcO", [128, 2, 2, 128], BF16); b_VcO = S.buf("VcO")
            for g in range(2):
                S.dma("pool", KsE[64:128, g, :], exp_d, W=[b_exp])
            for nt in range(2):
                for g in range(2):
                    S.dma("pool", VcO[:, nt, g, 64:128], ovl_d[:, nt, :], W=[b_VcO])
            S.op("pool", "memset", Vs[:, :, :, 64:65], 1.0, W=bVs)
            S.op("pool", "memset", Vw[:, :, :, 64:65], 1.0, W=bVw)

            xs = [sb("xs%d" % i, [128, D], F32) for i in range(2)]; b_xs = S.bufs("xs", 2)
            st = sb("st", [128, 64], F32); b_st = S.buf("st")
            xn = sb("xn", [128, D], BF16); b_xn = S.buf("xn")
            xnT = sb("xnT", [128, 8, 128], BF16); b_xnT = S.buf("xnT")
            u = sb("u", [128, IN_COLS], F32); b_u = S.buf("u")
            cqn = sb("cqn", [128, 256], BF16); b_cqn = S.buf("cqn")
            cqnT = sb("cqnT", [128, 2, 128], BF16); b_cqnT = S.buf("cqnT")
            q_sb = sb("q_sb", [128, 9, 96], F32); b_q = S.buf("q")
            qn_sb = sb("qn_sb", [128, 8, 64], BF16); b_qn = S.buf("qn")
            qpe_sb = sb("qpe_sb", [128, 9, 32], BF16); b_qpe = S.buf("qpe")
            rt = [sb("rt%d" % i, [128, 160], F32) for i in range(4)]; b_rt = S.buf("rt")
            QnT = sb("QnT", [64, 8, 128], BF16); b_QnT = S.buf("QnT")
            QpeT2 = [sb("QpeT%d" % i, [128, 8, 128], BF16) for i in range(2)]; b_QpeT2 = S.bufs("QpeT", 2)
            QabsT2 = [sb("QabsT%d" % i, [128, 8, 128], BF16) for i in range(2)]; b_Qabs2 = S.bufs("Qabs", 2)
            ub = sb("ub", [128, 20, 64], BF16); b_ub = S.buf("ub")
            QS2 = [sb("QS%d" % i, [128, 2, 4, 128], BF16) for i in range(2)]; b_QSq2 = S.bufs("QSq", 2); b_QSs2 = [S.bufs("QSs", 2) for _ in range(2)]
            rawT = [sb("rawT%d" % i, [64, 2, 2, 144], BF16) for i in range(2)]; b_rawT = S.bufs("rawT", 2)
            gate2 = [sb("gate%d" % i, [128, 3, 8], F32) for i in range(2)]; b_gate2 = S.bufs("gate", 2)
            z_sb = sb("z_sb", [128, 32], F32); z2_sb = sb("z2_sb", [128, 32], F32); b_z = S.buf("z")
            hid_sb = sb("hid_sb", [128, 32], BF16); b_hid = S.buf("hid")
            kc_f = sb("kc_f", [8, 2, 64], F32); kc_sb = sb("kc_sb", [8, 2, 64], BF16); b_kc = S.buf("kc")
            PT = [sb("PT%d" % i, [128, 512], BF16) for i in range(3)]; b_PT = S.bufs("PT", 3)
            PcT = [sb("PcT%d" % i, [128, 512], BF16) for i in range(2)]; b_PcT = S.bufs("PcT", 2)
            OlatT = sb("OlatT", [128, 4, 128], BF16); b_OlatT = S.buf("OlatT")
            y_sb = sb("y_sb", [128, D], F32); b_y = S.buf("y"); b_yn = S.buf("yn")
            imp = sb("imp", [128, 64], F32); sc1 = sb("sc1", [128, 64], F32); sc2 = sb("sc2", [128, 64], F32)
            sc3 = sb("sc3", [128, 64], F32); b_imp = S.buf("imp")
            selq = sb("selq", [128, 128], BF16); b_selq = S.buf("selq")
            tmp4 = sb("tmp4", [128, 4, 64], F32); b_tmp4 = S.buf("tmp4")
            mixed = sb("mixed", [128, D], BF16); b_mixed = S.buf("mixed")
            mixedT = sb("mixedT", [128, 8, 128], BF16); b_mixedT = S.buf("mixedT")
            h_sb = [sb("h_sb%d" % i, [128, D], F32) for i in range(2)]; b_h = S.bufs("h", 2)

            S.op("pool", "memset", selq[:], 0.0, W=[b_selq])
            S.op("pool", "memset", KpeT[:], 0.0, W=bKpe)
            S.op("pool", "memset", KwT[:], 0.0, W=bKw)
            for i in range(2):
                S.op("pool", "memset", QS2[i][:], 0.0, W=[b_QSq2[i]] + b_QSs2[i])
            for i in range(2):
                S.op("pool", "memset", QpeT2[i][:], 0.0, W=[b_QpeT2[i]])
            for i in range(2):
                S.op("pool", "memset", rawT[i][:], 0.0, W=[b_rawT[i]])
            S.op("pool", "memset", KcT[:], 0.0, W=[b_Kc])
            S.op("pool", "memset", VcT[:], 0.0, W=[b_VcT])
            S.op("pool", "memset", VcO[:, :, :, 0:64], 0.0, W=[b_VcO])

            b_stc = {0: S.buf("st0"), 4: S.buf("st4"), 12: S.buf("st12")}
            b_stm = S.buf("stm")
            b_stn = S.buf("stn")
            rl = sb("rl", [128, 8], F32); b_rl = S.buf("rl")
            lacc = sb("lacc", [128, 512], F32); b_lacc = S.buf("lacc")
            ones_f = sb("ones_f", [128, 1], F32); b_ones = S.buf("ones")
            S.op("pool", "memset", ones_f[:], 1.0, W=[b_ones])

            def rstd_multi(items, col, Rb, jk, b_jk):
                b_st = b_stc[col]
                k = len(items)
                S.op("dve", "memset", st[:, col:col + k], 0.0, W=[b_st])
                for i_, (src_ap, n) in enumerate(items):
                    S.op("dve", "scalar_tensor_tensor", jk[:, 0:n], src_ap, 1.0, src_ap, ALU.mult, ALU.mult,
                         accum_out=st[:, col + i_:col + i_ + 1], R=Rb + [b_st], W=[b_jk, b_st])
                    S.op("dve", "tensor_scalar", st[:, col + k + i_:col + k + i_ + 1], st[:, col + i_:col + i_ + 1], 1.0 / n, EPS,
                         ALU.mult, ALU.add, R=[b_st], W=[b_st])
                v_ = st[:, col + k:col + 2 * k]
                y_ = st[:, col + 2 * k:col + 3 * k]
                w_ = st[:, col + 3 * k:col + 4 * k]
                if not USE_MAGIC:
                    S.op("act", "activation", w_, v_, AF.Sqrt, R=[b_st], W=[b_st])
                    S.op("dve", "reciprocal", y_, w_, R=[b_st], W=[b_st])
                    return [st[:, col + 2 * k + i_:col + 2 * k + i_ + 1] for i_ in range(k)]
                ss_ = st[:, col:col + k]
                S.op("dve", "tensor_scalar", y_.bitcast(I32), v_.bitcast(I32), -0.5, 1597463007.0, ALU.mult, ALU.add, R=[b_st], W=[b_st])
                S.op("dve", "tensor_scalar", ss_, v_, -0.5, None, ALU.mult, R=[b_st], W=[b_st])
                for _it in range(2):
                    if k == 1:
                        S.op("dve", "scalar_tensor_tensor", w_, y_, ss_, y_, ALU.mult, ALU.mult, R=[b_st], W=[b_st])
                    else:
                        S.op("dve", "tensor_tensor", w_, y_, y_, ALU.mult, R=[b_st], W=[b_st])
                        S.op("dve", "tensor_tensor", w_, w_, ss_, ALU.mult, R=[b_st], W=[b_st])
                    S.op("dve", "scalar_tensor_tensor", y_, w_, 1.5, y_, ALU.add, ALU.mult, R=[b_st], W=[b_st])
                return [st[:, col + 2 * k + i_:col + 2 * k + i_ + 1] for i_ in range(k)]

            def rope(eng, out_ap, in_ap, cos_ap, sin_ap, nh, half, Rb, Wb):
                P = in_ap.shape[0]
                x1 = in_ap[:, :, 0:half]
                x2 = in_ap[:, :, half:2 * half]
                cb = cos_ap[:, None, :].to_broadcast([P, nh, half])
                sbb = sin_ap[:, None, :].to_broadcast([P, nh, half])
                t = [r_[0:P, 0:nh * half].rearrange("p (h d) -> p h d", h=nh) for r_ in rt]
                S.op(eng, "tensor_tensor", t[0], x1, cb, ALU.mult, R=Rb + [b_tab], W=[b_rt])
                S.op(eng, "tensor_tensor", t[1], x2, sbb, ALU.mult, R=Rb + [b_tab], W=[b_rt])
                S.op(eng, "tensor_tensor", t[2], x2, cb, ALU.mult, R=Rb + [b_tab], W=[b_rt])
                S.op(eng, "tensor_tensor", t[3], x1, sbb, ALU.mult, R=Rb + [b_tab], W=[b_rt])
                S.op(eng, "tensor_tensor", out_ap[:, :, 0:half], t[0], t[1], ALU.subtract, R=[b_rt], W=Wb)
                S.op(eng, "tensor_tensor", out_ap[:, :, half:2 * half], t[2], t[3], ALU.add, R=[b_rt], W=Wb)

            def transpose_to(ps_i, col0, in_ap, Rb):
                P, Fd = in_ap.shape[0], in_ap.shape[1]
                S.op("pe", "transpose", pT[ps_i][0:Fd, col0:col0 + P], in_ap, ident[0:P, 0:P],
                     R=Rb + [b_ident], W=[bpT[ps_i]])

            def phase1(s, t):
                par = t % 2
                QpeT, b_QpeT = QpeT2[par], b_QpeT2[par]
                QabsT, b_Qabs = QabsT2[par], b_Qabs2[par]
                QS, b_QSq, b_QSs = QS2[par], b_QSq2[par], b_QSs2[par]
                gate, b_gate = gate2[par], b_gate2[par]
                xb = t % 2
                ce, cm = ("act", "copy") if t < ACT_COPY_T else ("dve", "tensor_copy")
                S.dma("sp", xs[xb][:], x_d[s, t * 128:(t + 1) * 128, :], W=[b_xs[xb]])
                r0 = rstd_multi([(xs[xb][:], D)], 0, [b_xs[xb]], xn, b_xn)[0]
                S.op("dve", "tensor_scalar", xn[:], xs[xb][:], r0, None, ALU.mult, R=[b_xs[xb], b_stc[0]], W=[b_xn])
                ti = nxt("T", 2)
                for c in range(8):
                    transpose_to(ti, c * 128, xn[:, c * 128:(c + 1) * 128], [b_xn])
                S.op(ce, cm, xnT[:], pT[ti][:, 0:1024].rearrange("p (c n) -> p c n", c=8), R=[bpT[ti]], W=[b_xnT])
                for cg, (c0, c1) in enumerate(((0, 512), (512, 1024), (1024, 1536), (1536, IN_COLS))):
                    mi = nxt("M", 2)
                    for c in range(8):
                        S.op("pe", "matmul", pM[mi][:, 0:c1 - c0], lhsT=xnT[:, c, :], rhs=w_in[:, c, c0:c1],
                             start=(c == 0), stop=(c == 7), R=[b_xnT, b_win], W=[bpM[mi]])
                    S.op(ce, cm,
                         u[:, c0:c1], pM[mi][:, 0:c1 - c0], R=[bpM[mi]], W=[b_u])
                    yield

                yield
                r1, r2 = rstd_multi([(u[:, 0:256], 256), (u[:, 256:384], 128)], 4, [b_u], cqn, b_cqn)
                S.op("dve", "tensor_scalar", cqn[:], u[:, 0:256], r1, None, ALU.mult, R=[b_u, b_stc[4]], W=[b_cqn])
                ti = nxt("T", 2)
                for c in range(2):
                    transpose_to(ti, c * 128, cqn[:, c * 128:(c + 1) * 128], [b_cqn])
                S.op("dve", "tensor_copy", cqnT[:], pT[ti][:, 0:256].rearrange("p (c n) -> p c n", c=2), R=[bpT[ti]], W=[b_cqnT])
                yield
                for half in range(2):
                    mi = nxt("M", 2)
                    for c in range(2):
                        S.op("pe", "matmul", pM[mi][:, 0:384], lhsT=cqnT[:, c, :], rhs=w_uq[:, c, half * 384:(half + 1) * 384],
                             start=(c == 0), stop=(c == 1), R=[b_cqnT, b_wuq], W=[bpM[mi]])
                    S.op(ce, cm, q_sb[:, half * 4:(half + 1) * 4, :],
                         pM[mi][:, 0:384].rearrange("p (h d) -> p h d", h=4), R=[bpM[mi]], W=[b_q])
                yield
                S.op("dve", "tensor_copy", q_sb[:, 8, 64:96], u[:, 384:416], R=[b_u], W=[b_q])
                S.op("dve", "tensor_copy", qn_sb[:], q_sb[:, 0:8, 0:64], R=[b_q], W=[b_qn])
                rope("dve", qpe_sb[:], q_sb[:, :, 64:96], tabs["cos_m"][:, t, :], tabs["sin_m"][:, t, :], 9, 16, [b_q], [b_qpe])
                ti = nxt("T", 2)
                for h in range(8):
                    transpose_to(ti, h * 128, qn_sb[:, h, :], [b_qn])
                S.op(ce, cm, QnT[:], pT[ti][0:64, 0:1024].rearrange("p (c n) -> p c n", c=8), R=[bpT[ti]], W=[b_QnT])
                ti = nxt("T", 2)
                for h in range(8):
                    transpose_to(ti, h * 128, qpe_sb[:, h, :], [b_qpe])
                S.op(ce, cm, QpeT[0:32], pT[ti][0:32, 0:1024].rearrange("p (c n) -> p c n", c=8), R=[bpT[ti]], W=[b_QpeT])
                yield
                for hg in range(2):
                    mi = nxt("M", 2)
                    for j in range(4):
                        h = hg * 4 + j
                        S.op("pe", "matmul", pM[mi][:, j * 128:(j + 1) * 128], lhsT=wukT[:, h, :],
                             rhs=QnT[:, h, :], start=True, stop=True, R=[b_wkv, b_QnT], W=[bpM[mi]])
                    S.op(ce, cm, QabsT[:, hg * 4:(hg + 1) * 4, :],
                         pM[mi][:, 0:512].rearrange("p (c n) -> p c n", c=4), R=[bpM[mi]], W=[b_Qabs])

                yield
                S.op("dve", "scalar_tensor_tensor", Clat[:, t, 0:128], u[:, 256:384], r2, gckv[:], ALU.mult, ALU.mult,
                     R=[b_u, b_stc[4], b_tab], W=[bClat[t]])
                ti = nxt("T", 2)
                transpose_to(ti, 0, Clat[:, t, 0:128], [bClat[t]])
                S.op("dve", "tensor_copy", KlatT[:, t * 128:(t + 1) * 128], pT[ti][:, 0:128], R=[bpT[ti]], W=[bKlat[t]])
                ti = nxt("T", 2)
                transpose_to(ti, 0, qpe_sb[:, 8, :], [b_qpe])
                S.op("dve", "tensor_copy", KpeT[0:32, t * 128:(t + 1) * 128], pT[ti][0:32, 0:128], R=[bpT[ti]], W=[bKpe[t]])

                yield
                uv = u[:, 416:1696].rearrange("p (b d) -> p b d", b=20)
                S.op("dve", "tensor_copy", ub[:, :, 16:64], uv[:, :, 16:64], R=[b_u], W=[b_ub])
                rope("dve", ub[:, :, 0:16], uv[:, :, 0:16], tabs["cos_n"][:, t, :], tabs["sin_n"][:, t, :], 20, 8, [b_u], [b_ub])
                S.op("dve", "tensor_copy", ub[:, 8:12, 0:16], uv[:, 8:12, 0:16], R=[b_u, b_ub], W=[b_ub])
                ti = nxt("T", 2)
                for h in range(8):
                    transpose_to(ti, h * 128, ub[:, h, :], [b_ub])
                S.op(ce, cm, QS[0:64].rearrange("p g j n -> p (g j) n"),
                     pT[ti][0:64, 0:1024].rearrange("p (c n) -> p c n", c=8), R=[bpT[ti]], W=[b_QSq])
                yield
                kvv = u[:, 928:1696].rearrange("p (s g d) -> p s g d", s=6, g=2)
                S.op("dve", "tensor_copy", Vs[:, t, :, 0:64], kvv[:, 3], R=[b_u], W=[bVs[t]])
                S.op("dve", "tensor_copy", Vw[:, t % 8, :, 0:64], kvv[:, 5], R=[b_u], W=[bVw[t % 8]])
                ti = nxt("T", 2)
                for g in range(2):
                    transpose_to(ti, g * 128, ub[:, 12 + g, :], [b_ub])
                    transpose_to(ti, 256 + g * 128, ub[:, 16 + g, :], [b_ub])
                S.op(ce, cm, KsE[0:64, :, t * 128:(t + 1) * 128],
                     pT[ti][0:64, 0:256].rearrange("p (g n) -> p g n", g=2), R=[bpT[ti]], W=[bKs[t]])
                S.op("dve", "tensor_copy", KwT[0:64, :, (t % 8) * 128:(t % 8 + 1) * 128],
                     pT[ti][0:64, 256:512].rearrange("p (g n) -> p g n", g=2), R=[bpT[ti]], W=[bKw[t % 8]])
                yield
                rb = t % 2
                if t == 0:
                    S.op("pool", "memset", rawT[rb][:, :, :, 0:16], 0.0, W=[b_rawT[rb]])
                else:
                    S.op("dve", "tensor_copy", rawT[rb][:, :, :, 0:16], rawT[1 - rb][:, :, :, 128:144],
                         R=[b_rawT[1 - rb]], W=[b_rawT[rb]])
                ti = nxt("T", 2)
                for c in range(4):
                    transpose_to(ti, c * 128, ub[:, 8 + c, :], [b_ub])
                S.op(ce, cm, rawT[rb][:, :, :, 16:144],
                     pT[ti][0:64, 0:512].rearrange("p (k g n) -> p k g n", k=2, g=2), R=[bpT[ti]], W=[b_rawT[rb]])
                yield
                S.op("act", "activation", gate[:].rearrange("p b h -> p (b h)"), u[:, 1696:1720], AF.Tanh, scale=0.5, R=[b_u], W=[b_gate])
                S.op("dve", "tensor_scalar", gate[:].rearrange("p b h -> p (b h)"), gate[:].rearrange("p b h -> p (b h)"), 0.5, 0.5,
                     ALU.mult, ALU.add, R=[b_gate], W=[b_gate])

                yield
                mi = nxt("M", 2)
                for kv in range(2):
                    for g in range(2):
                        c0 = (kv * 2 + g) * 8
                        for l in range(32):
                            S.op("pe", "matmul", pM[mi][:, c0:c0 + 8], lhsT=w1[:, kv, l, :], rhs=rawT[rb][:, kv, g, l:l + 113:16],
                                 start=(l == 0), stop=(l == 31), R=[b_w1, b_rawT[rb]], W=[bpM[mi]])
                            if l % 8 == 7:
                                yield
                for kv in range(2):
                    S.op("dve", "tensor_scalar", z_sb[:, kv * 16:(kv + 1) * 16], pM[mi][:, kv * 16:(kv + 1) * 16],
                         bias_tot[:, kv:kv + 1], None, ALU.add, R=[bpM[mi], b_bt], W=[b_z])
                S.op("dve", "tensor_tensor", z2_sb[:], z_sb[:], z_sb[:], ALU.mult, R=[b_z], W=[b_z])
                S.op("dve", "tensor_scalar", z2_sb[:], z2_sb[:], 0.044715, 1.0, ALU.mult, ALU.add, R=[b_z], W=[b_z])
                S.op("dve", "tensor_tensor", z2_sb[:], z2_sb[:], z_sb[:], ALU.mult, R=[b_z], W=[b_z])
                S.op("act", "activation", z2_sb[:], z2_sb[:], AF.Tanh, scale=math.sqrt(2.0 / math.pi), R=[b_z], W=[b_z])
                S.op("dve", "tensor_scalar", z2_sb[:], z2_sb[:], 0.5, 0.5, ALU.mult, ALU.add, R=[b_z], W=[b_z])
                S.op("dve", "tensor_tensor", hid_sb[:], z_sb[:], z2_sb[:], ALU.mult, R=[b_z], W=[b_hid])
                yield
                n0 = 8 * t - 1
                m0 = 1 if t == 0 else 0
                mi = nxt("M", 2)
                for g in range(2):
                    S.op("pe", "matmul", pM[mi][0:8, g * 64:(g + 1) * 64], lhsT=hid_sb[:, g * 8:(g + 1) * 8], rhs=w2[:, 0, :],
                         start=True, stop=True, R=[b_hid, b_w1], W=[bpM[mi]])
                for g in range(2):
                    S.op("pe", "matmul", pM[mi][0:64, 128 + g * 8:136 + g * 8], lhsT=w2[:, 1, :], rhs=hid_sb[:, 16 + g * 8:24 + g * 8],
                         start=True, stop=True, R=[b_hid, b_w1], W=[bpM[mi]])
                S.op("dve", "tensor_tensor", kc_f[:].rearrange("p g d -> p (g d)"), pM[mi][0:8, 0:128], b2k[0:8, :], ALU.add,
                     R=[bpM[mi], b_tab], W=[b_kc])
                S.op("dve", "tensor_scalar", VcT[:, :, n0 + m0:n0 + 8], pM[mi][0:64, 128:144].rearrange("p (g m) -> p g m", g=2)[:, :, m0:8],
                     b2v[:, 0:1], None, ALU.add, R=[bpM[mi], b_tab], W=[b_VcT])
                S.op("dve", "tensor_copy", kc_sb[:, :, 16:64], kc_f[:, :, 16:64], R=[b_kc], W=[b_kc])
                rope("dve", kc_sb[:, :, 0:16], kc_f[:, :, 0:16], cos_e[:, t, :], sin_e[:, t, :], 2, 8, [b_kc], [b_kc])
                ti = nxt("T", 2)
                for g in range(2):
                    transpose_to(ti, g * 8, kc_sb[:, g, :], [b_kc])
                S.op("dve", "tensor_copy", KcT[:, :, n0 + m0:n0 + 8],
                     pT[ti][0:64, 0:16].rearrange("p (g m) -> p g m", g=2)[:, :, m0:8], R=[bpT[ti]], W=[b_Kc])
                yield
                nts = sorted(set([max(n0, 0) // 128, (n0 + 7) // 128]))
                for nt in nts:
                    ti = nxt("T", 2)
                    for g in range(2):
                        S.op("pe", "transpose", pT[ti][:, g * 64:(g + 1) * 64], VcT[:, g, nt * 128:(nt + 1) * 128], ident[0:64, 0:64],
                             R=[b_VcT, b_ident], W=[bpT[ti]])
                    S.op("dve", "tensor_copy", VcO[:, nt, :, 0:64], pT[ti][:, 0:128].rearrange("p (g d) -> p g d", g=2), R=[bpT[ti]], W=[b_VcO])

            def attn_loop(kts, qk_fn, post_fn):
                if not kts:
                    return
                si_next = qk_fn(kts[0])
                for i_, kt in enumerate(kts):
                    si = si_next
                    if i_ + 1 < len(kts):
                        si_next = qk_fn(kts[i_ + 1])
                    post_fn(kt, si)

            def mla(s, t):
                par = t % 2
                QpeT, b_QpeT = QpeT2[par], b_QpeT2[par]
                QabsT, b_Qabs = QabsT2[par], b_Qabs2[par]
                QS, b_QSq, b_QSs = QS2[par], b_QSq2[par], b_QSs2[par]
                gate, b_gate = gate2[par], b_gate2[par]
                mo = nxt("M", 2)
                for hg in range(2):
                    qa = QabsT[:, hg * 4:(hg + 1) * 4, :].rearrange("p c n -> p (c n)")
                    qp = QpeT[:, hg * 4:(hg + 1) * 4, :].rearrange("p c n -> p (c n)")

                    def qk(kt):
                        si = nxt("S", 2)
                        S.op("pe", "matmul", pS[si][:], lhsT=KlatT[:, kt * 128:(kt + 1) * 128], rhs=qa,
                             start=True, stop=False, R=[bKlat[kt], b_Qabs], W=[bpS[si]])
                        S.op("pe", "matmul", pS[si][:], lhsT=KpeT[:, kt * 128:(kt + 1) * 128], rhs=qp,
                             start=False, stop=True, R=[bKpe[kt], b_QpeT], W=[bpS[si]])
                        return si

                    def post(kt, si):
                        pi = nxt("P", 3)
                        S.op("act", "activation", PT[pi][:], pS[si][:], AF.Exp, R=[bpS[si]], W=[b_PT[pi]])
                        if kt == t:
                            S.op("dve", "tensor_tensor", PT[pi][:], PT[pi][:], tri4[:], ALU.mult, R=[b_PT[pi], b_msk], W=[b_PT[pi]])
                        S.op("pe", "matmul", pA[0][:], lhsT=Clat[:, kt, 0:128], rhs=PT[pi][:], start=(kt == 0), stop=(kt == t),
                             R=[b_PT[pi], bClat[kt]], W=[bpA[0]])
                        if kt == 0:
                            S.op("dve", "tensor_copy", lacc[:], PT[pi][:], R=[b_PT[pi]], W=[b_lacc])
                        else:
                            S.op("dve", "tensor_tensor", lacc[:], lacc[:], PT[pi][:], ALU.add, R=[b_PT[pi], b_lacc], W=[b_lacc])

                    attn_loop(list(range(t + 1)), qk, post)
                    for j in range(4):
                        S.op("pe", "matmul", pA[1][:, j:j + 1], lhsT=lacc[:, j * 128:(j + 1) * 128], rhs=ones_f[:, 0:1],
                             start=True, stop=True, R=[b_lacc, b_ones], W=[bpA[1]])
                    S.op("dve", "tensor_copy", OlatT[:], pA[0][:].rearrange("p (c n) -> p c n", c=4), R=[bpA[0]], W=[b_OlatT])
                    S.op("dve", "reciprocal", rl[:, hg * 4:(hg + 1) * 4], pA[1][:, 0:4], R=[bpA[1]], W=[b_rl])
                    for j in range(4):
                        h = hg * 4 + j
                        S.op("pe", "matmul", pM[mo][:, h * 64:(h + 1) * 64], lhsT=OlatT[:, j, :], rhs=wuv[:, h, :],
                             start=True, stop=True, R=[b_OlatT, b_wkv], W=[bpM[mo]])
                S.op("dve", "tensor_tensor", y_sb[:, 0:512].rearrange("p (h d) -> p h d", h=8),
                     pM[mo][:, 0:512].rearrange("p (h d) -> p h d", h=8), rl[:, 0:8, None].to_broadcast([128, 8, 64]), ALU.mult,
                     R=[bpM[mo], b_rl], W=[b_y])

            def nsa_sel(s, t, g):
                par = t % 2
                QpeT, b_QpeT = QpeT2[par], b_QpeT2[par]
                QabsT, b_Qabs = QabsT2[par], b_Qabs2[par]
                QS, b_QSq, b_QSs = QS2[par], b_QSq2[par], b_QSs2[par]
                gate, b_gate = gate2[par], b_gate2[par]
                QSg = QS[:, g].rearrange("p j n -> p (j n)")
                QSg = QS[:, g].rearrange("p j n -> p (j n)")
                nts = [0] + ([1] if t >= 16 else [])
                for nt in nts:
                    si = nxt("M", 2)
                    S.op("pe", "matmul", pM[si][:], lhsT=KcT[:, g, nt * 128:(nt + 1) * 128], rhs=QSg[0:64, :],
                         start=True, stop=True, R=[b_Kc, b_QSq], W=[bpM[si]])
                    S.op("act", "activation", PcT[nt][:], pM[si][:], AF.Exp, R=[bpM[si]], W=[b_PcT[nt]])
                    S.op("pool", "affine_select", PcT[nt][:].rearrange("p (j n) -> p j n", j=4),
                         PcT[nt][:].rearrange("p (j n) -> p j n", j=4), [[0, 4], [1, 128]], ALU.is_ge, 0.0,
                         base=128 * t - 31 - 2048 * nt, channel_multiplier=-16, R=[b_PcT[nt]], W=[b_PcT[nt]])
                yield
                mc = nxt("M", 2)
                for j in range(4):
                    for i_, nt in enumerate(nts):
                        S.op("pe", "matmul", pM[mc][:, j * 128:(j + 1) * 128], lhsT=PcT[nt][:, j * 128:(j + 1) * 128],
                             rhs=VcO[:, nt, g, :], start=(i_ == 0), stop=(i_ == len(nts) - 1),
                             R=[b_PcT[nt], b_VcO], W=[bpM[mc]])
                yield
                pc = pM[mc][:].rearrange("p (j c) -> p j c", j=4)
                S.op("dve", "tensor_reduce", st[:, 24:28], pc[:, :, 64:128], AX.X, ALU.add, R=[bpM[mc]], W=[b_stn])
                S.op("dve", "tensor_scalar", st[:, 24:28], st[:, 24:28], 1e-30, None, ALU.max, R=[b_stn], W=[b_stn])
                S.op("dve", "reciprocal", st[:, 28:32], st[:, 24:28], R=[b_stn], W=[b_stn])
                S.op("dve", "tensor_tensor", tmp4[:], pc[:, :, 64:128], st[:, 28:32, None].to_broadcast([128, 4, 64]), ALU.mult,
                     R=[bpM[mc], b_stn], W=[b_tmp4])
                S.op("dve", "tensor_reduce", imp[:], tmp4[:].rearrange("p j c -> p c j"), AX.X, ALU.add, R=[b_tmp4], W=[b_imp])
                yield
                S.op("dve", "tensor_tensor", st[:, 32:36], st[:, 28:32], gate[:, 0, g * 4:(g + 1) * 4], ALU.mult,
                     R=[b_stn, b_gate], W=[b_stn])
                S.op("dve", "tensor_tensor", y_sb[:, 512 + g * 256:768 + g * 256].rearrange("p (j c) -> p j c", j=4), pc[:, :, 0:64],
                     st[:, 32:36, None].to_broadcast([128, 4, 64]), ALU.mult, R=[bpM[mc], b_stn], W=[b_yn])
                yield
                S.op("pool", "affine_select", sc1[:], imp[:], [[-64, 64]], ALU.is_ge, 1e9, base=128 * t - 128, channel_multiplier=1,
                     R=[b_imp], W=[b_imp])
                S.op("pool", "affine_select", sc2[:], sc1[:], [[-64, 64]], ALU.is_ge, -1e9, base=128 * t, channel_multiplier=1,
                     R=[b_imp], W=[b_imp])
                S.op("pool", "memset", sc2[:, 0:1], 1e9, R=[b_imp], W=[b_imp])
                yield
                S.op("dve", "max", st[:, 40:48], sc2[:], R=[b_imp], W=[b_stn])
                S.op("dve", "match_replace", sc3[:], st[:, 40:48], sc2[:], -3e38, R=[b_imp, b_stn], W=[b_imp])
                S.op("dve", "max", st[:, 48:56], sc3[:], R=[b_imp], W=[b_stn])
                S.op("dve", "tensor_scalar", selq[:, 64:128], sc2[:], st[:, 55:56], NEG, ALU.is_lt, ALU.mult,
                     R=[b_imp, b_stn], W=[b_selq])
                yield
                ti = nxt("T", 2)
                transpose_to(ti, 0, selq[:], [b_selq])
                S.op("dve", "tensor_copy", QS[64:128, g], pT[ti][64:128, None, 0:128].to_broadcast([64, 4, 128]),
                     R=[bpT[ti]], W=[b_QSs[g]])
                yield

            def nsa_attn(s, t, g):
                par = t % 2
                QpeT, b_QpeT = QpeT2[par], b_QpeT2[par]
                QabsT, b_Qabs = QabsT2[par], b_Qabs2[par]
                QS, b_QSq, b_QSs = QS2[par], b_QSq2[par], b_QSs2[par]
                gate, b_gate = gate2[par], b_gate2[par]
                QSg = QS[:, g].rearrange("p j n -> p (j n)")
                S.op("dve", "memset", pA[0][:, 0:260], 0.0, W=[bpA[0]])
                S.op("dve", "memset", pA[1][:, 0:260], 0.0, W=[bpA[1]])
                def qk_s(kt):
                    si = nxt("S", 2)
                    S.op("pe", "matmul", pS[si][:], lhsT=KsE[:, g, kt * 128:(kt + 1) * 128], rhs=QSg,
                         start=True, stop=True, R=[bKs[kt], b_exp, b_QSq, b_QSs[g]], W=[bpS[si]])
                    return si

                def post_s(kt, si):
                    pi = nxt("P", 3)
                    S.op("act", "activation", PT[pi][:], pS[si][:], AF.Exp, R=[bpS[si]], W=[b_PT[pi]])
                    if kt == t:
                        S.op("dve", "tensor_tensor", PT[pi][:], PT[pi][:], tri4[:], ALU.mult, R=[b_PT[pi], b_msk], W=[b_PT[pi]])
                    for j in range(4):
                        S.op("pe", "matmul", pA[0][:, j * 65:j * 65 + 65], lhsT=PT[pi][:, j * 128:(j + 1) * 128],
                             rhs=Vs[:, kt, g, :], start=False, stop=(kt == t), skip_group_check=True,
                             R=[b_PT[pi], bVs[kt]], W=[bpA[0]])

                def qk_w(kt):
                    si = nxt("S", 2)
                    sl = kt % 8
                    S.op("pe", "matmul", pS[si][:], lhsT=KwT[:, g, sl * 128:(sl + 1) * 128], rhs=QSg,
                         start=True, stop=True, R=[bKw[sl], b_QSq, b_QSs[g]], W=[bpS[si]])
                    return si

                def post_w(kt, si):
                    sl = kt % 8
                    pi = nxt("P", 3)
                    S.op("act", "activation", PT[pi][:], pS[si][:], AF.Exp, R=[bpS[si]], W=[b_PT[pi]])
                    if kt == t:
                        S.op("dve", "tensor_tensor", PT[pi][:], PT[pi][:], tri4[:], ALU.mult, R=[b_PT[pi], b_msk], W=[b_PT[pi]])
                    if kt == t - 4:
                        S.op("dve", "tensor_tensor", PT[pi][:], PT[pi][:], anti4[:], ALU.mult, R=[b_PT[pi], b_msk], W=[b_PT[pi]])
                    for j in range(4):
                        S.op("pe", "matmul", pA[1][:, j * 65:j * 65 + 65], lhsT=PT[pi][:, j * 128:(j + 1) * 128],
                             rhs=Vw[:, sl, g, :], start=False, stop=(kt == t), skip_group_check=True,
                             R=[b_PT[pi], bVw[sl]], W=[bpA[1]])

                attn_loop(list(range(t + 1)), qk_s, post_s)
                attn_loop(list(range(max(0, t - 4), t + 1)), qk_w, post_w)
                for br in range(2):
                    pa = pA[br][:, 0:260].rearrange("p (j c) -> p j c", j=4)
                    S.op("dve", "reciprocal", st[:, 56:60], pa[:, :, 64], R=[bpA[br]], W=[b_stn])
                    S.op("dve", "tensor_tensor", st[:, 60:64], st[:, 56:60], gate[:, 1 + br, g * 4:(g + 1) * 4], ALU.mult,
                         R=[b_stn, b_gate], W=[b_stn])
                    yv = y_sb[:, 512 + g * 256:768 + g * 256].rearrange("p (j c) -> p j c", j=4)
                    S.op("dve", "tensor_tensor", tmp4[:], pa[:, :, 0:64], st[:, 60:64, None].to_broadcast([128, 4, 64]), ALU.mult,
                         R=[bpA[br], b_stn], W=[b_tmp4])
                    S.op("dve", "tensor_tensor", yv, yv, tmp4[:], ALU.add, R=[b_tmp4, b_yn], W=[b_yn])


            def outproj(s, t):
                xb = t % 2
                ra, rb_ = rstd_multi([(y_sb[:, 0:512], 512), (y_sb[:, 512:1024], 512)], 12, [b_y, b_yn], mixed, b_mixed)
                S.op("dve", "tensor_scalar", mixed[:, 0:512], y_sb[:, 0:512], ra, None, ALU.mult, R=[b_y, b_stc[12]], W=[b_mixed])
                S.op("dve", "tensor_scalar", mixed[:, 512:1024], y_sb[:, 512:1024], rb_, None, ALU.mult, R=[b_yn, b_stc[12]], W=[b_mixed])
                ti = nxt("T", 2)
                for c in range(8):
                    transpose_to(ti, c * 128, mixed[:, c * 128:(c + 1) * 128], [b_mixed])
                S.op("dve", "tensor_copy", mixedT[:], pT[ti][:, 0:1024].rearrange("p (c n) -> p c n", c=8), R=[bpT[ti]], W=[b_mixedT])
                for dh in range(2):
                    mi = nxt("S", 2)
                    for c in range(8):
                        S.op("pe", "matmul", pS[mi][:], lhsT=mixedT[:, c, :], rhs=w_o[:, c, dh * 512:(dh + 1) * 512],
                             start=(c == 0), stop=(c == 7), R=[b_mixedT, b_wo], W=[bpS[mi]])
                    S.op("dve", "tensor_tensor", h_sb[xb][:, dh * 512:(dh + 1) * 512], pS[mi][:], xs[xb][:, dh * 512:(dh + 1) * 512],
                         ALU.add, R=[bpS[mi], b_xs[xb]], W=[b_h[xb]])
                row = (s * SEQ + t * 128)
                S.dma("sp", hscr_d[row:row + 128, :], h_sb[xb][:], R=[b_h[xb]])

            bgst = {"gen": None, "credit": 0.0, "rate": 0.0}

            def bg_run(n=None):
                if bgst["gen"] is None:
                    return
                mode["bg"] = True
                try:
                    k = 0
                    while n is None or k < n:
                        next(bgst["gen"])
                        k += 1
                except StopIteration:
                    bgst["gen"] = None
                mode["bg"] = False

            def tick():
                if mode["bg"] or bgst["gen"] is None:
                    return
                bgst["credit"] += bgst["rate"]
                if bgst["credit"] >= 1.0:
                    n = int(bgst["credit"])
                    bgst["credit"] -= n
                    bg_run(n)

            S.tick = tick
            def chain(*gens):
                for g_ in gens:
                    yield from g_

            def set_bg(gen, nchunks, fg_ops):
                bgst["gen"] = gen
                bgst["credit"] = 0.0
                bgst["rate"] = nchunks / (0.7 * fg_ops)

            for s in range(nseq):
                bgst["gen"] = phase1(s, 0) if "p1" in stages else None
                bg_run(None)
                for t in range(ntiles):
                    if "nsa" in stages:
                        set_bg(chain(nsa_sel(s, t, 0), nsa_sel(s, t, 1)), 16.0, 30.0 + 18.0 * (t + 1))
                    if "mla" in stages:
                        mla(s, t)
                    bg_run(None)
                    if t + 1 < ntiles and "p1" in stages:
                        set_bg(phase1(s, t + 1), 40.0, 100.0 + 14.0 * (t + 1 + min(t + 1, 5)))
                    if "nsa" in stages:
                        nsa_attn(s, t, 0)
                        nsa_attn(s, t, 1)
                    if "out" in stages:
                        outproj(s, t)
                    bg_run(None)
            S.tick = None
            S.barrier()
        mode["B"] = True
        B = ExitStack()
        with B:
            def sb2(name, shape, dt):
                return B.enter_context(nc.sbuf_tensor("b_" + name, shape, dt))
            gb = sb2("gb", [128, 8], F32); b_gb = S.buf("gb")
            S.dma("sp", gb[:], g_mlp_d, W=[b_gb])
            gfin = sb2("gfin", [128, D], F32)
            S.dma("sp", gfin[:], gfin_d, W=[b_gb])
            ident2 = sb2("ident2", [128, 128], BF16); b_id2 = S.buf("ident2")
            S.dma("pool", ident2[:], ident_d, W=[b_id2])
            w_up = sb2("w_up", [128, 8, DFF], BF16); b_wup = S.buf("w_up")
            for c in range(8):
                S.dma("pool", w_up[:, c, :], w_up_d[c * 128:(c + 1) * 128, :], W=[b_wup])
                S.op("dve", "tensor_scalar", w_up[:, c, :], w_up[:, c, :], gb[:, c:c + 1], None, ALU.mult, R=[b_gb, b_wup], W=[b_wup])
            w_dn = sb2("w_dn", [128, 32, D], BF16); b_wdn = S.buf("w_dn")
            wdv = w_down_d.rearrange("(f p) n -> p f n", p=128)
            for f4 in range(8):
                S.dma("pool", w_dn[:, f4 * 4:(f4 + 1) * 4, :], wdv[:, f4 * 4:(f4 + 1) * 4, :], W=[b_wdn])
            hin = [sb2("hin%d" % i, [128, D], F32) for i in range(4)]; b_hin = S.bufs("hin", 4)
            st2 = sb2("st2", [128, 16], F32); b_st2 = S.buf("st2")
            junk2 = sb2("junk2", [128, D], BF16); b_junk2 = S.buf("junk2")
            hn = sb2("hn", [128, D], BF16); b_hn = S.buf("hn")
            hnT = sb2("hnT", [128, 8, 512], BF16); b_hnT = S.buf("hnT")
            rl = [sb2("rl%d" % i, [128, 512], BF16) for i in range(2)]; b_rl = S.bufs("rl", 2)
            aT = sb2("aT", [128, 32, 512], BF16); b_aT = S.buf("aT")
            yo = [sb2("yo%d" % i, [128, D], F32) for i in range(2)]; b_yo = S.bufs("yo", 2)
            S.op("pool", "memset", st2[:], 0.0, W=[b_st2])
            nT = (nseq * ntiles * 128) // 512 if do_mlp else 0

            def rstd2(src_ap, Rb):
                S.op("pool", "memset", st2[:, 0:1], 0.0, W=[b_st2])
                S.op("act", "activation", junk2[:], src_ap, AF.Square, accum_out=st2[:, 0:1], R=Rb, W=[b_junk2, b_st2])
                S.op("dve", "tensor_scalar", st2[:, 1:2], st2[:, 0:1], 1.0 / D, EPS, ALU.mult, ALU.add, R=[b_st2], W=[b_st2])
                S.op("act", "activation", st2[:, 2:3], st2[:, 1:2], AF.Sqrt, R=[b_st2], W=[b_st2])
                S.op("dve", "reciprocal", st2[:, 3:4], st2[:, 2:3], R=[b_st2], W=[b_st2])
                return st2[:, 3:4]

            oc = 0
            for T in range(nT):
                hb = T % 2
                row = T * 512 if nseq * ntiles * 128 == nseq * SEQ else None
                base = (T * 512 // (ntiles * 128)) * SEQ + (T * 512) % (ntiles * 128)
                for i in range(4):
                    S.dma("sp", hin[i][:], hscr_d[base + i * 128:base + (i + 1) * 128, :], W=[b_hin[i]])
                for i in range(4):
                    r = rstd2(hin[i][:], [b_hin[i]])
                    S.op("dve", "tensor_scalar", hn[:], hin[i][:], r, None, ALU.mult, R=[b_hin[i], b_st2], W=[b_hn])
                    ti = nxt("T", 2)
                    for c in range(8):
                        S.op("pe", "transpose", pT[ti][:, c * 128:(c + 1) * 128], hn[:, c * 128:(c + 1) * 128], ident2[:],
                             R=[b_hn, b_id2], W=[bpT[ti]])
                    S.op("act", "copy", hnT[:, :, i * 128:(i + 1) * 128], pT[ti][:, 0:1024].rearrange("p (c n) -> p c n", c=8),
                         R=[bpT[ti]], W=[b_hnT])
                for f in range(32):
                    si = nxt("S", 2)
                    for c in range(8):
                        S.op("pe", "matmul", pS[si][:], lhsT=w_up[:, c, f * 128:(f + 1) * 128], rhs=hnT[:, c, :],
                             start=(c == 0), stop=(c == 7), R=[b_wup, b_hnT], W=[bpS[si]])
                    ri = f % 2
                    S.op("act", "activation", rl[ri][:], pS[si][:], AF.Relu, R=[bpS[si]], W=[b_rl[ri]])
                    S.op("pool", "tensor_tensor", aT[:, f, :], rl[ri][:], rl[ri][:], ALU.mult, R=[b_rl[ri]], W=[b_aT])
                for i in range(4):
                    ob = oc % 2
                    oc += 1
                    for dh in range(2):
                        mi = nxt("M", 2)
                        for f in range(32):
                            S.op("pe", "matmul", pM[mi][:], lhsT=aT[:, f, i * 128:(i + 1) * 128], rhs=w_dn[:, f, dh * 512:(dh + 1) * 512],
                                 start=(f == 0), stop=(f == 31), R=[b_aT, b_wdn], W=[bpM[mi]])
                        S.op("dve", "tensor_tensor", yo[ob][:, dh * 512:(dh + 1) * 512], pM[mi][:], hin[i][:, dh * 512:(dh + 1) * 512],
                             ALU.add, R=[bpM[mi], b_hin[i]], W=[b_yo[ob]])
                    r = rstd2(yo[ob][:], [b_yo[ob]])
                    S.op("dve", "scalar_tensor_tensor", yo[ob][:], yo[ob][:], r, gfin[:], ALU.mult, ALU.mult,
                         R=[b_yo[ob], b_st2, b_gb], W=[b_yo[ob]])
                    S.dma("sp", out_d[base + i * 128:base + (i + 1) * 128, :], yo[ob][:], R=[b_yo[ob]])
            S.emit()
            print('NOPS', S.nops)
    return nc


def _rope_tab(pos, dim):
    inv = np.exp(np.float32(-math.log(500000.0)) * np.arange(0, dim, 2, dtype=np.float32) / np.float32(dim)).astype(np.float32)
    ang = pos.astype(np.float32)[:, None] * inv[None, :]
    return np.cos(ang).astype(np.float32), np.sin(ang).astype(np.float32)


def _tok_major(a):
    return np.ascontiguousarray(a.reshape(NT, 128, -1).transpose(1, 0, 2))


def host_consts():
    pos = np.arange(SEQ)
    cm, sm = _rope_tab(pos, 32)
    cn, sn = _rope_tab(pos, 16)
    c = {}
    c["cos_m"], c["sin_m"] = _tok_major(cm), _tok_major(sm)
    c["cos_n"], c["sin_n"] = _tok_major(cn), _tok_major(sn)
    c["cos_n8"], c["sin_n8"] = _tok_major(cn * np.float32(0.125)), _tok_major(sn * np.float32(0.125))
    ce = np.zeros((8, NT, 8), np.float32)
    se = np.zeros((8, NT, 8), np.float32)
    for t in range(NT):
        for m in range(8):
            n = 8 * t - 1 + m
            if 0 <= n < 255:
                p = 16 * n + 31
                ce[m, t], se[m, t] = cn[p], sn[p]
    c["cos_e"], c["sin_e"] = ce, se
    k = np.arange(128)[:, None]
    q = np.arange(128)[None, :]
    tri = (q >= k).astype(np.float32)
    anti = (q < k).astype(np.float32)
    c["tri4"] = np.ascontiguousarray(np.tile(tri, (1, 4)))
    c["anti4"] = np.ascontiguousarray(np.tile(anti, (1, 4)))
    n = np.arange(256)[:, None] * 16
    j = np.arange(64)[None, :] * 64
    ov = np.clip(np.minimum(n + 32, j + 64) - np.maximum(n, j), 0, None).astype(np.float32) / 32.0
    ov[255] = 0.0
    c["ovl"] = np.ascontiguousarray(ov.reshape(2, 128, 64).transpose(1, 0, 2))
    c["expand"] = (np.arange(SEQ)[None, :] // 64 == np.arange(64)[:, None]).astype(np.float32)
    c["ident"] = np.eye(128, dtype=np.float32)
    return c


def host_weights(inp):
    f = lambda a: np.ascontiguousarray(np.asarray(a, dtype=np.float32))
    pc = lambda v: np.ascontiguousarray(np.asarray(v, np.float32).reshape(-1, 128).T)
    w = {}
    w["w_in"] = f(inp["w_in"][0])
    w["g_mix"] = pc(inp["g_mix_norm"][0])
    w["w_uq"] = f(inp["w_uq"][0])
    w["g_cq"] = pc(inp["g_cq"][0])
    wukv = np.asarray(inp["w_ukv"][0], np.float32).reshape(128, 8, 2, 64)
    wuk = wukv[:, :, 0, :]
    w["wukT"] = np.ascontiguousarray(wuk.transpose(2, 1, 0))
    w["wuv"] = np.ascontiguousarray(wukv[:, :, 1, :])
    w["gckv_bc"] = np.ascontiguousarray(np.broadcast_to(np.asarray(inp["g_ckv"][0], np.float32)[None, :], (128, 128)))
    w1 = np.stack([np.asarray(inp["cmp_w1_k"][0], np.float32), np.asarray(inp["cmp_w1_v"][0], np.float32)])
    w["cmp_w1"] = np.ascontiguousarray(w1.reshape(2, 32, 64, 128).transpose(0, 2, 1, 3))
    pe = np.stack([np.asarray(inp["cmp_pe_k"][0], np.float32), np.asarray(inp["cmp_pe_v"][0], np.float32)])
    w["cmp_peT"] = np.ascontiguousarray(pe.transpose(0, 2, 1))
    w["cmp_b1"] = np.ascontiguousarray(np.stack([inp["cmp_b1_k"][0], inp["cmp_b1_v"][0]], axis=1).astype(np.float32))
    w["cmp_w2"] = np.ascontiguousarray(np.stack([inp["cmp_w2_k"][0], inp["cmp_w2_v"][0]], axis=1).astype(np.float32))
    b2k = np.asarray(inp["cmp_b2_k"][0], np.float32)
    w["cmp_b2k_bc"] = np.ascontiguousarray(np.broadcast_to(np.concatenate([b2k, b2k])[None, :], (128, 128)))
    w["cmp_b2v"] = np.ascontiguousarray(np.asarray(inp["cmp_b2_v"][0], np.float32).reshape(64, 1))
    w["w_o"] = f(inp["w_o"][0])
    w["g_out"] = pc(np.concatenate([np.asarray(inp["g_out_mla"][0]), np.asarray(inp["g_out_nsa"][0])]))
    w["w_up"] = f(inp["w_up"][0])
    w["g_mlp"] = pc(inp["g_mlp_norm"][0])
    w["w_down"] = f(inp["w_down"][0])
    w["gfin_bc"] = np.ascontiguousarray(np.broadcast_to(np.asarray(inp["g_final"], np.float32)[None, :], (128, D)))
    return w


_NC_CACHE = {}


def kernel(**inputs):
    x = np.asarray(inputs["x"], dtype=np.float32)
    shared = host_consts()
    shared.update(host_weights(inputs))
    if "full" not in _NC_CACHE:
        _NC_CACHE["full"] = build_program()
    nc = _NC_CACHE["full"]
    in_maps = []
    for c in range(NCORES):
        m = dict(shared)
        m["x"] = np.ascontiguousarray(x[c * NSEQ:(c + 1) * NSEQ])
        in_maps.append(m)
    res = run_bass_kernel_spmd(nc, in_maps, core_ids=list(range(NCORES)))
    outs = [np.asarray(r["out"]).reshape(NSEQ, SEQ, D) for r in res.results]
    return np.concatenate(outs, axis=0).astype(np.float32)
```

```python
import math
import numpy as np
from contextlib import ExitStack
import concourse.bass as bass
import concourse.mybir as mybir
from concourse.bass_utils import run_bass_kernel_spmd

F32 = mybir.dt.float32
BF16 = mybir.dt.bfloat16
I32 = mybir.dt.int32
AF = mybir.ActivationFunctionType
ALU = mybir.AluOpType
AX = mybir.AxisListType

NCORES = 8
SEQ = 4096
D = 1024
NSEQ = 2
NT = SEQ // 128
EPS = 1e-6
IN_COLS = 1720
DFF = 4096
NEG = -30000.0
STRICT_SAME = True
OP_LIMIT = None
BG_OVERLAP = True
USE_MAGIC = True
ACT_COPY_T = 20


class Buf:
    __slots__ = ("name", "w", "r", "dsem", "dcnt", "excl")

    def __init__(self, name):
        self.name = name
        self.excl = False
        self.w = None
        self.r = {}
        self.dsem = None
        self.dcnt = 0


class Sched:
    ENG = ("pe", "act", "dve", "pool", "sp")

    def __init__(self, nc, ctx):
        self.nc = nc
        self.ctx = ctx
        self.sem = {e: ctx.enter_context(nc.semaphore("s_" + e)) for e in self.ENG}
        self.cnt = {e: 0 for e in self.ENG}
        self.seen = {e: {} for e in self.ENG}
        self.prog = {e: [] for e in self.ENG}
        self.dbufs = []
        self.nb = 0
        self.nops = 0
        self.fillregs = {}
        self.tick = None
        self.limit = OP_LIMIT

    def buf(self, name):
        self.nb += 1
        return Buf("%s_%d" % (name, self.nb))

    def bufs(self, name, n):
        return [self.buf(name) for _ in range(n)]

    def _dsem(self, b):
        if b.dsem is None:
            b.dsem = self.ctx.enter_context(self.nc.semaphore("d_" + b.name))
            self.dbufs.append(b)
        return b.dsem

    def _deps(self, e, reads, writes, strict):
        toks = []
        for b in reads:
            if b.w is not None:
                toks.append(b.w)
            if b.excl:
                toks.extend(b.r.values())
        for b in writes:
            if b.w is not None:
                toks.append(b.w)
            toks.extend(b.r.values())
        need = {}
        for (key, sem, val) in toks:
            if key == e and not strict and (e == "pe" or not STRICT_SAME):
                continue
            if self.seen[e].get(key, 0) >= val:
                continue
            if key not in need or need[key][1] < val:
                need[key] = (sem, val)
        for key, (sem, val) in need.items():
            self.seen[e][key] = val
            self.prog[e].append(("wait", sem, val))

    def op(self, e, meth, *args, R=(), W=(), **kw):
        self.nops += 1
        if self.limit is not None and self.nops > self.limit:
            return None
        self._deps(e, R, W, False)
        self.cnt[e] += 1
        tok = (e, self.sem[e], self.cnt[e])
        self.prog[e].append(("op", meth, args, kw))
        for b in R:
            b.r[e] = tok
        for b in W:
            b.w = tok
            b.r = {}
        if self.tick is not None:
            self.tick()
        return tok

    def dma(self, q, out, in_, R=(), W=(), **kw):
        self.nops += 1
        if self.limit is not None and self.nops > self.limit:
            return None
        self._deps(q, R, W, True)
        owner = W[0] if W else R[0]
        sem = self._dsem(owner)
        owner.dcnt += 16
        tok = ("d_" + owner.name, sem, owner.dcnt)
        self.prog[q].append(("dma", out, in_, kw, sem))
        for b in R:
            b.r[tok[0]] = tok
        for b in W:
            b.w = tok
            b.r = {}
        return tok

    def barrier(self):
        toks = [(e, self.sem[e], self.cnt[e]) for e in self.ENG if self.cnt[e] > 0]
        toks += [("d_" + b.name, b.dsem, b.dcnt) for b in self.dbufs if b.dcnt > 0]
        for e in self.ENG:
            for (key, sem, val) in toks:
                if self.seen[e].get(key, 0) >= val:
                    continue
                self.seen[e][key] = val
                self.prog[e].append(("wait", sem, val))

    def flush(self):
        nc = self.nc
        with nc.Block() as block:
            def replay(e):
                def f(eng):
                    sem_e = self.sem[e]
                    for it in self.prog[e]:
                        if it[0] == "wait":
                            eng.wait_ge(it[1], it[2])
                        elif it[0] == "op":
                            args = it[2]
                            if it[1] == "affine_select":
                                args = list(args)
                                if args[4] not in self.fillregs:
                                    self.fillregs[args[4]] = eng.to_reg(args[4])
                                args[4] = self.fillregs[args[4]]
                            getattr(eng, it[1])(*args, **it[3]).then_inc(sem_e, 1)
                        else:
                            eng.dma_start(out=it[1], in_=it[2], **it[3]).then_inc(it[4], 16)
                return f
            block.tensor(replay("pe"))
            block.scalar(replay("act"))
            block.vector(replay("dve"))
            block.gpsimd(replay("pool"))
            block.sync(replay("sp"))
        self.prog = {e: [] for e in self.ENG}

    def emit(self):
        self.barrier()
        self.flush()


def build_program(nseq=NSEQ, ntiles=NT, do_mlp=True, stages=("p1", "mla", "nsa", "out")):
    nc = bass.Bass("TRN2", target_bir_lowering=False)

    def din(name, shape):
        return nc.dram_tensor(name, list(shape), F32, kind="ExternalInput").ap()

    x_d = din("x", [nseq, SEQ, D])
    w_in_d = din("w_in", [D, IN_COLS])
    g_mix_d = din("g_mix", [128, 8])
    w_uq_d = din("w_uq", [256, 768])
    g_cq_d = din("g_cq", [128, 2])
    wukT_d = din("wukT", [64, 8, 128])
    wuv_d = din("wuv", [128, 8, 64])
    gckv_d = din("gckv_bc", [128, 128])
    w1_d = din("cmp_w1", [2, 64, 32, 128])
    peT_d = din("cmp_peT", [2, 64, 32])
    b1_d = din("cmp_b1", [128, 2])
    w2_d = din("cmp_w2", [128, 2, 64])
    b2k_d = din("cmp_b2k_bc", [128, 128])
    b2v_d = din("cmp_b2v", [64, 1])
    w_o_d = din("w_o", [D, D])
    g_out_d = din("g_out", [128, 8])
    w_up_d = din("w_up", [D, DFF])
    g_mlp_d = din("g_mlp", [128, 8])
    w_down_d = din("w_down", [DFF, D])
    gfin_d = din("gfin_bc", [128, D])
    cosm_d = din("cos_m", [128, NT, 16])
    sinm_d = din("sin_m", [128, NT, 16])
    cosn_d = din("cos_n", [128, NT, 8])
    sinn_d = din("sin_n", [128, NT, 8])
    cosn8_d = din("cos_n8", [128, NT, 8])
    sinn8_d = din("sin_n8", [128, NT, 8])
    cose_d = din("cos_e", [8, NT, 8])
    sine_d = din("sin_e", [8, NT, 8])
    tri_d = din("tri4", [128, 512])
    anti_d = din("anti4", [128, 512])
    ovl_d = din("ovl", [128, 2, 64])
    exp_d = din("expand", [64, SEQ])
    ident_d = din("ident", [128, 128])
    out_d = nc.dram_tensor("out", [nseq * SEQ, D], F32, kind="ExternalOutput").ap()
    hscr_d = nc.dram_tensor("hscr", [nseq * SEQ, D], F32, kind="Internal").ap()

    top = ExitStack()
    with top:
        S = Sched(nc, top)
        def psum(name, shape, dt):
            return top.enter_context(nc.psum_tensor(name, shape, dt))
        pS = [psum("pS%d" % i, [128, 512], F32) for i in range(2)]
        pA = [psum("pA%d" % i, [128, 512], F32) for i in range(2)]
        pM = [psum("pM%d" % i, [128, 512], F32) for i in range(2)]
        pT = [psum("pT%d" % i, [128, 1024], BF16) for i in range(2)]
        bpS = S.bufs("pS", 2)
        bpA = S.bufs("pA", 2)
        bpM = S.bufs("pM", 2)
        bpT = S.bufs("pT", 2)
        for b_ in bpS + bpA + bpM + bpT:
            b_.excl = True
        rr = {"S": 0, "M": 0, "T": 0, "P": 0, "A": 0}
        mode = {"bg": False, "B": False}

        def nxt(kind, n):
            if kind in ("M", "T") and not mode["B"]:
                return 0 if mode["bg"] else 1
            i = rr[kind] % n
            rr[kind] += 1
            return i

        A = ExitStack()
        with A:
            def sb(name, shape, dt):
                return A.enter_context(nc.sbuf_tensor("a_" + name, shape, dt))

            ident = sb("ident", [128, 128], BF16); b_ident = S.buf("ident")
            S.dma("pool", ident[:], ident_d, W=[b_ident])
            tri4 = sb("tri4", [128, 512], BF16); anti4 = sb("anti4", [128, 512], BF16); b_msk = S.buf("msk")
            S.dma("pool", tri4[:], tri_d, W=[b_msk])
            S.dma("pool", anti4[:], anti_d, W=[b_msk])
            tabs = {}
            b_tab = S.buf("tab")
            for nm, d_, w_ in (("cos_m", cosm_d, 16), ("sin_m", sinm_d, 16), ("cos_n", cosn_d, 8), ("sin_n", sinn_d, 8)):
                tabs[nm] = sb(nm, [128, NT, w_], F32)
                S.dma("sp", tabs[nm][:], d_, W=[b_tab])
            cos_e = sb("cos_e", [8, NT, 8], F32); sin_e = sb("sin_e", [8, NT, 8], F32)
            S.dma("sp", cos_e[:], cose_d, W=[b_tab])
            S.dma("sp", sin_e[:], sine_d, W=[b_tab])
            gckv = sb("gckv", [128, 128], F32); b2k = sb("b2k", [128, 128], F32); b2v = sb("b2v", [64, 1], F32)
            b1 = sb("b1", [128, 2], F32)
            S.dma("sp", gckv[:], gckv_d, W=[b_tab])
            S.dma("sp", b2k[:], b2k_d, W=[b_tab])
            S.dma("sp", b2v[:], b2v_d, W=[b_tab])
            S.dma("sp", b1[:], b1_d, W=[b_tab])
            gvec = sb("gvec", [128, 24], F32)
            S.dma("sp", gvec[:, 0:8], g_mix_d, W=[b_tab])
            S.dma("sp", gvec[:, 8:10], g_cq_d, W=[b_tab])
            S.dma("sp", gvec[:, 10:18], g_out_d, W=[b_tab])

            w_in = sb("w_in", [128, 8, IN_COLS], BF16); b_win = S.buf("w_in")
            S.dma("pool", w_in[:], w_in_d.rearrange("(c p) n -> p c n", p=128), W=[b_win])
            for c in range(8):
                S.op("dve", "tensor_scalar", w_in[:, c, :], w_in[:, c, :], gvec[:, c:c + 1], None, ALU.mult,
                     R=[b_tab, b_win], W=[b_win])
                S.op("dve", "tensor_scalar", w_in[:, c, 416:928], w_in[:, c, 416:928], 0.125, None, ALU.mult,
                     R=[b_win], W=[b_win])
            w_o = sb("w_o", [128, 8, D], BF16); b_wo = S.buf("w_o")
            S.dma("pool", w_o[:], w_o_d.rearrange("(c p) n -> p c n", p=128), W=[b_wo])
            for c in range(8):
                S.op("dve", "tensor_scalar", w_o[:, c, :], w_o[:, c, :], gvec[:, 10 + c:11 + c], None, ALU.mult,
                     R=[b_tab, b_wo], W=[b_wo])
            w_uq = sb("w_uq", [128, 2, 768], BF16); b_wuq = S.buf("w_uq")
            S.dma("pool", w_uq[:], w_uq_d.rearrange("(c p) n -> p c n", p=128), W=[b_wuq])
            for c in range(2):
                S.op("dve", "tensor_scalar", w_uq[:, c, :], w_uq[:, c, :], gvec[:, 8 + c:9 + c], 96.0 ** -0.5,
                     ALU.mult, ALU.mult, R=[b_tab, b_wuq], W=[b_wuq])
            wukT = sb("wukT", [64, 8, 128], BF16); wuv = sb("wuv", [128, 8, 64], BF16); b_wkv = S.buf("wkv")
            S.dma("pool", wukT[:], wukT_d, W=[b_wkv])
            S.dma("pool", wuv[:], wuv_d, W=[b_wkv])
            w1 = sb("w1", [64, 2, 32, 128], BF16); b_w1 = S.buf("w1")
            for kv in range(2):
                S.dma("pool", w1[:, kv], w1_d[kv], W=[b_w1])
            peT = sb("peT", [64, 2, 32], BF16)
            for kv in range(2):
                S.dma("pool", peT[:, kv], peT_d[kv], W=[b_w1])
            w2 = sb("w2", [128, 2, 64], BF16)
            S.dma("pool", w2[:], w2_d, W=[b_w1])

            bias_tot = sb("bias_tot", [128, 2], F32); b_bt = S.buf("bias_tot")
            for kv in range(2):
                for l in range(32):
                    S.op("pe", "matmul", pM[0][:, kv:kv + 1], lhsT=w1[:, kv, l, :], rhs=peT[:, kv, l:l + 1],
                         start=(l == 0), stop=(l == 31), R=[b_w1], W=[bpM[0]])
            S.op("dve", "tensor_tensor", bias_tot[:], pM[0][:, 0:2], b1[:], ALU.add, R=[bpM[0], b_tab], W=[b_bt])

            KlatT = sb("KlatT", [128, SEQ], BF16); bKlat = S.bufs("Klat", NT)
            KpeT = sb("KpeT", [128, SEQ], BF16); bKpe = S.bufs("Kpe", NT)
            Clat = sb("Clat", [128, NT, 128], BF16); bClat = S.bufs("Clat", NT)
            KsE = sb("KsE", [128, 2, SEQ], BF16); bKs = S.bufs("Ks", NT); b_exp = S.buf("expand")
            Vs = sb("Vs", [128, NT, 2, 65], BF16); bVs = S.bufs("Vs", NT)
            KwT = sb("KwT", [128, 2, 8 * 128], BF16); bKw = S.bufs("Kw", 8)
            Vw = sb("Vw", [128, 8, 2, 65], BF16); bVw = S.bufs("Vw", 8)
            KcT = sb("KcT", [64, 2, 256], BF16); b_Kc = S.buf("Kc")
            VcT = sb("VcT", [64, 2, 256], BF16); b_VcT = S.buf("VcT")
            VcO = sb("VcO", [128, 2, 2, 128], BF16); b_VcO = S.buf("VcO")
            for g in range(2):
                S.dma("pool", KsE[64:128, g, :], exp_d, W=[b_exp])
            for nt in range(2):
                for g in range(2):
                    S.dma("pool", VcO[:, nt, g, 64:128], ovl_d[:, nt, :], W=[b_VcO])
            S.op("pool", "memset", Vs[:, :, :, 64:65], 1.0, W=bVs)
            S.op("pool", "memset", Vw[:, :, :, 64:65], 1.0, W=bVw)

            xs = [sb("xs%d" % i, [128, D], F32) for i in range(2)]; b_xs = S.bufs("xs", 2)
            st = sb("st", [128, 64], F32); b_st = S.buf("st")
            xn = sb("xn", [128, D], BF16); b_xn = S.buf("xn")
            xnT = sb("xnT", [128, 8, 128], BF16); b_xnT = S.buf("xnT")
            u = sb("u", [128, IN_COLS], F32); b_u = S.buf("u")
            cqn = sb("cqn", [128, 256], BF16); b_cqn = S.buf("cqn")
            cqnT = sb("cqnT", [128, 2, 128], BF16); b_cqnT = S.buf("cqnT")
            q_sb = sb("q_sb", [128, 9, 96], F32); b_q = S.buf("q")
            qn_sb = sb("qn_sb", [128, 8, 64], BF16); b_qn = S.buf("qn")
            qpe_sb = sb("qpe_sb", [128, 9, 32], BF16); b_qpe = S.buf("qpe")
            rt = [sb("rt%d" % i, [128, 160], F32) for i in range(4)]; b_rt = S.buf("rt")
            QnT = sb("QnT", [64, 8, 128], BF16); b_QnT = S.buf("QnT")
            QpeT2 = [sb("QpeT%d" % i, [128, 8, 128], BF16) for i in range(2)]; b_QpeT2 = S.bufs("QpeT", 2)
            QabsT2 = [sb("QabsT%d" % i, [128, 8, 128], BF16) for i in range(2)]; b_Qabs2 = S.bufs("Qabs", 2)
            ub = sb("ub", [128, 20, 64], BF16); b_ub = S.buf("ub")
            QS2 = [sb("QS%d" % i, [128, 2, 4, 128], BF16) for i in range(2)]; b_QSq2 = S.bufs("QSq", 2); b_QSs2 = [S.bufs("QSs", 2) for _ in range(2)]
            rawT = [sb("rawT%d" % i, [64, 2, 2, 144], BF16) for i in range(2)]; b_rawT = S.bufs("rawT", 2)
            gate2 = [sb("gate%d" % i, [128, 3, 8], F32) for i in range(2)]; b_gate2 = S.bufs("gate", 2)
            z_sb = sb("z_sb", [128, 32], F32); z2_sb = sb("z2_sb", [128, 32], F32); b_z = S.buf("z")
            hid_sb = sb("hid_sb", [128, 32], BF16); b_hid = S.buf("hid")
            kc_f = sb("kc_f", [8, 2, 64], F32); kc_sb = sb("kc_sb", [8, 2, 64], BF16); b_kc = S.buf("kc")
            PT = [sb("PT%d" % i, [128, 512], BF16) for i in range(3)]; b_PT = S.bufs("PT", 3)
            PcT = [sb("PcT%d" % i, [128, 512], BF16) for i in range(2)]; b_PcT = S.bufs("PcT", 2)
            OlatT = sb("OlatT", [128, 4, 128], BF16); b_OlatT = S.buf("OlatT")
            y_sb = sb("y_sb", [128, D], F32); b_y = S.buf("y"); b_yn = S.buf("yn")
            imp = sb("imp", [128, 64], F32); sc1 = sb("sc1", [128, 64], F32); sc2 = sb("sc2", [128, 64], F32)
            sc3 = sb("sc3", [128, 64], F32); b_imp = S.buf("imp")
            selq = sb("selq", [128, 128], BF16); b_selq = S.buf("selq")
            tmp4 = sb("tmp4", [128, 4, 64], F32); b_tmp4 = S.buf("tmp4")
            mixed = sb("mixed", [128, D], BF16); b_mixed = S.buf("mixed")
            mixedT = sb("mixedT", [128, 8, 128], BF16); b_mixedT = S.buf("mixedT")
            h_sb = [sb("h_sb%d" % i, [128, D], F32) for i in range(2)]; b_h = S.bufs("h", 2)

            S.op("pool", "memset", selq[:], 0.0, W=[b_selq])
            S.op("pool", "memset", KpeT[:], 0.0, W=bKpe)
            S.op("pool", "memset", KwT[:], 0.0, W=bKw)
            for i in range(2):
                S.op("pool", "memset", QS2[i][:], 0.0, W=[b_QSq2[i]] + b_QSs2[i])
            for i in range(2):
                S.op("pool", "memset", QpeT2[i][:], 0.0, W=[b_QpeT2[i]])
            for i in range(2):
                S.op("pool", "memset", rawT[i][:], 0.0, W=[b_rawT[i]])
            S.op("pool", "memset", KcT[:], 0.0, W=[b_Kc])
            S.op("pool", "memset", VcT[:], 0.0, W=[b_VcT])
            S.op("pool", "memset", VcO[:, :, :, 0:64], 0.0, W=[b_VcO])

            b_stc = {0: S.buf("st0"), 4: S.buf("st4"), 12: S.buf("st12")}
            b_stm = S.buf("stm")
            b_stn = S.buf("stn")
            rl = sb("rl", [128, 8], F32); b_rl = S.buf("rl")
            lacc = sb("lacc", [128, 512], F32); b_lacc = S.buf("lacc")
            ones_f = sb("ones_f", [128, 1], F32); b_ones = S.buf("ones")
            S.op("pool", "memset", ones_f[:], 1.0, W=[b_ones])

            def rstd_multi(items, col, Rb, jk, b_jk):
                b_st = b_stc[col]
                k = len(items)
                S.op("dve", "memset", st[:, col:col + k], 0.0, W=[b_st])
                for i_, (src_ap, n) in enumerate(items):
                    S.op("dve", "scalar_tensor_tensor", jk[:, 0:n], src_ap, 1.0, src_ap, ALU.mult, ALU.mult,
                         accum_out=st[:, col + i_:col + i_ + 1], R=Rb + [b_st], W=[b_jk, b_st])
                    S.op("dve", "tensor_scalar", st[:, col + k + i_:col + k + i_ + 1], st[:, col + i_:col + i_ + 1], 1.0 / n, EPS,
                         ALU.mult, ALU.add, R=[b_st], W=[b_st])
                v_ = st[:, col + k:col + 2 * k]
                y_ = st[:, col + 2 * k:col + 3 * k]
                w_ = st[:, col + 3 * k:col + 4 * k]
                if not USE_MAGIC:
                    S.op("act", "activation", w_, v_, AF.Sqrt, R=[b_st], W=[b_st])
                    S.op("dve", "reciprocal", y_, w_, R=[b_st], W=[b_st])
                    return [st[:, col + 2 * k + i_:col + 2 * k + i_ + 1] for i_ in range(k)]
                ss_ = st[:, col:col + k]
                S.op("dve", "tensor_scalar", y_.bitcast(I32), v_.bitcast(I32), -0.5, 1597463007.0, ALU.mult, ALU.add, R=[b_st], W=[b_st])
                S.op("dve", "tensor_scalar", ss_, v_, -0.5, None, ALU.mult, R=[b_st], W=[b_st])
                for _it in range(2):
                    if k == 1:
                        S.op("dve", "scalar_tensor_tensor", w_, y_, ss_, y_, ALU.mult, ALU.mult, R=[b_st], W=[b_st])
                    else:
                        S.op("dve", "tensor_tensor", w_, y_, y_, ALU.mult, R=[b_st], W=[b_st])
                        S.op("dve", "tensor_tensor", w_, w_, ss_, ALU.mult, R=[b_st], W=[b_st])
                    S.op("dve", "scalar_tensor_tensor", y_, w_, 1.5, y_, ALU.add, ALU.mult, R=[b_st], W=[b_st])
                return [st[:, col + 2 * k + i_:col + 2 * k + i_ + 1] for i_ in range(k)]

            def rope(eng, out_ap, in_ap, cos_ap, sin_ap, nh, half, Rb, Wb):
                P = in_ap.shape[0]
                x1 = in_ap[:, :, 0:half]
                x2 = in_ap[:, :, half:2 * half]
                cb = cos_ap[:, None, :].to_broadcast([P, nh, half])
                sbb = sin_ap[:, None, :].to_broadcast([P, nh, half])
                t = [r_[0:P, 0:nh * half].rearrange("p (h d) -> p h d", h=nh) for r_ in rt]
                S.op(eng, "tensor_tensor", t[0], x1, cb, ALU.mult, R=Rb + [b_tab], W=[b_rt])
                S.op(eng, "tensor_tensor", t[1], x2, sbb, ALU.mult, R=Rb + [b_tab], W=[b_rt])
                S.op(eng, "tensor_tensor", t[2], x2, cb, ALU.mult, R=Rb + [b_tab], W=[b_rt])
                S.op(eng, "tensor_tensor", t[3], x1, sbb, ALU.mult, R=Rb + [b_tab], W=[b_rt])
                S.op(eng, "tensor_tensor", out_ap[:, :, 0:half], t[0], t[1], ALU.subtract, R=[b_rt], W=Wb)
                S.op(eng, "tensor_tensor", out_ap[:, :, half:2 * half], t[2], t[3], ALU.add, R=[b_rt], W=Wb)

            def transpose_to(ps_i, col0, in_ap, Rb):
                P, Fd = in_ap.shape[0], in_ap.shape[1]
                S.op("pe", "transpose", pT[ps_i][0:Fd, col0:col0 + P], in_ap, ident[0:P, 0:P],
                     R=Rb + [b_ident], W=[bpT[ps_i]])

            def phase1(s, t):
                par = t % 2
                QpeT, b_QpeT = QpeT2[par], b_QpeT2[par]
                QabsT, b_Qabs = QabsT2[par], b_Qabs2[par]
                QS, b_QSq, b_QSs = QS2[par], b_QSq2[par], b_QSs2[par]
                gate, b_gate = gate2[par], b_gate2[par]
                xb = t % 2
                ce, cm = ("act", "copy") if t < ACT_COPY_T else ("dve", "tensor_copy")
                S.dma("sp", xs[xb][:], x_d[s, t * 128:(t + 1) * 128, :], W=[b_xs[xb]])
                r0 = rstd_multi([(xs[xb][:], D)], 0, [b_xs[xb]], xn, b_xn)[0]
                S.op("dve", "tensor_scalar", xn[:], xs[xb][:], r0, None, ALU.mult, R=[b_xs[xb], b_stc[0]], W=[b_xn])
                ti = nxt("T", 2)
                for c in range(8):
                    transpose_to(ti, c * 128, xn[:, c * 128:(c + 1) * 128], [b_xn])
                S.op(ce, cm, xnT[:], pT[ti][:, 0:1024].rearrange("p (c n) -> p c n", c=8), R=[bpT[ti]], W=[b_xnT])
                for cg, (c0, c1) in enumerate(((0, 512), (512, 1024), (1024, 1536), (1536, IN_COLS))):
                    mi = nxt("M", 2)
                    for c in range(8):
                        S.op("pe", "matmul", pM[mi][:, 0:c1 - c0], lhsT=xnT[:, c, :], rhs=w_in[:, c, c0:c1],
                             start=(c == 0), stop=(c == 7), R=[b_xnT, b_win], W=[bpM[mi]])
                    S.op(ce, cm,
                         u[:, c0:c1], pM[mi][:, 0:c1 - c0], R=[bpM[mi]], W=[b_u])
                    yield

                yield
                r1, r2 = rstd_multi([(u[:, 0:256], 256), (u[:, 256:384], 128)], 4, [b_u], cqn, b_cqn)
                S.op("dve", "tensor_scalar", cqn[:], u[:, 0:256], r1, None, ALU.mult, R=[b_u, b_stc[4]], W=[b_cqn])
                ti = nxt("T", 2)
                for c in range(2):
                    transpose_to(ti, c * 128, cqn[:, c * 128:(c + 1) * 128], [b_cqn])
                S.op("dve", "tensor_copy", cqnT[:], pT[ti][:, 0:256].rearrange("p (c n) -> p c n", c=2), R=[bpT[ti]], W=[b_cqnT])
                yield
                for half in range(2):
                    mi = nxt("M", 2)
                    for c in range(2):
                        S.op("pe", "matmul", pM[mi][:, 0:384], lhsT=cqnT[:, c, :], rhs=w_uq[:, c, half * 384:(half + 1) * 384],
                             start=(c == 0), stop=(c == 1), R=[b_cqnT, b_wuq], W=[bpM[mi]])
                    S.op(ce, cm, q_sb[:, half * 4:(half + 1) * 4, :],
                         pM[mi][:, 0:384].rearrange("p (h d) -> p h d", h=4), R=[bpM[mi]], W=[b_q])
                yield
                S.op("dve", "tensor_copy", q_sb[:, 8, 64:96], u[:, 384:416], R=[b_u], W=[b_q])
                S.op("dve", "tensor_copy", qn_sb[:], q_sb[:, 0:8, 0:64], R=[b_q], W=[b_qn])
                rope("dve", qpe_sb[:], q_sb[:, :, 64:96], tabs["cos_m"][:, t, :], tabs["sin_m"][:, t, :], 9, 16, [b_q], [b_qpe])
                ti = nxt("T", 2)
                for h in range(8):
                    transpose_to(ti, h * 128, qn_sb[:, h, :], [b_qn])
                S.op(ce, cm, QnT[:], pT[ti][0:64, 0:1024].rearrange("p (c n) -> p c n", c=8), R=[bpT[ti]], W=[b_QnT])
                ti = nxt("T", 2)
                for h in range(8):
                    transpose_to(ti, h * 128, qpe_sb[:, h, :], [b_qpe])
                S.op(ce, cm, QpeT[0:32], pT[ti][0:32, 0:1024].rearrange("p (c n) -> p c n", c=8), R=[bpT[ti]], W=[b_QpeT])
                yield
                for hg in range(2):
                    mi = nxt("M", 2)
                    for j in range(4):
                        h = hg * 4 + j
                        S.op("pe", "matmul", pM[mi][:, j * 128:(j + 1) * 128], lhsT=wukT[:, h, :],
                             rhs=QnT[:, h, :], start=True, stop=True, R=[b_wkv, b_QnT], W=[bpM[mi]])
                    S.op(ce, cm, QabsT[:, hg * 4:(hg + 1) * 4, :],
                         pM[mi][:, 0:512].rearrange("p (c n) -> p c n", c=4), R=[bpM[mi]], W=[b_Qabs])

                yield
                S.op("dve", "scalar_tensor_tensor", Clat[:, t, 0:128], u[:, 256:384], r2, gckv[:], ALU.mult, ALU.mult,
                     R=[b_u, b_stc[4], b_tab], W=[bClat[t]])
                ti = nxt("T", 2)
                transpose_to(ti, 0, Clat[:, t, 0:128], [bClat[t]])
                S.op("dve", "tensor_copy", KlatT[:, t * 128:(t + 1) * 128], pT[ti][:, 0:128], R=[bpT[ti]], W=[bKlat[t]])
                ti = nxt("T", 2)
                transpose_to(ti, 0, qpe_sb[:, 8, :], [b_qpe])
                S.op("dve", "tensor_copy", KpeT[0:32, t * 128:(t + 1) * 128], pT[ti][0:32, 0:128], R=[bpT[ti]], W=[bKpe[t]])

                yield
                uv = u[:, 416:1696].rearrange("p (b d) -> p b d", b=20)
                S.op("dve", "tensor_copy", ub[:, :, 16:64], uv[:, :, 16:64], R=[b_u], W=[b_ub])
                rope("dve", ub[:, :, 0:16], uv[:, :, 0:16], tabs["cos_n"][:, t, :], tabs["sin_n"][:, t, :], 20, 8, [b_u], [b_ub])
                S.op("dve", "tensor_copy", ub[:, 8:12, 0:16], uv[:, 8:12, 0:16], R=[b_u, b_ub], W=[b_ub])
                ti = nxt("T", 2)
                for h in range(8):
                    transpose_to(ti, h * 128, ub[:, h, :], [b_ub])
                S.op(ce, cm, QS[0:64].rearrange("p g j n -> p (g j) n"),
                     pT[ti][0:64, 0:1024].rearrange("p (c n) -> p c n", c=8), R=[bpT[ti]], W=[b_QSq])
                yield
                kvv = u[:, 928:1696].rearrange("p (s g d) -> p s g d", s=6, g=2)
                S.op("dve", "tensor_copy", Vs[:, t, :, 0:64], kvv[:, 3], R=[b_u], W=[bVs[t]])
                S.op("dve", "tensor_copy", Vw[:, t % 8, :, 0:64], kvv[:, 5], R=[b_u], W=[bVw[t % 8]])
                ti = nxt("T", 2)
                for g in range(2):
                    transpose_to(ti, g * 128, ub[:, 12 + g, :], [b_ub])
                    transpose_to(ti, 256 + g * 128, ub[:, 16 + g, :], [b_ub])
                S.op(ce, cm, KsE[0:64, :, t * 128:(t + 1) * 128],
                     pT[ti][0:64, 0:256].rearrange("p (g n) -> p g n", g=2), R=[bpT[ti]], W=[bKs[t]])
                S.op("dve", "tensor_copy", KwT[0:64, :, (t % 8) * 128:(t % 8 + 1) * 128],
                     pT[ti][0:64, 256:512].rearrange("p (g n) -> p g n", g=2), R=[bpT[ti]], W=[bKw[t % 8]])
                yield
                rb = t % 2
                if t == 0:
                    S.op("pool", "memset", rawT[rb][:, :, :, 0:16], 0.0, W=[b_rawT[rb]])
                else:
                    S.op("dve", "tensor_copy", rawT[rb][:, :, :, 0:16], rawT[1 - rb][:, :, :, 128:144],
                         R=[b_rawT[1 - rb]], W=[b_rawT[rb]])
                ti = nxt("T", 2)
                for c in range(4):
                    transpose_to(ti, c * 128, ub[:, 8 + c, :], [b_ub])
                S.op(ce, cm, rawT[rb][:, :, :, 16:144],
                     pT[ti][0:64, 0:512].rearrange("p (k g n) -> p k g n", k=2, g=2), R=[bpT[ti]], W=[b_rawT[rb]])
                yield
                S.op("act", "activation", gate[:].rearrange("p b h -> p (b h)"), u[:, 1696:1720], AF.Tanh, scale=0.5, R=[b_u], W=[b_gate])
                S.op("dve", "tensor_scalar", gate[:].rearrange("p b h -> p (b h)"), gate[:].rearrange("p b h -> p (b h)"), 0.5, 0.5,
                     ALU.mult, ALU.add, R=[b_gate], W=[b_gate])

                yield
                mi = nxt("M", 2)
                for kv in range(2):
                    for g in range(2):
                        c0 = (kv * 2 + g) * 8
                        for l in range(32):
                            S.op("pe", "matmul", pM[mi][:, c0:c0 + 8], lhsT=w1[:, kv, l, :], rhs=rawT[rb][:, kv, g, l:l + 113:16],
                                 start=(l == 0), stop=(l == 31), R=[b_w1, b_rawT[rb]], W=[bpM[mi]])
                            if l % 8 == 7:
                                yield
                for kv in range(2):
                    S.op("dve", "tensor_scalar", z_sb[:, kv * 16:(kv + 1) * 16], pM[mi][:, kv * 16:(kv + 1) * 16],
                         bias_tot[:, kv:kv + 1], None, ALU.add, R=[bpM[mi], b_bt], W=[b_z])
                S.op("dve", "tensor_tensor", z2_sb[:], z_sb[:], z_sb[:], ALU.mult, R=[b_z], W=[b_z])
                S.op("dve", "tensor_scalar", z2_sb[:], z2_sb[:], 0.044715, 1.0, ALU.mult, ALU.add, R=[b_z], W=[b_z])
                S.op("dve", "tensor_tensor", z2_sb[:], z2_sb[:], z_sb[:], ALU.mult, R=[b_z], W=[b_z])
                S.op("act", "activation", z2_sb[:], z2_sb[:], AF.Tanh, scale=math.sqrt(2.0 / math.pi), R=[b_z], W=[b_z])
                S.op("dve", "tensor_scalar", z2_sb[:], z2_sb[:], 0.5, 0.5, ALU.mult, ALU.add, R=[b_z], W=[b_z])
                S.op("dve", "tensor_tensor", hid_sb[:], z_sb[:], z2_sb[:], ALU.mult, R=[b_z], W=[b_hid])
                yield
                n0 = 8 * t - 1
                m0 = 1 if t == 0 else 0
                mi = nxt("M", 2)
                for g in range(2):
                    S.op("pe", "matmul", pM[mi][0:8, g * 64:(g + 1) * 64], lhsT=hid_sb[:, g * 8:(g + 1) * 8], rhs=w2[:, 0, :],
                         start=True, stop=True, R=[b_hid, b_w1], W=[bpM[mi]])
                for g in range(2):
                    S.op("pe", "matmul", pM[mi][0:64, 128 + g * 8:136 + g * 8], lhsT=w2[:, 1, :], rhs=hid_sb[:, 16 + g * 8:24 + g * 8],
                         start=True, stop=True, R=[b_hid, b_w1], W=[bpM[mi]])
                S.op("dve", "tensor_tensor", kc_f[:].rearrange("p g d -> p (g d)"), pM[mi][0:8, 0:128], b2k[0:8, :], ALU.add,
                     R=[bpM[mi], b_tab], W=[b_kc])
                S.op("dve", "tensor_scalar", VcT[:, :, n0 + m0:n0 + 8], pM[mi][0:64, 128:144].rearrange("p (g m) -> p g m", g=2)[:, :, m0:8],
                     b2v[:, 0:1], None, ALU.add, R=[bpM[mi], b_tab], W=[b_VcT])
                S.op("dve", "tensor_copy", kc_sb[:, :, 16:64], kc_f[:, :, 16:64], R=[b_kc], W=[b_kc])
                rope("dve", kc_sb[:, :, 0:16], kc_f[:, :, 0:16], cos_e[:, t, :], sin_e[:, t, :], 2, 8, [b_kc], [b_kc])
                ti = nxt("T", 2)
                for g in range(2):
                    transpose_to(ti, g * 8, kc_sb[:, g, :], [b_kc])
                S.op("dve", "tensor_copy", KcT[:, :, n0 + m0:n0 + 8],
                     pT[ti][0:64, 0:16].rearrange("p (g m) -> p g m", g=2)[:, :, m0:8], R=[bpT[ti]], W=[b_Kc])
                yield
                nts = sorted(set([max(n0, 0) // 128, (n0 + 7) // 128]))
                for nt in nts:
                    ti = nxt("T", 2)
                    for g in range(2):
                        S.op("pe", "transpose", pT[ti][:, g * 64:(g + 1) * 64], VcT[:, g, nt * 128:(nt + 1) * 128], ident[0:64, 0:64],
                             R=[b_VcT, b_ident], W=[bpT[ti]])
                    S.op("dve", "tensor_copy", VcO[:, nt, :, 0:64], pT[ti][:, 0:128].rearrange("p (g d) -> p g d", g=2), R=[bpT[ti]], W=[b_VcO])

            def attn_loop(kts, qk_fn, post_fn):
                if not kts:
                    return
                si_next = qk_fn(kts[0])
                for i_, kt in enumerate(kts):
                    si = si_next
                    if i_ + 1 < len(kts):
                        si_next = qk_fn(kts[i_ + 1])
                    post_fn(kt, si)

            def mla(s, t):
                par = t % 2
                QpeT, b_QpeT = QpeT2[par], b_QpeT2[par]
                QabsT, b_Qabs = QabsT2[par], b_Qabs2[par]
                QS, b_QSq, b_QSs = QS2[par], b_QSq2[par], b_QSs2[par]
                gate, b_gate = gate2[par], b_gate2[par]
                mo = nxt("M", 2)
                for hg in range(2):
                    qa = QabsT[:, hg * 4:(hg + 1) * 4, :].rearrange("p c n -> p (c n)")
                    qp = QpeT[:, hg * 4:(hg + 1) * 4, :].rearrange("p c n -> p (c n)")

                    def qk(kt):
                        si = nxt("S", 2)
                        S.op("pe", "matmul", pS[si][:], lhsT=KlatT[:, kt * 128:(kt + 1) * 128], rhs=qa,
                             start=True, stop=False, R=[bKlat[kt], b_Qabs], W=[bpS[si]])
                        S.op("pe", "matmul", pS[si][:], lhsT=KpeT[:, kt * 128:(kt + 1) * 128], rhs=qp,
                             start=False, stop=True, R=[bKpe[kt], b_QpeT], W=[bpS[si]])
                        return si

                    def post(kt, si):
                        pi = nxt("P", 3)
                        S.op("act", "activation", PT[pi][:], pS[si][:], AF.Exp, R=[bpS[si]], W=[b_PT[pi]])
                        if kt == t:
                            S.op("dve", "tensor_tensor", PT[pi][:], PT[pi][:], tri4[:], ALU.mult, R=[b_PT[pi], b_msk], W=[b_PT[pi]])
                        S.op("pe", "matmul", pA[0][:], lhsT=Clat[:, kt, 0:128], rhs=PT[pi][:], start=(kt == 0), stop=(kt == t),
                             R=[b_PT[pi], bClat[kt]], W=[bpA[0]])
                        if kt == 0:
                            S.op("dve", "tensor_copy", lacc[:], PT[pi][:], R=[b_PT[pi]], W=[b_lacc])
                        else:
                            S.op("dve", "tensor_tensor", lacc[:], lacc[:], PT[pi][:], ALU.add, R=[b_PT[pi], b_lacc], W=[b_lacc])

                    attn_loop(list(range(t + 1)), qk, post)
                    for j in range(4):
                        S.op("pe", "matmul", pA[1][:, j:j + 1], lhsT=lacc[:, j * 128:(j + 1) * 128], rhs=ones_f[:, 0:1],
                             start=True, stop=True, R=[b_lacc, b_ones], W=[bpA[1]])
                    S.op("dve", "tensor_copy", OlatT[:], pA[0][:].rearrange("p (c n) -> p c n", c=4), R=[bpA[0]], W=[b_OlatT])
                    S.op("dve", "reciprocal", rl[:, hg * 4:(hg + 1) * 4], pA[1][:, 0:4], R=[bpA[1]], W=[b_rl])
                    for j in range(4):
                        h = hg * 4 + j
                        S.op("pe", "matmul", pM[mo][:, h * 64:(h + 1) * 64], lhsT=OlatT[:, j, :], rhs=wuv[:, h, :],
                             start=True, stop=True, R=[b_OlatT, b_wkv], W=[bpM[mo]])
                S.op("dve", "tensor_tensor", y_sb[:, 0:512].rearrange("p (h d) -> p h d", h=8),
                     pM[mo][:, 0:512].rearrange("p (h d) -> p h d", h=8), rl[:, 0:8, None].to_broadcast([128, 8, 64]), ALU.mult,
                     R=[bpM[mo], b_rl], W=[b_y])

            def nsa_sel(s, t, g):
                par = t % 2
                QpeT, b_QpeT = QpeT2[par], b_QpeT2[par]
                QabsT, b_Qabs = QabsT2[par], b_Qabs2[par]
                QS, b_QSq, b_QSs = QS2[par], b_QSq2[par], b_QSs2[par]
                gate, b_gate = gate2[par], b_gate2[par]
                QSg = QS[:, g].rearrange("p j n -> p (j n)")
                QSg = QS[:, g].rearrange("p j n -> p (j n)")
                nts = [0] + ([1] if t >= 16 else [])
                for nt in nts:
                    si = nxt("M", 2)
                    S.op("pe", "matmul", pM[si][:], lhsT=KcT[:, g, nt * 128:(nt + 1) * 128], rhs=QSg[0:64, :],
                         start=True, stop=True, R=[b_Kc, b_QSq], W=[bpM[si]])
                    S.op("act", "activation", PcT[nt][:], pM[si][:], AF.Exp, R=[bpM[si]], W=[b_PcT[nt]])
                    S.op("pool", "affine_select", PcT[nt][:].rearrange("p (j n) -> p j n", j=4),
                         PcT[nt][:].rearrange("p (j n) -> p j n", j=4), [[0, 4], [1, 128]], ALU.is_ge, 0.0,
                         base=128 * t - 31 - 2048 * nt, channel_multiplier=-16, R=[b_PcT[nt]], W=[b_PcT[nt]])
                yield
                mc = nxt("M", 2)
                for j in range(4):
                    for i_, nt in enumerate(nts):
                        S.op("pe", "matmul", pM[mc][:, j * 128:(j + 1) * 128], lhsT=PcT[nt][:, j * 128:(j + 1) * 128],
                             rhs=VcO[:, nt, g, :], start=(i_ == 0), stop=(i_ == len(nts) - 1),
                             R=[b_PcT[nt], b_VcO], W=[bpM[mc]])
                yield
                pc = pM[mc][:].rearrange("p (j c) -> p j c", j=4)
                S.op("dve", "tensor_reduce", st[:, 24:28], pc[:, :, 64:128], AX.X, ALU.add, R=[bpM[mc]], W=[b_stn])
                S.op("dve", "tensor_scalar", st[:, 24:28], st[:, 24:28], 1e-30, None, ALU.max, R=[b_stn], W=[b_stn])
                S.op("dve", "reciprocal", st[:, 28:32], st[:, 24:28], R=[b_stn], W=[b_stn])
                S.op("dve", "tensor_tensor", tmp4[:], pc[:, :, 64:128], st[:, 28:32, None].to_broadcast([128, 4, 64]), ALU.mult,
                     R=[bpM[mc], b_stn], W=[b_tmp4])
                S.op("dve", "tensor_reduce", imp[:], tmp4[:].rearrange("p j c -> p c j"), AX.X, ALU.add, R=[b_tmp4], W=[b_imp])
                yield
                S.op("dve", "tensor_tensor", st[:, 32:36], st[:, 28:32], gate[:, 0, g * 4:(g + 1) * 4], ALU.mult,
                     R=[b_stn, b_gate], W=[b_stn])
                S.op("dve", "tensor_tensor", y_sb[:, 512 + g * 256:768 + g * 256].rearrange("p (j c) -> p j c", j=4), pc[:, :, 0:64],
                     st[:, 32:36, None].to_broadcast([128, 4, 64]), ALU.mult, R=[bpM[mc], b_stn], W=[b_yn])
                yield
                S.op("pool", "affine_select", sc1[:], imp[:], [[-64, 64]], ALU.is_ge, 1e9, base=128 * t - 128, channel_multiplier=1,
                     R=[b_imp], W=[b_imp])
                S.op("pool", "affine_select", sc2[:], sc1[:], [[-64, 64]], ALU.is_ge, -1e9, base=128 * t, channel_multiplier=1,
                     R=[b_imp], W=[b_imp])
                S.op("pool", "memset", sc2[:, 0:1], 1e9, R=[b_imp], W=[b_imp])
                yield
                S.op("dve", "max", st[:, 40:48], sc2[:], R=[b_imp], W=[b_stn])
                S.op("dve", "match_replace", sc3[:], st[:, 40:48], sc2[:], -3e38, R=[b_imp, b_stn], W=[b_imp])
                S.op("dve", "max", st[:, 48:56], sc3[:], R=[b_imp], W=[b_stn])
                S.op("dve", "tensor_scalar", selq[:, 64:128], sc2[:], st[:, 55:56], NEG, ALU.is_lt, ALU.mult,
                     R=[b_imp, b_stn], W=[b_selq])
                yield
                ti = nxt("T", 2)
                transpose_to(ti, 0, selq[:], [b_selq])
                S.op("dve", "tensor_copy", QS[64:128, g], pT[ti][64:128, None, 0:128].to_broadcast([64, 4, 128]),
                     R=[bpT[ti]], W=[b_QSs[g]])
                yield

            def nsa_attn(s, t, g):
                par = t % 2
                QpeT, b_QpeT = QpeT2[par], b_QpeT2[par]
                QabsT, b_Qabs = QabsT2[par], b_Qabs2[par]
                QS, b_QSq, b_QSs = QS2[par], b_QSq2[par], b_QSs2[par]
                gate, b_gate = gate2[par], b_gate2[par]
                QSg = QS[:, g].rearrange("p j n -> p (j n)")
                S.op("dve", "memset", pA[0][:, 0:260], 0.0, W=[bpA[0]])
                S.op("dve", "memset", pA[1][:, 0:260], 0.0, W=[bpA[1]])
                def qk_s(kt):
                    si = nxt("S", 2)
                    S.op("pe", "matmul", pS[si][:], lhsT=KsE[:, g, kt * 128:(kt + 1) * 128], rhs=QSg,
                         start=True, stop=True, R=[bKs[kt], b_exp, b_QSq, b_QSs[g]], W=[bpS[si]])
                    return si

                def post_s(kt, si):
                    pi = nxt("P", 3)
                    S.op("act", "activation", PT[pi][:], pS[si][:], AF.Exp, R=[bpS[si]], W=[b_PT[pi]])
                    if kt == t:
                        S.op("dve", "tensor_tensor", PT[pi][:], PT[pi][:], tri4[:], ALU.mult, R=[b_PT[pi], b_msk], W=[b_PT[pi]])
                    for j in range(4):
                        S.op("pe", "matmul", pA[0][:, j * 65:j * 65 + 65], lhsT=PT[pi][:, j * 128:(j + 1) * 128],
                             rhs=Vs[:, kt, g, :], start=False, stop=(kt == t), skip_group_check=True,
                             R=[b_PT[pi], bVs[kt]], W=[bpA[0]])

                def qk_w(kt):
                    si = nxt("S", 2)
                    sl = kt % 8
                    S.op("pe", "matmul", pS[si][:], lhsT=KwT[:, g, sl * 128:(sl + 1) * 128], rhs=QSg,
                         start=True, stop=True, R=[bKw[sl], b_QSq, b_QSs[g]], W=[bpS[si]])
                    return si

                def post_w(kt, si):
                    sl = kt % 8
                    pi = nxt("P", 3)
                    S.op("act", "activation", PT[pi][:], pS[si][:], AF.Exp, R=[bpS[si]], W=[b_PT[pi]])
                    if kt == t:
                        S.op("dve", "tensor_tensor", PT[pi][:], PT[pi][:], tri4[:], ALU.mult, R=[b_PT[pi], b_msk], W=[b_PT[pi]])
                    if kt == t - 4:
                        S.op("dve", "tensor_tensor", PT[pi][:], PT[pi][:], anti4[:], ALU.mult, R=[b_PT[pi], b_msk], W=[b_PT[pi]])
                    for j in range(4):
                        S.op("pe", "matmul", pA[1][:, j * 65:j * 65 + 65], lhsT=PT[pi][:, j * 128:(j + 1) * 128],
                             rhs=Vw[:, sl, g, :], start=False, stop=(kt == t), skip_group_check=True,
                             R=[b_PT[pi], bVw[sl]], W=[bpA[1]])

                attn_loop(list(range(t + 1)), qk_s, post_s)
                attn_loop(list(range(max(0, t - 4), t + 1)), qk_w, post_w)
                for br in range(2):
                    pa = pA[br][:, 0:260].rearrange("p (j c) -> p j c", j=4)
                    S.op("dve", "reciprocal", st[:, 56:60], pa[:, :, 64], R=[bpA[br]], W=[b_stn])
                    S.op("dve", "tensor_tensor", st[:, 60:64], st[:, 56:60], gate[:, 1 + br, g * 4:(g + 1) * 4], ALU.mult,
                         R=[b_stn, b_gate], W=[b_stn])
                    yv = y_sb[:, 512 + g * 256:768 + g * 256].rearrange("p (j c) -> p j c", j=4)
                    S.op("dve", "tensor_tensor", tmp4[:], pa[:, :, 0:64], st[:, 60:64, None].to_broadcast([128, 4, 64]), ALU.mult,
                         R=[bpA[br], b_stn], W=[b_tmp4])
                    S.op("dve", "tensor_tensor", yv, yv, tmp4[:], ALU.add, R=[b_tmp4, b_yn], W=[b_yn])


            def outproj(s, t):
                xb = t % 2
                ra, rb_ = rstd_multi([(y_sb[:, 0:512], 512), (y_sb[:, 512:1024], 512)], 12, [b_y, b_yn], mixed, b_mixed)
                S.op("dve", "tensor_scalar", mixed[:, 0:512], y_sb[:, 0:512], ra, None, ALU.mult, R=[b_y, b_stc[12]], W=[b_mixed])
                S.op("dve", "tensor_scalar", mixed[:, 512:1024], y_sb[:, 512:1024], rb_, None, ALU.mult, R=[b_yn, b_stc[12]], W=[b_mixed])
                ti = nxt("T", 2)
                for c in range(8):
                    transpose_to(ti, c * 128, mixed[:, c * 128:(c + 1) * 128], [b_mixed])
                S.op("dve", "tensor_copy", mixedT[:], pT[ti][:, 0:1024].rearrange("p (c n) -> p c n", c=8), R=[bpT[ti]], W=[b_mixedT])
                for dh in range(2):
                    mi = nxt("S", 2)
                    for c in range(8):
                        S.op("pe", "matmul", pS[mi][:], lhsT=mixedT[:, c, :], rhs=w_o[:, c, dh * 512:(dh + 1) * 512],
                             start=(c == 0), stop=(c == 7), R=[b_mixedT, b_wo], W=[bpS[mi]])
                    S.op("dve", "tensor_tensor", h_sb[xb][:, dh * 512:(dh + 1) * 512], pS[mi][:], xs[xb][:, dh * 512:(dh + 1) * 512],
                         ALU.add, R=[bpS[mi], b_xs[xb]], W=[b_h[xb]])
                row = (s * SEQ + t * 128)
                S.dma("sp", hscr_d[row:row + 128, :], h_sb[xb][:], R=[b_h[xb]])

            bgst = {"gen": None, "credit": 0.0, "rate": 0.0}

            def bg_run(n=None):
                if bgst["gen"] is None:
                    return
                mode["bg"] = True
                try:
                    k = 0
                    while n is None or k < n:
                        next(bgst["gen"])
                        k += 1
                except StopIteration:
                    bgst["gen"] = None
                mode["bg"] = False

            def tick():
                if mode["bg"] or bgst["gen"] is None:
                    return
                bgst["credit"] += bgst["rate"]
                if bgst["credit"] >= 1.0:
                    n = int(bgst["credit"])
                    bgst["credit"] -= n
                    bg_run(n)

            S.tick = tick
            def chain(*gens):
                for g_ in gens:
                    yield from g_

            def set_bg(gen, nchunks, fg_ops):
                bgst["gen"] = gen
                bgst["credit"] = 0.0
                bgst["rate"] = nchunks / (0.7 * fg_ops)

            for s in range(nseq):
                bgst["gen"] = phase1(s, 0) if "p1" in stages else None
                bg_run(None)
                for t in range(ntiles):
                    if "nsa" in stages:
                        set_bg(chain(nsa_sel(s, t, 0), nsa_sel(s, t, 1)), 16.0, 30.0 + 18.0 * (t + 1))
                    if "mla" in stages:
                        mla(s, t)
                    bg_run(None)
                    if t + 1 < ntiles and "p1" in stages:
                        set_bg(phase1(s, t + 1), 40.0, 100.0 + 14.0 * (t + 1 + min(t + 1, 5)))
                    if "nsa" in stages:
                        nsa_attn(s, t, 0)
                        nsa_attn(s, t, 1)
                    if "out" in stages:
                        outproj(s, t)
                    bg_run(None)
            S.tick = None
            S.barrier()
        mode["B"] = True
        B = ExitStack()
        with B:
            def sb2(name, shape, dt):
                return B.enter_context(nc.sbuf_tensor("b_" + name, shape, dt))
            gb = sb2("gb", [128, 8], F32); b_gb = S.buf("gb")
            S.dma("sp", gb[:], g_mlp_d, W=[b_gb])
            gfin = sb2("gfin", [128, D], F32)
            S.dma("sp", gfin[:], gfin_d, W=[b_gb])
            ident2 = sb2("ident2", [128, 128], BF16); b_id2 = S.buf("ident2")
            S.dma("pool", ident2[:], ident_d, W=[b_id2])
            w_up = sb2("w_up", [128, 8, DFF], BF16); b_wupb = S.bufs("w_up", 8)
            wuv_ = w_up_d.rearrange("(c p) n -> p c n", p=128)
            for fb in range(8):
                S.dma("pool", w_up[:, :, fb * 512:(fb + 1) * 512], wuv_[:, :, fb * 512:(fb + 1) * 512], W=[b_wupb[fb]])
                S.op("dve", "tensor_tensor", w_up[:, :, fb * 512:(fb + 1) * 512], w_up[:, :, fb * 512:(fb + 1) * 512],
                     gb[:, 0:8, None].to_broadcast([128, 8, 512]), ALU.mult, R=[b_gb, b_wupb[fb]], W=[b_wupb[fb]])
            w_dn = sb2("w_dn", [128, 32, D], BF16); b_wdn = S.buf("w_dn")
            wdv = w_down_d.rearrange("(f p) n -> p f n", p=128)
            for f4 in range(8):
                S.dma("pool", w_dn[:, f4 * 4:(f4 + 1) * 4, :], wdv[:, f4 * 4:(f4 + 1) * 4, :], W=[b_wdn])
            hin = [sb2("hin%d" % i, [128, D], F32) for i in range(4)]; b_hin = S.bufs("hin", 4)
            st2 = sb2("st2", [128, 16], F32); b_st2 = S.buf("st2")
            junk2 = sb2("junk2", [128, D], BF16); b_junk2 = S.buf("junk2")
            hn = sb2("hn", [128, D], BF16); b_hn = S.buf("hn")
            hnT = sb2("hnT", [128, 8, 512], BF16); b_hnT = S.buf("hnT")
            rl = [sb2("rl%d" % i, [128, 512], BF16) for i in range(2)]; b_rl = S.bufs("rl", 2)
            aT = sb2("aT", [128, 32, 512], BF16); b_aT = S.buf("aT")
            yo = [sb2("yo%d" % i, [128, D], F32) for i in range(2)]; b_yo = S.bufs("yo", 2)
            S.op("pool", "memset", st2[:], 0.0, W=[b_st2])
            nT = (nseq * ntiles * 128) // 512 if do_mlp else 0

            def rstd2(src_ap, Rb):
                S.op("pool", "memset", st2[:, 0:1], 0.0, W=[b_st2])
                S.op("act", "activation", junk2[:], src_ap, AF.Square, accum_out=st2[:, 0:1], R=Rb, W=[b_junk2, b_st2])
                S.op("dve", "tensor_scalar", st2[:, 1:2], st2[:, 0:1], 1.0 / D, EPS, ALU.mult, ALU.add, R=[b_st2], W=[b_st2])
                S.op("act", "activation", st2[:, 2:3], st2[:, 1:2], AF.Sqrt, R=[b_st2], W=[b_st2])
                S.op("dve", "reciprocal", st2[:, 3:4], st2[:, 2:3], R=[b_st2], W=[b_st2])
                return st2[:, 3:4]

            oc = 0
            for T in range(nT):
                hb = T % 2
                row = T * 512 if nseq * ntiles * 128 == nseq * SEQ else None
                base = (T * 512 // (ntiles * 128)) * SEQ + (T * 512) % (ntiles * 128)
                for i in range(4):
                    S.dma("sp", hin[i][:], hscr_d[base + i * 128:base + (i + 1) * 128, :], W=[b_hin[i]])
                for i in range(4):
                    r = rstd2(hin[i][:], [b_hin[i]])
                    S.op("dve", "tensor_scalar", hn[:], hin[i][:], r, None, ALU.mult, R=[b_hin[i], b_st2], W=[b_hn])
                    ti = nxt("T", 2)
                    for c in range(8):
                        S.op("pe", "transpose", pT[ti][:, c * 128:(c + 1) * 128], hn[:, c * 128:(c + 1) * 128], ident2[:],
                             R=[b_hn, b_id2], W=[bpT[ti]])
                    S.op("act", "copy", hnT[:, :, i * 128:(i + 1) * 128], pT[ti][:, 0:1024].rearrange("p (c n) -> p c n", c=8),
                         R=[bpT[ti]], W=[b_hnT])
                for f in range(32):
                    si = nxt("S", 2)
                    for c in range(8):
                        S.op("pe", "matmul", pS[si][:], lhsT=w_up[:, c, f * 128:(f + 1) * 128], rhs=hnT[:, c, :],
                             start=(c == 0), stop=(c == 7), R=[b_wupb[f // 4], b_hnT], W=[bpS[si]])
                    ri = f % 2
                    S.op("act", "activation", rl[ri][:], pS[si][:], AF.Relu, R=[bpS[si]], W=[b_rl[ri]])
                    S.op("pool", "tensor_tensor", aT[:, f, :], rl[ri][:], rl[ri][:], ALU.mult, R=[b_rl[ri]], W=[b_aT])
                for i in range(4):
                    ob = oc % 2
                    oc += 1
                    for dh in range(2):
                        mi = nxt("M", 2)
                        for f in range(32):
                            S.op("pe", "matmul", pM[mi][:], lhsT=aT[:, f, i * 128:(i + 1) * 128], rhs=w_dn[:, f, dh * 512:(dh + 1) * 512],
                                 start=(f == 0), stop=(f == 31), R=[b_aT, b_wdn], W=[bpM[mi]])
                        S.op("dve", "tensor_tensor", yo[ob][:, dh * 512:(dh + 1) * 512], pM[mi][:], hin[i][:, dh * 512:(dh + 1) * 512],
                             ALU.add, R=[bpM[mi], b_hin[i]], W=[b_yo[ob]])
                    r = rstd2(yo[ob][:], [b_yo[ob]])
                    S.op("dve", "scalar_tensor_tensor", yo[ob][:], yo[ob][:], r, gfin[:], ALU.mult, ALU.mult,
                         R=[b_yo[ob], b_st2, b_gb], W=[b_yo[ob]])
                    S.dma("sp", out_d[base + i * 128:base + (i + 1) * 128, :], yo[ob][:], R=[b_yo[ob]])
            S.emit()
            print('NOPS', S.nops)
    return nc


def _rope_tab(pos, dim):
    inv = np.exp(np.float32(-math.log(500000.0)) * np.arange(0, dim, 2, dtype=np.float32) / np.float32(dim)).astype(np.float32)
    ang = pos.astype(np.float32)[:, None] * inv[None, :]
    return np.cos(ang).astype(np.float32), np.sin(ang).astype(np.float32)


def _tok_major(a):
    return np.ascontiguousarray(a.reshape(NT, 128, -1).transpose(1, 0, 2))


def host_consts():
    pos = np.arange(SEQ)
    cm, sm = _rope_tab(pos, 32)
    cn, sn = _rope_tab(pos, 16)
    c = {}
    c["cos_m"], c["sin_m"] = _tok_major(cm), _tok_major(sm)
    c["cos_n"], c["sin_n"] = _tok_major(cn), _tok_major(sn)
    c["cos_n8"], c["sin_n8"] = _tok_major(cn * np.float32(0.125)), _tok_major(sn * np.float32(0.125))
    ce = np.zeros((8, NT, 8), np.float32)
    se = np.zeros((8, NT, 8), np.float32)
    for t in range(NT):
        for m in range(8):
            n = 8 * t - 1 + m
            if 0 <= n < 255:
                p = 16 * n + 31
                ce[m, t], se[m, t] = cn[p], sn[p]
    c["cos_e"], c["sin_e"] = ce, se
    k = np.arange(128)[:, None]
    q = np.arange(128)[None, :]
    tri = (q >= k).astype(np.float32)
    anti = (q < k).astype(np.float32)
    c["tri4"] = np.ascontiguousarray(np.tile(tri, (1, 4)))
    c["anti4"] = np.ascontiguousarray(np.tile(anti, (1, 4)))
    n = np.arange(256)[:, None] * 16
    j = np.arange(64)[None, :] * 64
    ov = np.clip(np.minimum(n + 32, j + 64) - np.maximum(n, j), 0, None).astype(np.float32) / 32.0
    ov[255] = 0.0
    c["ovl"] = np.ascontiguousarray(ov.reshape(2, 128, 64).transpose(1, 0, 2))
    c["expand"] = (np.arange(SEQ)[None, :] // 64 == np.arange(64)[:, None]).astype(np.float32)
    c["ident"] = np.eye(128, dtype=np.float32)
    return c


def host_weights(inp):
    f = lambda a: np.ascontiguousarray(np.asarray(a, dtype=np.float32))
    pc = lambda v: np.ascontiguousarray(np.asarray(v, np.float32).reshape(-1, 128).T)
    w = {}
    w["w_in"] = f(inp["w_in"][0])
    w["g_mix"] = pc(inp["g_mix_norm"][0])
    w["w_uq"] = f(inp["w_uq"][0])
    w["g_cq"] = pc(inp["g_cq"][0])
    wukv = np.asarray(inp["w_ukv"][0], np.float32).reshape(128, 8, 2, 64)
    wuk = wukv[:, :, 0, :]
    w["wukT"] = np.ascontiguousarray(wuk.transpose(2, 1, 0))
    w["wuv"] = np.ascontiguousarray(wukv[:, :, 1, :])
    w["gckv_bc"] = np.ascontiguousarray(np.broadcast_to(np.asarray(inp["g_ckv"][0], np.float32)[None, :], (128, 128)))
    w1 = np.stack([np.asarray(inp["cmp_w1_k"][0], np.float32), np.asarray(inp["cmp_w1_v"][0], np.float32)])
    w["cmp_w1"] = np.ascontiguousarray(w1.reshape(2, 32, 64, 128).transpose(0, 2, 1, 3))
    pe = np.stack([np.asarray(inp["cmp_pe_k"][0], np.float32), np.asarray(inp["cmp_pe_v"][0], np.float32)])
    w["cmp_peT"] = np.ascontiguousarray(pe.transpose(0, 2, 1))
    w["cmp_b1"] = np.ascontiguousarray(np.stack([inp["cmp_b1_k"][0], inp["cmp_b1_v"][0]], axis=1).astype(np.float32))
    w["cmp_w2"] = np.ascontiguousarray(np.stack([inp["cmp_w2_k"][0], inp["cmp_w2_v"][0]], axis=1).astype(np.float32))
    b2k = np.asarray(inp["cmp_b2_k"][0], np.float32)
    w["cmp_b2k_bc"] = np.ascontiguousarray(np.broadcast_to(np.concatenate([b2k, b2k])[None, :], (128, 128)))
    w["cmp_b2v"] = np.ascontiguousarray(np.asarray(inp["cmp_b2_v"][0], np.float32).reshape(64, 1))
    w["w_o"] = f(inp["w_o"][0])
    w["g_out"] = pc(np.concatenate([np.asarray(inp["g_out_mla"][0]), np.asarray(inp["g_out_nsa"][0])]))
    w["w_up"] = f(inp["w_up"][0])
    w["g_mlp"] = pc(inp["g_mlp_norm"][0])
    w["w_down"] = f(inp["w_down"][0])
    w["gfin_bc"] = np.ascontiguousarray(np.broadcast_to(np.asarray(inp["g_final"], np.float32)[None, :], (128, D)))
    return w


_NC_CACHE = {}


def kernel(**inputs):
    x = np.asarray(inputs["x"], dtype=np.float32)
    shared = host_consts()
    shared.update(host_weights(inputs))
    if "full" not in _NC_CACHE:
        _NC_CACHE["full"] = build_program()
    nc = _NC_CACHE["full"]
    in_maps = []
    for c in range(NCORES):
        m = dict(shared)
        m["x"] = np.ascontiguousarray(x[c * NSEQ:(c + 1) * NSEQ])
        in_maps.append(m)
    res = run_bass_kernel_spmd(nc, in_maps, core_ids=list(range(NCORES)))
    outs = [np.asarray(r["out"]).reshape(NSEQ, SEQ, D) for r in res.results]
    return np.concatenate(outs, axis=0).astype(np.float32)
```

```python
import math
import numpy as np
from contextlib import ExitStack
import concourse.bass as bass
import concourse.mybir as mybir
from concourse.bass_utils import run_bass_kernel_spmd

F32 = mybir.dt.float32
BF16 = mybir.dt.bfloat16
I32 = mybir.dt.int32
AF = mybir.ActivationFunctionType
ALU = mybir.AluOpType
AX = mybir.AxisListType

NCORES = 8
SEQ = 4096
D = 1024
NSEQ = 2
NT = SEQ // 128
EPS = 1e-6
IN_COLS = 1720
DFF = 4096
NEG = -30000.0
STRICT_SAME = True
OP_LIMIT = None
BG_OVERLAP = True
USE_MAGIC = True
ACT_COPY_T = 20


class Buf:
    __slots__ = ("name", "w", "r", "dsem", "dcnt", "excl")

    def __init__(self, name):
        self.name = name
        self.excl = False
        self.w = None
        self.r = {}
        self.dsem = None
        self.dcnt = 0


class Sched:
    ENG = ("pe", "act", "dve", "pool", "sp")

    def __init__(self, nc, ctx):
        self.nc = nc
        self.ctx = ctx
        self.sem = {e: ctx.enter_context(nc.semaphore("s_" + e)) for e in self.ENG}
        self.cnt = {e: 0 for e in self.ENG}
        self.seen = {e: {} for e in self.ENG}
        self.prog = {e: [] for e in self.ENG}
        self.dbufs = []
        self.nb = 0
        self.nops = 0
        self.fillregs = {}
        self.tick = None
        self.limit = OP_LIMIT

    def buf(self, name):
        self.nb += 1
        return Buf("%s_%d" % (name, self.nb))

    def bufs(self, name, n):
        return [self.buf(name) for _ in range(n)]

    def _dsem(self, b):
        if b.dsem is None:
            b.dsem = self.ctx.enter_context(self.nc.semaphore("d_" + b.name))
            self.dbufs.append(b)
        return b.dsem

    def _deps(self, e, reads, writes, strict):
        toks = []
        for b in reads:
            if b.w is not None:
                toks.append(b.w)
            if b.excl:
                toks.extend(b.r.values())
        for b in writes:
            if b.w is not None:
                toks.append(b.w)
            toks.extend(b.r.values())
        need = {}
        for (key, sem, val) in toks:
            if key == e and not strict and (e == "pe" or not STRICT_SAME):
                continue
            if self.seen[e].get(key, 0) >= val:
                continue
            if key not in need or need[key][1] < val:
                need[key] = (sem, val)
        for key, (sem, val) in need.items():
            self.seen[e][key] = val
            self.prog[e].append(("wait", sem, val))

    def op(self, e, meth, *args, R=(), W=(), **kw):
        self.nops += 1
        if self.limit is not None and self.nops > self.limit:
            return None
        self._deps(e, R, W, False)
        self.cnt[e] += 1
        tok = (e, self.sem[e], self.cnt[e])
        self.prog[e].append(("op", meth, args, kw))
        for b in R:
            b.r[e] = tok
        for b in W:
            b.w = tok
            b.r = {}
        if self.tick is not None:
            self.tick()
        return tok

    def dma(self, q, out, in_, R=(), W=(), **kw):
        self.nops += 1
        if self.limit is not None and self.nops > self.limit:
            return None
        self._deps(q, R, W, True)
        owner = W[0] if W else R[0]
        sem = self._dsem(owner)
        owner.dcnt += 16
        tok = ("d_" + owner.name, sem, owner.dcnt)
        self.prog[q].append(("dma", out, in_, kw, sem))
        for b in R:
            b.r[tok[0]] = tok
        for b in W:
            b.w = tok
            b.r = {}
        return tok

    def barrier(self):
        toks = [(e, self.sem[e], self.cnt[e]) for e in self.ENG if self.cnt[e] > 0]
        toks += [("d_" + b.name, b.dsem, b.dcnt) for b in self.dbufs if b.dcnt > 0]
        for e in self.ENG:
            for (key, sem, val) in toks:
                if self.seen[e].get(key, 0) >= val:
                    continue
                self.seen[e][key] = val
                self.prog[e].append(("wait", sem, val))

    def flush(self):
        nc = self.nc
        with nc.Block() as block:
            def replay(e):
                def f(eng):
                    sem_e = self.sem[e]
                    for it in self.prog[e]:
                        if it[0] == "wait":
                            eng.wait_ge(it[1], it[2])
                        elif it[0] == "op":
                            args = it[2]
                            if it[1] == "affine_select":
                                args = list(args)
                                if args[4] not in self.fillregs:
                                    self.fillregs[args[4]] = eng.to_reg(args[4])
                                args[4] = self.fillregs[args[4]]
                            getattr(eng, it[1])(*args, **it[3]).then_inc(sem_e, 1)
                        else:
                            eng.dma_start(out=it[1], in_=it[2], **it[3]).then_inc(it[4], 16)
                return f
            block.tensor(replay("pe"))
            block.scalar(replay("act"))
            block.vector(replay("dve"))
            block.gpsimd(replay("pool"))
            block.sync(replay("sp"))
        self.prog = {e: [] for e in self.ENG}

    def emit(self):
        self.barrier()
        self.flush()


def build_program(nseq=NSEQ, ntiles=NT, do_mlp=True, stages=("p1", "mla", "nsa", "out")):
    nc = bass.Bass("TRN2", target_bir_lowering=False)

    def din(name, shape):
        return nc.dram_tensor(name, list(shape), F32, kind="ExternalInput").ap()

    x_d = din("x", [nseq, SEQ, D])
    w_in_d = din("w_in", [D, IN_COLS])
    g_mix_d = din("g_mix", [128, 8])
    w_uq_d = din("w_uq", [256, 768])
    g_cq_d = din("g_cq", [128, 2])
    wukT_d = din("wukT", [64, 8, 128])
    wuv_d = din("wuv", [128, 8, 64])
    gckv_d = din("gckv_bc", [128, 128])
    w1_d = din("cmp_w1", [2, 64, 32, 128])
    peT_d = din("cmp_peT", [2, 64, 32])
    b1_d = din("cmp_b1", [128, 2])
    w2_d = din("cmp_w2", [128, 2, 64])
    b2k_d = din("cmp_b2k_bc", [128, 128])
    b2v_d = din("cmp_b2v", [64, 1])
    w_o_d = din("w_o", [D, D])
    g_out_d = din("g_out", [128, 8])
    w_up_d = din("w_up", [D, DFF])
    g_mlp_d = din("g_mlp", [128, 8])
    w_down_d = din("w_down", [DFF, D])
    gfin_d = din("gfin_bc", [128, D])
    cosm_d = din("cos_m", [128, NT, 16])
    sinm_d = din("sin_m", [128, NT, 16])
    cosn_d = din("cos_n", [128, NT, 8])
    sinn_d = din("sin_n", [128, NT, 8])
    cosn8_d = din("cos_n8", [128, NT, 8])
    sinn8_d = din("sin_n8", [128, NT, 8])
    cose_d = din("cos_e", [8, NT, 8])
    sine_d = din("sin_e", [8, NT, 8])
    tri_d = din("tri4", [128, 512])
    anti_d = din("anti4", [128, 512])
    ovl_d = din("ovl", [128, 2, 64])
    exp_d = din("expand", [64, SEQ])
    ident_d = din("ident", [128, 128])
    out_d = nc.dram_tensor("out", [nseq * SEQ, D], F32, kind="ExternalOutput").ap()
    hscr_d = nc.dram_tensor("hscr", [nseq * SEQ, D], F32, kind="Internal").ap()

    top = ExitStack()
    with top:
        S = Sched(nc, top)
        def psum(name, shape, dt):
            return top.enter_context(nc.psum_tensor(name, shape, dt))
        pS = [psum("pS%d" % i, [128, 512], F32) for i in range(2)]
        pA = [psum("pA%d" % i, [128, 512], F32) for i in range(2)]
        pM = [psum("pM%d" % i, [128, 512], F32) for i in range(2)]
        pT = [psum("pT%d" % i, [128, 1024], BF16) for i in range(2)]
        bpS = S.bufs("pS", 2)
        bpA = S.bufs("pA", 2)
        bpM = S.bufs("pM", 2)
        bpT = S.bufs("pT", 2)
        for b_ in bpS + bpA + bpM + bpT:
            b_.excl = True
        rr = {"S": 0, "M": 0, "T": 0, "P": 0, "A": 0}
        mode = {"bg": False, "B": False}

        def nxt(kind, n):
            if kind in ("M", "T") and not mode["B"]:
                return 0 if mode["bg"] else 1
            i = rr[kind] % n
            rr[kind] += 1
            return i

        A = ExitStack()
        with A:
            def sb(name, shape, dt):
                return A.enter_context(nc.sbuf_tensor("a_" + name, shape, dt))

            ident = sb("ident", [128, 128], BF16); b_ident = S.buf("ident")
            S.dma("pool", ident[:], ident_d, W=[b_ident])
            tri4 = sb("tri4", [128, 512], BF16); anti4 = sb("anti4", [128, 512], BF16); b_msk = S.buf("msk")
            S.dma("pool", tri4[:], tri_d, W=[b_msk])
            S.dma("pool", anti4[:], anti_d, W=[b_msk])
            tabs = {}
            b_tab = S.buf("tab")
            for nm, d_, w_ in (("cos_m", cosm_d, 16), ("sin_m", sinm_d, 16), ("cos_n", cosn_d, 8), ("sin_n", sinn_d, 8)):
                tabs[nm] = sb(nm, [128, NT, w_], F32)
                S.dma("sp", tabs[nm][:], d_, W=[b_tab])
            cos_e = sb("cos_e", [8, NT, 8], F32); sin_e = sb("sin_e", [8, NT, 8], F32)
            S.dma("sp", cos_e[:], cose_d, W=[b_tab])
            S.dma("sp", sin_e[:], sine_d, W=[b_tab])
            gckv = sb("gckv", [128, 128], F32); b2k = sb("b2k", [128, 128], F32); b2v = sb("b2v", [64, 1], F32)
            b1 = sb("b1", [128, 2], F32)
            S.dma("sp", gckv[:], gckv_d, W=[b_tab])
            S.dma("sp", b2k[:], b2k_d, W=[b_tab])
            S.dma("sp", b2v[:], b2v_d, W=[b_tab])
            S.dma("sp", b1[:], b1_d, W=[b_tab])
            gvec = sb("gvec", [128, 24], F32)
            S.dma("sp", gvec[:, 0:8], g_mix_d, W=[b_tab])
            S.dma("sp", gvec[:, 8:10], g_cq_d, W=[b_tab])
            S.dma("sp", gvec[:, 10:18], g_out_d, W=[b_tab])

            w_in = sb("w_in", [128, 8, IN_COLS], BF16); b_win = S.buf("w_in")
            S.dma("pool", w_in[:], w_in_d.rearrange("(c p) n -> p c n", p=128), W=[b_win])
            for c in range(8):
                S.op("dve", "tensor_scalar", w_in[:, c, :], w_in[:, c, :], gvec[:, c:c + 1], None, ALU.mult,
                     R=[b_tab, b_win], W=[b_win])
                S.op("dve", "tensor_scalar", w_in[:, c, 416:928], w_in[:, c, 416:928], 0.125, None, ALU.mult,
                     R=[b_win], W=[b_win])
            w_o = sb("w_o", [128, 8, D], BF16); b_wo = S.buf("w_o")
            S.dma("pool", w_o[:], w_o_d.rearrange("(c p) n -> p c n", p=128), W=[b_wo])
            for c in range(8):
                S.op("dve", "tensor_scalar", w_o[:, c, :], w_o[:, c, :], gvec[:, 10 + c:11 + c], None, ALU.mult,
                     R=[b_tab, b_wo], W=[b_wo])
            w_uq = sb("w_uq", [128, 2, 768], BF16); b_wuq = S.buf("w_uq")
            S.dma("pool", w_uq[:], w_uq_d.rearrange("(c p) n -> p c n", p=128), W=[b_wuq])
            for c in range(2):
                S.op("dve", "tensor_scalar", w_uq[:, c, :], w_uq[:, c, :], gvec[:, 8 + c:9 + c], 96.0 ** -0.5,
                     ALU.mult, ALU.mult, R=[b_tab, b_wuq], W=[b_wuq])
            wukT = sb("wukT", [128, 8, 128], BF16); wuv = sb("wuv", [128, 8, 64], BF16); b_wkv = S.buf("wkv")
            S.op("pool", "memset", wukT[:], 0.0, W=[b_wkv])
            S.dma("pool", wukT[0:64], wukT_d, W=[b_wkv])
            S.dma("pool", wuv[:], wuv_d, W=[b_wkv])
            w1 = sb("w1", [128, 2, 32, 128], BF16); b_w1 = S.buf("w1")
            S.op("pool", "memset", w1[:], 0.0, W=[b_w1])
            for kv in range(2):
                S.dma("pool", w1[0:64, kv], w1_d[kv], W=[b_w1])
            peT = sb("peT", [64, 2, 32], BF16)
            for kv in range(2):
                S.dma("pool", peT[:, kv], peT_d[kv], W=[b_w1])
            w2 = sb("w2", [128, 2, 64], BF16)
            S.dma("pool", w2[:], w2_d, W=[b_w1])

            bias_tot = sb("bias_tot", [128, 2], F32); b_bt = S.buf("bias_tot")
            for kv in range(2):
                for l in range(32):
                    S.op("pe", "matmul", pM[0][:, kv:kv + 1], lhsT=w1[0:64, kv, l, :], rhs=peT[:, kv, l:l + 1],
                         start=(l == 0), stop=(l == 31), R=[b_w1], W=[bpM[0]])
            S.op("dve", "tensor_tensor", bias_tot[:], pM[0][:, 0:2], b1[:], ALU.add, R=[bpM[0], b_tab], W=[b_bt])

            KlatT = sb("KlatT", [128, SEQ], BF16); bKlat = S.bufs("Klat", NT)
            KpeT = sb("KpeT", [128, SEQ], BF16); bKpe = S.bufs("Kpe", NT)
            Clat = sb("Clat", [128, NT, 128], BF16); bClat = S.bufs("Clat", NT)
            KsE = sb("KsE", [128, 2, SEQ], BF16); bKs = S.bufs("Ks", NT); b_exp = S.buf("expand")
            Vs = sb("Vs", [128, NT, 2, 65], BF16); bVs = S.bufs("Vs", NT)
            KwT = sb("KwT", [128, 2, 8 * 128], BF16); bKw = S.bufs("Kw", 8)
            Vw = sb("Vw", [128, 8, 2, 65], BF16); bVw = S.bufs("Vw", 8)
            KcT = sb("KcT", [128, 2, 256], BF16); b_Kc = S.buf("Kc")
            VcT = sb("VcT", [64, 2, 256], BF16); b_VcT = S.buf("VcT")
            VcO = sb("VcO", [128, 2, 2, 128], BF16); b_VcO = S.buf("VcO")
            for g in range(2):
                S.dma("pool", KsE[64:128, g, :], exp_d, W=[b_exp])
            for nt in range(2):
                for g in range(2):
                    S.dma("pool", VcO[:, nt, g, 64:128], ovl_d[:, nt, :], W=[b_VcO])
            S.op("pool", "memset", Vs[:, :, :, 64:65], 1.0, W=bVs)
            S.op("pool", "memset", Vw[:, :, :, 64:65], 1.0, W=bVw)

            xs = [sb("xs%d" % i, [128, D], F32) for i in range(2)]; b_xs = S.bufs("xs", 2)
            st = sb("st", [128, 64], F32); b_st = S.buf("st")
            xn = sb("xn", [128, D], BF16); b_xn = S.buf("xn")
            xnT = sb("xnT", [128, 8, 128], BF16); b_xnT = S.buf("xnT")
            u = sb("u", [128, IN_COLS], F32); b_u = S.buf("u")
            cqn = sb("cqn", [128, 256], BF16); b_cqn = S.buf("cqn")
            cqnT = sb("cqnT", [128, 2, 128], BF16); b_cqnT = S.buf("cqnT")
            q_sb = sb("q_sb", [128, 9, 96], F32); b_q = S.buf("q")
            qn_sb = sb("qn_sb", [128, 8, 64], BF16); b_qn = S.buf("qn")
            qpe_sb = sb("qpe_sb", [128, 9, 32], BF16); b_qpe = S.buf("qpe")
            rt = [sb("rt%d" % i, [128, 160], F32) for i in range(4)]; b_rt = S.buf("rt")
            QnT = sb("QnT", [128, 8, 128], BF16); b_QnT = S.buf("QnT")
            QpeT2 = [sb("QpeT%d" % i, [128, 8, 128], BF16) for i in range(2)]; b_QpeT2 = S.bufs("QpeT", 2)
            QabsT2 = [sb("QabsT%d" % i, [128, 8, 128], BF16) for i in range(2)]; b_Qabs2 = S.bufs("Qabs", 2)
            ub = sb("ub", [128, 20, 64], BF16); b_ub = S.buf("ub")
            QS2 = [sb("QS%d" % i, [128, 2, 4, 128], BF16) for i in range(2)]; b_QSq2 = S.bufs("QSq", 2); b_QSs2 = [S.bufs("QSs", 2) for _ in range(2)]
            rawT = [sb("rawT%d" % i, [128, 2, 2, 144], BF16) for i in range(2)]; b_rawT = S.bufs("rawT", 2)
            gate2 = [sb("gate%d" % i, [128, 3, 8], F32) for i in range(2)]; b_gate2 = S.bufs("gate", 2)
            z_sb = sb("z_sb", [128, 32], F32); z2_sb = sb("z2_sb", [128, 32], F32); b_z = S.buf("z")
            hid_sb = sb("hid_sb", [128, 32], BF16); b_hid = S.buf("hid")
            kc_f = sb("kc_f", [8, 2, 64], F32); kc_sb = sb("kc_sb", [8, 2, 64], BF16); b_kc = S.buf("kc")
            PT = [sb("PT%d" % i, [128, 512], BF16) for i in range(3)]; b_PT = S.bufs("PT", 3)
            PcT = [sb("PcT%d" % i, [128, 512], BF16) for i in range(2)]; b_PcT = S.bufs("PcT", 2)
            OlatT = sb("OlatT", [128, 4, 128], BF16); b_OlatT = S.buf("OlatT")
            y_sb = sb("y_sb", [128, D], F32); b_y = S.buf("y"); b_yn = S.buf("yn")
            imp = sb("imp", [128, 64], F32); sc1 = sb("sc1", [128, 64], F32); sc2 = sb("sc2", [128, 64], F32)
            sc3 = sb("sc3", [128, 64], F32); b_imp = S.buf("imp")
            selq = sb("selq", [128, 128], BF16); b_selq = S.buf("selq")
            tmp4 = sb("tmp4", [128, 4, 64], F32); b_tmp4 = S.buf("tmp4")
            mixed = sb("mixed", [128, D], BF16); b_mixed = S.buf("mixed")
            mixedT = sb("mixedT", [128, 8, 128], BF16); b_mixedT = S.buf("mixedT")
            h_sb = [sb("h_sb%d" % i, [128, D], F32) for i in range(2)]; b_h = S.bufs("h", 2)

            S.op("pool", "memset", selq[:], 0.0, W=[b_selq])
            S.op("pool", "memset", KpeT[:], 0.0, W=bKpe)
            S.op("pool", "memset", QnT[:], 0.0, W=[b_QnT])
            S.op("pool", "memset", KwT[:], 0.0, W=bKw)
            for i in range(2):
                S.op("pool", "memset", QS2[i][:], 0.0, W=[b_QSq2[i]] + b_QSs2[i])
            for i in range(2):
                S.op("pool", "memset", QpeT2[i][:], 0.0, W=[b_QpeT2[i]])
            for i in range(2):
                S.op("pool", "memset", rawT[i][:], 0.0, W=[b_rawT[i]])
            S.op("pool", "memset", KcT[:], 0.0, W=[b_Kc])
            S.op("pool", "memset", VcT[:], 0.0, W=[b_VcT])
            S.op("pool", "memset", VcO[:, :, :, 0:64], 0.0, W=[b_VcO])

            b_stc = {0: S.buf("st0"), 4: S.buf("st4"), 12: S.buf("st12")}
            b_stm = S.buf("stm")
            b_stn = S.buf("stn")
            rl = sb("rl", [128, 8], F32); b_rl = S.buf("rl")
            lacc = sb("lacc", [128, 512], F32); b_lacc = S.buf("lacc")
            ones_f = sb("ones_f", [128, 1], F32); b_ones = S.buf("ones")
            S.op("pool", "memset", ones_f[:], 1.0, W=[b_ones])

            def rstd_multi(items, col, Rb, jk, b_jk):
                b_st = b_stc[col]
                k = len(items)
                S.op("dve", "memset", st[:, col:col + k], 0.0, W=[b_st])
                for i_, (src_ap, n) in enumerate(items):
                    S.op("dve", "scalar_tensor_tensor", jk[:, 0:n], src_ap, 1.0, src_ap, ALU.mult, ALU.mult,
                         accum_out=st[:, col + i_:col + i_ + 1], R=Rb + [b_st], W=[b_jk, b_st])
                    S.op("dve", "tensor_scalar", st[:, col + k + i_:col + k + i_ + 1], st[:, col + i_:col + i_ + 1], 1.0 / n, EPS,
                         ALU.mult, ALU.add, R=[b_st], W=[b_st])
                v_ = st[:, col + k:col + 2 * k]
                y_ = st[:, col + 2 * k:col + 3 * k]
                w_ = st[:, col + 3 * k:col + 4 * k]
                if not USE_MAGIC:
                    S.op("act", "activation", w_, v_, AF.Sqrt, R=[b_st], W=[b_st])
                    S.op("dve", "reciprocal", y_, w_, R=[b_st], W=[b_st])
                    return [st[:, col + 2 * k + i_:col + 2 * k + i_ + 1] for i_ in range(k)]
                ss_ = st[:, col:col + k]
                S.op("dve", "tensor_scalar", y_.bitcast(I32), v_.bitcast(I32), -0.5, 1597463007.0, ALU.mult, ALU.add, R=[b_st], W=[b_st])
                S.op("dve", "tensor_scalar", ss_, v_, -0.5, None, ALU.mult, R=[b_st], W=[b_st])
                for _it in range(2):
                    if k == 1:
                        S.op("dve", "scalar_tensor_tensor", w_, y_, ss_, y_, ALU.mult, ALU.mult, R=[b_st], W=[b_st])
                    else:
                        S.op("dve", "tensor_tensor", w_, y_, y_, ALU.mult, R=[b_st], W=[b_st])
                        S.op("dve", "tensor_tensor", w_, w_, ss_, ALU.mult, R=[b_st], W=[b_st])
                    S.op("dve", "scalar_tensor_tensor", y_, w_, 1.5, y_, ALU.add, ALU.mult, R=[b_st], W=[b_st])
                return [st[:, col + 2 * k + i_:col + 2 * k + i_ + 1] for i_ in range(k)]

            def rope(eng, out_ap, in_ap, cos_ap, sin_ap, nh, half, Rb, Wb):
                P = in_ap.shape[0]
                x1 = in_ap[:, :, 0:half]
                x2 = in_ap[:, :, half:2 * half]
                cb = cos_ap[:, None, :].to_broadcast([P, nh, half])
                sbb = sin_ap[:, None, :].to_broadcast([P, nh, half])
                t = [r_[0:P, 0:nh * half].rearrange("p (h d) -> p h d", h=nh) for r_ in rt]
                S.op(eng, "tensor_tensor", t[0], x1, cb, ALU.mult, R=Rb + [b_tab], W=[b_rt])
                S.op(eng, "tensor_tensor", t[1], x2, sbb, ALU.mult, R=Rb + [b_tab], W=[b_rt])
                S.op(eng, "tensor_tensor", t[2], x2, cb, ALU.mult, R=Rb + [b_tab], W=[b_rt])
                S.op(eng, "tensor_tensor", t[3], x1, sbb, ALU.mult, R=Rb + [b_tab], W=[b_rt])
                S.op(eng, "tensor_tensor", out_ap[:, :, 0:half], t[0], t[1], ALU.subtract, R=[b_rt], W=Wb)
                S.op(eng, "tensor_tensor", out_ap[:, :, half:2 * half], t[2], t[3], ALU.add, R=[b_rt], W=Wb)

            def transpose_to(ps_i, col0, in_ap, Rb):
                P, Fd = in_ap.shape[0], in_ap.shape[1]
                S.op("pe", "transpose", pT[ps_i][0:Fd, col0:col0 + P], in_ap, ident[0:P, 0:P],
                     R=Rb + [b_ident], W=[bpT[ps_i]])

            def phase1(s, t):
                par = t % 2
                QpeT, b_QpeT = QpeT2[par], b_QpeT2[par]
                QabsT, b_Qabs = QabsT2[par], b_Qabs2[par]
                QS, b_QSq, b_QSs = QS2[par], b_QSq2[par], b_QSs2[par]
                gate, b_gate = gate2[par], b_gate2[par]
                xb = t % 2
                ce, cm = ("act", "copy") if t < ACT_COPY_T else ("dve", "tensor_copy")
                S.dma("sp", xs[xb][:], x_d[s, t * 128:(t + 1) * 128, :], W=[b_xs[xb]])
                r0 = rstd_multi([(xs[xb][:], D)], 0, [b_xs[xb]], xn, b_xn)[0]
                S.op("dve", "tensor_scalar", xn[:], xs[xb][:], r0, None, ALU.mult, R=[b_xs[xb], b_stc[0]], W=[b_xn])
                ti = nxt("T", 2)
                for c in range(8):
                    transpose_to(ti, c * 128, xn[:, c * 128:(c + 1) * 128], [b_xn])
                S.op(ce, cm, xnT[:], pT[ti][:, 0:1024].rearrange("p (c n) -> p c n", c=8), R=[bpT[ti]], W=[b_xnT])
                for cg, (c0, c1) in enumerate(((0, 512), (512, 1024), (1024, 1536), (1536, IN_COLS))):
                    mi = nxt("M", 2)
                    for c in range(8):
                        S.op("pe", "matmul", pM[mi][:, 0:c1 - c0], lhsT=xnT[:, c, :], rhs=w_in[:, c, c0:c1],
                             start=(c == 0), stop=(c == 7), R=[b_xnT, b_win], W=[bpM[mi]])
                    S.op(ce, cm,
                         u[:, c0:c1], pM[mi][:, 0:c1 - c0], R=[bpM[mi]], W=[b_u])
                    yield

                yield
                r1, r2 = rstd_multi([(u[:, 0:256], 256), (u[:, 256:384], 128)], 4, [b_u], cqn, b_cqn)
                S.op("dve", "tensor_scalar", cqn[:], u[:, 0:256], r1, None, ALU.mult, R=[b_u, b_stc[4]], W=[b_cqn])
                ti = nxt("T", 2)
                for c in range(2):
                    transpose_to(ti, c * 128, cqn[:, c * 128:(c + 1) * 128], [b_cqn])
                S.op("dve", "tensor_copy", cqnT[:], pT[ti][:, 0:256].rearrange("p (c n) -> p c n", c=2), R=[bpT[ti]], W=[b_cqnT])
                yield
                for half in range(2):
                    mi = nxt("M", 2)
                    for c in range(2):
                        S.op("pe", "matmul", pM[mi][:, 0:384], lhsT=cqnT[:, c, :], rhs=w_uq[:, c, half * 384:(half + 1) * 384],
                             start=(c == 0), stop=(c == 1), R=[b_cqnT, b_wuq], W=[bpM[mi]])
                    S.op(ce, cm, q_sb[:, half * 4:(half + 1) * 4, :],
                         pM[mi][:, 0:384].rearrange("p (h d) -> p h d", h=4), R=[bpM[mi]], W=[b_q])
                yield
                S.op("dve", "tensor_copy", q_sb[:, 8, 64:96], u[:, 384:416], R=[b_u], W=[b_q])
                S.op("dve", "tensor_copy", qn_sb[:], q_sb[:, 0:8, 0:64], R=[b_q], W=[b_qn])
                rope("dve", qpe_sb[:], q_sb[:, :, 64:96], tabs["cos_m"][:, t, :], tabs["sin_m"][:, t, :], 9, 16, [b_q], [b_qpe])
                ti = nxt("T", 2)
                for h in range(8):
                    transpose_to(ti, h * 128, qn_sb[:, h, :], [b_qn])
                S.op(ce, cm, QnT[0:64], pT[ti][0:64, 0:1024].rearrange("p (c n) -> p c n", c=8), R=[bpT[ti]], W=[b_QnT])
                ti = nxt("T", 2)
                for h in range(8):
                    transpose_to(ti, h * 128, qpe_sb[:, h, :], [b_qpe])
                S.op(ce, cm, QpeT[0:32], pT[ti][0:32, 0:1024].rearrange("p (c n) -> p c n", c=8), R=[bpT[ti]], W=[b_QpeT])
                yield
                for hg in range(2):
                    mi = nxt("M", 2)
                    for j in range(4):
                        h = hg * 4 + j
                        S.op("pe", "matmul", pM[mi][:, j * 128:(j + 1) * 128], lhsT=wukT[:, h, :],
                             rhs=QnT[:, h, :], start=True, stop=True, R=[b_wkv, b_QnT], W=[bpM[mi]])
                    S.op(ce, cm, QabsT[:, hg * 4:(hg + 1) * 4, :],
                         pM[mi][:, 0:512].rearrange("p (c n) -> p c n", c=4), R=[bpM[mi]], W=[b_Qabs])

                yield
                S.op("dve", "scalar_tensor_tensor", Clat[:, t, 0:128], u[:, 256:384], r2, gckv[:], ALU.mult, ALU.mult,
                     R=[b_u, b_stc[4], b_tab], W=[bClat[t]])
                ti = nxt("T", 2)
                transpose_to(ti, 0, Clat[:, t, 0:128], [bClat[t]])
                S.op("dve", "tensor_copy", KlatT[:, t * 128:(t + 1) * 128], pT[ti][:, 0:128], R=[bpT[ti]], W=[bKlat[t]])
                ti = nxt("T", 2)
                transpose_to(ti, 0, qpe_sb[:, 8, :], [b_qpe])
                S.op("dve", "tensor_copy", KpeT[0:32, t * 128:(t + 1) * 128], pT[ti][0:32, 0:128], R=[bpT[ti]], W=[bKpe[t]])

                yield
                uv = u[:, 416:1696].rearrange("p (b d) -> p b d", b=20)
                S.op("dve", "tensor_copy", ub[:, :, 16:64], uv[:, :, 16:64], R=[b_u], W=[b_ub])
                rope("dve", ub[:, :, 0:16], uv[:, :, 0:16], tabs["cos_n"][:, t, :], tabs["sin_n"][:, t, :], 20, 8, [b_u], [b_ub])
                S.op("dve", "tensor_copy", ub[:, 8:12, 0:16], uv[:, 8:12, 0:16], R=[b_u, b_ub], W=[b_ub])
                ti = nxt("T", 2)
                for h in range(8):
                    transpose_to(ti, h * 128, ub[:, h, :], [b_ub])
                S.op(ce, cm, QS[0:64].rearrange("p g j n -> p (g j) n"),
                     pT[ti][0:64, 0:1024].rearrange("p (c n) -> p c n", c=8), R=[bpT[ti]], W=[b_QSq])
                yield
                kvv = u[:, 928:1696].rearrange("p (s g d) -> p s g d", s=6, g=2)
                S.op("dve", "tensor_copy", Vs[:, t, :, 0:64], kvv[:, 3], R=[b_u], W=[bVs[t]])
                S.op("dve", "tensor_copy", Vw[:, t % 8, :, 0:64], kvv[:, 5], R=[b_u], W=[bVw[t % 8]])
                ti = nxt("T", 2)
                for g in range(2):
                    transpose_to(ti, g * 128, ub[:, 12 + g, :], [b_ub])
                    transpose_to(ti, 256 + g * 128, ub[:, 16 + g, :], [b_ub])
                S.op(ce, cm, KsE[0:64, :, t * 128:(t + 1) * 128],
                     pT[ti][0:64, 0:256].rearrange("p (g n) -> p g n", g=2), R=[bpT[ti]], W=[bKs[t]])
                S.op("dve", "tensor_copy", KwT[0:64, :, (t % 8) * 128:(t % 8 + 1) * 128],
                     pT[ti][0:64, 256:512].rearrange("p (g n) -> p g n", g=2), R=[bpT[ti]], W=[bKw[t % 8]])
                yield
                rb = t % 2
                if t == 0:
                    S.op("pool", "memset", rawT[rb][:, :, :, 0:16], 0.0, W=[b_rawT[rb]])
                else:
                    S.op("dve", "tensor_copy", rawT[rb][:, :, :, 0:16], rawT[1 - rb][:, :, :, 128:144],
                         R=[b_rawT[1 - rb]], W=[b_rawT[rb]])
                ti = nxt("T", 2)
                for c in range(4):
                    transpose_to(ti, c * 128, ub[:, 8 + c, :], [b_ub])
                S.op(ce, cm, rawT[rb][0:64, :, :, 16:144],
                     pT[ti][0:64, 0:512].rearrange("p (k g n) -> p k g n", k=2, g=2), R=[bpT[ti]], W=[b_rawT[rb]])
                yield
                S.op("act", "activation", gate[:].rearrange("p b h -> p (b h)"), u[:, 1696:1720], AF.Tanh, scale=0.5, R=[b_u], W=[b_gate])
                S.op("dve", "tensor_scalar", gate[:].rearrange("p b h -> p (b h)"), gate[:].rearrange("p b h -> p (b h)"), 0.5, 0.5,
                     ALU.mult, ALU.add, R=[b_gate], W=[b_gate])

                yield
                mi = nxt("M", 2)
                for kv in range(2):
                    for g in range(2):
                        c0 = (kv * 2 + g) * 8
                        for l in range(32):
                            S.op("pe", "matmul", pM[mi][:, c0:c0 + 8], lhsT=w1[:, kv, l, :], rhs=rawT[rb][:, kv, g, l:l + 113:16],
                                 start=(l == 0), stop=(l == 31), R=[b_w1, b_rawT[rb]], W=[bpM[mi]])
                            if l % 8 == 7:
                                yield
                for kv in range(2):
                    S.op("dve", "tensor_scalar", z_sb[:, kv * 16:(kv + 1) * 16], pM[mi][:, kv * 16:(kv + 1) * 16],
                         bias_tot[:, kv:kv + 1], None, ALU.add, R=[bpM[mi], b_bt], W=[b_z])
                S.op("dve", "tensor_tensor", z2_sb[:], z_sb[:], z_sb[:], ALU.mult, R=[b_z], W=[b_z])
                S.op("dve", "tensor_scalar", z2_sb[:], z2_sb[:], 0.044715, 1.0, ALU.mult, ALU.add, R=[b_z], W=[b_z])
                S.op("dve", "tensor_tensor", z2_sb[:], z2_sb[:], z_sb[:], ALU.mult, R=[b_z], W=[b_z])
                S.op("act", "activation", z2_sb[:], z2_sb[:], AF.Tanh, scale=math.sqrt(2.0 / math.pi), R=[b_z], W=[b_z])
                S.op("dve", "tensor_scalar", z2_sb[:], z2_sb[:], 0.5, 0.5, ALU.mult, ALU.add, R=[b_z], W=[b_z])
                S.op("dve", "tensor_tensor", hid_sb[:], z_sb[:], z2_sb[:], ALU.mult, R=[b_z], W=[b_hid])
                yield
                n0 = 8 * t - 1
                m0 = 1 if t == 0 else 0
                mi = nxt("M", 2)
                for g in range(2):
                    S.op("pe", "matmul", pM[mi][0:8, g * 64:(g + 1) * 64], lhsT=hid_sb[:, g * 8:(g + 1) * 8], rhs=w2[:, 0, :],
                         start=True, stop=True, R=[b_hid, b_w1], W=[bpM[mi]])
                for g in range(2):
                    S.op("pe", "matmul", pM[mi][0:64, 128 + g * 8:136 + g * 8], lhsT=w2[:, 1, :], rhs=hid_sb[:, 16 + g * 8:24 + g * 8],
                         start=True, stop=True, R=[b_hid, b_w1], W=[bpM[mi]])
                S.op("dve", "tensor_tensor", kc_f[:].rearrange("p g d -> p (g d)"), pM[mi][0:8, 0:128], b2k[0:8, :], ALU.add,
                     R=[bpM[mi], b_tab], W=[b_kc])
                S.op("dve", "tensor_scalar", VcT[:, :, n0 + m0:n0 + 8], pM[mi][0:64, 128:144].rearrange("p (g m) -> p g m", g=2)[:, :, m0:8],
                     b2v[:, 0:1], None, ALU.add, R=[bpM[mi], b_tab], W=[b_VcT])
                S.op("dve", "tensor_copy", kc_sb[:, :, 16:64], kc_f[:, :, 16:64], R=[b_kc], W=[b_kc])
                rope("dve", kc_sb[:, :, 0:16], kc_f[:, :, 0:16], cos_e[:, t, :], sin_e[:, t, :], 2, 8, [b_kc], [b_kc])
                ti = nxt("T", 2)
                for g in range(2):
                    transpose_to(ti, g * 8, kc_sb[:, g, :], [b_kc])
                S.op("dve", "tensor_copy", KcT[0:64, :, n0 + m0:n0 + 8],
                     pT[ti][0:64, 0:16].rearrange("p (g m) -> p g m", g=2)[:, :, m0:8], R=[bpT[ti]], W=[b_Kc])
                yield
                nts = sorted(set([max(n0, 0) // 128, (n0 + 7) // 128]))
                for nt in nts:
                    ti = nxt("T", 2)
                    for g in range(2):
                        S.op("pe", "transpose", pT[ti][:, g * 64:(g + 1) * 64], VcT[:, g, nt * 128:(nt + 1) * 128], ident[0:64, 0:64],
                             R=[b_VcT, b_ident], W=[bpT[ti]])
                    S.op("dve", "tensor_copy", VcO[:, nt, :, 0:64], pT[ti][:, 0:128].rearrange("p (g d) -> p g d", g=2), R=[bpT[ti]], W=[b_VcO])

            def attn_loop(kts, qk_fn, post_fn):
                if not kts:
                    return
                si_next = qk_fn(kts[0])
                for i_, kt in enumerate(kts):
                    si = si_next
                    if i_ + 1 < len(kts):
                        si_next = qk_fn(kts[i_ + 1])
                    post_fn(kt, si)

            def mla(s, t):
                par = t % 2
                QpeT, b_QpeT = QpeT2[par], b_QpeT2[par]
                QabsT, b_Qabs = QabsT2[par], b_Qabs2[par]
                QS, b_QSq, b_QSs = QS2[par], b_QSq2[par], b_QSs2[par]
                gate, b_gate = gate2[par], b_gate2[par]
                mo = nxt("M", 2)
                for hg in range(2):
                    qa = QabsT[:, hg * 4:(hg + 1) * 4, :].rearrange("p c n -> p (c n)")
                    qp = QpeT[:, hg * 4:(hg + 1) * 4, :].rearrange("p c n -> p (c n)")

                    def qk(kt):
                        si = nxt("S", 2)
                        S.op("pe", "matmul", pS[si][:], lhsT=KlatT[:, kt * 128:(kt + 1) * 128], rhs=qa,
                             start=True, stop=False, R=[bKlat[kt], b_Qabs], W=[bpS[si]])
                        S.op("pe", "matmul", pS[si][:], lhsT=KpeT[:, kt * 128:(kt + 1) * 128], rhs=qp,
                             start=False, stop=True, R=[bKpe[kt], b_QpeT], W=[bpS[si]])
                        return si

                    def post(kt, si):
                        pi = nxt("P", 3)
                        S.op("act", "activation", PT[pi][:], pS[si][:], AF.Exp, R=[bpS[si]], W=[b_PT[pi]])
                        if kt == t:
                            S.op("dve", "tensor_tensor", PT[pi][:], PT[pi][:], tri4[:], ALU.mult, R=[b_PT[pi], b_msk], W=[b_PT[pi]])
                        S.op("pe", "matmul", pA[0][:], lhsT=Clat[:, kt, 0:128], rhs=PT[pi][:], start=(kt == 0), stop=(kt == t),
                             R=[b_PT[pi], bClat[kt]], W=[bpA[0]])
                        if kt == 0:
                            S.op("dve", "tensor_copy", lacc[:], PT[pi][:], R=[b_PT[pi]], W=[b_lacc])
                        else:
                            S.op("dve", "tensor_tensor", lacc[:], lacc[:], PT[pi][:], ALU.add, R=[b_PT[pi], b_lacc], W=[b_lacc])

                    attn_loop(list(range(t + 1)), qk, post)
                    for j in range(4):
                        S.op("pe", "matmul", pA[1][:, j:j + 1], lhsT=lacc[:, j * 128:(j + 1) * 128], rhs=ones_f[:, 0:1],
                             start=True, stop=True, R=[b_lacc, b_ones], W=[bpA[1]])
                    S.op("dve", "tensor_copy", OlatT[:], pA[0][:].rearrange("p (c n) -> p c n", c=4), R=[bpA[0]], W=[b_OlatT])
                    S.op("dve", "reciprocal", rl[:, hg * 4:(hg + 1) * 4], pA[1][:, 0:4], R=[bpA[1]], W=[b_rl])
                    for j in range(4):
                        h = hg * 4 + j
                        S.op("pe", "matmul", pM[mo][:, h * 64:(h + 1) * 64], lhsT=OlatT[:, j, :], rhs=wuv[:, h, :],
                             start=True, stop=True, R=[b_OlatT, b_wkv], W=[bpM[mo]])
                S.op("dve", "tensor_tensor", y_sb[:, 0:512].rearrange("p (h d) -> p h d", h=8),
                     pM[mo][:, 0:512].rearrange("p (h d) -> p h d", h=8), rl[:, 0:8, None].to_broadcast([128, 8, 64]), ALU.mult,
                     R=[bpM[mo], b_rl], W=[b_y])

            def nsa_sel(s, t, g):
                par = t % 2
                QpeT, b_QpeT = QpeT2[par], b_QpeT2[par]
                QabsT, b_Qabs = QabsT2[par], b_Qabs2[par]
                QS, b_QSq, b_QSs = QS2[par], b_QSq2[par], b_QSs2[par]
                gate, b_gate = gate2[par], b_gate2[par]
                QSg = QS[:, g].rearrange("p j n -> p (j n)")
                QSg = QS[:, g].rearrange("p j n -> p (j n)")
                nts = [0] + ([1] if t >= 16 else [])
                for nt in nts:
                    si = nxt("M", 2)
                    S.op("pe", "matmul", pM[si][:], lhsT=KcT[:, g, nt * 128:(nt + 1) * 128], rhs=QSg,
                         start=True, stop=True, R=[b_Kc, b_QSq, b_QSs[g]], W=[bpM[si]])
                    S.op("act", "activation", PcT[nt][:], pM[si][:], AF.Exp, R=[bpM[si]], W=[b_PcT[nt]])
                    S.op("pool", "affine_select", PcT[nt][:].rearrange("p (j n) -> p j n", j=4),
                         PcT[nt][:].rearrange("p (j n) -> p j n", j=4), [[0, 4], [1, 128]], ALU.is_ge, 0.0,
                         base=128 * t - 31 - 2048 * nt, channel_multiplier=-16, R=[b_PcT[nt]], W=[b_PcT[nt]])
                yield
                mc = nxt("M", 2)
                for j in range(4):
                    for i_, nt in enumerate(nts):
                        S.op("pe", "matmul", pM[mc][:, j * 128:(j + 1) * 128], lhsT=PcT[nt][:, j * 128:(j + 1) * 128],
                             rhs=VcO[:, nt, g, :], start=(i_ == 0), stop=(i_ == len(nts) - 1),
                             R=[b_PcT[nt], b_VcO], W=[bpM[mc]])
                yield
                pc = pM[mc][:].rearrange("p (j c) -> p j c", j=4)
                S.op("dve", "tensor_reduce", st[:, 24:28], pc[:, :, 64:128], AX.X, ALU.add, R=[bpM[mc]], W=[b_stn])
                S.op("dve", "tensor_scalar", st[:, 24:28], st[:, 24:28], 1e-30, None, ALU.max, R=[b_stn], W=[b_stn])
                S.op("dve", "reciprocal", st[:, 28:32], st[:, 24:28], R=[b_stn], W=[b_stn])
                S.op("dve", "tensor_tensor", tmp4[:], pc[:, :, 64:128], st[:, 28:32, None].to_broadcast([128, 4, 64]), ALU.mult,
                     R=[bpM[mc], b_stn], W=[b_tmp4])
                S.op("dve", "tensor_reduce", imp[:], tmp4[:].rearrange("p j c -> p c j"), AX.X, ALU.add, R=[b_tmp4], W=[b_imp])
                yield
                S.op("dve", "tensor_tensor", st[:, 32:36], st[:, 28:32], gate[:, 0, g * 4:(g + 1) * 4], ALU.mult,
                     R=[b_stn, b_gate], W=[b_stn])
                S.op("dve", "tensor_tensor", y_sb[:, 512 + g * 256:768 + g * 256].rearrange("p (j c) -> p j c", j=4), pc[:, :, 0:64],
                     st[:, 32:36, None].to_broadcast([128, 4, 64]), ALU.mult, R=[bpM[mc], b_stn], W=[b_yn])
                yield
                S.op("pool", "affine_select", sc1[:], imp[:], [[-64, 64]], ALU.is_ge, 1e9, base=128 * t - 128, channel_multiplier=1,
                     R=[b_imp], W=[b_imp])
                S.op("pool", "affine_select", sc2[:], sc1[:], [[-64, 64]], ALU.is_ge, -1e9, base=128 * t, channel_multiplier=1,
                     R=[b_imp], W=[b_imp])
                S.op("pool", "memset", sc2[:, 0:1], 1e9, R=[b_imp], W=[b_imp])
                yield
                S.op("dve", "max", st[:, 40:48], sc2[:], R=[b_imp], W=[b_stn])
                S.op("dve", "match_replace", sc3[:], st[:, 40:48], sc2[:], -3e38, R=[b_imp, b_stn], W=[b_imp])
                S.op("dve", "max", st[:, 48:56], sc3[:], R=[b_imp], W=[b_stn])
                S.op("dve", "tensor_scalar", selq[:, 64:128], sc2[:], st[:, 55:56], NEG, ALU.is_lt, ALU.mult,
                     R=[b_imp, b_stn], W=[b_selq])
                yield
                ti = nxt("T", 2)
                transpose_to(ti, 0, selq[:], [b_selq])
                S.op("dve", "tensor_copy", QS[64:128, g], pT[ti][64:128, None, 0:128].to_broadcast([64, 4, 128]),
                     R=[bpT[ti]], W=[b_QSs[g]])
                yield

            def nsa_attn(s, t, g):
                par = t % 2
                QpeT, b_QpeT = QpeT2[par], b_QpeT2[par]
                QabsT, b_Qabs = QabsT2[par], b_Qabs2[par]
                QS, b_QSq, b_QSs = QS2[par], b_QSq2[par], b_QSs2[par]
                gate, b_gate = gate2[par], b_gate2[par]
                QSg = QS[:, g].rearrange("p j n -> p (j n)")
                S.op("dve", "memset", pA[0][:, 0:260], 0.0, W=[bpA[0]])
                S.op("dve", "memset", pA[1][:, 0:260], 0.0, W=[bpA[1]])
                def qk_s(kt):
                    si = nxt("S", 2)
                    S.op("pe", "matmul", pS[si][:], lhsT=KsE[:, g, kt * 128:(kt + 1) * 128], rhs=QSg,
                         start=True, stop=True, R=[bKs[kt], b_exp, b_QSq, b_QSs[g]], W=[bpS[si]])
                    return si

                def post_s(kt, si):
                    pi = nxt("P", 3)
                    S.op("act", "activation", PT[pi][:], pS[si][:], AF.Exp, R=[bpS[si]], W=[b_PT[pi]])
                    if kt == t:
                        S.op("dve", "tensor_tensor", PT[pi][:], PT[pi][:], tri4[:], ALU.mult, R=[b_PT[pi], b_msk], W=[b_PT[pi]])
                    for j in range(4):
                        S.op("pe", "matmul", pA[0][:, j * 65:j * 65 + 65], lhsT=PT[pi][:, j * 128:(j + 1) * 128],
                             rhs=Vs[:, kt, g, :], start=False, stop=(kt == t), skip_group_check=True,
                             R=[b_PT[pi], bVs[kt]], W=[bpA[0]])

                def qk_w(kt):
                    si = nxt("S", 2)
                    sl = kt % 8
                    S.op("pe", "matmul", pS[si][:], lhsT=KwT[:, g, sl * 128:(sl + 1) * 128], rhs=QSg,
                         start=True, stop=True, R=[bKw[sl], b_QSq, b_QSs[g]], W=[bpS[si]])
                    return si

                def post_w(kt, si):
                    sl = kt % 8
                    pi = nxt("P", 3)
                    S.op("act", "activation", PT[pi][:], pS[si][:], AF.Exp, R=[bpS[si]], W=[b_PT[pi]])
                    if kt == t:
                        S.op("dve", "tensor_tensor", PT[pi][:], PT[pi][:], tri4[:], ALU.mult, R=[b_PT[pi], b_msk], W=[b_PT[pi]])
                    if kt == t - 4:
                        S.op("dve", "tensor_tensor", PT[pi][:], PT[pi][:], anti4[:], ALU.mult, R=[b_PT[pi], b_msk], W=[b_PT[pi]])
                    for j in range(4):
                        S.op("pe", "matmul", pA[1][:, j * 65:j * 65 + 65], lhsT=PT[pi][:, j * 128:(j + 1) * 128],
                             rhs=Vw[:, sl, g, :], start=False, stop=(kt == t), skip_group_check=True,
                             R=[b_PT[pi], bVw[sl]], W=[bpA[1]])

                attn_loop(list(range(t + 1)), qk_s, post_s)
                attn_loop(list(range(max(0, t - 4), t + 1)), qk_w, post_w)
                for br in range(2):
                    pa = pA[br][:, 0:260].rearrange("p (j c) -> p j c", j=4)
                    S.op("dve", "reciprocal", st[:, 56:60], pa[:, :, 64], R=[bpA[br]], W=[b_stn])
                    S.op("dve", "tensor_tensor", st[:, 60:64], st[:, 56:60], gate[:, 1 + br, g * 4:(g + 1) * 4], ALU.mult,
                         R=[b_stn, b_gate], W=[b_stn])
                    yv = y_sb[:, 512 + g * 256:768 + g * 256].rearrange("p (j c) -> p j c", j=4)
                    S.op("dve", "tensor_tensor", tmp4[:], pa[:, :, 0:64], st[:, 60:64, None].to_broadcast([128, 4, 64]), ALU.mult,
                         R=[bpA[br], b_stn], W=[b_tmp4])
                    S.op("dve", "tensor_tensor", yv, yv, tmp4[:], ALU.add, R=[b_tmp4, b_yn], W=[b_yn])


            def outproj(s, t):
                xb = t % 2
                ra, rb_ = rstd_multi([(y_sb[:, 0:512], 512), (y_sb[:, 512:1024], 512)], 12, [b_y, b_yn], mixed, b_mixed)
                S.op("dve", "tensor_scalar", mixed[:, 0:512], y_sb[:, 0:512], ra, None, ALU.mult, R=[b_y, b_stc[12]], W=[b_mixed])
                S.op("dve", "tensor_scalar", mixed[:, 512:1024], y_sb[:, 512:1024], rb_, None, ALU.mult, R=[b_yn, b_stc[12]], W=[b_mixed])
                ti = nxt("T", 2)
                for c in range(8):
                    transpose_to(ti, c * 128, mixed[:, c * 128:(c + 1) * 128], [b_mixed])
                S.op("dve", "tensor_copy", mixedT[:], pT[ti][:, 0:1024].rearrange("p (c n) -> p c n", c=8), R=[bpT[ti]], W=[b_mixedT])
                for dh in range(2):
                    mi = nxt("S", 2)
                    for c in range(8):
                        S.op("pe", "matmul", pS[mi][:], lhsT=mixedT[:, c, :], rhs=w_o[:, c, dh * 512:(dh + 1) * 512],
                             start=(c == 0), stop=(c == 7), R=[b_mixedT, b_wo], W=[bpS[mi]])
                    S.op("dve", "tensor_tensor", h_sb[xb][:, dh * 512:(dh + 1) * 512], pS[mi][:], xs[xb][:, dh * 512:(dh + 1) * 512],
                         ALU.add, R=[bpS[mi], b_xs[xb]], W=[b_h[xb]])
                row = (s * SEQ + t * 128)
                S.dma("sp", hscr_d[row:row + 128, :], h_sb[xb][:], R=[b_h[xb]])

            bgst = {"gen": None, "credit": 0.0, "rate": 0.0}

            def bg_run(n=None):
                if bgst["gen"] is None:
                    return
                mode["bg"] = True
                try:
                    k = 0
                    while n is None or k < n:
                        next(bgst["gen"])
                        k += 1
                except StopIteration:
                    bgst["gen"] = None
                mode["bg"] = False

            def tick():
                if mode["bg"] or bgst["gen"] is None:
                    return
                bgst["credit"] += bgst["rate"]
                if bgst["credit"] >= 1.0:
                    n = int(bgst["credit"])
                    bgst["credit"] -= n
                    bg_run(n)

            S.tick = tick
            def chain(*gens):
                for g_ in gens:
                    yield from g_

            def set_bg(gen, nchunks, fg_ops):
                bgst["gen"] = gen
                bgst["credit"] = 0.0
                bgst["rate"] = nchunks / (0.7 * fg_ops)

            for s in range(nseq):
                bgst["gen"] = phase1(s, 0) if "p1" in stages else None
                bg_run(None)
                for t in range(ntiles):
                    if "nsa" in stages:
                        set_bg(chain(nsa_sel(s, t, 0), nsa_sel(s, t, 1)), 16.0, 30.0 + 18.0 * (t + 1))
                    if "mla" in stages:
                        mla(s, t)
                    bg_run(None)
                    if t + 1 < ntiles and "p1" in stages:
                        set_bg(phase1(s, t + 1), 40.0, 100.0 + 14.0 * (t + 1 + min(t + 1, 5)))
                    if "nsa" in stages:
                        nsa_attn(s, t, 0)
                        nsa_attn(s, t, 1)
                    if "out" in stages:
                        outproj(s, t)
                    bg_run(None)
            S.tick = None
            S.barrier()
        mode["B"] = True
        B = ExitStack()
        with B:
            def sb2(name, shape, dt):
                return B.enter_context(nc.sbuf_tensor("b_" + name, shape, dt))
            gb = sb2("gb", [128, 8], F32); b_gb = S.buf("gb")
            S.dma("sp", gb[:], g_mlp_d, W=[b_gb])
            gfin = sb2("gfin", [128, D], F32)
            S.dma("sp", gfin[:], gfin_d, W=[b_gb])
            ident2 = sb2("ident2", [128, 128], BF16); b_id2 = S.buf("ident2")
            S.dma("pool", ident2[:], ident_d, W=[b_id2])
            w_up = sb2("w_up", [128, 8, DFF], BF16); b_wupb = S.bufs("w_up", 8)
            wuv_ = w_up_d.rearrange("(c p) n -> p c n", p=128)
            for fb in range(8):
                S.dma("pool", w_up[:, :, fb * 512:(fb + 1) * 512], wuv_[:, :, fb * 512:(fb + 1) * 512], W=[b_wupb[fb]])
                S.op("dve", "tensor_tensor", w_up[:, :, fb * 512:(fb + 1) * 512], w_up[:, :, fb * 512:(fb + 1) * 512],
                     gb[:, 0:8, None].to_broadcast([128, 8, 512]), ALU.mult, R=[b_gb, b_wupb[fb]], W=[b_wupb[fb]])
            w_dn = sb2("w_dn", [128, 32, D], BF16); b_wdn = S.buf("w_dn")
            wdv = w_down_d.rearrange("(f p) n -> p f n", p=128)
            for f4 in range(8):
                S.dma("pool", w_dn[:, f4 * 4:(f4 + 1) * 4, :], wdv[:, f4 * 4:(f4 + 1) * 4, :], W=[b_wdn])
            hin = [sb2("hin%d" % i, [128, D], F32) for i in range(4)]; b_hin = S.bufs("hin", 4)
            st2 = sb2("st2", [128, 16], F32); b_st2 = S.buf("st2")
            junk2 = sb2("junk2", [128, D], BF16); b_junk2 = S.buf("junk2")
            hn = sb2("hn", [128, D], BF16); b_hn = S.buf("hn")
            hnT = sb2("hnT", [128, 8, 512], BF16); b_hnT = S.buf("hnT")
            rl = [sb2("rl%d" % i, [128, 512], BF16) for i in range(2)]; b_rl = S.bufs("rl", 2)
            aT = sb2("aT", [128, 32, 512], BF16); b_aT = S.buf("aT")
            yo = [sb2("yo%d" % i, [128, D], F32) for i in range(2)]; b_yo = S.bufs("yo", 2)
            S.op("pool", "memset", st2[:], 0.0, W=[b_st2])
            nT = (nseq * ntiles * 128) // 512 if do_mlp else 0

            def rstd2(src_ap, Rb):
                S.op("pool", "memset", st2[:, 0:1], 0.0, W=[b_st2])
                S.op("act", "activation", junk2[:], src_ap, AF.Square, accum_out=st2[:, 0:1], R=Rb, W=[b_junk2, b_st2])
                S.op("dve", "tensor_scalar", st2[:, 1:2], st2[:, 0:1], 1.0 / D, EPS, ALU.mult, ALU.add, R=[b_st2], W=[b_st2])
                S.op("act", "activation", st2[:, 2:3], st2[:, 1:2], AF.Sqrt, R=[b_st2], W=[b_st2])
                S.op("dve", "reciprocal", st2[:, 3:4], st2[:, 2:3], R=[b_st2], W=[b_st2])
                return st2[:, 3:4]

            oc = 0
            for T in range(nT):
                hb = T % 2
                row = T * 512 if nseq * ntiles * 128 == nseq * SEQ else None
                base = (T * 512 // (ntiles * 128)) * SEQ + (T * 512) % (ntiles * 128)
                for i in range(4):
                    S.dma("sp", hin[i][:], hscr_d[base + i * 128:base + (i + 1) * 128, :], W=[b_hin[i]])
                for i in range(4):
                    r = rstd2(hin[i][:], [b_hin[i]])
                    S.op("dve", "tensor_scalar", hn[:], hin[i][:], r, None, ALU.mult, R=[b_hin[i], b_st2], W=[b_hn])
                    ti = nxt("T", 2)
                    for c in range(8):
                        S.op("pe", "transpose", pT[ti][:, c * 128:(c + 1) * 128], hn[:, c * 128:(c + 1) * 128], ident2[:],
                             R=[b_hn, b_id2], W=[bpT[ti]])
                    S.op("act", "copy", hnT[:, :, i * 128:(i + 1) * 128], pT[ti][:, 0:1024].rearrange("p (c n) -> p c n", c=8),
                         R=[bpT[ti]], W=[b_hnT])
                for f in range(32):
                    si = nxt("S", 2)
                    for c in range(8):
                        S.op("pe", "matmul", pS[si][:], lhsT=w_up[:, c, f * 128:(f + 1) * 128], rhs=hnT[:, c, :],
                             start=(c == 0), stop=(c == 7), R=[b_wupb[f // 4], b_hnT], W=[bpS[si]])
                    ri = f % 2
                    S.op("act", "activation", rl[ri][:], pS[si][:], AF.Relu, R=[bpS[si]], W=[b_rl[ri]])
                    S.op("pool", "tensor_tensor", aT[:, f, :], rl[ri][:], rl[ri][:], ALU.mult, R=[b_rl[ri]], W=[b_aT])
                for i in range(4):
                    ob = oc % 2
                    oc += 1
                    for dh in range(2):
                        mi = nxt("M", 2)
                        for f in range(32):
                            S.op("pe", "matmul", pM[mi][:], lhsT=aT[:, f, i * 128:(i + 1) * 128], rhs=w_dn[:, f, dh * 512:(dh + 1) * 512],
                                 start=(f == 0), stop=(f == 31), R=[b_aT, b_wdn], W=[bpM[mi]])
                        S.op("dve", "tensor_tensor", yo[ob][:, dh * 512:(dh + 1) * 512], pM[mi][:], hin[i][:, dh * 512:(dh + 1) * 512],
                             ALU.add, R=[bpM[mi], b_hin[i]], W=[b_yo[ob]])
                    r = rstd2(yo[ob][:], [b_yo[ob]])
                    S.op("dve", "scalar_tensor_tensor", yo[ob][:], yo[ob][:], r, gfin[:], ALU.mult, ALU.mult,
                         R=[b_yo[ob], b_st2, b_gb], W=[b_yo[ob]])
                    S.dma("sp", out_d[base + i * 128:base + (i + 1) * 128, :], yo[ob][:], R=[b_yo[ob]])
            S.emit()
            print('NOPS', S.nops)
    return nc


def _rope_tab(pos, dim):
    inv = np.exp(np.float32(-math.log(500000.0)) * np.arange(0, dim, 2, dtype=np.float32) / np.float32(dim)).astype(np.float32)
    ang = pos.astype(np.float32)[:, None] * inv[None, :]
    return np.cos(ang).astype(np.float32), np.sin(ang).astype(np.float32)


def _tok_major(a):
    return np.ascontiguousarray(a.reshape(NT, 128, -1).transpose(1, 0, 2))


def host_consts():
    pos = np.arange(SEQ)
    cm, sm = _rope_tab(pos, 32)
    cn, sn = _rope_tab(pos, 16)
    c = {}
    c["cos_m"], c["sin_m"] = _tok_major(cm), _tok_major(sm)
    c["cos_n"], c["sin_n"] = _tok_major(cn), _tok_major(sn)
    c["cos_n8"], c["sin_n8"] = _tok_major(cn * np.float32(0.125)), _tok_major(sn * np.float32(0.125))
    ce = np.zeros((8, NT, 8), np.float32)
    se = np.zeros((8, NT, 8), np.float32)
    for t in range(NT):
        for m in range(8):
            n = 8 * t - 1 + m
            if 0 <= n < 255:
                p = 16 * n + 31
                ce[m, t], se[m, t] = cn[p], sn[p]
    c["cos_e"], c["sin_e"] = ce, se
    k = np.arange(128)[:, None]
    q = np.arange(128)[None, :]
    tri = (q >= k).astype(np.float32)
    anti = (q < k).astype(np.float32)
    c["tri4"] = np.ascontiguousarray(np.tile(tri, (1, 4)))
    c["anti4"] = np.ascontiguousarray(np.tile(anti, (1, 4)))
    n = np.arange(256)[:, None] * 16
    j = np.arange(64)[None, :] * 64
    ov = np.clip(np.minimum(n + 32, j + 64) - np.maximum(n, j), 0, None).astype(np.float32) / 32.0
    ov[255] = 0.0
    c["ovl"] = np.ascontiguousarray(ov.reshape(2, 128, 64).transpose(1, 0, 2))
    c["expand"] = (np.arange(SEQ)[None, :] // 64 == np.arange(64)[:, None]).astype(np.float32)
    c["ident"] = np.eye(128, dtype=np.float32)
    return c


def host_weights(inp):
    f = lambda a: np.ascontiguousarray(np.asarray(a, dtype=np.float32))
    pc = lambda v: np.ascontiguousarray(np.asarray(v, np.float32).reshape(-1, 128).T)
    w = {}
    w["w_in"] = f(inp["w_in"][0])
    w["g_mix"] = pc(inp["g_mix_norm"][0])
    w["w_uq"] = f(inp["w_uq"][0])
    w["g_cq"] = pc(inp["g_cq"][0])
    wukv = np.asarray(inp["w_ukv"][0], np.float32).reshape(128, 8, 2, 64)
    wuk = wukv[:, :, 0, :]
    w["wukT"] = np.ascontiguousarray(wuk.transpose(2, 1, 0))
    w["wuv"] = np.ascontiguousarray(wukv[:, :, 1, :])
    w["gckv_bc"] = np.ascontiguousarray(np.broadcast_to(np.asarray(inp["g_ckv"][0], np.float32)[None, :], (128, 128)))
    w1 = np.stack([np.asarray(inp["cmp_w1_k"][0], np.float32), np.asarray(inp["cmp_w1_v"][0], np.float32)])
    w["cmp_w1"] = np.ascontiguousarray(w1.reshape(2, 32, 64, 128).transpose(0, 2, 1, 3))
    pe = np.stack([np.asarray(inp["cmp_pe_k"][0], np.float32), np.asarray(inp["cmp_pe_v"][0], np.float32)])
    w["cmp_peT"] = np.ascontiguousarray(pe.transpose(0, 2, 1))
    w["cmp_b1"] = np.ascontiguousarray(np.stack([inp["cmp_b1_k"][0], inp["cmp_b1_v"][0]], axis=1).astype(np.float32))
    w["cmp_w2"] = np.ascontiguousarray(np.stack([inp["cmp_w2_k"][0], inp["cmp_w2_v"][0]], axis=1).astype(np.float32))
    b2k = np.asarray(inp["cmp_b2_k"][0], np.float32)
    w["cmp_b2k_bc"] = np.ascontiguousarray(np.broadcast_to(np.concatenate([b2k, b2k])[None, :], (128, 128)))
    w["cmp_b2v"] = np.ascontiguousarray(np.asarray(inp["cmp_b2_v"][0], np.float32).reshape(64, 1))
    w["w_o"] = f(inp["w_o"][0])
    w["g_out"] = pc(np.concatenate([np.asarray(inp["g_out_mla"][0]), np.asarray(inp["g_out_nsa"][0])]))
    w["w_up"] = f(inp["w_up"][0])
    w["g_mlp"] = pc(inp["g_mlp_norm"][0])
    w["w_down"] = f(inp["w_down"][0])
    w["gfin_bc"] = np.ascontiguousarray(np.broadcast_to(np.asarray(inp["g_final"], np.float32)[None, :], (128, D)))
    return w


_NC_CACHE = {}


def kernel(**inputs):
    x = np.asarray(inputs["x"], dtype=np.float32)
    shared = host_consts()
    shared.update(host_weights(inputs))
    if "full" not in _NC_CACHE:
        _NC_CACHE["full"] = build_program()
    nc = _NC_CACHE["full"]
    in_maps = []
    for c in range(NCORES):
        m = dict(shared)
        m["x"] = np.ascontiguousarray(x[c * NSEQ:(c + 1) * NSEQ])
        in_maps.append(m)
    res = run_bass_kernel_spmd(nc, in_maps, core_ids=list(range(NCORES)))
    outs = [np.asarray(r["out"]).reshape(NSEQ, SEQ, D) for r in res.results]
    return np.concatenate(outs, axis=0).astype(np.float32)
```
